# Optimizing a Trainium2 kernel written in Bass

```python
import jax, jax.numpy as jnp
from jax import lax
import numpy as np

D_MODEL = 1024
BATCH = 16
SEQ = 2048
DEPTH = 4

GRID_W = 64
CTX_LEN = 256
N_MIXERS = 3
ROPE_THETA = 10000.0
EPS = 1e-6
Q_BLOCK = 128
NEG_INF = -1e30

A_HEADS = 16
A_KV_HEADS = 4
A_HEAD_DIM = 64
B_HEADS = 16
B_Q_RANK = 384
B_KV_RANK = 256
B_NOPE_DIM = 64
B_ROPE_DIM = 32
B_V_DIM = 64
C_HEADS = 16
C_HEAD_DIM = 64
NA_ROWS = 8
NA_COLS = 16
N_EXPERTS = 16
EXPERT_FF = 1024
EC_CAPACITY_FACTOR = 2

N_A = (DEPTH + 2) // 3
N_B = (DEPTH + 1) // 3
N_C = DEPTH // 3

kernel_name = "hybrid_diffusion_gqa_mla_natten_ecmoe"


def rms_norm(x, w):
    xf = x.astype(jnp.float32)
    y = xf * lax.rsqrt(jnp.mean(xf * xf, axis=-1, keepdims=True) + EPS)
    return (y * w.astype(jnp.float32)).astype(x.dtype)


def modulate(h, shift, scale):
    return h * (1.0 + scale[:, None]) + shift[:, None]


def axial_rope_tables(n_tokens, rot_dim, dtype):
    n_freq = rot_dim // 4
    inv = jnp.float32(ROPE_THETA) ** (-jnp.arange(n_freq, dtype=jnp.float32) / n_freq)
    t = jnp.arange(n_tokens, dtype=jnp.int32)
    row = (t // GRID_W).astype(jnp.float32)
    col = (t % GRID_W).astype(jnp.float32)
    ang = jnp.concatenate([row[:, None] * inv, col[:, None] * inv], axis=-1)
    return jnp.cos(ang).astype(dtype), jnp.sin(ang).astype(dtype)


def apply_rope(x, cos, sin):
    half = x.shape[-1] // 2
    x1, x2 = x[..., :half], x[..., half:]
    shp = (1, x.shape[1]) + (1,) * (x.ndim - 3) + (half,)
    c, s = cos.reshape(shp), sin.reshape(shp)
    return jnp.concatenate([x1 * c - x2 * s, x1 * s + x2 * c], axis=-1)


def attend(q, k, v):
    scale = q.shape[-1] ** -0.5
    s = jnp.einsum('bqhgd,bkhd->bhgqk', q, k, preferred_element_type=jnp.float32) * scale
    p = jax.nn.softmax(s, axis=-1).astype(v.dtype)
    return jnp.einsum('bhgqk,bkhd->bqhgd', p, v)


def blocked_attend(q, k, v):
    B, T = q.shape[:2]
    nb = T // Q_BLOCK
    qb = jnp.moveaxis(q.reshape((B, nb, Q_BLOCK) + q.shape[2:]), 1, 0)
    ob = lax.map(lambda qi: attend(qi, k, v), qb)
    return jnp.moveaxis(ob, 0, 1).reshape((B, T) + ob.shape[3:])


def gqa_axial_mixer(h_lat, h_ctx, w_qkv, q_norm, k_norm, w_o, need_ctx):
    G = A_HEADS // A_KV_HEADS
    nq = A_HEADS * A_HEAD_DIM
    nk = A_KV_HEADS * A_HEAD_DIM

    def project(h):
        B, T, _ = h.shape
        qkv = h @ w_qkv
        q = qkv[..., :nq].reshape(B, T, A_KV_HEADS, G, A_HEAD_DIM)
        k = qkv[..., nq:nq + nk].reshape(B, T, A_KV_HEADS, A_HEAD_DIM)
        v = qkv[..., nq + nk:].reshape(B, T, A_KV_HEADS, A_HEAD_DIM)
        return rms_norm(q, q_norm), rms_norm(k, k_norm), v

    B, T = h_lat.shape[:2]
    q_l, k_l, v_l = project(h_lat)
    q_c, k_c, v_c = project(h_ctx)
    cos, sin = axial_rope_tables(T, A_HEAD_DIM, h_lat.dtype)
    q_l = apply_rope(q_l, cos, sin)
    k_l = apply_rope(k_l, cos, sin)
    k_all = jnp.concatenate([k_c, k_l], axis=1)
    v_all = jnp.concatenate([v_c, v_l], axis=1)
    out_l = blocked_attend(q_l, k_all, v_all).reshape(B, T, nq) @ w_o
    out_c = None
    if need_ctx:
        out_c = attend(q_c, k_c, v_c).reshape(B, h_ctx.shape[1], nq) @ w_o
    return out_l, out_c


def mla_mixer(h_lat, h_ctx, w_dq, q_lat_norm, w_uq, w_dkv, kv_lat_norm, w_ukv, q_norm, k_norm, w_o, need_ctx):
    qd = B_NOPE_DIM + B_ROPE_DIM

    def project(h):
        B, T, _ = h.shape
        cq = rms_norm(h @ w_dq, q_lat_norm)
        q = (cq @ w_uq).reshape(B, T, B_HEADS, qd)
        kv_a = h @ w_dkv
        ckv = rms_norm(kv_a[..., :B_KV_RANK], kv_lat_norm)
        k_rope = kv_a[..., B_KV_RANK:]
        kv = (ckv @ w_ukv).reshape(B, T, B_HEADS, B_NOPE_DIM + B_V_DIM)
        q_nope = rms_norm(q[..., :B_NOPE_DIM], q_norm[:B_NOPE_DIM])
        q_rope = rms_norm(q[..., B_NOPE_DIM:], q_norm[B_NOPE_DIM:])
        k_nope = rms_norm(kv[..., :B_NOPE_DIM], k_norm[:B_NOPE_DIM])
        k_rope = rms_norm(k_rope, k_norm[B_NOPE_DIM:])
        return q_nope, q_rope, k_nope, k_rope, kv[..., B_NOPE_DIM:]

    def assemble(q_nope, q_rope, k_nope, k_rope, v):
        q = jnp.concatenate([q_nope, q_rope], axis=-1)[:, :, :, None, :]
        k_rope_h = jnp.broadcast_to(k_rope[:, :, None, :], k_nope.shape[:3] + (B_ROPE_DIM,))
        k = jnp.concatenate([k_nope, k_rope_h], axis=-1)
        return q, k, v

    B, T = h_lat.shape[:2]
    q_nope_l, q_rope_l, k_nope_l, k_rope_l, v_l = project(h_lat)
    cos, sin = axial_rope_tables(T, B_ROPE_DIM, h_lat.dtype)
    q_l, k_l, v_l = assemble(q_nope_l, apply_rope(q_rope_l, cos, sin), k_nope_l, apply_rope(k_rope_l, cos, sin), v_l)
    q_c, k_c, v_c = assemble(*project(h_ctx))
    k_all = jnp.concatenate([k_c, k_l], axis=1)
    v_all = jnp.concatenate([v_c, v_l], axis=1)
    out_l = blocked_attend(q_l, k_all, v_all).reshape(B, T, B_HEADS * B_V_DIM) @ w_o
    out_c = None
    if need_ctx:
        out_c = attend(q_c, k_c, v_c).reshape(B, h_ctx.shape[1], B_HEADS * B_V_DIM) @ w_o
    return out_l, out_c


def neighbourhood_mixer(h_lat, h_ctx, w_qkv, q_norm, k_norm, rpb, w_o, need_ctx):
    hd = C_HEADS * C_HEAD_DIM

    def project(h):
        B, T, _ = h.shape
        qkv = h @ w_qkv
        q = qkv[..., :hd].reshape(B, T, C_HEADS, C_HEAD_DIM)
        k = qkv[..., hd:2 * hd].reshape(B, T, C_HEADS, C_HEAD_DIM)
        v = qkv[..., 2 * hd:].reshape(B, T, C_HEADS, C_HEAD_DIM)
        return rms_norm(q, q_norm), rms_norm(k, k_norm), v

    B, T = h_lat.shape[:2]
    q_l, k_l, v_l = project(h_lat)
    q_c, k_c, v_c = project(h_ctx)
    rows = T // GRID_W
    kr = min(NA_ROWS, rows)
    kc = min(NA_COLS, GRID_W)
    scale = C_HEAD_DIM ** -0.5
    k_grid = k_l.reshape(B, rows, GRID_W, C_HEADS, C_HEAD_DIM)
    v_grid = v_l.reshape(B, rows, GRID_W, C_HEADS, C_HEAD_DIM)
    q_rows = jnp.moveaxis(q_l.reshape(B, rows, GRID_W, C_HEADS, C_HEAD_DIM), 1, 0)
    col = jnp.arange(GRID_W)
    col_start = jnp.clip(col - kc // 2, 0, GRID_W - kc)
    col_mask = (col[None, :] >= col_start[:, None]) & (col[None, :] < col_start[:, None] + kc)
    win_mask = jnp.tile(col_mask, (1, kr))
    dc_idx = jnp.clip(col[None, :] - col[:, None] + NA_COLS - 1, 0, 2 * NA_COLS - 2)
    rpb_f = rpb.astype(jnp.float32)
    n_win = kr * GRID_W

    def row_fn(args):
        r, q_row = args
        rs = jnp.clip(r - kr // 2, 0, rows - kr)
        k_band = lax.dynamic_slice_in_dim(k_grid, rs, kr, axis=1).reshape(B, n_win, C_HEADS, C_HEAD_DIM)
        v_band = lax.dynamic_slice_in_dim(v_grid, rs, kr, axis=1).reshape(B, n_win, C_HEADS, C_HEAD_DIM)
        dr_idx = rs + jnp.arange(kr) - r + NA_ROWS - 1
        bias = rpb_f[:, dr_idx[None, :, None], dc_idx[:, None, :]].reshape(C_HEADS, GRID_W, n_win)
        s_win = jnp.einsum('bqhd,bkhd->bhqk', q_row, k_band, preferred_element_type=jnp.float32) * scale + bias[None]
        s_win = jnp.where(win_mask[None, None], s_win, NEG_INF)
        s_ctx = jnp.einsum('bqhd,bkhd->bhqk', q_row, k_c, preferred_element_type=jnp.float32) * scale
        p = jax.nn.softmax(jnp.concatenate([s_win, s_ctx], axis=-1), axis=-1).astype(v_l.dtype)
        return (jnp.einsum('bhqk,bkhd->bqhd', p[..., :n_win], v_band)
                + jnp.einsum('bhqk,bkhd->bqhd', p[..., n_win:], v_c))

    o_l = lax.map(row_fn, (jnp.arange(rows, dtype=jnp.int32), q_rows))
    out_l = jnp.moveaxis(o_l, 0, 1).reshape(B, T, hd) @ w_o
    out_c = None
    if need_ctx:
        out_c = attend(q_c[:, :, :, None, :], k_c, v_c).reshape(B, h_ctx.shape[1], hd) @ w_o
    return out_l, out_c


def expert_choice_ffn(h, router, w_gate, w_up, w_down):
    B, T, _ = h.shape
    cap = max(1, min(T, EC_CAPACITY_FACTOR * T // N_EXPERTS))
    aff = jax.nn.softmax((h @ router).astype(jnp.float32), axis=-1)
    gate, idx = lax.top_k(jnp.swapaxes(aff, 1, 2), cap)
    bidx = jnp.arange(B)[:, None, None]
    xg = h[bidx, idx]
    hid = jax.nn.silu(jnp.einsum('becd,edf->becf', xg, w_gate)) * jnp.einsum('becd,edf->becf', xg, w_up)
    y = jnp.einsum('becf,efd->becd', hid, w_down) * gate[..., None].astype(h.dtype)
    return jnp.zeros_like(h).at[bidx, idx].add(y)


def setup_inputs(seed: int = 0) -> dict:
    key = jax.random.key(seed)
    ks = iter(jax.random.split(key, 40))
    D = D_MODEL

    def nrm(shape, fan_in, scale=1.0):
        return jax.random.normal(next(ks), shape, jnp.float32) * (scale * fan_in ** -0.5)

    def gain(shape):
        return 1.0 + 0.1 * jax.random.normal(next(ks), shape, jnp.float32)

    return {
        "x": jax.random.normal(next(ks), (BATCH, SEQ, D), jnp.float32),
        "c": jax.random.normal(next(ks), (BATCH, D), jnp.float32),
        "ctx": jax.random.normal(next(ks), (BATCH, CTX_LEN, D), jnp.float32),
        "c_ctx": jax.random.normal(next(ks), (D,), jnp.float32),
        "ada_w": nrm((DEPTH, D, 6 * D), D, 0.5),
        "ada_b": 0.02 * jax.random.normal(next(ks), (DEPTH, 6 * D), jnp.float32),
        "norm1_w": gain((DEPTH, D)),
        "norm2_w": gain((DEPTH, D)),
        "a_wqkv": nrm((N_A, D, (A_HEADS + 2 * A_KV_HEADS) * A_HEAD_DIM), D),
        "a_qnorm": gain((N_A, A_HEAD_DIM)),
        "a_knorm": gain((N_A, A_HEAD_DIM)),
        "a_wo": nrm((N_A, A_HEADS * A_HEAD_DIM, D), A_HEADS * A_HEAD_DIM),
        "b_wdq": nrm((N_B, D, B_Q_RANK), D),
        "b_qnorm_lat": gain((N_B, B_Q_RANK)),
        "b_wuq": nrm((N_B, B_Q_RANK, B_HEADS * (B_NOPE_DIM + B_ROPE_DIM)), B_Q_RANK),
        "b_wdkv": nrm((N_B, D, B_KV_RANK + B_ROPE_DIM), D),
        "b_kvnorm_lat": gain((N_B, B_KV_RANK)),
        "b_wukv": nrm((N_B, B_KV_RANK, B_HEADS * (B_NOPE_DIM + B_V_DIM)), B_KV_RANK),
        "b_qnorm": gain((N_B, B_NOPE_DIM + B_ROPE_DIM)),
        "b_knorm": gain((N_B, B_NOPE_DIM + B_ROPE_DIM)),
        "b_wo": nrm((N_B, B_HEADS * B_V_DIM, D), B_HEADS * B_V_DIM),
        "c_wqkv": nrm((N_C, D, 3 * C_HEADS * C_HEAD_DIM), D),
        "c_qnorm": gain((N_C, C_HEAD_DIM)),
        "c_knorm": gain((N_C, C_HEAD_DIM)),
        "c_rpb": 0.1 * jax.random.normal(next(ks), (N_C, C_HEADS, 2 * NA_ROWS - 1, 2 * NA_COLS - 1), jnp.float32),
        "c_wo": nrm((N_C, C_HEADS * C_HEAD_DIM, D), C_HEADS * C_HEAD_DIM),
        "moe_router": nrm((DEPTH, D, N_EXPERTS), D),
        "moe_wg": nrm((DEPTH, N_EXPERTS, D, EXPERT_FF), D),
        "moe_wu": nrm((DEPTH, N_EXPERTS, D, EXPERT_FF), D),
        "moe_wd": nrm((DEPTH, N_EXPERTS, EXPERT_FF, D), EXPERT_FF),
    }


def reference(x, c, ctx, c_ctx, ada_w, ada_b, norm1_w, norm2_w,
              a_wqkv, a_qnorm, a_knorm, a_wo,
              b_wdq, b_qnorm_lat, b_wuq, b_wdkv, b_kvnorm_lat, b_wukv, b_qnorm, b_knorm, b_wo,
              c_wqkv, c_qnorm, c_knorm, c_rpb, c_wo,
              moe_router, moe_wg, moe_wu, moe_wd):
    s_lat = jax.nn.silu(c)
    s_ctx = jax.nn.silu(c_ctx)[None]
    for i in range(DEPTH):
        need_ctx = i < DEPTH - 1
        m_l = jnp.split(s_lat @ ada_w[i] + ada_b[i], 6, axis=-1)
        m_c = jnp.split(s_ctx @ ada_w[i] + ada_b[i], 6, axis=-1)
        h_l = modulate(rms_norm(x, norm1_w[i]), m_l[0], m_l[1])
        h_c = modulate(rms_norm(ctx, norm1_w[i]), m_c[0], m_c[1])
        kind, j = i % N_MIXERS, i // N_MIXERS
        if kind == 0:
            out_l, out_c = gqa_axial_mixer(h_l, h_c, a_wqkv[j], a_qnorm[j], a_knorm[j], a_wo[j], need_ctx)
        elif kind == 1:
            out_l, out_c = mla_mixer(h_l, h_c, b_wdq[j], b_qnorm_lat[j], b_wuq[j], b_wdkv[j], b_kvnorm_lat[j],
                                     b_wukv[j], b_qnorm[j], b_knorm[j], b_wo[j], need_ctx)
        else:
            out_l, out_c = neighbourhood_mixer(h_l, h_c, c_wqkv[j], c_qnorm[j], c_knorm[j], c_rpb[j], c_wo[j], need_ctx)
        x = x + m_l[2][:, None] * out_l
        x = x + m_l[5][:, None] * expert_choice_ffn(modulate(rms_norm(x, norm2_w[i]), m_l[3], m_l[4]),
                                                    moe_router[i], moe_wg[i], moe_wu[i], moe_wd[i])
        if need_ctx:
            ctx = ctx + m_c[2][:, None] * out_c
            ctx = ctx + m_c[5][:, None] * expert_choice_ffn(modulate(rms_norm(ctx, norm2_w[i]), m_c[3], m_c[4]),
                                                            moe_router[i], moe_wg[i], moe_wu[i], moe_wd[i])
    return x
```

```python
import numpy as np
import ml_dtypes
from contextlib import ExitStack
import concourse.bass as bass
import concourse.mybir as mybir
from concourse.bass_utils import run_bass_kernel_spmd

F32 = mybir.dt.float32
BF16 = mybir.dt.bfloat16
AF = mybir.ActivationFunctionType
ALU = mybir.AluOpType
AX = mybir.AxisListType

D = 1024
T = 2048
TC = 256
TT = 2304
NT = 18
NE = 16
CAP = 256
CAPC = 32
EPS = 1e-6
NEG = -30000.0
N_CORES = 8
SPC = 2

XT_OFF = 0
RING_OFF = 73728
UNIT = 8192
NUNIT = 5
HT_OFF = RING_OFF + UNIT * NUNIT
R_OFF = HT_OFF + 36864
R_SIZE = 45056
PERS_OFF = R_OFF + R_SIZE
ARENA_BYTES = 212000
R2_OFF = RING_OFF + 2 * UNIT
R2_SIZE = 3 * UNIT


def _dsize(dt):
    return 4 if dt == F32 else 2


class Prog:
    ENG = ("pe", "act", "dve", "pool", "sp")

    def __init__(self, nc):
        self.nc = nc
        self.eng = {"pe": nc.tensor, "act": nc.scalar, "dve": nc.vector, "pool": nc.gpsimd, "sp": nc.sync}
        self.ops = []
        self.eops = {e: [] for e in self.ENG}
        self.res = {}
        self.slots = {}
        self.pending = {e: set() for e in self.ENG}

    def _deps(self, eng, reads, writes):
        deps = set(self.pending[eng])
        self.pending[eng] = set()
        for r in reads:
            st = self.res.get(r)
            if st is not None and st["w"] is not None:
                deps.add(st["w"])
        for w in writes:
            st = self.res.get(w)
            if st is not None:
                if st["w"] is not None:
                    deps.add(st["w"])
                deps.update(st["r"].values())
        return deps

    def _mark(self, tok, key, reads, writes):
        for r in reads:
            st = self.res.get(r)
            if st is None:
                st = self.res[r] = {"w": None, "r": {}}
            st["r"][key] = tok
        for w in writes:
            self.res[w] = {"w": tok, "r": {}}

    def op(self, eng, fn, reads=(), writes=()):
        deps = self._deps(eng, reads, writes)
        idx = len(self.eops[eng])
        rec = {"eng": eng, "kind": "op", "fn": fn, "deps": deps, "idx": idx, "sig": False}
        self.eops[eng].append(rec)
        self.ops.append(rec)
        self._mark(("e", eng, idx), eng, reads, writes)

    def dma(self, queue, slot, out, in_, reads=(), writes=(), group=False, bar=True, **kw):
        deps = self._deps(queue, reads, writes)
        s = self.slots.get(slot)
        if s is None:
            s = self.slots[slot] = {"n": 0, "groups": [], "bar": bar}
        s["n"] += 1
        n = s["n"]
        if group and s["groups"]:
            s["groups"][-1] = n
        else:
            if n > 1:
                deps.add(("d", slot, n - 1))
            s["groups"].append(n)
        idx = len(self.eops[queue])
        rec = {"eng": queue, "kind": "dma", "slot": slot, "n": n, "out": out, "in_": in_, "deps": deps,
               "idx": idx, "kw": kw, "sig": False}
        self.eops[queue].append(rec)
        self.ops.append(rec)
        self._mark(("d", slot, n), ("d", slot), reads, writes)

    def barrier(self):
        toks = set()
        for e in ("pe", "act", "dve", "pool"):
            if self.eops[e]:
                for rec in reversed(self.eops[e]):
                    if rec["kind"] == "op":
                        toks.add(("e", e, rec["idx"]))
                        break
        for name, s in self.slots.items():
            if s["bar"] and s["n"] > 0:
                toks.add(("d", name, s["n"]))
        for e in ("pe", "act", "dve", "pool", "sp"):
            self.pending[e] |= {t for t in toks if not (t[0] == "e" and t[1] == e == "pe")}

    def _gend(self, slot, n):
        for g in self.slots[slot]["groups"]:
            if g >= n:
                return g
        raise AssertionError

    def emit(self, es):
        nc = self.nc
        for rec in self.ops:
            for d in rec["deps"]:
                if d[0] == "e":
                    if d[1] == rec["eng"] == "pe":
                        continue
                    self.eops[d[1]][d[2]]["sig"] = True
        for e in self.ENG:
            c = 0
            for rec in self.eops[e]:
                if rec["sig"]:
                    c += 1
                rec["sigval"] = c
        sem = {}
        for e in self.ENG:
            sem[("e", e)] = es.enter_context(nc.semaphore("g_" + e))
        for name in self.slots:
            sem[("d", name)] = es.enter_context(nc.semaphore("d_" + name))
        waited = {e: {} for e in self.ENG}
        nwait = 0
        for rec in self.ops:
            e = rec["eng"]
            engine = self.eng[e]
            need = {}
            for d in rec["deps"]:
                if d[0] == "e":
                    if d[1] == e == "pe":
                        continue
                    key = ("e", d[1])
                    val = self.eops[d[1]][d[2]]["sigval"]
                else:
                    if rec["kind"] == "dma" and d[1] == rec["slot"] and self._gend(d[1], d[2]) == self._gend(rec["slot"], rec["n"]):
                        continue
                    key = ("d", d[1])
                    val = 16 * self._gend(d[1], d[2])
                if need.get(key, 0) < val:
                    need[key] = val
            for key, val in need.items():
                if waited[e].get(key, 0) < val:
                    engine.wait_ge(sem[key], val)
                    waited[e][key] = val
                    nwait += 1
            if rec["kind"] == "op":
                inst = rec["fn"](engine)
                if rec["sig"]:
                    inst.then_inc(sem[("e", e)], 1)
            else:
                inst = engine.dma_start(out=rec["out"], in_=rec["in_"], **rec["kw"])
                inst.then_inc(sem[("d", rec["slot"])], 16)
        sp = self.eng["sp"]
        for name, s in self.slots.items():
            if s["n"] > 0:
                sp.wait_ge(sem[("d", name)], 16 * s["n"])
        self.stats = {e: len(self.eops[e]) for e in self.ENG}
        self.stats["waits"] = nwait


class Region:
    def __init__(self, off, size):
        self.off, self.size, self.cur = off, size, 0

    def reset(self):
        self.cur = 0

    def take(self, nbytes):
        nbytes = (nbytes + 31) // 32 * 32
        o = self.off + self.cur
        self.cur += nbytes
        assert self.cur <= self.size, (self.cur, self.size)
        return o


class Buf:
    def __init__(self, name, ap):
        self.name = name
        self.ap = ap

    def __getitem__(self, idx):
        return self.ap[idx]

    def r(self, *k):
        return (self.name,) + k


GROUPS = [(0, 512), (512, 512), (1024, 512), (1536, 512), (2048, 256)]


def na_tables(rpb):
    blocks = [(5, 5 + r) for r in (-2, -1, 0, 1, 2)]
    for qi in (0, 1):
        blocks += [(qi, kt) for kt in range(4)]
    for qi in (14, 15):
        blocks += [(qi, kt) for kt in range(12, 16)]
    nb = len(blocks)
    H = rpb.shape[0]
    bias = np.zeros((H, 128, nb, 128), np.float32)
    mask = np.zeros((128, nb, 128), np.float32)
    kk = np.arange(128)
    for bi, (qi, kt) in enumerate(blocks):
        k_r = 2 * kt + kk // 64
        k_c = kk % 64
        q_r = 2 * qi + kk // 64
        q_c = kk % 64
        rs = np.clip(q_r - 4, 0, 24)
        cs = np.clip(q_c - 8, 0, 48)
        ok = ((k_r[:, None] >= rs[None, :]) & (k_r[:, None] < rs[None, :] + 8)
              & (k_c[:, None] >= cs[None, :]) & (k_c[:, None] < cs[None, :] + 16))
        dr = np.clip(k_r[:, None] - q_r[None, :] + 7, 0, 14)
        dc = np.clip(k_c[:, None] - q_c[None, :] + 15, 0, 30)
        bias[:, :, bi, :] = rpb[:, dr, dc]
        mask[:, bi, :] = np.where(ok, 0.0, NEG)
    return bias.reshape(H, 128, nb * 128), mask.reshape(128, nb * 128)


def na_block_ids(qi):
    if 2 <= qi <= 13:
        return [(qi + r, 2 + r) for r in (-2, -1, 0, 1, 2)]
    if qi in (0, 1):
        return [(kt, 5 + 4 * qi + kt) for kt in range(4)]
    base = 13 if qi == 14 else 17
    return [(kt, base + (kt - 12)) for kt in range(12, 16)]


def rope_tables(rot_dim):
    n_freq = rot_dim // 4
    inv = np.float32(10000.0) ** (-np.arange(n_freq, dtype=np.float32) / np.float32(n_freq))
    t = np.arange(T, dtype=np.int32)
    row = (t // 64).astype(np.float32)
    col = (t % 64).astype(np.float32)
    ang = np.concatenate([row[:, None] * inv, col[:, None] * inv], axis=-1).astype(np.float32)
    return np.cos(ang).astype(np.float32), np.sin(ang).astype(np.float32)


class Builder:
    def __init__(self, cfg):
        self.cfg = cfg
        nc = self.nc = bass.Bass("TRN2", target_bir_lowering=False)
        self.es = ExitStack()
        self.P = Prog(nc)
        self.dbg_outs = []
        self._dram()
        self.arena = self.es.enter_context(nc.sbuf_tensor("arena", [128, ARENA_BYTES // 4], F32))
        self.psb = [self.es.enter_context(nc.psum_tensor("psb%d" % i, [128, 512], F32)) for i in range(8)]
        self.R = Region(R_OFF, R_SIZE)
        self.R2 = Region(R2_OFF, R2_SIZE)
        self.HTR = Region(HT_OFF, 36864)
        self.PERS = Region(PERS_OFF, ARENA_BYTES - PERS_OFF)
        self.unit_ctr = 0
        self.ot_ctr = 0
        self.op_ctr = 0
        self._persistent()

    def _dram(self):
        nc = self.nc

        def inp(name, shape, dt=F32):
            return nc.dram_tensor(name, list(shape), dt, kind="ExternalInput").ap()
        self.d = d = {}
        d["x2"] = inp("x2", [SPC, T, D])
        d["ctx2"] = inp("ctx2", [SPC, TC, D])
        d["cvec"] = inp("cvec", [3, D])
        d["ada_w"] = inp("ada_w", [4, D, 6 * D])
        d["ada_b"] = inp("ada_b", [4, 6 * D])
        d["norm1_w"] = inp("norm1_w", [4, D])
        d["norm2_w"] = inp("norm2_w", [4, D])
        d["a_wqkv"] = inp("a_wqkv", [2, D, 1536])
        d["a_qnorm"] = inp("a_qnorm", [2, 64])
        d["a_knorm"] = inp("a_knorm", [2, 64])
        d["a_wo"] = inp("a_wo", [2, D, D])
        d["b_wdq"] = inp("b_wdq", [1, D, 384])
        d["b_qnorm_lat"] = inp("b_qnorm_lat", [1, 384])
        d["b_wuq"] = inp("b_wuq", [1, 384, 1536])
        d["b_wdkv"] = inp("b_wdkv", [1, D, 288])
        d["b_kvnorm_lat"] = inp("b_kvnorm_lat", [1, 256])
        d["b_wukv"] = inp("b_wukv", [1, 256, 2048])
        d["b_qnorm"] = inp("b_qnorm", [1, 96])
        d["b_knorm"] = inp("b_knorm", [1, 96])
        d["b_wo"] = inp("b_wo", [1, D, D])
        d["c_wqkv"] = inp("c_wqkv", [1, D, 3072])
        d["c_qnorm"] = inp("c_qnorm", [1, 64])
        d["c_knorm"] = inp("c_knorm", [1, 64])
        d["c_wo"] = inp("c_wo", [1, D, D])
        d["moe_router"] = inp("moe_router", [4, D, NE])
        d["moe_wg"] = inp("moe_wg", [4, NE, D, D])
        d["moe_wu"] = inp("moe_wu", [4, NE, D, D])
        d["moe_wd"] = inp("moe_wd", [4, NE, D, D])
        d["cst"] = inp("cst", [128, 388])
        d["selc"] = inp("selc", [16, 2048], BF16)
        d["cosA"] = inp("cosA", [T, 32])
        d["sinA"] = inp("sinA", [T, 32])
        d["cosB"] = inp("cosB", [T, 16])
        d["sinB"] = inp("sinB", [T, 16])
        d["na_bias"] = inp("na_bias", [16, 128, 21 * 128])
        d["na_mask"] = inp("na_mask", [128, 21 * 128])
        self.y2 = nc.dram_tensor("y2", [SPC, T, D], F32, kind="ExternalOutput").ap()

    def dbg_out(self, name, shape, dt=F32):
        ap = self.nc.dram_tensor("dbg_" + name, list(shape), dt, kind="ExternalOutput").ap()
        self.dbg_outs.append("dbg_" + name)
        return ap

    def carve(self, name, off, shape, dt):
        n = int(np.prod(shape[1:]))
        nbytes = n * _dsize(dt)
        assert off % 4 == 0 and nbytes % 4 == 0, (name, off, nbytes)
        assert off + nbytes <= ARENA_BYTES, (name, off, nbytes)
        ap = self.arena[0:shape[0], off // 4:(off + nbytes) // 4]
        if dt != F32:
            ap = ap.bitcast(dt)
        if len(shape) == 3:
            ap = ap.rearrange("p (a b) -> p a b", b=shape[2])
        elif len(shape) == 4:
            ap = ap.rearrange("p (a b c) -> p a b c", b=shape[2], c=shape[3])
        elif len(shape) == 5:
            ap = ap.rearrange("p (a b c d) -> p a b c d", b=shape[2], c=shape[3], d=shape[4])
        return Buf(name, ap)

    def alloc(self, region, name, shape, dt):
        n = int(np.prod(shape[1:])) * _dsize(dt)
        return self.carve(name, region.take(n), shape, dt)

    def psbf(self, i):
        return self.psb[i][:, :].bitcast(BF16)

    def mm(self, out, lhsT, rhs, start=True, stop=True, R=(), W=()):
        self.P.op("pe", lambda e: e.matmul(out, lhsT, rhs, start=start, stop=stop), R, W)

    def tr(self, out, in_, ident, R=(), W=()):
        self.P.op("pe", lambda e: e.transpose(out, in_, ident), R, W)

    def act(self, out, in_, func, bias=None, scale=None, accum=None, R=(), W=()):
        kw = {}
        if bias is not None:
            kw["bias"] = bias
        if scale is not None:
            kw["scale"] = scale
        if accum is not None:
            kw["accum_out"] = accum
        self.P.op("act", lambda e: e.activation(out=out, in_=in_, func=func, **kw), R, W)

    def tt(self, eng, out, in0, in1, op, R=(), W=()):
        self.P.op(eng, lambda e: e.tensor_tensor(out=out, in0=in0, in1=in1, op=op), R, W)

    def ts(self, eng, out, in0, s1, op0, s2=None, op1=None, R=(), W=()):
        if op1 is None:
            self.P.op(eng, lambda e: e.tensor_scalar(out=out, in0=in0, scalar1=s1, scalar2=None, op0=op0), R, W)
        else:
            self.P.op(eng, lambda e: e.tensor_scalar(out=out, in0=in0, scalar1=s1, scalar2=s2, op0=op0, op1=op1), R, W)

    def stt(self, out, in0, scalar, in1, op0, op1, R=(), W=()):
        self.P.op("dve", lambda e: e.scalar_tensor_tensor(out=out, in0=in0, scalar=scalar, in1=in1, op0=op0, op1=op1), R, W)

    def cp(self, eng, out, in_, R=(), W=()):
        if eng == "act":
            self.P.op("act", lambda e: e.activation(out=out, in_=in_, func=AF.Copy), R, W)
        else:
            self.P.op(eng, lambda e: e.tensor_copy(out=out, in_=in_), R, W)

    def red(self, out, in_, op, R=(), W=()):
        self.P.op("dve", lambda e: e.tensor_reduce(out=out, in_=in_, axis=AX.X, op=op), R, W)

    def memset(self, eng, ap, val, R=(), W=()):
        self.P.op(eng, lambda e: e.memset(ap, val), R, W)

    def dma(self, queue, slot, out, in_, R=(), W=(), group=False, bar=True, **kw):
        self.P.dma(queue, slot, out, in_, R, W, group=group, bar=bar, **kw)

    def _persistent(self):
        A = self.alloc
        PR = self.PERS
        self.xT = self.carve("xT", XT_OFF, [128, 8, TT], F32)
        self.ring = [self.carve("ring%d" % i, RING_OFF + i * UNIT, [128, 8, 512], BF16) for i in range(NUNIT)]
        self.cstb = A(PR, "cstb", [128, 388], F32)
        self.identf = self.cstb[:, 0:128]
        self.iotac = self.cstb[:, 128:384]
        self.misc = self.cstb[:, 384:388]
        self.identb = A(PR, "identb", [128, 128], BF16)
        self.onesb = A(PR, "onesb", [128, 128], BF16)
        self.sel = A(PR, "sel", [16, 16, 128], BF16)
        self.modv = A(PR, "modv", [128, 4, 3, 6, 8], F32)
        self.cosA = A(PR, "cosA", [128, 16, 32], F32)
        self.sinA = A(PR, "sinA", [128, 16, 32], F32)
        self.cosB = A(PR, "cosB", [128, 16, 16], F32)
        self.sinB = A(PR, "sinB", [128, 16, 16], F32)
        self.smallp = A(PR, "smallp", [128, 64], F32)

    def prologue(self):
        d = self.d
        self.dma("sp", "c0", self.cstb[:, :], d["cst"][:, :], W=[("cst",)])
        self.dma("sp", "c0", self.sel[:, :, :], d["selc"].rearrange("k (e m) -> k e m", m=128), W=[("sel",)], group=True)
        for nm, buf in (("cosA", self.cosA), ("sinA", self.sinA), ("cosB", self.cosB), ("sinB", self.sinB)):
            self.dma("sp", "c0", buf[:, :, :], d[nm].rearrange("(t p) n -> p t n", p=128), W=[(nm,)], group=True)
        self.cp("dve", self.identb[:, :], self.identf, R=[("cst",)], W=[("identb",)])
        self.memset("pool", self.onesb[:, :], 1.0, W=[("onesb",)])
        R = self.R
        R.reset()
        vr = self.alloc(R, "vr", [128, 128], F32)
        vr2 = self.alloc(R, "vr2", [128, 128], F32)
        vr3 = self.alloc(R, "vr3", [128, 128], F32)
        vT = self.alloc(R, "vT", [128, 88], F32)
        abT = self.alloc(R, "abT", [128, 192], F32)
        sT = self.alloc(R, "sT", [128, 8, 4], F32)
        modraw = self.alloc(R, "modraw", [128, 4, 48, 3], F32)
        wslot = [self.carve("adaw0", HT_OFF, [128, 8, 768], F32),
                 self.alloc(R, "adaw1", [128, 8, 768], F32)]
        self.dma("sp", "p0", vr[0:24, :], d["cvec"].rearrange("s (c p) -> (s c) p", p=128), W=[("vr",)])
        self.dma("sp", "p0", vr[24:56, :], d["norm1_w"].rearrange("l (c p) -> (l c) p", p=128), W=[("vr",)], group=True)
        self.dma("sp", "p0", vr[56:88, :], d["norm2_w"].rearrange("l (c p) -> (l c) p", p=128), W=[("vr",)], group=True)
        abrows = d["ada_b"].rearrange("l (k p) -> (l k) p", p=128)
        self.dma("sp", "p0", vr2[:, :], abrows[0:128, :], W=[("vr2",)], group=True)
        self.dma("sp", "p0", vr3[0:64, :], abrows[128:192, :], W=[("vr3",)], group=True)
        ps = self.psb[0]
        self.tr(ps[:, 0:88], vr[0:88, :], self.identf[0:88, 0:88], R=[("vr",), ("cst",)], W=[("ps", 0)])
        self.cp("dve", vT[:, :], ps[:, 0:88], R=[("ps", 0)], W=[("vT",)])
        ps1 = self.psb[1]
        self.tr(ps1[:, 0:128], vr2[:, :], self.identf, R=[("vr2",), ("cst",)], W=[("ps", 1)])
        self.tr(ps1[:, 128:192], vr3[0:64, :], self.identf[0:64, 0:64], R=[("vr3",), ("cst",)], W=[("ps", 1)])
        self.cp("dve", abT[:, :], ps1[:, 0:192], R=[("ps", 1)], W=[("abT",)])
        self.memset("dve", sT[:, :, :], 0.0, W=[("sT",)])
        cTv = vT[:, 0:24].rearrange("p (s c) -> p c s", c=8)
        self.act(sT[:, :, 0:3], cTv, AF.Silu, R=[("vT",)], W=[("sT",)])
        n1 = vT[:, 24:56].rearrange("p (l c) -> p l c", c=8)
        n2 = vT[:, 56:88].rearrange("p (l c) -> p l c", c=8)
        for l in range(4):
            mps = self.psb[2 + (l % 2)]
            for cb in range(8):
                u = l * 8 + cb
                w = wslot[u % 2]
                self.dma("sp", "aw%d" % (u % 2), w[:, :, :],
                         d["ada_w"][l].rearrange("(c p) f -> p c f", p=128)[:, :, cb * 768:(cb + 1) * 768],
                         W=[("adaw", u % 2)])
                for fc in range(6):
                    col = (cb * 6 + fc) * 4
                    for dc in range(8):
                        self.mm(mps[:, col:col + 4], w[:, dc, fc * 128:(fc + 1) * 128], sT[:, dc, :],
                                start=(dc == 0), stop=(dc == 7),
                                R=[("adaw", u % 2), ("sT",)], W=[("ps", 2 + (l % 2))])
            mv = mps[:, 0:192].rearrange("p (k s) -> p k s", s=4)[:, :, 0:3]
            bv = abT[:, l * 48:(l + 1) * 48].unsqueeze(2).to_broadcast([128, 48, 3])
            self.tt("dve", modraw[:, l, :, :], mv, bv, ALU.add, R=[("ps", 2 + (l % 2)), ("abT",)], W=[("modraw", l)])
            mr = modraw[:, l, :, :].rearrange("p (k c) s -> p k s c", c=8)
            for kind, k in ((1, 0), (2, 2), (4, 3), (5, 5)):
                self.cp("dve", self.modv[:, l, :, kind, :], mr[:, k, :, :], R=[("modraw", l)], W=[("modv",)])
            for kind, k, nn in ((0, 1, n1), (3, 4, n2)):
                nb = nn[:, l, :].unsqueeze(1).to_broadcast([128, 3, 8])
                self.stt(self.modv[:, l, :, kind, :], mr[:, k, :, :], 1.0, nb, ALU.add, ALU.mult,
                         R=[("modraw", l), ("vT",)], W=[("modv",)])
        self.P.barrier()

    def mod(self, l, s, kind, c=None):
        if c is None:
            return self.modv[:, l, s, kind, :]
        return self.modv[:, l, s, kind, c:c + 1]

    def load_sample(self, b):
        R = self.R
        R.reset()
        stg = [self.alloc(R, "stg%d" % i, [128, D], F32) for i in range(2)]
        for t in range(NT):
            src = self.d["x2"][b, t * 128:(t + 1) * 128, :] if t < 16 else self.d["ctx2"][b, (t - 16) * 128:(t - 15) * 128, :]
            s = stg[t % 2]
            self.dma("sp", "ld%d" % (t % 2), s[:, :], src, W=[("stg", t % 2)])
            for half in range(2):
                bank = (t % 2) * 2 + half
                ps = self.psb[bank]
                for j in range(4):
                    c = half * 4 + j
                    self.tr(ps[:, j * 128:(j + 1) * 128], s[:, c * 128:(c + 1) * 128], self.identf,
                            R=[("stg", t % 2), ("cst",)], W=[("ps", bank)])
                self.cp("act" if half == 0 else "dve", self.xT[:, half * 4:(half + 1) * 4, t * 128:(t + 1) * 128],
                        ps[:, :].rearrange("p (j n) -> p j n", n=128),
                        R=[("ps", bank)], W=[("xT", min(t // 4, 4), half * 4 + j) for j in range(4)])
        self.P.barrier()

    def store_sample(self, dst, ntiles):
        R = self.R
        R.reset()
        stg = [self.alloc(R, "ostg%d" % i, [128, D], F32) for i in range(2)]
        for t in range(ntiles):
            s = stg[t % 2]
            for half in range(2):
                bank = (t % 2) * 2 + half
                ps = self.psb[bank]
                for j in range(4):
                    c = half * 4 + j
                    self.tr(ps[:, j * 128:(j + 1) * 128], self.xT[:, c, t * 128:(t + 1) * 128], self.identf,
                            R=[("xT", min(t // 4, 4), c), ("cst",)], W=[("ps", bank)])
                self.cp("act" if half == 0 else "dve", s[:, half * 512:(half + 1) * 512], ps[:, :],
                        R=[("ps", bank)], W=[("ostg", t % 2)])
            self.dma("sp", "st%d" % (t % 2), dst[t * 128:(t + 1) * 128, :], s[:, :], R=[("ostg", t % 2)])
        self.P.barrier()

    def norm_bufs(self):
        R = self.R
        self.sq = [self.alloc(R, "sq%d" % i, [128, 8, 512], BF16) for i in range(2)]
        self.lnv = self.alloc(R, "lnv", [128, 512], F32)
        self.rstd = [self.alloc(R, "rstd%d" % i, [128, 512], F32) for i in range(2)]
        self.tmpn = [self.alloc(R, "tmpn%d" % i, [128, 512], F32) for i in range(2)]

    def norm_group(self, gi, l, b, which, dst_of):
        t0, w = GROUPS[gi]
        s = b if gi < 4 else 2
        ka, kb = (0, 1) if which == 1 else (3, 4)
        sq = self.sq[gi % 2]
        self.act(sq[:, :, 0:w], self.xT[:, :, t0:t0 + w], AF.Square, R=[("xT", gi, c) for c in range(8)], W=[("sq", gi % 2)])
        bank = 5 + (gi % 2)
        ps = self.psb[bank]
        for c in range(8):
            self.mm(ps[:, 0:w], self.onesb[:, :], sq[:, c, 0:w], start=(c == 0), stop=(c == 7),
                    R=[("sq", gi % 2), ("onesb",)], W=[("ps", bank)])
        self.act(self.lnv[:, 0:w], ps[:, 0:w], AF.Ln, bias=self.misc[:, 2:3], scale=1.0 / D,
                 R=[("ps", bank), ("cst",)], W=[("lnv",)])
        rstd = self.rstd[gi % 2]
        self.act(rstd[:, 0:w], self.lnv[:, 0:w], AF.Exp, scale=-0.5, R=[("lnv",)], W=[("rstd", gi % 2)])
        for c in range(8):
            tm = self.tmpn[c % 2]
            self.tt("dve", tm[:, 0:w], self.xT[:, c, t0:t0 + w], rstd[:, 0:w], ALU.mult,
                    R=[("xT", gi, c), ("rstd", gi % 2)], W=[("tmpn", c % 2)])
            dst, dres = dst_of(c)
            self.ts("pool", dst, tm[:, 0:w], self.mod(l, s, ka, c), ALU.mult, self.mod(l, s, kb, c), ALU.add,
                    R=[("tmpn", c % 2), ("modv",)], W=[dres])

    def attn_core(self, steps_cfg, scale):
        steps = []
        for hi, hc in enumerate(steps_cfg):
            nk = len(hc["ktiles"])
            for ki, k in enumerate(hc["ktiles"]):
                steps.append((hi, hc, ki, k, ki == 0, ki == nk - 1))
        n = len(steps)

        def qk(i):
            hi, hc, ki, k, first, last = steps[i]
            bank = i % 3
            self.mm(self.psb[bank][:, 0:hc["w"]], hc["kt"][:, k * 128:(k + 1) * 128], hc["qt"][:, hc["t0"]:hc["t0"] + hc["w"]],
                    R=[hc["kres"], hc["qres"]], W=[("ps", bank)])

        def ex(i):
            hi, hc, ki, k, first, last = steps[i]
            bank = i % 3
            self.act(self.PT[:, bank, 0:hc["w"]], self.psb[bank][:, 0:hc["w"]], AF.Exp, scale=scale,
                     R=[("ps", bank)], W=[("pt", bank)])

        def pv(i):
            hi, hc, ki, k, first, last = steps[i]
            w = hc["w"]
            ob = 3 + (hi + self.ot_ctr) % 2
            self.mm(self.psb[ob][:, 0:w], hc["va"](k), self.PT[:, i % 3, 0:w], start=first, stop=last,
                    R=[("pt", i % 3), hc["vres"]], W=[("ps", ob)])
            if last:
                lo, hi_ = (slice(0, 64), slice(64, 128))
                o_sl, d_sl = (lo, hi_) if hc["o_lo"] else (hi_, lo)
                rec_o = self.rec[d_sl, 0:w]
                rec_i = self.psb[ob][d_sl, 0:w]
                self.P.op("dve", lambda e: e.reciprocal(out=rec_o, in_=rec_i), [("ps", ob)], [("rec",)])
                self.tt("dve", hc["att"][o_sl, hc["t0"]:hc["t0"] + w], self.psb[ob][o_sl, 0:w], self.rec[d_sl, 0:w], ALU.mult,
                        R=[("ps", ob), ("rec",)], W=[hc["ares"]])

        if n == 0:
            return
        qk(0)
        for i in range(n):
            if i + 1 < n:
                qk(i + 1)
            ex(i)
            pv(i)
        self.ot_ctr += len(steps_cfg)

    def out_proj(self, wo, wres, att, ares, l, b, need_ctx):
        ng = 5 if need_ctx else 4
        n = 0
        for gi in range(ng):
            t0, w = GROUPS[gi]
            s = b if gi < 4 else 2
            for dc in range(8):
                bank = 5 + (self.op_ctr % 2)
                self.op_ctr += 1
                ps = self.psb[bank]
                self.mm(ps[:, 0:w], wo[:, dc * 128:(dc + 1) * 128], att[:, t0:t0 + w], R=[wres, ares], W=[("ps", bank)])
                xs = self.xT[:, dc, t0:t0 + w]
                self.stt(xs, ps[:, 0:w], self.mod(l, s, 2, dc), xs, ALU.mult, ALU.add,
                         R=[("ps", bank), ("modv",), ("xT", gi, dc)], W=[("xT", gi, dc)])

    def head_rstd(self, src3, nh, hd, sqt, ssq, lnv, rs, res_src):
        self.tt("dve", sqt, src3, src3, ALU.mult, R=[res_src], W=[("sqt",)])
        self.red(ssq[:, 0:nh], sqt, ALU.add, R=[("sqt",)], W=[("ssq",)])
        self.act(lnv[:, 0:nh], ssq[:, 0:nh], AF.Ln, bias=self.misc[:, 2:3], scale=1.0 / hd, R=[("ssq",), ("cst",)], W=[("lnvh",)])
        self.act(rs[:, 0:nh], lnv[:, 0:nh], AF.Exp, scale=-0.5, R=[("lnvh",)], W=[("rsh",)])

    def bcast_row(self, queue, slot, dst, src_row, n, W, group=False):
        self.dma(queue, slot, dst, src_row.unsqueeze(0).to_broadcast([128, n]), W=W, group=group)

    def gqa_layer(self, l, b, need_ctx):
        d = self.d
        j = l // 3
        R, R2 = self.R, self.R2
        R.reset()
        R2.reset()
        self.norm_bufs()
        hT = self.carve("hT", HT_OFF, [128, 8, TT], BF16)
        for gi in range(5):
            t0, w = GROUPS[gi]
            self.norm_group(gi, l, b, 1, lambda c, t0=t0, w=w, gi=gi: (hT[:, c, t0:t0 + w], ("hT", gi)))
        self.P.barrier()
        R.reset()
        QKT = self.alloc(R, "QKT", [128, 5, TT], BF16)
        VA = self.alloc(R, "VA", [128, NT, 192], BF16)
        attT = self.alloc(R, "attT", [128, TT], BF16)
        Wg = self.alloc(R2, "Wg", [128, 8, 384], BF16)
        WoP = self.alloc(R2, "WoP", [128, 2, D], BF16)
        self.PT = self.alloc(R, "PT", [128, 3, 512], BF16)
        self.rec = self.alloc(R, "rec", [128, 512], F32)
        gq = self.alloc(R, "gq", [128, 5, 64], F32)
        raw = [self.alloc(R2, "raw%d" % i, [128, 384], F32) for i in range(2)]
        self.memset("pool", QKT[64:128, 0:2, :], 0.0, W=[("QKTz",)])
        self.memset("pool", QKT[0:64, 2:4, :], 0.0, W=[("QKTz",)])
        sqt = self.alloc(R2, "sqt", [128, 5, 64], F32)
        t1 = self.alloc(R2, "t1", [128, 5, 64], F32)
        t2 = self.alloc(R2, "t2", [128, 5, 64], F32)
        ra = self.alloc(R2, "ra", [128, 5, 32], F32)
        rb = self.alloc(R2, "rb", [128, 5, 32], F32)
        rc = self.alloc(R2, "rc", [128, 5, 32], F32)
        rd = self.alloc(R2, "rd", [128, 5, 32], F32)
        qkb = [self.alloc(R2, "qkb%d" % i, [128, 6, 64], BF16) for i in range(2)]
        ssq = self.smallp[:, 0:8]
        lnv = self.smallp[:, 8:16]
        rs = self.smallp[:, 16:24]
        for i in range(4):
            self.bcast_row("sp", "gq", gq[:, i, :], d["a_qnorm"][j], 64, W=[("gq",)], group=(i > 0))
        self.bcast_row("sp", "gq", gq[:, 4, :], d["a_knorm"][j], 64, W=[("gq",)], group=True)
        self.memset("pool", VA[:, :, 0:64], 1.0, W=[("VA", t) for t in range(NT)])
        self.memset("pool", VA[:, :, 128:192], 1.0, W=[("VA", t) for t in range(NT)])
        wq = d["a_wqkv"][j].rearrange("(c p) f -> p c f", p=128)
        ptb = self.psbf(7)
        ntl = NT
        for g in range(4):
            self.dma("pool", "wg", Wg[:, :, 0:256], wq[:, :, 256 * g:256 * g + 256], W=[("Wg",)], bar=False)
            self.dma("pool", "wg", Wg[:, :, 256:320], wq[:, :, 1024 + 64 * g:1024 + 64 * g + 64], W=[("Wg",)], group=True, bar=False)
            self.dma("pool", "wg", Wg[:, :, 320:384], wq[:, :, 1280 + 64 * g:1280 + 64 * g + 64], W=[("Wg",)], group=True, bar=False)
            for t in range(ntl):
                bank = 5 + (t % 2)
                ps = self.psb[bank]
                for c in range(8):
                    self.mm(ps[:, 0:384], hT[:, c, t * 128:(t + 1) * 128], Wg[:, c, :], start=(c == 0), stop=(c == 7),
                            R=[("hT", min(t // 4, 4)), ("Wg",)], W=[("ps", bank)])
                rw = raw[t % 2]
                self.cp("act", rw[:, :], ps[:, 0:384], R=[("ps", bank)], W=[("raw", t % 2)])
                r3 = rw[:, 0:320].rearrange("p (h e) -> p h e", e=64)
                self.head_rstd(r3, 5, 64, sqt[:, :, :], ssq, lnv, rs, ("raw", t % 2))
                self.tt("dve", t1[:, :, :], r3, rs[:, 0:5].unsqueeze(2).to_broadcast([128, 5, 64]), ALU.mult,
                        R=[("raw", t % 2), ("rsh",)], W=[("t1",)])
                self.tt("dve", t2[:, :, :], t1[:, :, :], gq[:, :, :], ALU.mult, R=[("t1",), ("gq",)], W=[("t2",)])
                qb = qkb[t % 2]
                if t < 16:
                    x1 = t2[:, :, 0:32]
                    x2 = t2[:, :, 32:64]
                    cs = self.cosA[:, t, :].unsqueeze(1).to_broadcast([128, 5, 32])
                    sn = self.sinA[:, t, :].unsqueeze(1).to_broadcast([128, 5, 32])
                    self.tt("dve", ra[:, :, :], x1, cs, ALU.mult, R=[("t2",), ("cosA",)], W=[("ra",)])
                    self.tt("pool", rb[:, :, :], x2, sn, ALU.mult, R=[("t2",), ("sinA",)], W=[("rb",)])
                    self.tt("dve", qb[:, 0:5, 0:32], ra[:, :, :], rb[:, :, :], ALU.subtract, R=[("ra",), ("rb",)], W=[("qkb", t % 2)])
                    self.tt("pool", rc[:, :, :], x1, sn, ALU.mult, R=[("t2",), ("sinA",)], W=[("rc",)])
                    self.tt("dve", rd[:, :, :], x2, cs, ALU.mult, R=[("t2",), ("cosA",)], W=[("rd",)])
                    self.tt("dve", qb[:, 0:5, 32:64], rc[:, :, :], rd[:, :, :], ALU.add, R=[("rc",), ("rd",)], W=[("qkb", t % 2)])
                else:
                    self.cp("dve", qb[:, 0:5, :], t2[:, :, :], R=[("t2",)], W=[("qkb", t % 2)])
                self.cp("pool", qb[:, 5, :], qb[:, 4, :], R=[("qkb", t % 2)], W=[("qkb", t % 2)])
                self.cp("pool", VA[:, t, 64:128], rw[:, 320:384], R=[("raw", t % 2)], W=[("VA", t)])
                qf = qb[:, :, :].rearrange("p h e -> p (h e)")
                for i in range(3):
                    self.tr(ptb[:, i * 128:(i + 1) * 128], qf[:, i * 128:(i + 1) * 128], self.identb[:, :],
                            R=[("qkb", t % 2), ("identb",)], W=[("ps", 7)])
                self.cp("act", QKT[0:64, 0:2, t * 128:(t + 1) * 128], ptb[0:64, 0:256].rearrange("p (i n) -> p i n", n=128),
                        R=[("ps", 7)], W=[("QKT", t)])
                self.cp("act", QKT[64:128, 2:4, t * 128:(t + 1) * 128], ptb[64:128, 0:256].rearrange("p (i n) -> p i n", n=128),
                        R=[("ps", 7)], W=[("QKT", t)])
                self.cp("dve", QKT[:, 4, t * 128:(t + 1) * 128], ptb[:, 256:384], R=[("ps", 7)], W=[("QKT", t)])
            for p in range(2):
                slot = (g * 2 + p) % 2
                h0 = 4 * g + 2 * p
                self.dma("pool", "wo%d" % slot, WoP[:, slot, :], d["a_wo"][j][h0 * 64:h0 * 64 + 128, :], W=[("WoP", slot)], bar=False)
                cfgs = []
                qgroups = [0, 1, 2, 3] + ([4] if need_ctx else [])
                for gi in qgroups:
                    t0, w = GROUPS[gi]
                    ktiles = list(range(NT)) if gi < 4 else [16, 17]
                    for hs in range(2):
                        psl = slice(0, 64) if hs == 0 else slice(64, 128)
                        cfgs.append(dict(
                            qt=QKT[:, p + 2 * hs, :], kt=QKT[:, 4, :],
                            va=(lambda k, hs=hs: VA[:, k, 64:192] if hs == 0 else VA[:, k, 0:128]),
                            o_lo=(hs == 0), t0=t0, w=w, ktiles=ktiles, att=attT,
                            qres=("QKTall",), kres=("QKTall",), vres=("VAall",), ares=("attT",)))
                self._alias([("QKT", t) for t in range(ntl)] + [("QKTz",)], ("QKTall",))
                self._alias([("VA", t) for t in range(ntl)], ("VAall",))
                self.attn_core(cfgs, 0.125)
                self.out_proj(WoP[:, slot, :], ("WoP", slot), attT, ("attT",), l, b, need_ctx)
            self._alias_release([("QKT", t) for t in range(ntl)], ("QKTall",))
            self._alias_release([("VA", t) for t in range(ntl)], ("VAall",))
        self.P.barrier()

    def _alias(self, fine, coarse):
        P = self.P
        toks = set()
        for f in fine:
            st = P.res.get(f)
            if st is not None and st["w"] is not None:
                toks.add(st["w"])
        P.res[coarse] = {"w": None, "r": {}, "ws": toks}
        for e in ("pe",):
            P.pending[e] |= toks

    def _alias_release(self, fine, coarse):
        P = self.P
        st = P.res.get(coarse)
        if st is None:
            return
        for f in fine:
            fs = P.res.get(f)
            if fs is None:
                fs = P.res[f] = {"w": None, "r": {}}
            for k, v in st["r"].items():
                fs["r"][("al", coarse, k)] = v

    def mla_layer(self, l, b, need_ctx):
        d = self.d
        R, R2, HTR = self.R, self.R2, self.HTR
        R.reset()
        R2.reset()
        HTR.reset()
        self.norm_bufs()
        hT = self.carve("hT", HT_OFF, [128, 8, TT], BF16)
        for gi in range(5):
            t0, w = GROUPS[gi]
            self.norm_group(gi, l, b, 1, lambda c, t0=t0, w=w, gi=gi: (hT[:, c, t0:t0 + w], ("hT", gi)))
        self.P.barrier()
        R.reset()
        cT = self.alloc(R, "cT", [128, 5, TT], BF16)
        krope = self.alloc(R, "krope", [128, NT, 32], BF16)
        self.PT = self.alloc(R, "PT", [128, 3, 512], BF16)
        self.rec = self.alloc(R, "rec", [128, 512], F32)
        gq2 = self.alloc(R, "gq2", [128, 2, 96], F32)
        gk2 = self.alloc(R, "gk2", [128, 2, 64], F32)
        gk = self.alloc(R, "gkr", [128, 32], F32)
        W1 = self.alloc(R, "W1", [128, 8, 672], BF16)
        g1 = self.alloc(R, "g1", [128, 640], F32)
        raw1 = [self.alloc(R2, "rawm%d" % i, [128, 672], F32) for i in range(2)]
        cn = [self.alloc(R2, "cn%d" % i, [128, 640], BF16) for i in range(2)]
        sq1 = self.alloc(R2, "sq1", [128, 384], F32)
        kr1 = self.alloc(R2, "kr1", [128, 32], F32)
        rt = [self.alloc(R2, "rt%d" % i, [128, 2, 16], F32) for i in range(4)]
        raw2 = [self.alloc(R2, "rawn%d" % i, [128, 448], F32) for i in range(2)]
        sqt = self.alloc(R2, "sqt", [128, 2, 64], F32)
        t1 = self.alloc(R2, "t1", [128, 2, 96], F32)
        tq = self.alloc(R2, "tq", [128, 2, 32], F32)
        k1 = self.alloc(R2, "k1", [128, 2, 64], F32)
        qa = [self.alloc(R2, "qa%d" % i, [128, 2, 96], BF16) for i in range(2)]
        ka = [self.alloc(R2, "ka%d" % i, [128, 2, 96], BF16) for i in range(2)]
        ssq = self.smallp[:, 0:8]
        lnv = self.smallp[:, 8:16]
        rs = self.smallp[:, 16:24]
        ssq1 = self.smallp[:, 24:28]
        lnv1 = self.smallp[:, 28:32]
        rs1 = self.smallp[:, 32:36]
        self.bcast_row("sp", "gq", g1[:, 0:384], d["b_qnorm_lat"][0], 384, W=[("g1",)])
        self.bcast_row("sp", "gq", g1[:, 384:640], d["b_kvnorm_lat"][0], 256, W=[("g1",)], group=True)
        self.bcast_row("sp", "gq", gk[:, :], d["b_knorm"][0, 64:96], 32, W=[("gk",)], group=True)
        for i in range(2):
            self.bcast_row("sp", "gq", gq2[:, i, :], d["b_qnorm"][0], 96, W=[("gq2",)], group=True)
            self.bcast_row("sp", "gq", gk2[:, i, :], d["b_knorm"][0, 0:64], 64, W=[("gk2",)], group=True)
        self.dma("pool", "wg", W1[:, :, 0:384], d["b_wdq"][0].rearrange("(c p) f -> p c f", p=128), W=[("W1",)], bar=False)
        self.dma("pool", "wg", W1[:, :, 384:672], d["b_wdkv"][0].rearrange("(c p) f -> p c f", p=128), W=[("W1",)], group=True, bar=False)
        ptb = self.psbf(7)
        for t in range(NT):
            ba = 3 + 2 * (t % 2)
            bb = ba + 1
            for c in range(8):
                self.mm(self.psb[ba][:, :], hT[:, c, t * 128:(t + 1) * 128], W1[:, c, 0:512], start=(c == 0), stop=(c == 7),
                        R=[("hT", min(t // 4, 4)), ("W1",)], W=[("ps", ba)])
            for c in range(8):
                self.mm(self.psb[bb][:, 0:160], hT[:, c, t * 128:(t + 1) * 128], W1[:, c, 512:672], start=(c == 0), stop=(c == 7),
                        R=[("hT", min(t // 4, 4)), ("W1",)], W=[("ps", bb)])
            rw = raw1[t % 2]
            self.cp("act", rw[:, 0:512], self.psb[ba][:, :], R=[("ps", ba)], W=[("rawm", t % 2)])
            self.cp("act", rw[:, 512:672], self.psb[bb][:, 0:160], R=[("ps", bb)], W=[("rawm", t % 2)])
            for i, (c0, n) in enumerate(((0, 384), (384, 256), (640, 32))):
                self.act(sq1[:, 0:n], rw[:, c0:c0 + n], AF.Square, accum=ssq1[:, i:i + 1], R=[("rawm", t % 2)], W=[("sq1",), ("ssq1", i)])
                self.act(lnv1[:, i:i + 1], ssq1[:, i:i + 1], AF.Ln, bias=self.misc[:, 2:3], scale=1.0 / n, R=[("ssq1", i), ("cst",)], W=[("lnv1", i)])
            self.act(rs1[:, 0:3], lnv1[:, 0:3], AF.Exp, scale=-0.5, R=[("lnv1", 0), ("lnv1", 1), ("lnv1", 2)], W=[("rs1",)])
            cnb = cn[t % 2]
            self.stt(cnb[:, 0:384], rw[:, 0:384], rs1[:, 0:1], g1[:, 0:384], ALU.mult, ALU.mult, R=[("rawm", t % 2), ("rs1",), ("g1",)], W=[("cn", t % 2)])
            self.stt(cnb[:, 384:640], rw[:, 384:640], rs1[:, 1:2], g1[:, 384:640], ALU.mult, ALU.mult, R=[("rawm", t % 2), ("rs1",), ("g1",)], W=[("cn", t % 2)])
            self.stt(kr1[:, :], rw[:, 640:672], rs1[:, 2:3], gk[:, :], ALU.mult, ALU.mult, R=[("rawm", t % 2), ("rs1",), ("gk",)], W=[("kr1",)])
            if t < 16:
                x1 = kr1[:, 0:16]
                x2 = kr1[:, 16:32]
                cs = self.cosB[:, t, :]
                sn = self.sinB[:, t, :]
                self.tt("dve", rt[0][:, 0, :], x1, cs, ALU.mult, R=[("kr1",), ("cosB",)], W=[("rt", 0)])
                self.tt("dve", rt[1][:, 0, :], x2, sn, ALU.mult, R=[("kr1",), ("sinB",)], W=[("rt", 1)])
                self.tt("dve", krope[:, t, 0:16], rt[0][:, 0, :], rt[1][:, 0, :], ALU.subtract, R=[("rt", 0), ("rt", 1)], W=[("krope", t)])
                self.tt("dve", rt[2][:, 0, :], x1, sn, ALU.mult, R=[("kr1",), ("sinB",)], W=[("rt", 2)])
                self.tt("dve", rt[3][:, 0, :], x2, cs, ALU.mult, R=[("kr1",), ("cosB",)], W=[("rt", 3)])
                self.tt("dve", krope[:, t, 16:32], rt[2][:, 0, :], rt[3][:, 0, :], ALU.add, R=[("rt", 2), ("rt", 3)], W=[("krope", t)])
            else:
                self.cp("dve", krope[:, t, :], kr1[:, :], R=[("kr1",)], W=[("krope", t)])
            for i in range(5):
                self.tr(ptb[:, i * 128:(i + 1) * 128], cnb[:, i * 128:(i + 1) * 128], self.identb[:, :],
                        R=[("cn", t % 2), ("identb",)], W=[("ps", 7)])
            self.cp("dve", cT[:, :, t * 128:(t + 1) * 128], ptb[:, 0:640].rearrange("p (i n) -> p i n", n=128),
                    R=[("ps", 7)], W=[("cT", t)])
        self.P.barrier()
        QKT = self.alloc(HTR, "QKT", [128, 4, TT], BF16)
        VA = self.alloc(HTR, "VA", [128, NT, 192], BF16)
        attT = self.alloc(HTR, "attT", [128, TT], BF16)
        W2q = self.alloc(HTR, "W2q", [128, 3, 192], BF16)
        W2kv = self.alloc(HTR, "W2kv", [128, 2, 256], BF16)
        WoP = self.alloc(HTR, "WoP", [128, 2, D], BF16)
        self.memset("pool", VA[:, :, 64:128], 1.0, W=[("VA", t) for t in range(NT)])
        wuq = d["b_wuq"][0].rearrange("(c p) f -> p c f", p=128)
        wukv = d["b_wukv"][0].rearrange("(c p) f -> p c f", p=128)
        sc = 96.0 ** -0.5
        for pp in range(8):
            self.dma("pool", "wg", W2q[:, :, :], wuq[:, :, pp * 192:(pp + 1) * 192], W=[("W2",)], bar=False)
            self.dma("pool", "wg", W2kv[:, :, :], wukv[:, :, pp * 256:(pp + 1) * 256], W=[("W2",)], group=True, bar=False)
            for t in range(NT):
                bank = 5 + (t % 2)
                ps = self.psb[bank]
                for c in range(3):
                    self.mm(ps[:, 0:192], cT[:, c, t * 128:(t + 1) * 128], W2q[:, c, :], start=(c == 0), stop=(c == 2),
                            R=[("cT", t), ("W2",)], W=[("ps", bank)])
                for c in range(2):
                    self.mm(ps[:, 192:448], cT[:, 3 + c, t * 128:(t + 1) * 128], W2kv[:, c, :], start=(c == 0), stop=(c == 1),
                            R=[("cT", t), ("W2",)], W=[("ps", bank)])
                rw = raw2[t % 2]
                self.cp("act", rw[:, :], ps[:, 0:448], R=[("ps", bank)], W=[("rawn", t % 2)])
                rq = rw[:, 0:192].rearrange("p (h e) -> p h e", e=96)
                rkv = rw[:, 192:448].rearrange("p (h e) -> p h e", e=128)
                qab = qa[t % 2]
                kab = ka[t % 2]
                self.head_rstd(rq[:, :, 0:64], 2, 64, sqt[:, :, :], ssq, lnv, rs, ("rawn", t % 2))
                self.tt("dve", t1[:, :, 0:64], rq[:, :, 0:64], rs[:, 0:2].unsqueeze(2).to_broadcast([128, 2, 64]), ALU.mult,
                        R=[("rawn", t % 2), ("rsh",)], W=[("t1",)])
                self.tt("pool", qab[:, :, 0:64], t1[:, :, 0:64], gq2[:, :, 0:64], ALU.mult, R=[("t1",), ("gq2",)], W=[("qa", t % 2)])
                self.head_rstd(rq[:, :, 64:96], 2, 32, sqt[:, :, 0:32], ssq, lnv, rs, ("rawn", t % 2))
                self.tt("dve", t1[:, :, 64:96], rq[:, :, 64:96], rs[:, 0:2].unsqueeze(2).to_broadcast([128, 2, 32]), ALU.mult,
                        R=[("rawn", t % 2), ("rsh",)], W=[("t1",)])
                if t < 16:
                    self.tt("dve", tq[:, :, :], t1[:, :, 64:96], gq2[:, :, 64:96], ALU.mult, R=[("t1",), ("gq2",)], W=[("tq",)])
                    x1 = tq[:, :, 0:16]
                    x2 = tq[:, :, 16:32]
                    cs = self.cosB[:, t, :].unsqueeze(1).to_broadcast([128, 2, 16])
                    sn = self.sinB[:, t, :].unsqueeze(1).to_broadcast([128, 2, 16])
                    self.tt("dve", rt[0][:, :, :], x1, cs, ALU.mult, R=[("tq",), ("cosB",)], W=[("rt", 0)])
                    self.tt("pool", rt[1][:, :, :], x2, sn, ALU.mult, R=[("tq",), ("sinB",)], W=[("rt", 1)])
                    self.tt("dve", qab[:, :, 64:80], rt[0][:, :, :], rt[1][:, :, :], ALU.subtract, R=[("rt", 0), ("rt", 1)], W=[("qa", t % 2)])
                    self.tt("pool", rt[2][:, :, :], x1, sn, ALU.mult, R=[("tq",), ("sinB",)], W=[("rt", 2)])
                    self.tt("dve", rt[3][:, :, :], x2, cs, ALU.mult, R=[("tq",), ("cosB",)], W=[("rt", 3)])
                    self.tt("dve", qab[:, :, 80:96], rt[2][:, :, :], rt[3][:, :, :], ALU.add, R=[("rt", 2), ("rt", 3)], W=[("qa", t % 2)])
                else:
                    self.tt("dve", qab[:, :, 64:96], t1[:, :, 64:96], gq2[:, :, 64:96], ALU.mult, R=[("t1",), ("gq2",)], W=[("qa", t % 2)])
                self.head_rstd(rkv[:, :, 0:64], 2, 64, sqt[:, :, :], ssq, lnv, rs, ("rawn", t % 2))
                self.tt("dve", k1[:, :, :], rkv[:, :, 0:64], rs[:, 0:2].unsqueeze(2).to_broadcast([128, 2, 64]), ALU.mult,
                        R=[("rawn", t % 2), ("rsh",)], W=[("k1",)])
                self.tt("pool", kab[:, :, 0:64], k1[:, :, :], gk2[:, :, :], ALU.mult, R=[("k1",), ("gk2",)], W=[("ka", t % 2)])
                self.cp("pool", kab[:, :, 64:96], krope[:, t, :].unsqueeze(1).to_broadcast([128, 2, 32]), R=[("krope", t)], W=[("ka", t % 2)])
                self.cp("pool", VA[:, t, 0:64], rkv[:, 0, 64:128], R=[("rawn", t % 2)], W=[("VA", t)])
                self.cp("pool", VA[:, t, 128:192], rkv[:, 1, 64:128], R=[("rawn", t % 2)], W=[("VA", t)])
                for i in range(2):
                    self.tr(ptb[0:96, i * 128:(i + 1) * 128], qab[:, i, :], self.identb[:, :], R=[("qa", t % 2), ("identb",)], W=[("ps", 7)])
                for i in range(2):
                    self.tr(ptb[0:96, (2 + i) * 128:(3 + i) * 128], kab[:, i, :], self.identb[:, :], R=[("ka", t % 2), ("identb",)], W=[("ps", 7)])
                self.cp("act", QKT[0:96, :, t * 128:(t + 1) * 128], ptb[0:96, 0:512].rearrange("p (i n) -> p i n", n=128),
                        R=[("ps", 7)], W=[("QKT", t)])
            slot = pp % 2
            self.dma("pool", "wo%d" % slot, WoP[:, slot, :], d["b_wo"][0][pp * 128:(pp + 1) * 128, :], W=[("WoP", slot)], bar=False)
            cfgs = []
            qgroups = [0, 1, 2, 3] + ([4] if need_ctx else [])
            for gi in qgroups:
                t0, w = GROUPS[gi]
                ktiles = list(range(NT)) if gi < 4 else [16, 17]
                for hs in range(2):
                    cfgs.append(dict(
                        qt=QKT[0:96, hs, :], kt=QKT[0:96, 2 + hs, :],
                        va=(lambda k, hs=hs: VA[:, k, 0:128] if hs == 0 else VA[:, k, 64:192]),
                        o_lo=(hs == 0), t0=t0, w=w, ktiles=ktiles, att=attT,
                        qres=("QKTall",), kres=("QKTall",), vres=("VAall",), ares=("attT",)))
            self._alias([("QKT", t) for t in range(NT)], ("QKTall",))
            self._alias([("VA", t) for t in range(NT)], ("VAall",))
            self.attn_core(cfgs, sc)
            self.out_proj(WoP[:, slot, :], ("WoP", slot), attT, ("attT",), l, b, need_ctx)
            self._alias_release([("QKT", t) for t in range(NT)], ("QKTall",))
            self._alias_release([("VA", t) for t in range(NT)], ("VAall",))
        self.P.barrier()

    def na_layer(self, l, b, need_ctx):
        d = self.d
        R, R2 = self.R, self.R2
        R.reset()
        R2.reset()
        self.norm_bufs()
        hT = self.carve("hT", HT_OFF, [128, 8, TT], BF16)
        for gi in range(5):
            t0, w = GROUPS[gi]
            self.norm_group(gi, l, b, 1, lambda c, t0=t0, w=w, gi=gi: (hT[:, c, t0:t0 + w], ("hT", gi)))
        self.P.barrier()
        R.reset()
        QKT = self.alloc(R, "QKT", [128, 3, TT], BF16)
        VA = self.alloc(R, "VA", [128, NT, 192], BF16)
        attT = self.alloc(R, "attT", [128, TT], BF16)
        Wp = self.alloc(R, "Wg", [128, 8, 384], BF16)
        WoP = self.alloc(R, "WoP", [128, 2, D], BF16)
        self.PT = self.alloc(R, "PT", [128, 3, 512], BF16)
        self.rec = self.alloc(R2, "rec", [128, 512], F32)
        gq4 = self.alloc(R2, "gq4", [128, 4, 64], F32)
        maskb = self.alloc(R, "maskb", [128, 21 * 128], BF16)
        self.memset("pool", QKT[64:128, 0, :], 0.0, W=[("QKTz",)])
        self.memset("pool", QKT[0:64, 1, :], 0.0, W=[("QKTz",)])
        BM = self.alloc(R2, "BM", [128, 21 * 128], F32)
        tmpb = [self.alloc(R2, "tmpb%d" % i, [128, 512], F32) for i in range(2)]
        raw = [self.alloc(R2, "raw%d" % i, [128, 384], F32) for i in range(2)]
        sqt = self.alloc(R2, "sqt", [128, 4, 64], F32)
        t1 = self.alloc(R2, "t1", [128, 4, 64], F32)
        qkb = [self.alloc(R2, "qkb%d" % i, [128, 4, 64], BF16) for i in range(2)]
        ssq = self.smallp[:, 0:8]
        lnv = self.smallp[:, 8:16]
        rs = self.smallp[:, 16:24]
        for i in range(2):
            self.bcast_row("sp", "gq", gq4[:, i, :], d["c_qnorm"][0], 64, W=[("gq4",)], group=(i > 0))
            self.bcast_row("sp", "gq", gq4[:, 2 + i, :], d["c_knorm"][0], 64, W=[("gq4",)], group=True)
        self.dma("pool", "mk", maskb[:, 0:1344], d["na_mask"][:, 0:1344], W=[("maskb",)])
        self.dma("pool", "mk", maskb[:, 1344:2688], d["na_mask"][:, 1344:2688], W=[("maskb",)], group=True)
        self.memset("pool", VA[:, :, 64:128], 1.0, W=[("VA", t) for t in range(NT)])
        wq = d["c_wqkv"][0].rearrange("(c p) f -> p c f", p=128)
        ptb = self.psbf(7)
        sc = 0.125
        for pp in range(8):
            self.dma("pool", "wg", Wp[:, :, 0:128], wq[:, :, 128 * pp:128 * pp + 128], W=[("Wg",)], bar=False)
            self.dma("pool", "wg", Wp[:, :, 128:256], wq[:, :, 1024 + 128 * pp:1024 + 128 * pp + 128], W=[("Wg",)], group=True, bar=False)
            self.dma("pool", "wg", Wp[:, :, 256:384], wq[:, :, 2048 + 128 * pp:2048 + 128 * pp + 128], W=[("Wg",)], group=True, bar=False)
            for t in range(NT):
                bank = 5 + (t % 2)
                ps = self.psb[bank]
                for c in range(8):
                    self.mm(ps[:, 0:384], hT[:, c, t * 128:(t + 1) * 128], Wp[:, c, :], start=(c == 0), stop=(c == 7),
                            R=[("hT", min(t // 4, 4)), ("Wg",)], W=[("ps", bank)])
                rw = raw[t % 2]
                self.cp("act", rw[:, :], ps[:, 0:384], R=[("ps", bank)], W=[("raw", t % 2)])
                r3 = rw[:, 0:256].rearrange("p (h e) -> p h e", e=64)
                self.head_rstd(r3, 4, 64, sqt[:, :, :], ssq, lnv, rs, ("raw", t % 2))
                self.tt("dve", t1[:, :, :], r3, rs[:, 0:4].unsqueeze(2).to_broadcast([128, 4, 64]), ALU.mult,
                        R=[("raw", t % 2), ("rsh",)], W=[("t1",)])
                qb = qkb[t % 2]
                self.tt("dve", qb[:, :, :], t1[:, :, :], gq4[:, :, :], ALU.mult, R=[("t1",), ("gq4",)], W=[("qkb", t % 2)])
                self.cp("pool", VA[:, t, 0:64], rw[:, 256:320], R=[("raw", t % 2)], W=[("VA", t)])
                self.cp("pool", VA[:, t, 128:192], rw[:, 320:384], R=[("raw", t % 2)], W=[("VA", t)])
                qf = qb[:, :, :].rearrange("p h e -> p (h e)")
                for i in range(2):
                    self.tr(ptb[:, i * 128:(i + 1) * 128], qf[:, i * 128:(i + 1) * 128], self.identb[:, :],
                            R=[("qkb", t % 2), ("identb",)], W=[("ps", 7)])
                self.cp("act", QKT[0:64, 0, t * 128:(t + 1) * 128], ptb[0:64, 0:128], R=[("ps", 7)], W=[("QKT", t)])
                self.cp("act", QKT[64:128, 1, t * 128:(t + 1) * 128], ptb[64:128, 0:128], R=[("ps", 7)], W=[("QKT", t)])
                self.cp("dve", QKT[:, 2, t * 128:(t + 1) * 128], ptb[:, 128:256], R=[("ps", 7)], W=[("QKT", t)])
            slot = pp % 2
            self.dma("pool", "wo%d" % slot, WoP[:, slot, :], d["c_wo"][0][pp * 128:(pp + 1) * 128, :], W=[("WoP", slot)], bar=False)
            self._alias([("QKT", t) for t in range(NT)] + [("QKTz",)], ("QKTall",))
            self._alias([("VA", t) for t in range(NT)], ("VAall",))
            for hs in range(2):
                h = 2 * pp + hs
                psl = slice(0, 64) if hs == 0 else slice(64, 128)
                self.dma("sp", "bm", BM[:, :], d["na_bias"][h], W=[("BM",)])
                self.tt("pool", BM[:, :], BM[:, :], maskb[:, :], ALU.add, R=[("BM",), ("maskb",)], W=[("BM",)])
                va = (lambda k, hs=hs: VA[:, k, 0:128] if hs == 0 else VA[:, k, 64:192])
                self.na_core(QKT[:, hs, :], QKT[:, 2, :], va, hs == 0, attT, BM, tmpb, sc)
            if need_ctx:
                cfgs = []
                t0, w = GROUPS[4]
                for hs in range(2):
                    psl = slice(0, 64) if hs == 0 else slice(64, 128)
                    cfgs.append(dict(
                        qt=QKT[:, hs, :], kt=QKT[:, 2, :],
                        va=(lambda k, hs=hs: VA[:, k, 0:128] if hs == 0 else VA[:, k, 64:192]),
                        o_lo=(hs == 0), t0=t0, w=w, ktiles=[16, 17], att=attT,
                        qres=("QKTall",), kres=("QKTall",), vres=("VAall",), ares=("attT",)))
                self.attn_core(cfgs, sc)
            self.out_proj(WoP[:, slot, :], ("WoP", slot), attT, ("attT",), l, b, need_ctx)
            self._alias_release([("QKT", t) for t in range(NT)], ("QKTall",))
            self._alias_release([("VA", t) for t in range(NT)], ("VAall",))
        self.P.barrier()

    def na_core(self, qt, kt, va, o_lo, attT, BM, tmpb, sc):
        packs = []
        for qi in range(16):
            blocks = na_block_ids(qi)
            items = [(kt_, blk) for (kt_, blk) in blocks] + [(16, None), (17, None)]
            plist = [items[0:4], items[4:]]
            for pi, pk in enumerate(plist):
                packs.append((qi, pk, pi == 0, pi == len(plist) - 1))
        n = len(packs)
        lo, hi_ = slice(0, 64), slice(64, 128)
        o_sl, d_sl = (lo, hi_) if o_lo else (hi_, lo)

        def qk(i):
            qi, pk, first, last = packs[i]
            bank = i % 3
            for j, (k, blk) in enumerate(pk):
                self.mm(self.psb[bank][:, j * 128:(j + 1) * 128], kt[:, k * 128:(k + 1) * 128], qt[:, qi * 128:(qi + 1) * 128],
                        R=[("QKTall",)], W=[("ps", bank)])

        def ex(i):
            qi, pk, first, last = packs[i]
            bank = i % 3
            nb = sum(1 for (_, blk) in pk if blk is not None)
            nk = len(pk)
            if nb > 0:
                b0 = pk[0][1]
                tb = tmpb[i % 2]
                self.stt(tb[:, 0:nb * 128], self.psb[bank][:, 0:nb * 128], sc, BM[:, b0 * 128:(b0 + nb) * 128], ALU.mult, ALU.add,
                         R=[("ps", bank), ("BM",)], W=[("tmpb", i % 2)])
                self.act(self.PT[:, bank, 0:nb * 128], tb[:, 0:nb * 128], AF.Exp, R=[("tmpb", i % 2)], W=[("pt", bank)])
            if nk > nb:
                self.act(self.PT[:, bank, nb * 128:nk * 128], self.psb[bank][:, nb * 128:nk * 128], AF.Exp, scale=sc,
                         R=[("ps", bank)], W=[("pt", bank)])

        def pv(i):
            qi, pk, first, last = packs[i]
            ob = 3 + (qi + self.ot_ctr) % 2
            for j, (k, blk) in enumerate(pk):
                self.mm(self.psb[ob][:, 0:128], va(k), self.PT[:, i % 3, j * 128:(j + 1) * 128],
                        start=(first and j == 0), stop=(last and j == len(pk) - 1),
                        R=[("pt", i % 3), ("VAall",)], W=[("ps", ob)])
            if last:
                rec_o = self.rec[d_sl, 0:128]
                rec_i = self.psb[ob][d_sl, 0:128]
                self.P.op("dve", lambda e: e.reciprocal(out=rec_o, in_=rec_i), [("ps", ob)], [("rec",)])
                self.tt("dve", attT[o_sl, qi * 128:(qi + 1) * 128], self.psb[ob][o_sl, 0:128], self.rec[d_sl, 0:128], ALU.mult,
                        R=[("ps", ob), ("rec",)], W=[("attT",)])

        qk(0)
        for i in range(n):
            if i + 1 < n:
                qk(i + 1)
            ex(i)
            pv(i)
        self.ot_ctr += 16

    def ring_load(self, src):
        i = self.unit_ctr % NUNIT
        self.unit_ctr += 1
        buf = self.ring[i]
        self.dma("pool", "ring%d" % i, buf[:, :, :], src, W=[("ring", i)], bar=False)
        return buf, ("ring", i)

    def moe_units(self, l, e):
        d = self.d
        wg = d["moe_wg"][l, e].rearrange("(c p) f -> p c f", p=128)
        wu = d["moe_wu"][l, e].rearrange("(c p) f -> p c f", p=128)
        wd = d["moe_wd"][l, e].rearrange("(c p) f -> p c f", p=128)
        return [wg[:, :, 0:512], wu[:, :, 0:512], wg[:, :, 512:1024], wu[:, :, 512:1024], wd[:, :, 0:512], wd[:, :, 512:1024]]

    def moe_layer(self, l, b, need_ctx, pre):
        d = self.d
        R = self.R
        R.reset()
        ROFF = R_OFF
        self.norm_bufs()
        h2T = [self.alloc(R, "h2T%d" % i, [128, 8, 512], BF16) for i in range(2)]
        aff = self.carve("aff", ROFF + 43008, [128, NT, 16], F32)
        rw = self.carve("rw", ROFF + 44160, [128, 8, 16], BF16)
        m8 = self.carve("m8", ROFF + 44160 + 256, [16, 8], F32)
        thr = self.carve("thr", ROFF + 44160 + 288, [16, 2], F32)
        gate = self.carve("gate", ROFF + 44160 + 320, [128, 4], F32)
        ee = self.carve("ee", ROFF + 44160 + 352, [128, 16], F32)
        sst = self.carve("sst", ROFF + 44160 + 416, [128, 8], F32)
        h2 = self.carve("h2", HT_OFF, [128, NT, D], BF16)
        self.dma("pool", "rw", rw[:, :, :], d["moe_router"][l].rearrange("(c p) e -> p c e", p=128), W=[("rw",)])
        ngrp = 5 if need_ctx else 4
        ntl = NT if need_ctx else 16
        ptb = self.psbf(7)
        for gi in range(ngrp):
            t0, w = GROUPS[gi]
            hb = h2T[gi % 2]
            self.norm_group(gi, l, b, 2, lambda c, hb=hb, w=w, gi=gi: (hb[:, c, 0:w], ("h2T", gi % 2)))
            for tt_ in range(w // 128):
                t = t0 // 128 + tt_
                bank = 3 + (t % 2)
                ps = self.psb[bank]
                for c in range(8):
                    self.mm(ps[:, 0:16], hb[:, c, tt_ * 128:(tt_ + 1) * 128], rw[:, c, :], start=(c == 0), stop=(c == 7),
                            R=[("h2T", gi % 2), ("rw",)], W=[("ps", bank)])
                self.red(sst[:, 0:1], ps[:, 0:16], ALU.max, R=[("ps", bank)], W=[("sst",)])
                self.ts("dve", sst[:, 1:2], sst[:, 0:1], -1.0, ALU.mult, R=[("sst",)], W=[("sst",)])
                self.act(ee[:, :], ps[:, 0:16], AF.Exp, bias=sst[:, 1:2], accum=sst[:, 2:3],
                         R=[("ps", bank), ("sst",)], W=[("ee",), ("sst",)])
                self.P.op("dve", lambda e: e.reciprocal(out=sst[:, 3:4], in_=sst[:, 2:3]), [("sst",)], [("sst",)])
                self.ts("dve", aff[:, t, :], ee[:, :], sst[:, 3:4], ALU.mult, R=[("ee",), ("sst",)], W=[("aff",)])
                for c in range(8):
                    self.tr(ptb[:, c * 128:(c + 1) * 128], hb[:, c, tt_ * 128:(tt_ + 1) * 128], self.identb[:, :],
                            R=[("h2T", gi % 2), ("identb",)], W=[("ps", 7)])
                self.cp("act", h2[:, t, :], ptb[:, :], R=[("ps", 7)], W=[("h2", t)])
        self.P.barrier()
        affT = self.carve("affT", ROFF + 0, [16, TT], F32)
        work = self.carve("work", ROFF + 9216, [16, TT], F32)
        B3 = self.carve("B3", ROFF + 18432, [16, TT], F32)
        vb16 = self.carve("vb16", ROFF + 27648, [16, TT], BF16)
        gmtok = self.carve("gmtok", ROFF + 38400, [128, NT, 16], F32)
        vtok = self.carve("vtok", ROFF + 40704, [128, NT, 16], F32)
        gmhl = self.carve("gmhl", ROFF + 41856, [128, NT, 16, 2], BF16)
        for t in range(ntl):
            bank = (t // 4) % 2
            self.tr(self.psb[bank][0:16, (t % 4) * 128:(t % 4 + 1) * 128], aff[:, t, :], self.identf,
                    R=[("aff",), ("cst",)], W=[("ps", bank)])
            if t % 4 == 3 or t == ntl - 1:
                t_lo = (t // 4) * 4
                n = (t - t_lo + 1) * 128
                self.cp("dve", affT[:, t_lo * 128:t_lo * 128 + n], self.psb[bank][0:16, 0:n], R=[("ps", bank)], W=[("affT",)])
        segs = [(0, T, CAP // 8, 0)] + ([(T, TC, CAPC // 8, 1)] if need_ctx else [])
        ncols = T + (TC if need_ctx else 0)
        self.cp("dve", work[:, 0:ncols], affT[:, 0:ncols], R=[("affT",)], W=[("work",)])
        for (c0, n, rounds, ti) in segs:
            wv = work[:, c0:c0 + n]
            for r in range(rounds):
                self.P.op("dve", lambda e, wv=wv: e.max(out=m8[:, :], in_=wv), [("work",)], [("m8",)])
                if r < rounds - 1:
                    self.P.op("dve", lambda e, wv=wv: e.match_replace(out=wv, in_to_replace=m8[:, :], in_values=wv, imm_value=-1.0),
                              [("work",), ("m8",)], [("work",)])
            self.cp("dve", thr[:, ti:ti + 1], m8[:, 7:8], R=[("m8",)], W=[("thr",)])
        ones_col = self.cstb[0:16, 128 + 1:128 + 2]
        for (c0, n, rounds, ti) in segs:
            self.ts("dve", work[:, c0:c0 + n], affT[:, c0:c0 + n], thr[:, ti:ti + 1], ALU.is_ge, R=[("affT",), ("thr",)], W=[("work",)])
            self.P.op("dve", lambda e, c0=c0, n=n: e.tensor_tensor_scan(out=B3[:, c0:c0 + n], data0=ones_col.to_broadcast([16, n]),
                                                                     data1=work[:, c0:c0 + n], initial=0.0, op0=ALU.mult, op1=ALU.add),
                      [("work",), ("cst",)], [("B3",)])
        self.tt("dve", B3[:, 0:ncols], B3[:, 0:ncols], work[:, 0:ncols], ALU.mult, R=[("B3",), ("work",)], W=[("B3",)])
        self.ts("dve", B3[:, 0:ncols], B3[:, 0:ncols], -1.0, ALU.add, R=[("B3",)], W=[("B3",)])
        self.tt("dve", affT[:, 0:ncols], affT[:, 0:ncols], work[:, 0:ncols], ALU.mult, R=[("affT",), ("work",)], W=[("affT",)])
        self.cp("dve", vb16[:, 0:ncols], B3[:, 0:ncols], R=[("B3",)], W=[("vb16",)])
        for (src, dst, nm, bank) in ((B3, vtok, "vtok", 2), (affT, gmtok, "gmtok", 3)):
            for t in range(ntl):
                self.tr(self.psb[bank][:, t * 16:(t + 1) * 16], src[:, t * 128:(t + 1) * 128], self.identf[0:16, 0:16],
                        R=[(src.name,), ("cst",)], W=[("ps", bank)])
            self.cp("dve", dst[:, 0:ntl, :], self.psb[bank][:, 0:ntl * 16].rearrange("p (t e) -> p t e", e=16),
                    R=[("ps", bank)], W=[(nm,)])
        self.cp("dve", gmhl[:, 0:ntl, :, 0], gmtok[:, 0:ntl, :], R=[("gmtok",)], W=[("gmhl",)])
        self.tt("dve", gmtok[:, 0:ntl, :], gmtok[:, 0:ntl, :], gmhl[:, 0:ntl, :, 0], ALU.subtract, R=[("gmtok",), ("gmhl",)], W=[("gmtok",)])
        self.cp("dve", gmhl[:, 0:ntl, :, 1], gmtok[:, 0:ntl, :], R=[("gmtok",)], W=[("gmhl",)])
        self.P.barrier()
        S = self.carve("S", ROFF + 0, [128, 16, 256], BF16)
        Sc = self.carve("Sc", ROFF + 8192, [128, 2, 32], BF16)
        ST = self.carve("ST", ROFF + 9216, [128, 2, T], BF16)
        STc = self.carve("STc", ROFF + 9216 + 8192, [32, 256], BF16)
        xgT = self.carve("xgT", ROFF + 18432, [128, 8, 288], BF16)
        hidT = self.carve("hidT", ROFF + 18432 + 4608, [128, 8, 288], BF16)
        y = self.carve("y", ROFF + 32256, [128, 3, D], BF16)
        sg = self.carve("sg", ROFF + 38400, [128, 2, 288], F32)
        Wn = 288 if need_ctx else 256
        units = list(pre)
        upos = [0]

        def next_units(n):
            out = []
            for _ in range(n):
                out.append(units[upos[0]])
                upos[0] += 1
            return out

        srcs = []
        for e in range(NE):
            srcs += self.moe_units(l, e)
        issued = [len(pre)]

        def issue(upto):
            while issued[0] < min(upto, len(srcs)):
                units.append(self.ring_load(srcs[issued[0]]))
                issued[0] += 1

        chunks = [(0, 128), (128, 128)] + ([(256, 32)] if need_ctx else [])
        nch = 3 if need_ctx else 2

        def build_S(e):
            for t in range(16):
                self.ts("dve", S[:, t, :], self.iotac, vtok[:, t, e:e + 1], ALU.is_equal, R=[("vtok",), ("cst",)], W=[("S", t)])
            if need_ctx:
                for t in range(2):
                    self.ts("dve", Sc[:, t, :], self.iotac[:, 0:32], vtok[:, 16 + t, e:e + 1], ALU.is_equal, R=[("vtok",), ("cst",)], W=[("Sc",)])

        def slot_gates(e):
            gb = 4
            gps = self.psb[gb]
            for ch in range(2):
                for t in range(16):
                    self.mm(gps[:, ch * 2:ch * 2 + 2], S[:, t, ch * 128:(ch + 1) * 128], gmhl[:, t, e, :], start=(t == 0), stop=(t == 15),
                            R=[("S", t), ("gmhl",)], W=[("ps", gb)])
            if need_ctx:
                for t in range(2):
                    self.mm(gps[0:32, 4:6], Sc[:, t, :], gmhl[:, 16 + t, e, :], start=(t == 0), stop=(t == 1),
                            R=[("Sc",), ("gmhl",)], W=[("ps", gb)])
            self.red(gate[:, 0:nch], gps[:, 0:2 * nch].rearrange("p (c two) -> p c two", two=2), ALU.add, R=[("ps", gb)], W=[("gate",)])

        def gather_c(e, c):
            bank = c % 2
            ps = self.psb[bank]
            for t in range(16):
                self.mm(ps[:, 0:256], h2[:, t, c * 128:(c + 1) * 128], S[:, t, :], start=(t == 0), stop=(t == 15),
                        R=[("h2", t), ("S", t)], W=[("ps", bank)])
            if need_ctx:
                for t in range(2):
                    self.mm(ps[:, 256:288], h2[:, 16 + t, c * 128:(c + 1) * 128], Sc[:, t, :], start=(t == 0), stop=(t == 1),
                            R=[("h2", 16 + t), ("Sc",)], W=[("ps", bank)])
            self.cp("act", xgT[:, c, 0:Wn], ps[:, 0:Wn], R=[("ps", bank)], W=[("xgT",)])

        def scatter_blocks(e):
            out = []
            for grp in range(4):
                for dc in range(8):
                    def blk(grp=grp, dc=dc):
                        bank = 2 + (dc % 2)
                        ps = self.psb[bank]
                        for ch in range(2):
                            self.mm(ps[:, :], y[:, ch, dc * 128:(dc + 1) * 128], ST[:, ch, grp * 512:(grp + 1) * 512],
                                    start=(ch == 0), stop=(ch == 1), R=[("y",), ("ST", grp)], W=[("ps", bank)])
                        xs = self.xT[:, dc, grp * 512:(grp + 1) * 512]
                        self.stt(xs, ps[:, :], self.mod(l, b, 5, dc), xs, ALU.mult, ALU.add,
                                 R=[("ps", bank), ("modv",), ("xT", grp, dc)], W=[("xT", grp, dc)])
                    out.append(blk)
            if need_ctx:
                for dc in range(8):
                    def blk(dc=dc):
                        bank = 2 + (dc % 2)
                        ps = self.psb[bank]
                        self.mm(ps[:, 0:256], y[0:32, 2, dc * 128:(dc + 1) * 128], STc[:, :], R=[("y",), ("STc",)], W=[("ps", bank)])
                        xs = self.xT[:, dc, T:TT]
                        self.stt(xs, ps[:, 0:256], self.mod(l, 2, 5, dc), xs, ALU.mult, ALU.add,
                                 R=[("ps", bank), ("modv",), ("xT", 4, dc)], W=[("xT", 4, dc)])
                    out.append(blk)
            return out

        def build_ST(e):
            for grp in range(4):
                bank = 2 + (grp % 2)
                ps = self.psb[bank]
                self.mm(ps[:, :], self.sel[:, e, :], vb16[:, grp * 512:(grp + 1) * 512], R=[("sel",), ("vb16",)], W=[("ps", bank)])
                for ch in range(2):
                    self.ts("dve", ST[:, ch, grp * 512:(grp + 1) * 512], ps[:, :], self.misc[:, ch:ch + 1], ALU.is_equal,
                            R=[("ps", bank), ("cst",)], W=[("ST", grp)])
            if need_ctx:
                ps = self.psb[2]
                self.mm(ps[0:32, 0:256], self.sel[:, e, 0:32], vb16[:, T:TT], R=[("sel",), ("vb16",)], W=[("ps", 2)])
                self.ts("dve", STc[:, :], ps[0:32, 0:256], self.misc[0:32, 0:1], ALU.is_equal, R=[("ps", 2), ("cst",)], W=[("STc",)])

        def gate_up(e, wg0, wu0, wg1, wu1):
            for fc in range(8):
                gbuf, gres = (wg0 if fc < 4 else wg1)
                ubuf, ures = (wu0 if fc < 4 else wu1)
                fo = (fc % 4) * 128
                bg = 4 + 2 * (fc % 2)
                bu = bg + 1
                for c in range(8):
                    self.mm(self.psb[bg][:, 0:Wn], gbuf[:, c, fo:fo + 128], xgT[:, c, 0:Wn], start=(c == 0), stop=(c == 7),
                            R=[gres, ("xgT",)], W=[("ps", bg)])
                for c in range(8):
                    self.mm(self.psb[bu][:, 0:Wn], ubuf[:, c, fo:fo + 128], xgT[:, c, 0:Wn], start=(c == 0), stop=(c == 7),
                            R=[ures, ("xgT",)], W=[("ps", bu)])
                self.act(sg[:, fc % 2, 0:Wn], self.psb[bg][:, 0:Wn], AF.Silu, R=[("ps", bg)], W=[("sg", fc % 2)])
                self.tt("dve", hidT[:, fc, 0:Wn], sg[:, fc % 2, 0:Wn], self.psb[bu][:, 0:Wn], ALU.mult,
                        R=[("sg", fc % 2), ("ps", bu)], W=[("hidT",)])

        def down(e, wd0, wd1):
            for half, (dbuf, dres) in enumerate((wd0, wd1)):
                for ci, (c0, m) in enumerate(chunks):
                    bank = (half * 3 + ci) % 2
                    ps = self.psb[bank]
                    for fcn in range(8):
                        self.mm(ps[0:m, :], hidT[:, fcn, c0:c0 + m], dbuf[:, fcn, :], start=(fcn == 0), stop=(fcn == 7),
                                R=[("hidT",), dres], W=[("ps", bank)])
                    self.ts("dve", y[0:m, ci, half * 512:(half + 1) * 512], ps[0:m, :], gate2[0:m, ci:ci + 1], ALU.mult,
                            R=[("ps", bank), ("gate2",)], W=[("y",)])

        gate2 = self.carve("gate2", ROFF + 44160 + 480, [128, 4], F32)
        issue(4)
        build_S(0)
        slot_gates(0)
        for c in range(8):
            gather_c(0, c)
        for e in range(NE):
            issue(e * 6 + 4)
            wg0, wu0, wg1, wu1 = next_units(4)
            self.cp("dve", gate2[:, 0:nch], gate[:, 0:nch], R=[("gate",)], W=[("gate2",)])
            build_ST(e)
            gate_up(e, wg0, wu0, wg1, wu1)
            issue(e * 6 + 6)
            wd0, wd1 = next_units(2)
            down(e, wd0, wd1)
            issue(e * 6 + 10)
            blocks = scatter_blocks(e)
            if e + 1 < NE:
                build_S(e + 1)
                slot_gates(e + 1)
                nb = len(blocks)
                per = (nb + 7) // 8
                bi = 0
                for c in range(8):
                    gather_c(e + 1, c)
                    for _ in range(per):
                        if bi < nb:
                            blocks[bi]()
                            bi += 1
                while bi < nb:
                    blocks[bi]()
                    bi += 1
            else:
                for blk in blocks:
                    blk()
        self.P.barrier()

    def moe_prefetch(self, l):
        self.unit_ctr = 0
        srcs = self.moe_units(l, 0)
        return [self.ring_load(srcs[i]) for i in range(2)]

    def build(self):
        cfg = self.cfg
        self.prologue()
        for b in range(cfg.get("nsamples", SPC)):
            self.load_sample(b)
            for l in cfg.get("layers", [0, 1, 2, 3]):
                need_ctx = l < 3
                pre = self.moe_prefetch(l) if cfg.get("moe", True) else []
                kind = l % 3
                if cfg.get("attn", True):
                    if kind == 0:
                        self.gqa_layer(l, b, need_ctx)
                    elif kind == 1:
                        self.mla_layer(l, b, need_ctx)
                    else:
                        self.na_layer(l, b, need_ctx)
                if cfg.get("dump_xa") == (b, l):
                    self.store_sample(self.dbg_out("xa", [TT, D]), NT)
                if cfg.get("moe", True):
                    self.moe_layer(l, b, need_ctx, pre)
                if cfg.get("dump_x") == (b, l):
                    self.store_sample(self.dbg_out("x", [TT, D]), NT)
            self.store_sample(self.y2[b], 16)
        self.P.emit(self.es)
        return self.nc


def host_inputs(inp):
    cst = np.zeros((128, 388), np.float32)
    cst[:, 0:128] = np.eye(128, dtype=np.float32)
    cst[:, 128:384] = np.arange(256, dtype=np.float32)[None, :]
    cst[:, 384] = np.arange(128, dtype=np.float32)
    cst[:, 385] = np.arange(128, dtype=np.float32) + 128.0
    cst[:, 386] = EPS
    selc = np.zeros((16, 16, 128), np.float32)
    for e in range(16):
        selc[e, e, :] = 1.0
    selc = selc.reshape(16, 2048).astype(ml_dtypes.bfloat16)
    cosA, sinA = rope_tables(64)
    cosB, sinB = rope_tables(32)
    na_bias, na_mask = na_tables(np.asarray(inp["c_rpb"], np.float32)[0])
    shared = {k: np.ascontiguousarray(np.asarray(inp[k], np.float32)) for k in (
        "ada_w", "ada_b", "norm1_w", "norm2_w", "a_wqkv", "a_qnorm", "a_knorm", "a_wo",
        "b_wdq", "b_qnorm_lat", "b_wuq", "b_wdkv", "b_kvnorm_lat", "b_wukv", "b_qnorm", "b_knorm", "b_wo",
        "c_wqkv", "c_qnorm", "c_knorm", "c_wo", "moe_router", "moe_wg", "moe_wu", "moe_wd")}
    shared.update(cst=cst, selc=selc, cosA=cosA, sinA=sinA, cosB=cosB, sinB=sinB, na_bias=na_bias, na_mask=na_mask)
    x = np.asarray(inp["x"], np.float32)
    ctx = np.asarray(inp["ctx"], np.float32)
    c = np.asarray(inp["c"], np.float32)
    c_ctx = np.asarray(inp["c_ctx"], np.float32)
    maps = []
    for core in range(N_CORES):
        m = dict(shared)
        m["x2"] = np.ascontiguousarray(x[SPC * core:SPC * core + SPC])
        m["ctx2"] = np.ascontiguousarray(ctx[SPC * core:SPC * core + SPC])
        m["cvec"] = np.ascontiguousarray(np.concatenate([c[SPC * core:SPC * core + SPC], c_ctx[None, :]], axis=0))
        maps.append(m)
    return maps


def kernel(**inp):
    maps = host_inputs(inp)
    nc = Builder({}).build()
    res = run_bass_kernel_spmd(nc, maps, core_ids=list(range(N_CORES)))
    out = np.concatenate([np.asarray(r["y2"], np.float32) for r in res.results], axis=0)
    return out
```

```python
import numpy as np
import ml_dtypes
from contextlib import ExitStack
import concourse.bass as bass
import concourse.mybir as mybir
from concourse.bass_utils import run_bass_kernel_spmd

F32 = mybir.dt.float32
BF16 = mybir.dt.bfloat16
AF = mybir.ActivationFunctionType
ALU = mybir.AluOpType
AX = mybir.AxisListType

D = 1024
T = 2048
TC = 256
TT = 2304
NT = 18
NE = 16
CAP = 256
CAPC = 32
EPS = 1e-6
NEG = -30000.0
N_CORES = 8
SPC = 2

XT_OFF = 0
RING_OFF = 73728
UNIT = 8192
NUNIT = 5
HT_OFF = RING_OFF + UNIT * NUNIT
R_OFF = HT_OFF + 36864
R_SIZE = 45056
PERS_OFF = R_OFF + R_SIZE
ARENA_BYTES = 212000
R2_OFF = RING_OFF + 2 * UNIT
R2_SIZE = 3 * UNIT


def _dsize(dt):
    return 4 if dt == F32 else 2


class Prog:
    ENG = ("pe", "act", "dve", "pool", "sp")

    def __init__(self, nc):
        self.nc = nc
        self.eng = {"pe": nc.tensor, "act": nc.scalar, "dve": nc.vector, "pool": nc.gpsimd, "sp": nc.sync}
        self.ops = []
        self.eops = {e: [] for e in self.ENG}
        self.res = {}
        self.slots = {}
        self.pending = {e: set() for e in self.ENG}

    def _deps(self, eng, reads, writes):
        deps = set(self.pending[eng])
        self.pending[eng] = set()
        for r in reads:
            st = self.res.get(r)
            if st is not None and st["w"] is not None:
                deps.add(st["w"])
        for w in writes:
            st = self.res.get(w)
            if st is not None:
                if st["w"] is not None:
                    deps.add(st["w"])
                deps.update(st["r"].values())
        return deps

    def _mark(self, tok, key, reads, writes):
        for r in reads:
            st = self.res.get(r)
            if st is None:
                st = self.res[r] = {"w": None, "r": {}}
            st["r"][key] = tok
        for w in writes:
            self.res[w] = {"w": tok, "r": {}}

    def op(self, eng, fn, reads=(), writes=()):
        deps = self._deps(eng, reads, writes)
        idx = len(self.eops[eng])
        rec = {"eng": eng, "kind": "op", "fn": fn, "deps": deps, "idx": idx, "sig": False}
        self.eops[eng].append(rec)
        self.ops.append(rec)
        self._mark(("e", eng, idx), eng, reads, writes)

    def dma(self, queue, slot, out, in_, reads=(), writes=(), group=False, bar=True, **kw):
        deps = self._deps(queue, reads, writes)
        s = self.slots.get(slot)
        if s is None:
            s = self.slots[slot] = {"n": 0, "groups": [], "bar": bar}
        s["n"] += 1
        n = s["n"]
        if group and s["groups"]:
            s["groups"][-1] = n
        else:
            if n > 1:
                deps.add(("d", slot, n - 1))
            s["groups"].append(n)
        idx = len(self.eops[queue])
        rec = {"eng": queue, "kind": "dma", "slot": slot, "n": n, "out": out, "in_": in_, "deps": deps,
               "idx": idx, "kw": kw, "sig": False}
        self.eops[queue].append(rec)
        self.ops.append(rec)
        self._mark(("d", slot, n), ("d", slot), reads, writes)

    def barrier(self):
        toks = set()
        for e in ("pe", "act", "dve", "pool"):
            if self.eops[e]:
                for rec in reversed(self.eops[e]):
                    if rec["kind"] == "op":
                        toks.add(("e", e, rec["idx"]))
                        break
        for name, s in self.slots.items():
            if s["bar"] and s["n"] > 0:
                toks.add(("d", name, s["n"]))
        for e in ("pe", "act", "dve", "pool", "sp"):
            self.pending[e] |= {t for t in toks if not (t[0] == "e" and t[1] == e == "pe")}

    def _gend(self, slot, n):
        for g in self.slots[slot]["groups"]:
            if g >= n:
                return g
        raise AssertionError

    def emit(self, es):
        nc = self.nc
        for rec in self.ops:
            for d in rec["deps"]:
                if d[0] == "e":
                    if d[1] == rec["eng"] == "pe":
                        continue
                    self.eops[d[1]][d[2]]["sig"] = True
        for e in self.ENG:
            c = 0
            for rec in self.eops[e]:
                if rec["sig"]:
                    c += 1
                rec["sigval"] = c
        sem = {}
        for e in self.ENG:
            sem[("e", e)] = es.enter_context(nc.semaphore("g_" + e))
        for name in self.slots:
            sem[("d", name)] = es.enter_context(nc.semaphore("d_" + name))
        waited = {e: {} for e in self.ENG}
        nwait = 0
        for rec in self.ops:
            e = rec["eng"]
            engine = self.eng[e]
            need = {}
            for d in rec["deps"]:
                if d[0] == "e":
                    if d[1] == e == "pe":
                        continue
                    key = ("e", d[1])
                    val = self.eops[d[1]][d[2]]["sigval"]
                else:
                    if rec["kind"] == "dma" and d[1] == rec["slot"] and self._gend(d[1], d[2]) == self._gend(rec["slot"], rec["n"]):
                        continue
                    key = ("d", d[1])
                    val = 16 * self._gend(d[1], d[2])
                if need.get(key, 0) < val:
                    need[key] = val
            for key, val in need.items():
                if waited[e].get(key, 0) < val:
                    engine.wait_ge(sem[key], val)
                    waited[e][key] = val
                    nwait += 1
            if rec["kind"] == "op":
                inst = rec["fn"](engine)
                if rec["sig"]:
                    inst.then_inc(sem[("e", e)], 1)
            else:
                inst = engine.dma_start(out=rec["out"], in_=rec["in_"], **rec["kw"])
                inst.then_inc(sem[("d", rec["slot"])], 16)
        sp = self.eng["sp"]
        for name, s in self.slots.items():
            if s["n"] > 0:
                sp.wait_ge(sem[("d", name)], 16 * s["n"])
        self.stats = {e: len(self.eops[e]) for e in self.ENG}
        self.stats["waits"] = nwait


class Region:
    def __init__(self, off, size):
        self.off, self.size, self.cur = off, size, 0

    def reset(self):
        self.cur = 0

    def take(self, nbytes):
        nbytes = (nbytes + 31) // 32 * 32
        o = self.off + self.cur
        self.cur += nbytes
        assert self.cur <= self.size, (self.cur, self.size)
        return o


class Buf:
    def __init__(self, name, ap):
        self.name = name
        self.ap = ap

    def __getitem__(self, idx):
        return self.ap[idx]

    def r(self, *k):
        return (self.name,) + k


GROUPS = [(0, 512), (512, 512), (1024, 512), (1536, 512), (2048, 256)]


def na_tables(rpb):
    blocks = [(5, 5 + r) for r in (-2, -1, 0, 1, 2)]
    for qi in (0, 1):
        blocks += [(qi, kt) for kt in range(4)]
    for qi in (14, 15):
        blocks += [(qi, kt) for kt in range(12, 16)]
    nb = len(blocks)
    H = rpb.shape[0]
    bias = np.zeros((H, 128, nb, 128), np.float32)
    mask = np.zeros((128, nb, 128), np.float32)
    kk = np.arange(128)
    for bi, (qi, kt) in enumerate(blocks):
        k_r = 2 * kt + kk // 64
        k_c = kk % 64
        q_r = 2 * qi + kk // 64
        q_c = kk % 64
        rs = np.clip(q_r - 4, 0, 24)
        cs = np.clip(q_c - 8, 0, 48)
        ok = ((k_r[:, None] >= rs[None, :]) & (k_r[:, None] < rs[None, :] + 8)
              & (k_c[:, None] >= cs[None, :]) & (k_c[:, None] < cs[None, :] + 16))
        dr = np.clip(k_r[:, None] - q_r[None, :] + 7, 0, 14)
        dc = np.clip(k_c[:, None] - q_c[None, :] + 15, 0, 30)
        bias[:, :, bi, :] = rpb[:, dr, dc]
        mask[:, bi, :] = np.where(ok, 0.0, NEG)
    return bias.reshape(H, 128, nb * 128), mask.reshape(128, nb * 128)


def na_block_ids(qi):
    if 2 <= qi <= 13:
        return [(qi + r, 2 + r) for r in (-2, -1, 0, 1, 2)]
    if qi in (0, 1):
        return [(kt, 5 + 4 * qi + kt) for kt in range(4)]
    base = 13 if qi == 14 else 17
    return [(kt, base + (kt - 12)) for kt in range(12, 16)]


def rope_tables(rot_dim):
    n_freq = rot_dim // 4
    inv = np.float32(10000.0) ** (-np.arange(n_freq, dtype=np.float32) / np.float32(n_freq))
    t = np.arange(T, dtype=np.int32)
    row = (t // 64).astype(np.float32)
    col = (t % 64).astype(np.float32)
    ang = np.concatenate([row[:, None] * inv, col[:, None] * inv], axis=-1).astype(np.float32)
    return np.cos(ang).astype(np.float32), np.sin(ang).astype(np.float32)


class Builder:
    def __init__(self, cfg):
        self.cfg = cfg
        nc = self.nc = bass.Bass("TRN2", target_bir_lowering=False)
        self.es = ExitStack()
        self.P = Prog(nc)
        self.dbg_outs = []
        self._dram()
        self.arena = self.es.enter_context(nc.sbuf_tensor("arena", [128, ARENA_BYTES // 4], F32))
        self.psall = self.es.enter_context(nc.psum_tensor("psall", [128, 4096], F32))
        self.psb = [self.psall[:, i * 512:(i + 1) * 512] for i in range(8)]
        self.R = Region(R_OFF, R_SIZE)
        self.R2 = Region(R2_OFF, R2_SIZE)
        self.HTR = Region(HT_OFF, 36864)
        self.PERS = Region(PERS_OFF, ARENA_BYTES - PERS_OFF)
        self.unit_ctr = 0
        self.ot_ctr = 0
        self.op_ctr = 0
        self._persistent()

    def _dram(self):
        nc = self.nc

        def inp(name, shape, dt=F32):
            return nc.dram_tensor(name, list(shape), dt, kind="ExternalInput").ap()
        self.d = d = {}
        d["x2"] = inp("x2", [SPC, T, D])
        d["ctx2"] = inp("ctx2", [SPC, TC, D])
        d["cvec"] = inp("cvec", [3, D])
        d["ada_w"] = inp("ada_w", [4, D, 6 * D])
        d["ada_b"] = inp("ada_b", [4, 6 * D])
        d["norm1_w"] = inp("norm1_w", [4, D])
        d["norm2_w"] = inp("norm2_w", [4, D])
        d["a_wqkv"] = inp("a_wqkv", [2, D, 1536])
        d["a_qnorm"] = inp("a_qnorm", [2, 64])
        d["a_knorm"] = inp("a_knorm", [2, 64])
        d["a_wo"] = inp("a_wo", [2, D, D])
        d["b_wdq"] = inp("b_wdq", [1, D, 384])
        d["b_qnorm_lat"] = inp("b_qnorm_lat", [1, 384])
        d["b_wuq"] = inp("b_wuq", [1, 384, 1536])
        d["b_wdkv"] = inp("b_wdkv", [1, D, 288])
        d["b_kvnorm_lat"] = inp("b_kvnorm_lat", [1, 256])
        d["b_wukv"] = inp("b_wukv", [1, 256, 2048])
        d["b_qnorm"] = inp("b_qnorm", [1, 96])
        d["b_knorm"] = inp("b_knorm", [1, 96])
        d["b_wo"] = inp("b_wo", [1, D, D])
        d["c_wqkv"] = inp("c_wqkv", [1, D, 3072])
        d["c_qnorm"] = inp("c_qnorm", [1, 64])
        d["c_knorm"] = inp("c_knorm", [1, 64])
        d["c_wo"] = inp("c_wo", [1, D, D])
        d["moe_router"] = inp("moe_router", [4, D, NE])
        d["moe_wg"] = inp("moe_wg", [4, NE, D, D])
        d["moe_wu"] = inp("moe_wu", [4, NE, D, D])
        d["moe_wd"] = inp("moe_wd", [4, NE, D, D])
        d["cst"] = inp("cst", [128, 388])
        d["selc"] = inp("selc", [16, 2048], BF16)
        d["cosA"] = inp("cosA", [T, 32])
        d["sinA"] = inp("sinA", [T, 32])
        d["cosB"] = inp("cosB", [T, 16])
        d["sinB"] = inp("sinB", [T, 16])
        d["na_bias"] = inp("na_bias", [16, 128, 21 * 128])
        d["na_mask"] = inp("na_mask", [128, 21 * 128])
        self.y2 = nc.dram_tensor("y2", [SPC, T, D], F32, kind="ExternalOutput").ap()

    def dbg_out(self, name, shape, dt=F32):
        ap = self.nc.dram_tensor("dbg_" + name, list(shape), dt, kind="ExternalOutput").ap()
        self.dbg_outs.append("dbg_" + name)
        return ap

    def carve(self, name, off, shape, dt):
        n = int(np.prod(shape[1:]))
        nbytes = n * _dsize(dt)
        assert off % 4 == 0 and nbytes % 4 == 0, (name, off, nbytes)
        assert off + nbytes <= ARENA_BYTES, (name, off, nbytes)
        ap = self.arena[0:shape[0], off // 4:(off + nbytes) // 4]
        if dt != F32:
            ap = ap.bitcast(dt)
        if len(shape) == 3:
            ap = ap.rearrange("p (a b) -> p a b", b=shape[2])
        elif len(shape) == 4:
            ap = ap.rearrange("p (a b c) -> p a b c", b=shape[2], c=shape[3])
        elif len(shape) == 5:
            ap = ap.rearrange("p (a b c d) -> p a b c d", b=shape[2], c=shape[3], d=shape[4])
        return Buf(name, ap)

    def alloc(self, region, name, shape, dt):
        n = int(np.prod(shape[1:])) * _dsize(dt)
        return self.carve(name, region.take(n), shape, dt)

    def psbf(self, i):
        return self.psall[:, i * 512:(i + 1) * 512].bitcast(BF16)

    def mm(self, out, lhsT, rhs, start=True, stop=True, R=(), W=()):
        self.P.op("pe", lambda e: e.matmul(out, lhsT, rhs, start=start, stop=stop), R, W)

    def tr(self, out, in_, ident, R=(), W=()):
        self.P.op("pe", lambda e: e.transpose(out, in_, ident), R, W)

    def act(self, out, in_, func, bias=None, scale=None, accum=None, R=(), W=()):
        kw = {}
        if bias is not None:
            kw["bias"] = bias
        if scale is not None:
            kw["scale"] = scale
        if accum is not None:
            kw["accum_out"] = accum
        self.P.op("act", lambda e: e.activation(out=out, in_=in_, func=func, **kw), R, W)

    def tt(self, eng, out, in0, in1, op, R=(), W=()):
        self.P.op(eng, lambda e: e.tensor_tensor(out=out, in0=in0, in1=in1, op=op), R, W)

    def ts(self, eng, out, in0, s1, op0, s2=None, op1=None, R=(), W=()):
        if op1 is None:
            self.P.op(eng, lambda e: e.tensor_scalar(out=out, in0=in0, scalar1=s1, scalar2=None, op0=op0), R, W)
        else:
            self.P.op(eng, lambda e: e.tensor_scalar(out=out, in0=in0, scalar1=s1, scalar2=s2, op0=op0, op1=op1), R, W)

    def stt(self, out, in0, scalar, in1, op0, op1, R=(), W=()):
        self.P.op("dve", lambda e: e.scalar_tensor_tensor(out=out, in0=in0, scalar=scalar, in1=in1, op0=op0, op1=op1), R, W)

    def cp(self, eng, out, in_, R=(), W=()):
        if eng == "act":
            self.P.op("act", lambda e: e.activation(out=out, in_=in_, func=AF.Copy), R, W)
        else:
            self.P.op(eng, lambda e: e.tensor_copy(out=out, in_=in_), R, W)

    def red(self, out, in_, op, R=(), W=()):
        self.P.op("dve", lambda e: e.tensor_reduce(out=out, in_=in_, axis=AX.X, op=op), R, W)

    def memset(self, eng, ap, val, R=(), W=()):
        self.P.op(eng, lambda e: e.memset(ap, val), R, W)

    def dma(self, queue, slot, out, in_, R=(), W=(), group=False, bar=True, **kw):
        self.P.dma(queue, slot, out, in_, R, W, group=group, bar=bar, **kw)

    def _persistent(self):
        A = self.alloc
        PR = self.PERS
        self.xT = self.carve("xT", XT_OFF, [128, 8, TT], F32)
        self.ring = [self.carve("ring%d" % i, RING_OFF + i * UNIT, [128, 8, 512], BF16) for i in range(NUNIT)]
        self.cstb = A(PR, "cstb", [128, 388], F32)
        self.identf = self.cstb[:, 0:128]
        self.iotac = self.cstb[:, 128:384]
        self.misc = self.cstb[:, 384:388]
        self.identb = A(PR, "identb", [128, 128], BF16)
        self.onesb = A(PR, "onesb", [128, 128], BF16)
        self.sel = A(PR, "sel", [16, 16, 128], BF16)
        self.modv = A(PR, "modv", [128, 4, 3, 6, 8], F32)
        self.cosA = A(PR, "cosA", [128, 16, 32], F32)
        self.sinA = A(PR, "sinA", [128, 16, 32], F32)
        self.cosB = A(PR, "cosB", [128, 16, 16], F32)
        self.sinB = A(PR, "sinB", [128, 16, 16], F32)
        self.smallp = A(PR, "smallp", [128, 64], F32)

    def prologue(self):
        d = self.d
        self.dma("sp", "c0", self.cstb[:, :], d["cst"][:, :], W=[("cst",)])
        self.dma("sp", "c0", self.sel[:, :, :], d["selc"].rearrange("k (e m) -> k e m", m=128), W=[("sel",)], group=True)
        for nm, buf in (("cosA", self.cosA), ("sinA", self.sinA), ("cosB", self.cosB), ("sinB", self.sinB)):
            self.dma("sp", "c0", buf[:, :, :], d[nm].rearrange("(t p) n -> p t n", p=128), W=[(nm,)], group=True)
        self.cp("dve", self.identb[:, :], self.identf, R=[("cst",)], W=[("identb",)])
        self.memset("pool", self.onesb[:, :], 1.0, W=[("onesb",)])
        R = self.R
        R.reset()
        vr = self.alloc(R, "vr", [128, 128], F32)
        vr2 = self.alloc(R, "vr2", [128, 128], F32)
        vr3 = self.alloc(R, "vr3", [128, 128], F32)
        vT = self.alloc(R, "vT", [128, 88], F32)
        abT = self.alloc(R, "abT", [128, 192], F32)
        sT = self.alloc(R, "sT", [128, 8, 4], F32)
        modraw = self.alloc(R, "modraw", [128, 4, 48, 3], F32)
        wslot = [self.carve("adaw0", HT_OFF, [128, 8, 768], F32),
                 self.alloc(R, "adaw1", [128, 8, 768], F32)]
        self.dma("sp", "p0", vr[0:24, :], d["cvec"].rearrange("s (c p) -> (s c) p", p=128), W=[("vr",)])
        self.dma("sp", "p0", vr[24:56, :], d["norm1_w"].rearrange("l (c p) -> (l c) p", p=128), W=[("vr",)], group=True)
        self.dma("sp", "p0", vr[56:88, :], d["norm2_w"].rearrange("l (c p) -> (l c) p", p=128), W=[("vr",)], group=True)
        abrows = d["ada_b"].rearrange("l (k p) -> (l k) p", p=128)
        self.dma("sp", "p0", vr2[:, :], abrows[0:128, :], W=[("vr2",)], group=True)
        self.dma("sp", "p0", vr3[0:64, :], abrows[128:192, :], W=[("vr3",)], group=True)
        ps = self.psb[0]
        self.tr(ps[:, 0:88], vr[0:88, :], self.identf[0:88, 0:88], R=[("vr",), ("cst",)], W=[("ps", 0)])
        self.cp("dve", vT[:, :], ps[:, 0:88], R=[("ps", 0)], W=[("vT",)])
        ps1 = self.psb[1]
        self.tr(ps1[:, 0:128], vr2[:, :], self.identf, R=[("vr2",), ("cst",)], W=[("ps", 1)])
        self.tr(ps1[:, 128:192], vr3[0:64, :], self.identf[0:64, 0:64], R=[("vr3",), ("cst",)], W=[("ps", 1)])
        self.cp("dve", abT[:, :], ps1[:, 0:192], R=[("ps", 1)], W=[("abT",)])
        self.memset("dve", sT[:, :, :], 0.0, W=[("sT",)])
        cTv = vT[:, 0:24].rearrange("p (s c) -> p c s", c=8)
        self.act(sT[:, :, 0:3], cTv, AF.Silu, R=[("vT",)], W=[("sT",)])
        n1 = vT[:, 24:56].rearrange("p (l c) -> p l c", c=8)
        n2 = vT[:, 56:88].rearrange("p (l c) -> p l c", c=8)
        for l in range(4):
            mps = self.psb[2 + (l % 2)]
            for cb in range(8):
                u = l * 8 + cb
                w = wslot[u % 2]
                self.dma("sp", "aw%d" % (u % 2), w[:, :, :],
                         d["ada_w"][l].rearrange("(c p) f -> p c f", p=128)[:, :, cb * 768:(cb + 1) * 768],
                         W=[("adaw", u % 2)])
                for fc in range(6):
                    col = (cb * 6 + fc) * 4
                    for dc in range(8):
                        self.mm(mps[:, col:col + 4], w[:, dc, fc * 128:(fc + 1) * 128], sT[:, dc, :],
                                start=(dc == 0), stop=(dc == 7),
                                R=[("adaw", u % 2), ("sT",)], W=[("ps", 2 + (l % 2))])
            mv = mps[:, 0:192].rearrange("p (k s) -> p k s", s=4)[:, :, 0:3]
            bv = abT[:, l * 48:(l + 1) * 48].unsqueeze(2).to_broadcast([128, 48, 3])
            self.tt("dve", modraw[:, l, :, :], mv, bv, ALU.add, R=[("ps", 2 + (l % 2)), ("abT",)], W=[("modraw", l)])
            mr = modraw[:, l, :, :].rearrange("p (k c) s -> p k s c", c=8)
            for kind, k in ((1, 0), (2, 2), (4, 3), (5, 5)):
                self.cp("dve", self.modv[:, l, :, kind, :], mr[:, k, :, :], R=[("modraw", l)], W=[("modv",)])
            for kind, k, nn in ((0, 1, n1), (3, 4, n2)):
                nb = nn[:, l, :].unsqueeze(1).to_broadcast([128, 3, 8])
                self.stt(self.modv[:, l, :, kind, :], mr[:, k, :, :], 1.0, nb, ALU.add, ALU.mult,
                         R=[("modraw", l), ("vT",)], W=[("modv",)])
        self.P.barrier()

    def mod(self, l, s, kind, c=None):
        if c is None:
            return self.modv[:, l, s, kind, :]
        return self.modv[:, l, s, kind, c:c + 1]

    def load_sample(self, b):
        R = self.R
        R.reset()
        stg = [self.alloc(R, "stg%d" % i, [128, D], F32) for i in range(2)]
        for t in range(NT):
            src = self.d["x2"][b, t * 128:(t + 1) * 128, :] if t < 16 else self.d["ctx2"][b, (t - 16) * 128:(t - 15) * 128, :]
            s = stg[t % 2]
            self.dma("sp", "ld%d" % (t % 2), s[:, :], src, W=[("stg", t % 2)])
            for half in range(2):
                bank = (t % 2) * 2 + half
                ps = self.psb[bank]
                for j in range(4):
                    c = half * 4 + j
                    self.tr(ps[:, j * 128:(j + 1) * 128], s[:, c * 128:(c + 1) * 128], self.identf,
                            R=[("stg", t % 2), ("cst",)], W=[("ps", bank)])
                self.cp("act" if half == 0 else "dve", self.xT[:, half * 4:(half + 1) * 4, t * 128:(t + 1) * 128],
                        ps[:, :].rearrange("p (j n) -> p j n", n=128),
                        R=[("ps", bank)], W=[("xT", min(t // 4, 4), half * 4 + j) for j in range(4)])
        self.P.barrier()

    def store_sample(self, dst, ntiles):
        R = self.R
        R.reset()
        stg = [self.alloc(R, "ostg%d" % i, [128, D], F32) for i in range(2)]
        for t in range(ntiles):
            s = stg[t % 2]
            for half in range(2):
                bank = (t % 2) * 2 + half
                ps = self.psb[bank]
                for j in range(4):
                    c = half * 4 + j
                    self.tr(ps[:, j * 128:(j + 1) * 128], self.xT[:, c, t * 128:(t + 1) * 128], self.identf,
                            R=[("xT", min(t // 4, 4), c), ("cst",)], W=[("ps", bank)])
                self.cp("act" if half == 0 else "dve", s[:, half * 512:(half + 1) * 512], ps[:, :],
                        R=[("ps", bank)], W=[("ostg", t % 2)])
            self.dma("sp", "st%d" % (t % 2), dst[t * 128:(t + 1) * 128, :], s[:, :], R=[("ostg", t % 2)])
        self.P.barrier()

    def norm_bufs(self):
        R = self.R
        self.sq = [self.alloc(R, "sq%d" % i, [128, 8, 512], BF16) for i in range(2)]
        self.lnv = self.alloc(R, "lnv", [128, 512], F32)
        self.rstd = [self.alloc(R, "rstd%d" % i, [128, 512], F32) for i in range(2)]
        self.tmpn = [self.alloc(R, "tmpn%d" % i, [128, 512], F32) for i in range(2)]

    def norm_group(self, gi, l, b, which, dst_of):
        t0, w = GROUPS[gi]
        s = b if gi < 4 else 2
        ka, kb = (0, 1) if which == 1 else (3, 4)
        sq = self.sq[gi % 2]
        self.act(sq[:, :, 0:w], self.xT[:, :, t0:t0 + w], AF.Square, R=[("xT", gi, c) for c in range(8)], W=[("sq", gi % 2)])
        bank = 5 + (gi % 2)
        ps = self.psb[bank]
        for c in range(8):
            self.mm(ps[:, 0:w], self.onesb[:, :], sq[:, c, 0:w], start=(c == 0), stop=(c == 7),
                    R=[("sq", gi % 2), ("onesb",)], W=[("ps", bank)])
        self.act(self.lnv[:, 0:w], ps[:, 0:w], AF.Ln, bias=self.misc[:, 2:3], scale=1.0 / D,
                 R=[("ps", bank), ("cst",)], W=[("lnv",)])
        rstd = self.rstd[gi % 2]
        self.act(rstd[:, 0:w], self.lnv[:, 0:w], AF.Exp, scale=-0.5, R=[("lnv",)], W=[("rstd", gi % 2)])
        for c in range(8):
            tm = self.tmpn[c % 2]
            self.tt("dve", tm[:, 0:w], self.xT[:, c, t0:t0 + w], rstd[:, 0:w], ALU.mult,
                    R=[("xT", gi, c), ("rstd", gi % 2)], W=[("tmpn", c % 2)])
            dst, dres = dst_of(c)
            self.ts("pool", dst, tm[:, 0:w], self.mod(l, s, ka, c), ALU.mult, self.mod(l, s, kb, c), ALU.add,
                    R=[("tmpn", c % 2), ("modv",)], W=[dres])

    def attn_core(self, steps_cfg, scale):
        steps = []
        for hi, hc in enumerate(steps_cfg):
            kts = hc["ktiles"]
            npair = (len(kts) + 1) // 2
            for pi in range(npair):
                ks = kts[2 * pi:2 * pi + 2]
                steps.append((hi, hc, ks, pi == 0, pi == npair - 1))
        n = len(steps)

        def qk(i):
            hi, hc, ks, first, last = steps[i]
            s_ = i % 2
            for j, k in enumerate(ks):
                bank = 2 * s_ + j
                self.mm(self.psb[bank][:, 0:hc["w"]], hc["kt"][:, k * 128:(k + 1) * 128], hc["qt"][:, hc["t0"]:hc["t0"] + hc["w"]],
                        R=[hc["kres"], hc["qres"]], W=[("ps", bank)])

        def ex(i):
            hi, hc, ks, first, last = steps[i]
            s_ = i % 2
            w = hc["w"]
            nk = len(ks)
            src = self.psall[:, s_ * 1024:s_ * 1024 + nk * 512].rearrange("p (b n) -> p b n", n=512)[:, :, 0:w]
            dst = self.PT[:, s_, 0:nk * 512].rearrange("p (b n) -> p b n", n=512)[:, :, 0:w]
            self.act(dst, src, AF.Exp, scale=scale, R=[("ps", 2 * s_ + j) for j in range(nk)], W=[("pt", s_)])

        def pv(i):
            hi, hc, ks, first, last = steps[i]
            w = hc["w"]
            s_ = i % 2
            ob = 4 + (hi + self.ot_ctr) % 2
            for j, k in enumerate(ks):
                self.mm(self.psb[ob][:, 0:w], hc["va"](k), self.PT[:, s_, j * 512:j * 512 + w],
                        start=(first and j == 0), stop=(last and j == len(ks) - 1),
                        R=[("pt", s_), hc["vres"]], W=[("ps", ob)])
            if last:
                lo, hi_ = (slice(0, 64), slice(64, 128))
                o_sl, d_sl = (lo, hi_) if hc["o_lo"] else (hi_, lo)
                rec_o = self.rec[d_sl, 0:w]
                rec_i = self.psb[ob][d_sl, 0:w]
                self.P.op("dve", lambda e: e.reciprocal(out=rec_o, in_=rec_i), [("ps", ob)], [("rec",)])
                self.tt("dve", hc["att"][o_sl, hc["t0"]:hc["t0"] + w], self.psb[ob][o_sl, 0:w], self.rec[d_sl, 0:w], ALU.mult,
                        R=[("ps", ob), ("rec",)], W=[hc["ares"]])

        if n == 0:
            return
        qk(0)
        for i in range(n):
            if i + 1 < n:
                qk(i + 1)
            ex(i)
            pv(i)
        self.ot_ctr += len(steps_cfg)

    def out_proj(self, wo, wres, att, ares, l, b, need_ctx):
        ng = 5 if need_ctx else 4
        n = 0
        for gi in range(ng):
            t0, w = GROUPS[gi]
            s = b if gi < 4 else 2
            for dc in range(8):
                bank = 5 + (self.op_ctr % 2)
                self.op_ctr += 1
                ps = self.psb[bank]
                self.mm(ps[:, 0:w], wo[:, dc * 128:(dc + 1) * 128], att[:, t0:t0 + w], R=[wres, ares], W=[("ps", bank)])
                xs = self.xT[:, dc, t0:t0 + w]
                self.stt(xs, ps[:, 0:w], self.mod(l, s, 2, dc), xs, ALU.mult, ALU.add,
                         R=[("ps", bank), ("modv",), ("xT", gi, dc)], W=[("xT", gi, dc)])

    def head_rstd(self, src3, nh, hd, sqt, ssq, lnv, rs, res_src):
        self.tt("dve", sqt, src3, src3, ALU.mult, R=[res_src], W=[("sqt",)])
        self.red(ssq[:, 0:nh], sqt, ALU.add, R=[("sqt",)], W=[("ssq",)])
        self.act(lnv[:, 0:nh], ssq[:, 0:nh], AF.Ln, bias=self.misc[:, 2:3], scale=1.0 / hd, R=[("ssq",), ("cst",)], W=[("lnvh",)])
        self.act(rs[:, 0:nh], lnv[:, 0:nh], AF.Exp, scale=-0.5, R=[("lnvh",)], W=[("rsh",)])

    def bcast_row(self, queue, slot, dst, src_row, n, W, group=False):
        self.dma(queue, slot, dst, src_row.unsqueeze(0).to_broadcast([128, n]), W=W, group=group)

    def gqa_layer(self, l, b, need_ctx):
        d = self.d
        j = l // 3
        R, R2 = self.R, self.R2
        R.reset()
        R2.reset()
        self.norm_bufs()
        hT = self.carve("hT", HT_OFF, [128, 8, TT], BF16)
        for gi in range(5):
            t0, w = GROUPS[gi]
            self.norm_group(gi, l, b, 1, lambda c, t0=t0, w=w, gi=gi: (hT[:, c, t0:t0 + w], ("hT", gi)))
        self.P.barrier()
        R.reset()
        QKT = self.alloc(R, "QKT", [128, 5, TT], BF16)
        VA = self.alloc(R, "VA", [128, NT, 192], BF16)
        attT = self.alloc(R, "attT", [128, TT], BF16)
        Wg = self.alloc(R2, "Wg", [128, 8, 384], BF16)
        WoP = self.alloc(R2, "WoP", [128, 2, D], BF16)
        self.PT = self.alloc(R, "PT", [128, 2, 1024], BF16)
        self.rec = self.alloc(R, "rec", [128, 512], F32)
        gq = self.alloc(R, "gq", [128, 5, 64], F32)
        raw = [self.alloc(R2, "raw%d" % i, [128, 384], F32) for i in range(3)]
        self.memset("pool", QKT[64:128, 0:2, :], 0.0, W=[("QKTz",)])
        self.memset("pool", QKT[0:64, 2:4, :], 0.0, W=[("QKTz",)])
        sqt = self.alloc(R2, "sqt", [128, 5, 64], F32)
        t1 = self.alloc(R2, "t1", [128, 5, 64], F32)
        t2 = self.alloc(R2, "t2", [128, 5, 64], F32)
        ra = self.alloc(R2, "ra", [128, 5, 32], F32)
        rb = self.alloc(R2, "rb", [128, 5, 32], F32)
        rc = self.alloc(R2, "rc", [128, 5, 32], F32)
        rd = self.alloc(R2, "rd", [128, 5, 32], F32)
        qkb = [self.alloc(R2, "qkb%d" % i, [128, 6, 64], BF16) for i in range(2)]
        ssq = [self.smallp[:, 0:8], self.smallp[:, 8:16]]
        lnv = [self.smallp[:, 16:24], self.smallp[:, 24:32]]
        rs = [self.smallp[:, 32:40], self.smallp[:, 40:48]]
        for i in range(4):
            self.bcast_row("sp", "gq", gq[:, i, :], d["a_qnorm"][j], 64, W=[("gq",)], group=(i > 0))
        self.bcast_row("sp", "gq", gq[:, 4, :], d["a_knorm"][j], 64, W=[("gq",)], group=True)
        self.memset("pool", VA[:, :, 0:64], 1.0, W=[("VA", t) for t in range(NT)])
        self.memset("pool", VA[:, :, 128:192], 1.0, W=[("VA", t) for t in range(NT)])
        wq = d["a_wqkv"][j].rearrange("(c p) f -> p c f", p=128)
        ptb = self.psbf(7)
        ntl = NT
        for g in range(4):
            self.dma("pool", "wg", Wg[:, :, 0:256], wq[:, :, 256 * g:256 * g + 256], W=[("Wg",)], bar=False)
            self.dma("pool", "wg", Wg[:, :, 256:320], wq[:, :, 1024 + 64 * g:1024 + 64 * g + 64], W=[("Wg",)], group=True, bar=False)
            self.dma("pool", "wg", Wg[:, :, 320:384], wq[:, :, 1280 + 64 * g:1280 + 64 * g + 64], W=[("Wg",)], group=True, bar=False)
            def S1(t):
                bank = 5 + (t % 2)
                ps = self.psb[bank]
                for c in range(8):
                    self.mm(ps[:, 0:384], hT[:, c, t * 128:(t + 1) * 128], Wg[:, c, :], start=(c == 0), stop=(c == 7),
                            R=[("hT", min(t // 4, 4)), ("Wg",)], W=[("ps", bank)])
                self.cp("act", raw[t % 3][:, :], ps[:, 0:384], R=[("ps", bank)], W=[("raw", t % 3)])

            def S2(t):
                rw = raw[t % 3]
                r3 = rw[:, 0:320].rearrange("p (h e) -> p h e", e=64)
                self.stt(sqt[:, :, :], r3, 1.0 / 64, r3, ALU.mult, ALU.mult, R=[("raw", t % 3)], W=[("sqt",)])
                self.red(ssq[t % 2][:, 0:5], sqt[:, :, :], ALU.add, R=[("sqt",)], W=[("ssq", t % 2)])

            def S3(t):
                self.act(lnv[t % 2][:, 0:5], ssq[t % 2][:, 0:5], AF.Ln, bias=self.misc[:, 2:3], scale=1.0,
                         R=[("ssq", t % 2), ("cst",)], W=[("lnvh", t % 2)])
                self.act(rs[t % 2][:, 0:5], lnv[t % 2][:, 0:5], AF.Exp, scale=-0.5, R=[("lnvh", t % 2)], W=[("rsh", t % 2)])

            def S4(t):
                rw = raw[t % 3]
                r3 = rw[:, 0:320].rearrange("p (h e) -> p h e", e=64)
                self.tt("dve", t1[:, :, :], r3, rs[t % 2][:, 0:5].unsqueeze(2).to_broadcast([128, 5, 64]), ALU.mult,
                        R=[("raw", t % 3), ("rsh", t % 2)], W=[("t1",)])
                self.tt("dve", t2[:, :, :], t1[:, :, :], gq[:, :, :], ALU.mult, R=[("t1",), ("gq",)], W=[("t2",)])
                qb = qkb[t % 2]
                if t < 16:
                    x1 = t2[:, :, 0:32]
                    x2 = t2[:, :, 32:64]
                    cs = self.cosA[:, t, :].unsqueeze(1).to_broadcast([128, 5, 32])
                    sn = self.sinA[:, t, :].unsqueeze(1).to_broadcast([128, 5, 32])
                    self.tt("dve", ra[:, :, :], x1, cs, ALU.mult, R=[("t2",), ("cosA",)], W=[("ra",)])
                    self.tt("pool", rb[:, :, :], x2, sn, ALU.mult, R=[("t2",), ("sinA",)], W=[("rb",)])
                    self.tt("dve", qb[:, 0:5, 0:32], ra[:, :, :], rb[:, :, :], ALU.subtract, R=[("ra",), ("rb",)], W=[("qkb", t % 2)])
                    self.tt("pool", rc[:, :, :], x1, sn, ALU.mult, R=[("t2",), ("sinA",)], W=[("rc",)])
                    self.tt("dve", rd[:, :, :], x2, cs, ALU.mult, R=[("t2",), ("cosA",)], W=[("rd",)])
                    self.tt("dve", qb[:, 0:5, 32:64], rc[:, :, :], rd[:, :, :], ALU.add, R=[("rc",), ("rd",)], W=[("qkb", t % 2)])
                else:
                    self.cp("dve", qb[:, 0:5, :], t2[:, :, :], R=[("t2",)], W=[("qkb", t % 2)])
                self.cp("pool", qb[:, 5, :], qb[:, 4, :], R=[("qkb", t % 2)], W=[("qkb", t % 2)])
                self.cp("pool", VA[:, t, 64:128], rw[:, 320:384], R=[("raw", t % 3)], W=[("VA", t)])

            def S5(t):
                qb = qkb[t % 2]
                qf = qb[:, :, :].rearrange("p h e -> p (h e)")
                for i in range(3):
                    self.tr(ptb[:, i * 128:(i + 1) * 128], qf[:, i * 128:(i + 1) * 128], self.identb[:, :],
                            R=[("qkb", t % 2), ("identb",)], W=[("ps", 7)])
                self.cp("act", QKT[0:64, 0:2, t * 128:(t + 1) * 128], ptb[0:64, 0:256].rearrange("p (i n) -> p i n", n=128),
                        R=[("ps", 7)], W=[("QKT", t)])
                self.cp("act", QKT[64:128, 2:4, t * 128:(t + 1) * 128], ptb[64:128, 0:256].rearrange("p (i n) -> p i n", n=128),
                        R=[("ps", 7)], W=[("QKT", t)])
                self.cp("dve", QKT[:, 4, t * 128:(t + 1) * 128], ptb[:, 256:384], R=[("ps", 7)], W=[("QKT", t)])

            self.pipeline(ntl, S1, S2, S3, S4, S5)
            for p in range(2):
                slot = (g * 2 + p) % 2
                h0 = 4 * g + 2 * p
                self.dma("pool", "wo%d" % slot, WoP[:, slot, :], d["a_wo"][j][h0 * 64:h0 * 64 + 128, :], W=[("WoP", slot)], bar=False)
                cfgs = []
                qgroups = [0, 1, 2, 3] + ([4] if need_ctx else [])
                for gi in qgroups:
                    t0, w = GROUPS[gi]
                    ktiles = list(range(NT)) if gi < 4 else [16, 17]
                    for hs in range(2):
                        psl = slice(0, 64) if hs == 0 else slice(64, 128)
                        cfgs.append(dict(
                            qt=QKT[:, p + 2 * hs, :], kt=QKT[:, 4, :],
                            va=(lambda k, hs=hs: VA[:, k, 64:192] if hs == 0 else VA[:, k, 0:128]),
                            o_lo=(hs == 0), t0=t0, w=w, ktiles=ktiles, att=attT,
                            qres=("QKTall",), kres=("QKTall",), vres=("VAall",), ares=("attT",)))
                self._alias([("QKT", t) for t in range(ntl)] + [("QKTz",)], ("QKTall",))
                self._alias([("VA", t) for t in range(ntl)], ("VAall",))
                self.attn_core(cfgs, 0.125)
                self.out_proj(WoP[:, slot, :], ("WoP", slot), attT, ("attT",), l, b, need_ctx)
            self._alias_release([("QKT", t) for t in range(ntl)], ("QKTall",))
            self._alias_release([("VA", t) for t in range(ntl)], ("VAall",))
        self.P.barrier()

    def pipeline(self, n, S1, S2, S3, S4, S5):
        for i in range(-1, n + 1):
            if 0 <= i + 1 < n:
                S1(i + 1)
            if 0 <= i < n:
                S2(i)
                S3(i)
            if 0 <= i - 1 < n:
                S4(i - 1)
                S5(i - 1)

    def _alias(self, fine, coarse):
        P = self.P
        toks = set()
        for f in fine:
            st = P.res.get(f)
            if st is not None and st["w"] is not None:
                toks.add(st["w"])
        P.res[coarse] = {"w": None, "r": {}, "ws": toks}
        for e in ("pe",):
            P.pending[e] |= toks

    def _alias_release(self, fine, coarse):
        P = self.P
        st = P.res.get(coarse)
        if st is None:
            return
        for f in fine:
            fs = P.res.get(f)
            if fs is None:
                fs = P.res[f] = {"w": None, "r": {}}
            for k, v in st["r"].items():
                fs["r"][("al", coarse, k)] = v

    def mla_layer(self, l, b, need_ctx):
        d = self.d
        R, R2, HTR = self.R, self.R2, self.HTR
        R.reset()
        R2.reset()
        HTR.reset()
        self.norm_bufs()
        hT = self.carve("hT", HT_OFF, [128, 8, TT], BF16)
        for gi in range(5):
            t0, w = GROUPS[gi]
            self.norm_group(gi, l, b, 1, lambda c, t0=t0, w=w, gi=gi: (hT[:, c, t0:t0 + w], ("hT", gi)))
        self.P.barrier()
        R.reset()
        cT = self.alloc(R, "cT", [128, 5, TT], BF16)
        krope = self.alloc(R, "krope", [128, NT, 32], BF16)
        self.PT = self.alloc(R, "PT", [128, 2, 1024], BF16)
        self.rec = self.alloc(R, "rec", [128, 512], F32)
        gq2 = self.alloc(R, "gq2", [128, 2, 96], F32)
        gk2 = self.alloc(R, "gk2", [128, 2, 64], F32)
        gk = self.alloc(R, "gkr", [128, 32], F32)
        W1 = self.alloc(R, "W1", [128, 8, 672], BF16)
        g1 = self.alloc(R, "g1", [128, 640], F32)
        raw1 = [self.alloc(R2, "rawm%d" % i, [128, 672], F32) for i in range(2)]
        cn = [self.alloc(R2, "cn%d" % i, [128, 640], BF16) for i in range(2)]
        sq1 = self.alloc(R2, "sq1", [128, 384], F32)
        kr1 = self.alloc(R2, "kr1", [128, 32], F32)
        rt = [self.alloc(R2, "rt%d" % i, [128, 2, 16], F32) for i in range(4)]
        raw2 = [self.alloc(R2, "rawn%d" % i, [128, 448], F32) for i in range(3)]
        ssq2 = [self.smallp[:, 36:44], self.smallp[:, 44:52]]
        lnv2 = [self.smallp[:, 52:58], self.smallp[:, 58:64]]
        rs2 = [self.alloc(R2, "rs2_%d" % i, [128, 8], F32) for i in range(2)]
        sqt = self.alloc(R2, "sqt", [128, 2, 64], F32)
        t1 = self.alloc(R2, "t1", [128, 2, 96], F32)
        tq = self.alloc(R2, "tq", [128, 2, 32], F32)
        k1 = self.alloc(R2, "k1", [128, 2, 64], F32)
        qa = [self.alloc(R2, "qa%d" % i, [128, 2, 96], BF16) for i in range(2)]
        ka = [self.alloc(R2, "ka%d" % i, [128, 2, 96], BF16) for i in range(2)]
        ssq = self.smallp[:, 0:8]
        lnv = self.smallp[:, 8:16]
        rs = self.smallp[:, 16:24]
        ssq1 = self.smallp[:, 24:28]
        lnv1 = self.smallp[:, 28:32]
        rs1 = self.smallp[:, 32:36]
        self.bcast_row("sp", "gq", g1[:, 0:384], d["b_qnorm_lat"][0], 384, W=[("g1",)])
        self.bcast_row("sp", "gq", g1[:, 384:640], d["b_kvnorm_lat"][0], 256, W=[("g1",)], group=True)
        self.bcast_row("sp", "gq", gk[:, :], d["b_knorm"][0, 64:96], 32, W=[("gk",)], group=True)
        for i in range(2):
            self.bcast_row("sp", "gq", gq2[:, i, :], d["b_qnorm"][0], 96, W=[("gq2",)], group=True)
            self.bcast_row("sp", "gq", gk2[:, i, :], d["b_knorm"][0, 0:64], 64, W=[("gk2",)], group=True)
        self.dma("pool", "wg", W1[:, :, 0:384], d["b_wdq"][0].rearrange("(c p) f -> p c f", p=128), W=[("W1",)], bar=False)
        self.dma("pool", "wg", W1[:, :, 384:672], d["b_wdkv"][0].rearrange("(c p) f -> p c f", p=128), W=[("W1",)], group=True, bar=False)
        ptb = self.psbf(7)
        for t in range(NT):
            ba = 3 + 2 * (t % 2)
            bb = ba + 1
            for c in range(8):
                self.mm(self.psb[ba][:, :], hT[:, c, t * 128:(t + 1) * 128], W1[:, c, 0:512], start=(c == 0), stop=(c == 7),
                        R=[("hT", min(t // 4, 4)), ("W1",)], W=[("ps", ba)])
            for c in range(8):
                self.mm(self.psb[bb][:, 0:160], hT[:, c, t * 128:(t + 1) * 128], W1[:, c, 512:672], start=(c == 0), stop=(c == 7),
                        R=[("hT", min(t // 4, 4)), ("W1",)], W=[("ps", bb)])
            rw = raw1[t % 2]
            self.cp("act", rw[:, 0:512], self.psb[ba][:, :], R=[("ps", ba)], W=[("rawm", t % 2)])
            self.cp("act", rw[:, 512:672], self.psb[bb][:, 0:160], R=[("ps", bb)], W=[("rawm", t % 2)])
            for i, (c0, n) in enumerate(((0, 384), (384, 256), (640, 32))):
                self.act(sq1[:, 0:n], rw[:, c0:c0 + n], AF.Square, accum=ssq1[:, i:i + 1], R=[("rawm", t % 2)], W=[("sq1",), ("ssq1", i)])
                self.act(lnv1[:, i:i + 1], ssq1[:, i:i + 1], AF.Ln, bias=self.misc[:, 2:3], scale=1.0 / n, R=[("ssq1", i), ("cst",)], W=[("lnv1", i)])
            self.act(rs1[:, 0:3], lnv1[:, 0:3], AF.Exp, scale=-0.5, R=[("lnv1", 0), ("lnv1", 1), ("lnv1", 2)], W=[("rs1",)])
            cnb = cn[t % 2]
            self.stt(cnb[:, 0:384], rw[:, 0:384], rs1[:, 0:1], g1[:, 0:384], ALU.mult, ALU.mult, R=[("rawm", t % 2), ("rs1",), ("g1",)], W=[("cn", t % 2)])
            self.stt(cnb[:, 384:640], rw[:, 384:640], rs1[:, 1:2], g1[:, 384:640], ALU.mult, ALU.mult, R=[("rawm", t % 2), ("rs1",), ("g1",)], W=[("cn", t % 2)])
            self.stt(kr1[:, :], rw[:, 640:672], rs1[:, 2:3], gk[:, :], ALU.mult, ALU.mult, R=[("rawm", t % 2), ("rs1",), ("gk",)], W=[("kr1",)])
            if t < 16:
                x1 = kr1[:, 0:16]
                x2 = kr1[:, 16:32]
                cs = self.cosB[:, t, :]
                sn = self.sinB[:, t, :]
                self.tt("dve", rt[0][:, 0, :], x1, cs, ALU.mult, R=[("kr1",), ("cosB",)], W=[("rt", 0)])
                self.tt("dve", rt[1][:, 0, :], x2, sn, ALU.mult, R=[("kr1",), ("sinB",)], W=[("rt", 1)])
                self.tt("dve", krope[:, t, 0:16], rt[0][:, 0, :], rt[1][:, 0, :], ALU.subtract, R=[("rt", 0), ("rt", 1)], W=[("krope", t)])
                self.tt("dve", rt[2][:, 0, :], x1, sn, ALU.mult, R=[("kr1",), ("sinB",)], W=[("rt", 2)])
                self.tt("dve", rt[3][:, 0, :], x2, cs, ALU.mult, R=[("kr1",), ("cosB",)], W=[("rt", 3)])
                self.tt("dve", krope[:, t, 16:32], rt[2][:, 0, :], rt[3][:, 0, :], ALU.add, R=[("rt", 2), ("rt", 3)], W=[("krope", t)])
            else:
                self.cp("dve", krope[:, t, :], kr1[:, :], R=[("kr1",)], W=[("krope", t)])
            for i in range(5):
                self.tr(ptb[:, i * 128:(i + 1) * 128], cnb[:, i * 128:(i + 1) * 128], self.identb[:, :],
                        R=[("cn", t % 2), ("identb",)], W=[("ps", 7)])
            self.cp("dve", cT[:, :, t * 128:(t + 1) * 128], ptb[:, 0:640].rearrange("p (i n) -> p i n", n=128),
                    R=[("ps", 7)], W=[("cT", t)])
        self.P.barrier()
        QKT = self.alloc(HTR, "QKT", [128, 4, TT], BF16)
        VA = self.alloc(HTR, "VA", [128, NT, 192], BF16)
        attT = self.alloc(HTR, "attT", [128, TT], BF16)
        W2q = self.alloc(HTR, "W2q", [128, 3, 192], BF16)
        W2kv = self.alloc(HTR, "W2kv", [128, 2, 256], BF16)
        WoP = self.alloc(HTR, "WoP", [128, 2, D], BF16)
        self.memset("pool", VA[:, :, 64:128], 1.0, W=[("VA", t) for t in range(NT)])
        wuq = d["b_wuq"][0].rearrange("(c p) f -> p c f", p=128)
        wukv = d["b_wukv"][0].rearrange("(c p) f -> p c f", p=128)
        sc = 96.0 ** -0.5
        for pp in range(8):
            self.dma("pool", "wg", W2q[:, :, :], wuq[:, :, pp * 192:(pp + 1) * 192], W=[("W2",)], bar=False)
            self.dma("pool", "wg", W2kv[:, :, :], wukv[:, :, pp * 256:(pp + 1) * 256], W=[("W2",)], group=True, bar=False)
            def S1(t):
                bank = 5 + (t % 2)
                ps = self.psb[bank]
                for c in range(3):
                    self.mm(ps[:, 0:192], cT[:, c, t * 128:(t + 1) * 128], W2q[:, c, :], start=(c == 0), stop=(c == 2),
                            R=[("cT", t), ("W2",)], W=[("ps", bank)])
                for c in range(2):
                    self.mm(ps[:, 192:448], cT[:, 3 + c, t * 128:(t + 1) * 128], W2kv[:, c, :], start=(c == 0), stop=(c == 1),
                            R=[("cT", t), ("W2",)], W=[("ps", bank)])
                self.cp("act", raw2[t % 3][:, :], ps[:, 0:448], R=[("ps", bank)], W=[("rawn", t % 3)])

            def views(t):
                rw = raw2[t % 3]
                rq = rw[:, 0:192].rearrange("p (h e) -> p h e", e=96)
                rkv = rw[:, 192:448].rearrange("p (h e) -> p h e", e=128)
                return rw, rq, rkv

            def S2(t):
                rw, rq, rkv = views(t)
                sq = ssq2[t % 2]
                for i, (src, hd) in enumerate(((rq[:, :, 0:64], 64), (rq[:, :, 64:96], 32), (rkv[:, :, 0:64], 64))):
                    self.stt(sqt[:, :, 0:hd], src, 1.0 / hd, src, ALU.mult, ALU.mult, R=[("rawn", t % 3)], W=[("sqt",)])
                    self.red(sq[:, 2 * i:2 * i + 2], sqt[:, :, 0:hd], ALU.add, R=[("sqt",)], W=[("ssq", t % 2)])

            def S3(t):
                self.act(lnv2[t % 2][:, 0:6], ssq2[t % 2][:, 0:6], AF.Ln, bias=self.misc[:, 2:3], scale=1.0,
                         R=[("ssq", t % 2), ("cst",)], W=[("lnvh", t % 2)])
                self.act(rs2[t % 2][:, 0:6], lnv2[t % 2][:, 0:6], AF.Exp, scale=-0.5, R=[("lnvh", t % 2)], W=[("rsh", t % 2)])

            def S4(t):
                rw, rq, rkv = views(t)
                rsv = rs2[t % 2]
                qab = qa[t % 2]
                kab = ka[t % 2]
                self.tt("dve", t1[:, :, 0:64], rq[:, :, 0:64], rsv[:, 0:2].unsqueeze(2).to_broadcast([128, 2, 64]), ALU.mult,
                        R=[("rawn", t % 3), ("rsh", t % 2)], W=[("t1",)])
                self.tt("pool", qab[:, :, 0:64], t1[:, :, 0:64], gq2[:, :, 0:64], ALU.mult, R=[("t1",), ("gq2",)], W=[("qa", t % 2)])
                self.tt("dve", t1[:, :, 64:96], rq[:, :, 64:96], rsv[:, 2:4].unsqueeze(2).to_broadcast([128, 2, 32]), ALU.mult,
                        R=[("rawn", t % 3), ("rsh", t % 2)], W=[("t1",)])
                if t < 16:
                    self.tt("dve", tq[:, :, :], t1[:, :, 64:96], gq2[:, :, 64:96], ALU.mult, R=[("t1",), ("gq2",)], W=[("tq",)])
                    x1 = tq[:, :, 0:16]
                    x2 = tq[:, :, 16:32]
                    cs = self.cosB[:, t, :].unsqueeze(1).to_broadcast([128, 2, 16])
                    sn = self.sinB[:, t, :].unsqueeze(1).to_broadcast([128, 2, 16])
                    self.tt("dve", rt[0][:, :, :], x1, cs, ALU.mult, R=[("tq",), ("cosB",)], W=[("rt", 0)])
                    self.tt("pool", rt[1][:, :, :], x2, sn, ALU.mult, R=[("tq",), ("sinB",)], W=[("rt", 1)])
                    self.tt("dve", qab[:, :, 64:80], rt[0][:, :, :], rt[1][:, :, :], ALU.subtract, R=[("rt", 0), ("rt", 1)], W=[("qa", t % 2)])
                    self.tt("pool", rt[2][:, :, :], x1, sn, ALU.mult, R=[("tq",), ("sinB",)], W=[("rt", 2)])
                    self.tt("dve", rt[3][:, :, :], x2, cs, ALU.mult, R=[("tq",), ("cosB",)], W=[("rt", 3)])
                    self.tt("dve", qab[:, :, 80:96], rt[2][:, :, :], rt[3][:, :, :], ALU.add, R=[("rt", 2), ("rt", 3)], W=[("qa", t % 2)])
                else:
                    self.tt("dve", qab[:, :, 64:96], t1[:, :, 64:96], gq2[:, :, 64:96], ALU.mult, R=[("t1",), ("gq2",)], W=[("qa", t % 2)])
                self.tt("dve", k1[:, :, :], rkv[:, :, 0:64], rsv[:, 4:6].unsqueeze(2).to_broadcast([128, 2, 64]), ALU.mult,
                        R=[("rawn", t % 3), ("rsh", t % 2)], W=[("k1",)])
                self.tt("pool", kab[:, :, 0:64], k1[:, :, :], gk2[:, :, :], ALU.mult, R=[("k1",), ("gk2",)], W=[("ka", t % 2)])
                self.cp("pool", kab[:, :, 64:96], krope[:, t, :].unsqueeze(1).to_broadcast([128, 2, 32]), R=[("krope", t)], W=[("ka", t % 2)])
                self.cp("pool", VA[:, t, 0:64], rkv[:, 0, 64:128], R=[("rawn", t % 3)], W=[("VA", t)])
                self.cp("pool", VA[:, t, 128:192], rkv[:, 1, 64:128], R=[("rawn", t % 3)], W=[("VA", t)])

            def S5(t):
                qab = qa[t % 2]
                kab = ka[t % 2]
                for i in range(2):
                    self.tr(ptb[0:96, i * 128:(i + 1) * 128], qab[:, i, :], self.identb[:, :], R=[("qa", t % 2), ("identb",)], W=[("ps", 7)])
                for i in range(2):
                    self.tr(ptb[0:96, (2 + i) * 128:(3 + i) * 128], kab[:, i, :], self.identb[:, :], R=[("ka", t % 2), ("identb",)], W=[("ps", 7)])
                self.cp("act", QKT[0:96, :, t * 128:(t + 1) * 128], ptb[0:96, 0:512].rearrange("p (i n) -> p i n", n=128),
                        R=[("ps", 7)], W=[("QKT", t)])

            self.pipeline(NT, S1, S2, S3, S4, S5)
            slot = pp % 2
            self.dma("pool", "wo%d" % slot, WoP[:, slot, :], d["b_wo"][0][pp * 128:(pp + 1) * 128, :], W=[("WoP", slot)], bar=False)
            cfgs = []
            qgroups = [0, 1, 2, 3] + ([4] if need_ctx else [])
            for gi in qgroups:
                t0, w = GROUPS[gi]
                ktiles = list(range(NT)) if gi < 4 else [16, 17]
                for hs in range(2):
                    cfgs.append(dict(
                        qt=QKT[0:96, hs, :], kt=QKT[0:96, 2 + hs, :],
                        va=(lambda k, hs=hs: VA[:, k, 0:128] if hs == 0 else VA[:, k, 64:192]),
                        o_lo=(hs == 0), t0=t0, w=w, ktiles=ktiles, att=attT,
                        qres=("QKTall",), kres=("QKTall",), vres=("VAall",), ares=("attT",)))
            self._alias([("QKT", t) for t in range(NT)], ("QKTall",))
            self._alias([("VA", t) for t in range(NT)], ("VAall",))
            self.attn_core(cfgs, sc)
            self.out_proj(WoP[:, slot, :], ("WoP", slot), attT, ("attT",), l, b, need_ctx)
            self._alias_release([("QKT", t) for t in range(NT)], ("QKTall",))
            self._alias_release([("VA", t) for t in range(NT)], ("VAall",))
        self.P.barrier()

    def na_layer(self, l, b, need_ctx):
        d = self.d
        R, R2 = self.R, self.R2
        R.reset()
        R2.reset()
        self.norm_bufs()
        hT = self.carve("hT", HT_OFF, [128, 8, TT], BF16)
        for gi in range(5):
            t0, w = GROUPS[gi]
            self.norm_group(gi, l, b, 1, lambda c, t0=t0, w=w, gi=gi: (hT[:, c, t0:t0 + w], ("hT", gi)))
        self.P.barrier()
        R.reset()
        QKT = self.alloc(R, "QKT", [128, 3, TT], BF16)
        VA = self.alloc(R, "VA", [128, NT, 192], BF16)
        attT = self.alloc(R, "attT", [128, TT], BF16)
        Wp = self.alloc(R, "Wg", [128, 8, 384], BF16)
        WoP = self.alloc(R, "WoP", [128, 2, D], BF16)
        self.PT = self.alloc(R, "PT", [128, 2, 1024], BF16)
        self.rec = self.alloc(R2, "rec", [128, 512], F32)
        gq4 = self.alloc(R2, "gq4", [128, 4, 64], F32)
        maskb = self.alloc(R, "maskb", [128, 21 * 128], BF16)
        self.memset("pool", QKT[64:128, 0, :], 0.0, W=[("QKTz",)])
        self.memset("pool", QKT[0:64, 1, :], 0.0, W=[("QKTz",)])
        BM = self.alloc(R2, "BM", [128, 21 * 128], F32)
        tmpb = [self.alloc(R2, "tmpb%d" % i, [128, 512], F32) for i in range(2)]
        raw = [self.alloc(R2, "raw%d" % i, [128, 384], F32) for i in range(3)]
        sqt = self.alloc(R2, "sqt", [128, 4, 64], F32)
        qkb = [self.alloc(R2, "qkb%d" % i, [128, 4, 64], BF16) for i in range(2)]
        ssq = [self.smallp[:, 0:8], self.smallp[:, 8:16]]
        lnv = [self.smallp[:, 16:24], self.smallp[:, 24:32]]
        rs = [self.smallp[:, 32:40], self.smallp[:, 40:48]]
        for i in range(2):
            self.bcast_row("sp", "gq", gq4[:, i, :], d["c_qnorm"][0], 64, W=[("gq4",)], group=(i > 0))
            self.bcast_row("sp", "gq", gq4[:, 2 + i, :], d["c_knorm"][0], 64, W=[("gq4",)], group=True)
        self.dma("pool", "mk", maskb[:, 0:1344], d["na_mask"][:, 0:1344], W=[("maskb",)])
        self.dma("pool", "mk", maskb[:, 1344:2688], d["na_mask"][:, 1344:2688], W=[("maskb",)], group=True)
        self.memset("pool", VA[:, :, 64:128], 1.0, W=[("VA", t) for t in range(NT)])
        wq = d["c_wqkv"][0].rearrange("(c p) f -> p c f", p=128)
        ptb = self.psbf(7)
        sc = 0.125
        for pp in range(8):
            self.dma("pool", "wg", Wp[:, :, 0:128], wq[:, :, 128 * pp:128 * pp + 128], W=[("Wg",)], bar=False)
            self.dma("pool", "wg", Wp[:, :, 128:256], wq[:, :, 1024 + 128 * pp:1024 + 128 * pp + 128], W=[("Wg",)], group=True, bar=False)
            self.dma("pool", "wg", Wp[:, :, 256:384], wq[:, :, 2048 + 128 * pp:2048 + 128 * pp + 128], W=[("Wg",)], group=True, bar=False)
            def S1(t):
                bank = 5 + (t % 2)
                ps = self.psb[bank]
                for c in range(8):
                    self.mm(ps[:, 0:384], hT[:, c, t * 128:(t + 1) * 128], Wp[:, c, :], start=(c == 0), stop=(c == 7),
                            R=[("hT", min(t // 4, 4)), ("Wg",)], W=[("ps", bank)])
                self.cp("act", raw[t % 3][:, :], ps[:, 0:384], R=[("ps", bank)], W=[("raw", t % 3)])

            def S2(t):
                r3 = raw[t % 3][:, 0:256].rearrange("p (h e) -> p h e", e=64)
                self.stt(sqt[:, :, :], r3, 1.0 / 64, r3, ALU.mult, ALU.mult, R=[("raw", t % 3)], W=[("sqt",)])
                self.red(ssq[t % 2][:, 0:4], sqt[:, :, :], ALU.add, R=[("sqt",)], W=[("ssq", t % 2)])

            def S3(t):
                self.act(lnv[t % 2][:, 0:4], ssq[t % 2][:, 0:4], AF.Ln, bias=self.misc[:, 2:3], scale=1.0,
                         R=[("ssq", t % 2), ("cst",)], W=[("lnvh", t % 2)])
                self.act(rs[t % 2][:, 0:4], lnv[t % 2][:, 0:4], AF.Exp, scale=-0.5, R=[("lnvh", t % 2)], W=[("rsh", t % 2)])

            def S4(t):
                rw = raw[t % 3]
                r3 = rw[:, 0:256].rearrange("p (h e) -> p h e", e=64)
                self.tt("dve", sqt[:, :, :], r3, rs[t % 2][:, 0:4].unsqueeze(2).to_broadcast([128, 4, 64]), ALU.mult,
                        R=[("raw", t % 3), ("rsh", t % 2)], W=[("sqt",)])
                qb = qkb[t % 2]
                self.tt("dve", qb[:, :, :], sqt[:, :, :], gq4[:, :, :], ALU.mult, R=[("sqt",), ("gq4",)], W=[("qkb", t % 2)])
                self.cp("pool", VA[:, t, 0:64], rw[:, 256:320], R=[("raw", t % 3)], W=[("VA", t)])
                self.cp("pool", VA[:, t, 128:192], rw[:, 320:384], R=[("raw", t % 3)], W=[("VA", t)])

            def S5(t):
                qb = qkb[t % 2]
                qf = qb[:, :, :].rearrange("p h e -> p (h e)")
                for i in range(2):
                    self.tr(ptb[:, i * 128:(i + 1) * 128], qf[:, i * 128:(i + 1) * 128], self.identb[:, :],
                            R=[("qkb", t % 2), ("identb",)], W=[("ps", 7)])
                self.cp("act", QKT[0:64, 0, t * 128:(t + 1) * 128], ptb[0:64, 0:128], R=[("ps", 7)], W=[("QKT", t)])
                self.cp("act", QKT[64:128, 1, t * 128:(t + 1) * 128], ptb[64:128, 0:128], R=[("ps", 7)], W=[("QKT", t)])
                self.cp("dve", QKT[:, 2, t * 128:(t + 1) * 128], ptb[:, 128:256], R=[("ps", 7)], W=[("QKT", t)])

            self.pipeline(NT, S1, S2, S3, S4, S5)
            slot = pp % 2
            self.dma("pool", "wo%d" % slot, WoP[:, slot, :], d["c_wo"][0][pp * 128:(pp + 1) * 128, :], W=[("WoP", slot)], bar=False)
            self._alias([("QKT", t) for t in range(NT)] + [("QKTz",)], ("QKTall",))
            self._alias([("VA", t) for t in range(NT)], ("VAall",))
            for hs in range(2):
                h = 2 * pp + hs
                psl = slice(0, 64) if hs == 0 else slice(64, 128)
                self.dma("sp", "bm", BM[:, :], d["na_bias"][h], W=[("BM",)])
                self.tt("pool", BM[:, :], BM[:, :], maskb[:, :], ALU.add, R=[("BM",), ("maskb",)], W=[("BM",)])
                va = (lambda k, hs=hs: VA[:, k, 0:128] if hs == 0 else VA[:, k, 64:192])
                self.na_core(QKT[:, hs, :], QKT[:, 2, :], va, hs == 0, attT, BM, tmpb, sc)
            if need_ctx:
                cfgs = []
                t0, w = GROUPS[4]
                for hs in range(2):
                    psl = slice(0, 64) if hs == 0 else slice(64, 128)
                    cfgs.append(dict(
                        qt=QKT[:, hs, :], kt=QKT[:, 2, :],
                        va=(lambda k, hs=hs: VA[:, k, 0:128] if hs == 0 else VA[:, k, 64:192]),
                        o_lo=(hs == 0), t0=t0, w=w, ktiles=[16, 17], att=attT,
                        qres=("QKTall",), kres=("QKTall",), vres=("VAall",), ares=("attT",)))
                self.attn_core(cfgs, sc)
            self.out_proj(WoP[:, slot, :], ("WoP", slot), attT, ("attT",), l, b, need_ctx)
            self._alias_release([("QKT", t) for t in range(NT)], ("QKTall",))
            self._alias_release([("VA", t) for t in range(NT)], ("VAall",))
        self.P.barrier()

    def na_core(self, qt, kt, va, o_lo, attT, BM, tmpb, sc):
        packs = []
        for qi in range(16):
            blocks = na_block_ids(qi)
            items = [(kt_, blk) for (kt_, blk) in blocks] + [(16, None), (17, None)]
            plist = [items[0:4], items[4:]]
            for pi, pk in enumerate(plist):
                packs.append((qi, pk, pi == 0, pi == len(plist) - 1))
        n = len(packs)
        pt3 = self.PT[:, :, :].rearrange("p a (b n) -> p (a b) n", n=512)
        lo, hi_ = slice(0, 64), slice(64, 128)
        o_sl, d_sl = (lo, hi_) if o_lo else (hi_, lo)

        def qk(i):
            qi, pk, first, last = packs[i]
            bank = i % 3
            for j, (k, blk) in enumerate(pk):
                self.mm(self.psb[bank][:, j * 128:(j + 1) * 128], kt[:, k * 128:(k + 1) * 128], qt[:, qi * 128:(qi + 1) * 128],
                        R=[("QKTall",)], W=[("ps", bank)])

        def ex(i):
            qi, pk, first, last = packs[i]
            bank = i % 3
            nb = sum(1 for (_, blk) in pk if blk is not None)
            nk = len(pk)
            if nb > 0:
                b0 = pk[0][1]
                tb = tmpb[i % 2]
                self.stt(tb[:, 0:nb * 128], self.psb[bank][:, 0:nb * 128], sc, BM[:, b0 * 128:(b0 + nb) * 128], ALU.mult, ALU.add,
                         R=[("ps", bank), ("BM",)], W=[("tmpb", i % 2)])
                self.act(pt3[:, bank, 0:nb * 128], tb[:, 0:nb * 128], AF.Exp, R=[("tmpb", i % 2)], W=[("pt", bank)])
            if nk > nb:
                self.act(pt3[:, bank, nb * 128:nk * 128], self.psb[bank][:, nb * 128:nk * 128], AF.Exp, scale=sc,
                         R=[("ps", bank)], W=[("pt", bank)])

        def pv(i):
            qi, pk, first, last = packs[i]
            ob = 3 + (qi + self.ot_ctr) % 2
            for j, (k, blk) in enumerate(pk):
                self.mm(self.psb[ob][:, 0:128], va(k), pt3[:, i % 3, j * 128:(j + 1) * 128],
                        start=(first and j == 0), stop=(last and j == len(pk) - 1),
                        R=[("pt", i % 3), ("VAall",)], W=[("ps", ob)])
            if last:
                rec_o = self.rec[d_sl, 0:128]
                rec_i = self.psb[ob][d_sl, 0:128]
                self.P.op("dve", lambda e: e.reciprocal(out=rec_o, in_=rec_i), [("ps", ob)], [("rec",)])
                self.tt("dve", attT[o_sl, qi * 128:(qi + 1) * 128], self.psb[ob][o_sl, 0:128], self.rec[d_sl, 0:128], ALU.mult,
                        R=[("ps", ob), ("rec",)], W=[("attT",)])

        qk(0)
        for i in range(n):
            if i + 1 < n:
                qk(i + 1)
            ex(i)
            pv(i)
        self.ot_ctr += 16

    def ring_load(self, src):
        i = self.unit_ctr % NUNIT
        self.unit_ctr += 1
        buf = self.ring[i]
        self.dma("pool", "ring%d" % i, buf[:, :, :], src, W=[("ring", i)], bar=False)
        return buf, ("ring", i)

    def moe_units(self, l, e):
        d = self.d
        wg = d["moe_wg"][l, e].rearrange("(c p) f -> p c f", p=128)
        wu = d["moe_wu"][l, e].rearrange("(c p) f -> p c f", p=128)
        wd = d["moe_wd"][l, e].rearrange("(c p) f -> p c f", p=128)
        return [wg[:, :, 0:512], wu[:, :, 0:512], wg[:, :, 512:1024], wu[:, :, 512:1024], wd[:, :, 0:512], wd[:, :, 512:1024]]

    def moe_layer(self, l, b, need_ctx, pre):
        d = self.d
        R = self.R
        R.reset()
        ROFF = R_OFF
        self.norm_bufs()
        h2T = [self.alloc(R, "h2T%d" % i, [128, 8, 512], BF16) for i in range(2)]
        aff = self.carve("aff", ROFF + 43008, [128, NT, 16], F32)
        rw = self.carve("rw", ROFF + 44160, [128, 8, 16], BF16)
        m8 = self.carve("m8", ROFF + 44160 + 256, [16, 8], F32)
        thr = self.carve("thr", ROFF + 44160 + 288, [16, 2], F32)
        gate = self.carve("gate", ROFF + 44160 + 320, [128, 4], F32)
        ee = self.carve("ee", ROFF + 44160 + 352, [128, 16], F32)
        sst = self.carve("sst", ROFF + 44160 + 416, [128, 8], F32)
        h2 = self.carve("h2", HT_OFF, [128, NT, D], BF16)
        self.dma("pool", "rw", rw[:, :, :], d["moe_router"][l].rearrange("(c p) e -> p c e", p=128), W=[("rw",)])
        ngrp = 5 if need_ctx else 4
        ntl = NT if need_ctx else 16
        ptb = self.psbf(7)
        for gi in range(ngrp):
            t0, w = GROUPS[gi]
            hb = h2T[gi % 2]
            self.norm_group(gi, l, b, 2, lambda c, hb=hb, w=w, gi=gi: (hb[:, c, 0:w], ("h2T", gi % 2)))
            for tt_ in range(w // 128):
                t = t0 // 128 + tt_
                bank = 3 + (t % 2)
                ps = self.psb[bank]
                for c in range(8):
                    self.mm(ps[:, 0:16], hb[:, c, tt_ * 128:(tt_ + 1) * 128], rw[:, c, :], start=(c == 0), stop=(c == 7),
                            R=[("h2T", gi % 2), ("rw",)], W=[("ps", bank)])
                self.red(sst[:, 0:1], ps[:, 0:16], ALU.max, R=[("ps", bank)], W=[("sst",)])
                self.ts("dve", sst[:, 1:2], sst[:, 0:1], -1.0, ALU.mult, R=[("sst",)], W=[("sst",)])
                self.act(ee[:, :], ps[:, 0:16], AF.Exp, bias=sst[:, 1:2], accum=sst[:, 2:3],
                         R=[("ps", bank), ("sst",)], W=[("ee",), ("sst",)])
                self.P.op("dve", lambda e: e.reciprocal(out=sst[:, 3:4], in_=sst[:, 2:3]), [("sst",)], [("sst",)])
                self.ts("dve", aff[:, t, :], ee[:, :], sst[:, 3:4], ALU.mult, R=[("ee",), ("sst",)], W=[("aff",)])
                for c in range(8):
                    self.tr(ptb[:, c * 128:(c + 1) * 128], hb[:, c, tt_ * 128:(tt_ + 1) * 128], self.identb[:, :],
                            R=[("h2T", gi % 2), ("identb",)], W=[("ps", 7)])
                self.cp("act", h2[:, t, :], ptb[:, :], R=[("ps", 7)], W=[("h2", t)])
        self.P.barrier()
        affT = self.carve("affT", ROFF + 0, [16, TT], F32)
        work = self.carve("work", ROFF + 9216, [16, TT], F32)
        B3 = self.carve("B3", ROFF + 18432, [16, TT], F32)
        vb16 = self.carve("vb16", ROFF + 27648, [16, TT], BF16)
        gmtok = self.carve("gmtok", ROFF + 38400, [128, NT, 16], F32)
        vtok = self.carve("vtok", ROFF + 40704, [128, NT, 16], F32)
        gmhl = self.carve("gmhl", ROFF + 41856, [128, NT, 16, 2], BF16)
        for t in range(ntl):
            bank = (t // 4) % 2
            self.tr(self.psb[bank][0:16, (t % 4) * 128:(t % 4 + 1) * 128], aff[:, t, :], self.identf,
                    R=[("aff",), ("cst",)], W=[("ps", bank)])
            if t % 4 == 3 or t == ntl - 1:
                t_lo = (t // 4) * 4
                n = (t - t_lo + 1) * 128
                self.cp("dve", affT[:, t_lo * 128:t_lo * 128 + n], self.psb[bank][0:16, 0:n], R=[("ps", bank)], W=[("affT",)])
        segs = [(0, T, CAP // 8, 0)] + ([(T, TC, CAPC // 8, 1)] if need_ctx else [])
        ncols = T + (TC if need_ctx else 0)
        self.cp("dve", work[:, 0:ncols], affT[:, 0:ncols], R=[("affT",)], W=[("work",)])
        for (c0, n, rounds, ti) in segs:
            wv = work[:, c0:c0 + n]
            for r in range(rounds):
                self.P.op("dve", lambda e, wv=wv: e.max(out=m8[:, :], in_=wv), [("work",)], [("m8",)])
                if r < rounds - 1:
                    self.P.op("dve", lambda e, wv=wv: e.match_replace(out=wv, in_to_replace=m8[:, :], in_values=wv, imm_value=-1.0),
                              [("work",), ("m8",)], [("work",)])
            self.cp("dve", thr[:, ti:ti + 1], m8[:, 7:8], R=[("m8",)], W=[("thr",)])
        ones_col = self.cstb[0:16, 128 + 1:128 + 2]
        for (c0, n, rounds, ti) in segs:
            self.ts("dve", work[:, c0:c0 + n], affT[:, c0:c0 + n], thr[:, ti:ti + 1], ALU.is_ge, R=[("affT",), ("thr",)], W=[("work",)])
            self.P.op("dve", lambda e, c0=c0, n=n: e.tensor_tensor_scan(out=B3[:, c0:c0 + n], data0=ones_col.to_broadcast([16, n]),
                                                                     data1=work[:, c0:c0 + n], initial=0.0, op0=ALU.mult, op1=ALU.add),
                      [("work",), ("cst",)], [("B3",)])
        self.tt("dve", B3[:, 0:ncols], B3[:, 0:ncols], work[:, 0:ncols], ALU.mult, R=[("B3",), ("work",)], W=[("B3",)])
        self.ts("dve", B3[:, 0:ncols], B3[:, 0:ncols], -1.0, ALU.add, R=[("B3",)], W=[("B3",)])
        self.tt("dve", affT[:, 0:ncols], affT[:, 0:ncols], work[:, 0:ncols], ALU.mult, R=[("affT",), ("work",)], W=[("affT",)])
        self.cp("dve", vb16[:, 0:ncols], B3[:, 0:ncols], R=[("B3",)], W=[("vb16",)])
        for (src, dst, nm, bank) in ((B3, vtok, "vtok", 2), (affT, gmtok, "gmtok", 3)):
            for t in range(ntl):
                self.tr(self.psb[bank][:, t * 16:(t + 1) * 16], src[:, t * 128:(t + 1) * 128], self.identf[0:16, 0:16],
                        R=[(src.name,), ("cst",)], W=[("ps", bank)])
            self.cp("dve", dst[:, 0:ntl, :], self.psb[bank][:, 0:ntl * 16].rearrange("p (t e) -> p t e", e=16),
                    R=[("ps", bank)], W=[(nm,)])
        self.cp("dve", gmhl[:, 0:ntl, :, 0], gmtok[:, 0:ntl, :], R=[("gmtok",)], W=[("gmhl",)])
        self.tt("dve", gmtok[:, 0:ntl, :], gmtok[:, 0:ntl, :], gmhl[:, 0:ntl, :, 0], ALU.subtract, R=[("gmtok",), ("gmhl",)], W=[("gmtok",)])
        self.cp("dve", gmhl[:, 0:ntl, :, 1], gmtok[:, 0:ntl, :], R=[("gmtok",)], W=[("gmhl",)])
        self.P.barrier()
        S = self.carve("S", ROFF + 0, [128, 16, 256], BF16)
        Sc = self.carve("Sc", ROFF + 8192, [128, 2, 32], BF16)
        ST = self.carve("ST", ROFF + 9216, [128, 2, T], BF16)
        STc = self.carve("STc", ROFF + 9216 + 8192, [32, 256], BF16)
        xgT = self.carve("xgT", ROFF + 18432, [128, 8, 288], BF16)
        hidT = self.carve("hidT", ROFF + 18432 + 4608, [128, 8, 288], BF16)
        y = self.carve("y", ROFF + 32256, [128, 3, D], BF16)
        sg = self.carve("sg", ROFF + 38400, [128, 2, 288], F32)
        Wn = 288 if need_ctx else 256
        units = list(pre)
        upos = [0]

        def next_units(n):
            out = []
            for _ in range(n):
                out.append(units[upos[0]])
                upos[0] += 1
            return out

        srcs = []
        for e in range(NE):
            srcs += self.moe_units(l, e)
        issued = [len(pre)]

        def issue(upto):
            while issued[0] < min(upto, len(srcs)):
                units.append(self.ring_load(srcs[issued[0]]))
                issued[0] += 1

        chunks = [(0, 128), (128, 128)] + ([(256, 32)] if need_ctx else [])
        nch = 3 if need_ctx else 2

        def build_S(e):
            for t in range(16):
                self.ts("dve", S[:, t, :], self.iotac, vtok[:, t, e:e + 1], ALU.is_equal, R=[("vtok",), ("cst",)], W=[("S", t)])
            if need_ctx:
                for t in range(2):
                    self.ts("dve", Sc[:, t, :], self.iotac[:, 0:32], vtok[:, 16 + t, e:e + 1], ALU.is_equal, R=[("vtok",), ("cst",)], W=[("Sc",)])

        def slot_gates(e):
            gb = 4
            gps = self.psb[gb]
            for ch in range(2):
                for t in range(16):
                    self.mm(gps[:, ch * 2:ch * 2 + 2], S[:, t, ch * 128:(ch + 1) * 128], gmhl[:, t, e, :], start=(t == 0), stop=(t == 15),
                            R=[("S", t), ("gmhl",)], W=[("ps", gb)])
            if need_ctx:
                for t in range(2):
                    self.mm(gps[0:32, 4:6], Sc[:, t, :], gmhl[:, 16 + t, e, :], start=(t == 0), stop=(t == 1),
                            R=[("Sc",), ("gmhl",)], W=[("ps", gb)])
            self.red(gate[:, 0:nch], gps[:, 0:2 * nch].rearrange("p (c two) -> p c two", two=2), ALU.add, R=[("ps", gb)], W=[("gate",)])

        def gather_c(e, c):
            bank = c % 2
            ps = self.psb[bank]
            for t in range(16):
                self.mm(ps[:, 0:256], h2[:, t, c * 128:(c + 1) * 128], S[:, t, :], start=(t == 0), stop=(t == 15),
                        R=[("h2", t), ("S", t)], W=[("ps", bank)])
            if need_ctx:
                for t in range(2):
                    self.mm(ps[:, 256:288], h2[:, 16 + t, c * 128:(c + 1) * 128], Sc[:, t, :], start=(t == 0), stop=(t == 1),
                            R=[("h2", 16 + t), ("Sc",)], W=[("ps", bank)])
            self.cp("act", xgT[:, c, 0:Wn], ps[:, 0:Wn], R=[("ps", bank)], W=[("xgT",)])

        def scatter_blocks(e):
            out = []
            for grp in range(4):
                for dc in range(8):
                    def blk(grp=grp, dc=dc):
                        bank = 2 + (dc % 2)
                        ps = self.psb[bank]
                        for ch in range(2):
                            self.mm(ps[:, :], y[:, ch, dc * 128:(dc + 1) * 128], ST[:, ch, grp * 512:(grp + 1) * 512],
                                    start=(ch == 0), stop=(ch == 1), R=[("y",), ("ST", grp)], W=[("ps", bank)])
                        xs = self.xT[:, dc, grp * 512:(grp + 1) * 512]
                        self.stt(xs, ps[:, :], self.mod(l, b, 5, dc), xs, ALU.mult, ALU.add,
                                 R=[("ps", bank), ("modv",), ("xT", grp, dc)], W=[("xT", grp, dc)])
                    out.append(blk)
            if need_ctx:
                for dc in range(8):
                    def blk(dc=dc):
                        bank = 2 + (dc % 2)
                        ps = self.psb[bank]
                        self.mm(ps[:, 0:256], y[0:32, 2, dc * 128:(dc + 1) * 128], STc[:, :], R=[("y",), ("STc",)], W=[("ps", bank)])
                        xs = self.xT[:, dc, T:TT]
                        self.stt(xs, ps[:, 0:256], self.mod(l, 2, 5, dc), xs, ALU.mult, ALU.add,
                                 R=[("ps", bank), ("modv",), ("xT", 4, dc)], W=[("xT", 4, dc)])
                    out.append(blk)
            return out

        def build_ST(e):
            for grp in range(4):
                bank = 2 + (grp % 2)
                ps = self.psb[bank]
                self.mm(ps[:, :], self.sel[:, e, :], vb16[:, grp * 512:(grp + 1) * 512], R=[("sel",), ("vb16",)], W=[("ps", bank)])
                for ch in range(2):
                    self.ts("dve", ST[:, ch, grp * 512:(grp + 1) * 512], ps[:, :], self.misc[:, ch:ch + 1], ALU.is_equal,
                            R=[("ps", bank), ("cst",)], W=[("ST", grp)])
            if need_ctx:
                ps = self.psb[2]
                self.mm(ps[0:32, 0:256], self.sel[:, e, 0:32], vb16[:, T:TT], R=[("sel",), ("vb16",)], W=[("ps", 2)])
                self.ts("dve", STc[:, :], ps[0:32, 0:256], self.misc[0:32, 0:1], ALU.is_equal, R=[("ps", 2), ("cst",)], W=[("STc",)])

        def gate_up(e, wg0, wu0, wg1, wu1):
            for fc in range(8):
                gbuf, gres = (wg0 if fc < 4 else wg1)
                ubuf, ures = (wu0 if fc < 4 else wu1)
                fo = (fc % 4) * 128
                bg = 4 + 2 * (fc % 2)
                bu = bg + 1
                for c in range(8):
                    self.mm(self.psb[bg][:, 0:Wn], gbuf[:, c, fo:fo + 128], xgT[:, c, 0:Wn], start=(c == 0), stop=(c == 7),
                            R=[gres, ("xgT",)], W=[("ps", bg)])
                for c in range(8):
                    self.mm(self.psb[bu][:, 0:Wn], ubuf[:, c, fo:fo + 128], xgT[:, c, 0:Wn], start=(c == 0), stop=(c == 7),
                            R=[ures, ("xgT",)], W=[("ps", bu)])
                self.act(sg[:, fc % 2, 0:Wn], self.psb[bg][:, 0:Wn], AF.Silu, R=[("ps", bg)], W=[("sg", fc % 2)])
                self.tt("dve", hidT[:, fc, 0:Wn], sg[:, fc % 2, 0:Wn], self.psb[bu][:, 0:Wn], ALU.mult,
                        R=[("sg", fc % 2), ("ps", bu)], W=[("hidT",)])

        def down(e, wd0, wd1):
            for half, (dbuf, dres) in enumerate((wd0, wd1)):
                for ci, (c0, m) in enumerate(chunks):
                    bank = (half * 3 + ci) % 2
                    ps = self.psb[bank]
                    for fcn in range(8):
                        self.mm(ps[0:m, :], hidT[:, fcn, c0:c0 + m], dbuf[:, fcn, :], start=(fcn == 0), stop=(fcn == 7),
                                R=[("hidT",), dres], W=[("ps", bank)])
                    self.ts("dve", y[0:m, ci, half * 512:(half + 1) * 512], ps[0:m, :], gate2[0:m, ci:ci + 1], ALU.mult,
                            R=[("ps", bank), ("gate2",)], W=[("y",)])

        gate2 = self.carve("gate2", ROFF + 44160 + 480, [128, 4], F32)
        issue(4)
        build_S(0)
        slot_gates(0)
        for c in range(8):
            gather_c(0, c)
        for e in range(NE):
            issue(e * 6 + 4)
            wg0, wu0, wg1, wu1 = next_units(4)
            self.cp("dve", gate2[:, 0:nch], gate[:, 0:nch], R=[("gate",)], W=[("gate2",)])
            build_ST(e)
            gate_up(e, wg0, wu0, wg1, wu1)
            issue(e * 6 + 6)
            wd0, wd1 = next_units(2)
            down(e, wd0, wd1)
            issue(e * 6 + 10)
            blocks = scatter_blocks(e)
            if e + 1 < NE:
                build_S(e + 1)
                slot_gates(e + 1)
                nb = len(blocks)
                per = (nb + 7) // 8
                bi = 0
                for c in range(8):
                    gather_c(e + 1, c)
                    for _ in range(per):
                        if bi < nb:
                            blocks[bi]()
                            bi += 1
                while bi < nb:
                    blocks[bi]()
                    bi += 1
            else:
                for blk in blocks:
                    blk()
        self.P.barrier()

    def moe_prefetch(self, l):
        self.unit_ctr = 0
        srcs = self.moe_units(l, 0)
        return [self.ring_load(srcs[i]) for i in range(2)]

    def build(self):
        cfg = self.cfg
        self.prologue()
        for b in range(cfg.get("nsamples", SPC)):
            self.load_sample(b)
            for l in cfg.get("layers", [0, 1, 2, 3]):
                need_ctx = l < 3
                pre = self.moe_prefetch(l) if cfg.get("moe", True) else []
                kind = l % 3
                if cfg.get("attn", True):
                    if kind == 0:
                        self.gqa_layer(l, b, need_ctx)
                    elif kind == 1:
                        self.mla_layer(l, b, need_ctx)
                    else:
                        self.na_layer(l, b, need_ctx)
                if cfg.get("dump_xa") == (b, l):
                    self.store_sample(self.dbg_out("xa", [TT, D]), NT)
                if cfg.get("moe", True):
                    self.moe_layer(l, b, need_ctx, pre)
                if cfg.get("dump_x") == (b, l):
                    self.store_sample(self.dbg_out("x", [TT, D]), NT)
            self.store_sample(self.y2[b], 16)
        self.P.emit(self.es)
        return self.nc


def host_inputs(inp):
    cst = np.zeros((128, 388), np.float32)
    cst[:, 0:128] = np.eye(128, dtype=np.float32)
    cst[:, 128:384] = np.arange(256, dtype=np.float32)[None, :]
    cst[:, 384] = np.arange(128, dtype=np.float32)
    cst[:, 385] = np.arange(128, dtype=np.float32) + 128.0
    cst[:, 386] = EPS
    selc = np.zeros((16, 16, 128), np.float32)
    for e in range(16):
        selc[e, e, :] = 1.0
    selc = selc.reshape(16, 2048).astype(ml_dtypes.bfloat16)
    cosA, sinA = rope_tables(64)
    cosB, sinB = rope_tables(32)
    na_bias, na_mask = na_tables(np.asarray(inp["c_rpb"], np.float32)[0])
    shared = {k: np.ascontiguousarray(np.asarray(inp[k], np.float32)) for k in (
        "ada_w", "ada_b", "norm1_w", "norm2_w", "a_wqkv", "a_qnorm", "a_knorm", "a_wo",
        "b_wdq", "b_qnorm_lat", "b_wuq", "b_wdkv", "b_kvnorm_lat", "b_wukv", "b_qnorm", "b_knorm", "b_wo",
        "c_wqkv", "c_qnorm", "c_knorm", "c_wo", "moe_router", "moe_wg", "moe_wu", "moe_wd")}
    shared.update(cst=cst, selc=selc, cosA=cosA, sinA=sinA, cosB=cosB, sinB=sinB, na_bias=na_bias, na_mask=na_mask)
    x = np.asarray(inp["x"], np.float32)
    ctx = np.asarray(inp["ctx"], np.float32)
    c = np.asarray(inp["c"], np.float32)
    c_ctx = np.asarray(inp["c_ctx"], np.float32)
    maps = []
    for core in range(N_CORES):
        m = dict(shared)
        m["x2"] = np.ascontiguousarray(x[SPC * core:SPC * core + SPC])
        m["ctx2"] = np.ascontiguousarray(ctx[SPC * core:SPC * core + SPC])
        m["cvec"] = np.ascontiguousarray(np.concatenate([c[SPC * core:SPC * core + SPC], c_ctx[None, :]], axis=0))
        maps.append(m)
    return maps


def kernel(**inp):
    maps = host_inputs(inp)
    nc = Builder({}).build()
    res = run_bass_kernel_spmd(nc, maps, core_ids=list(range(N_CORES)))
    out = np.concatenate([np.asarray(r["y2"], np.float32) for r in res.results], axis=0)
    return out
```

```python
import numpy as np
import ml_dtypes
from contextlib import ExitStack
import concourse.bass as bass
import concourse.mybir as mybir
from concourse.bass_utils import run_bass_kernel_spmd

F32 = mybir.dt.float32
BF16 = mybir.dt.bfloat16
AF = mybir.ActivationFunctionType
ALU = mybir.AluOpType
AX = mybir.AxisListType

D = 1024
T = 2048
TC = 256
TT = 2304
NT = 18
NE = 16
CAP = 256
CAPC = 32
EPS = 1e-6
NEG = -30000.0
N_CORES = 8
SPC = 2

XT_OFF = 0
RING_OFF = 73728
UNIT = 8192
NUNIT = 5
HT_OFF = RING_OFF + UNIT * NUNIT
R_OFF = HT_OFF + 36864
R_SIZE = 45056
PERS_OFF = R_OFF + R_SIZE
ARENA_BYTES = 212000
R2_OFF = RING_OFF + 2 * UNIT
R2_SIZE = 3 * UNIT


def _dsize(dt):
    return 4 if dt == F32 else 2


class Prog:
    ENG = ("pe", "act", "dve", "pool", "sp")

    def __init__(self, nc):
        self.nc = nc
        self.eng = {"pe": nc.tensor, "act": nc.scalar, "dve": nc.vector, "pool": nc.gpsimd, "sp": nc.sync}
        self.ops = []
        self.eops = {e: [] for e in self.ENG}
        self.res = {}
        self.slots = {}
        self.pending = {e: set() for e in self.ENG}

    def _deps(self, eng, reads, writes):
        deps = set(self.pending[eng])
        self.pending[eng] = set()
        for r in reads:
            st = self.res.get(r)
            if st is not None and st["w"] is not None:
                deps.add(st["w"])
        for w in writes:
            st = self.res.get(w)
            if st is not None:
                if st["w"] is not None:
                    deps.add(st["w"])
                deps.update(st["r"].values())
        return deps

    def _mark(self, tok, key, reads, writes):
        for r in reads:
            st = self.res.get(r)
            if st is None:
                st = self.res[r] = {"w": None, "r": {}}
            st["r"][key] = tok
        for w in writes:
            self.res[w] = {"w": tok, "r": {}}

    def op(self, eng, fn, reads=(), writes=()):
        deps = self._deps(eng, reads, writes)
        idx = len(self.eops[eng])
        rec = {"eng": eng, "kind": "op", "fn": fn, "deps": deps, "idx": idx, "sig": False}
        self.eops[eng].append(rec)
        self.ops.append(rec)
        self._mark(("e", eng, idx), eng, reads, writes)

    def dma(self, queue, slot, out, in_, reads=(), writes=(), group=False, bar=True, **kw):
        deps = self._deps(queue, reads, writes)
        s = self.slots.get(slot)
        if s is None:
            s = self.slots[slot] = {"n": 0, "groups": [], "bar": bar}
        s["n"] += 1
        n = s["n"]
        if group and s["groups"]:
            s["groups"][-1] = n
        else:
            if n > 1:
                deps.add(("d", slot, n - 1))
            s["groups"].append(n)
        idx = len(self.eops[queue])
        rec = {"eng": queue, "kind": "dma", "slot": slot, "n": n, "out": out, "in_": in_, "deps": deps,
               "idx": idx, "kw": kw, "sig": False}
        self.eops[queue].append(rec)
        self.ops.append(rec)
        self._mark(("d", slot, n), ("d", slot), reads, writes)

    def barrier(self):
        toks = set()
        for e in ("pe", "act", "dve", "pool"):
            if self.eops[e]:
                for rec in reversed(self.eops[e]):
                    if rec["kind"] == "op":
                        toks.add(("e", e, rec["idx"]))
                        break
        for name, s in self.slots.items():
            if s["bar"] and s["n"] > 0:
                toks.add(("d", name, s["n"]))
        for e in ("pe", "act", "dve", "pool", "sp"):
            self.pending[e] |= {t for t in toks if not (t[0] == "e" and t[1] == e == "pe")}

    def _gend(self, slot, n):
        for g in self.slots[slot]["groups"]:
            if g >= n:
                return g
        raise AssertionError

    def emit(self, es):
        nc = self.nc
        for rec in self.ops:
            for d in rec["deps"]:
                if d[0] == "e":
                    if d[1] == rec["eng"] == "pe":
                        continue
                    self.eops[d[1]][d[2]]["sig"] = True
        for e in self.ENG:
            c = 0
            for rec in self.eops[e]:
                if rec["sig"]:
                    c += 1
                rec["sigval"] = c
        sem = {}
        for e in self.ENG:
            sem[("e", e)] = es.enter_context(nc.semaphore("g_" + e))
        for name in self.slots:
            sem[("d", name)] = es.enter_context(nc.semaphore("d_" + name))
        waited = {e: {} for e in self.ENG}
        nwait = 0
        for rec in self.ops:
            e = rec["eng"]
            engine = self.eng[e]
            need = {}
            for d in rec["deps"]:
                if d[0] == "e":
                    if d[1] == e == "pe":
                        continue
                    key = ("e", d[1])
                    val = self.eops[d[1]][d[2]]["sigval"]
                else:
                    if rec["kind"] == "dma" and d[1] == rec["slot"] and self._gend(d[1], d[2]) == self._gend(rec["slot"], rec["n"]):
                        continue
                    key = ("d", d[1])
                    val = 16 * self._gend(d[1], d[2])
                if need.get(key, 0) < val:
                    need[key] = val
            for key, val in need.items():
                if waited[e].get(key, 0) < val:
                    engine.wait_ge(sem[key], val)
                    waited[e][key] = val
                    nwait += 1
            if rec["kind"] == "op":
                inst = rec["fn"](engine)
                if rec["sig"]:
                    inst.then_inc(sem[("e", e)], 1)
            else:
                inst = engine.dma_start(out=rec["out"], in_=rec["in_"], **rec["kw"])
                inst.then_inc(sem[("d", rec["slot"])], 16)
        sp = self.eng["sp"]
        for name, s in self.slots.items():
            if s["n"] > 0:
                sp.wait_ge(sem[("d", name)], 16 * s["n"])
        self.stats = {e: len(self.eops[e]) for e in self.ENG}
        self.stats["waits"] = nwait


class Region:
    def __init__(self, off, size):
        self.off, self.size, self.cur = off, size, 0

    def reset(self):
        self.cur = 0

    def take(self, nbytes):
        nbytes = (nbytes + 31) // 32 * 32
        o = self.off + self.cur
        self.cur += nbytes
        assert self.cur <= self.size, (self.cur, self.size)
        return o


class Buf:
    def __init__(self, name, ap):
        self.name = name
        self.ap = ap

    def __getitem__(self, idx):
        return self.ap[idx]

    def r(self, *k):
        return (self.name,) + k


GROUPS = [(0, 512), (512, 512), (1024, 512), (1536, 512), (2048, 256)]


def na_tables(rpb):
    blocks = [(5, 5 + r) for r in (-2, -1, 0, 1, 2)]
    for qi in (0, 1):
        blocks += [(qi, kt) for kt in range(4)]
    for qi in (14, 15):
        blocks += [(qi, kt) for kt in range(12, 16)]
    nb = len(blocks)
    H = rpb.shape[0]
    bias = np.zeros((H, 128, nb, 128), np.float32)
    mask = np.zeros((128, nb, 128), np.float32)
    kk = np.arange(128)
    for bi, (qi, kt) in enumerate(blocks):
        k_r = 2 * kt + kk // 64
        k_c = kk % 64
        q_r = 2 * qi + kk // 64
        q_c = kk % 64
        rs = np.clip(q_r - 4, 0, 24)
        cs = np.clip(q_c - 8, 0, 48)
        ok = ((k_r[:, None] >= rs[None, :]) & (k_r[:, None] < rs[None, :] + 8)
              & (k_c[:, None] >= cs[None, :]) & (k_c[:, None] < cs[None, :] + 16))
        dr = np.clip(k_r[:, None] - q_r[None, :] + 7, 0, 14)
        dc = np.clip(k_c[:, None] - q_c[None, :] + 15, 0, 30)
        bias[:, :, bi, :] = rpb[:, dr, dc]
        mask[:, bi, :] = np.where(ok, 0.0, NEG)
    return bias.reshape(H, 128, nb * 128), mask.reshape(128, nb * 128)


def na_block_ids(qi):
    if 2 <= qi <= 13:
        return [(qi + r, 2 + r) for r in (-2, -1, 0, 1, 2)]
    if qi in (0, 1):
        return [(kt, 5 + 4 * qi + kt) for kt in range(4)]
    base = 13 if qi == 14 else 17
    return [(kt, base + (kt - 12)) for kt in range(12, 16)]


def rope_tables(rot_dim):
    n_freq = rot_dim // 4
    inv = np.float32(10000.0) ** (-np.arange(n_freq, dtype=np.float32) / np.float32(n_freq))
    t = np.arange(T, dtype=np.int32)
    row = (t // 64).astype(np.float32)
    col = (t % 64).astype(np.float32)
    ang = np.concatenate([row[:, None] * inv, col[:, None] * inv], axis=-1).astype(np.float32)
    return np.cos(ang).astype(np.float32), np.sin(ang).astype(np.float32)


class Builder:
    def __init__(self, cfg):
        self.cfg = cfg
        nc = self.nc = bass.Bass("TRN2", target_bir_lowering=False)
        self.es = ExitStack()
        self.P = Prog(nc)
        self.dbg_outs = []
        self._dram()
        self.arena = self.es.enter_context(nc.sbuf_tensor("arena", [128, ARENA_BYTES // 4], F32))
        self.psall = self.es.enter_context(nc.psum_tensor("psall", [128, 4096], F32))
        self.psb = [self.psall[:, i * 512:(i + 1) * 512] for i in range(8)]
        self.R = Region(R_OFF, R_SIZE)
        self.R2 = Region(R2_OFF, R2_SIZE)
        self.HTR = Region(HT_OFF, 36864)
        self.PERS = Region(PERS_OFF, ARENA_BYTES - PERS_OFF)
        self.unit_ctr = 0
        self.ot_ctr = 0
        self.op_ctr = 0
        self._persistent()

    def _dram(self):
        nc = self.nc

        def inp(name, shape, dt=F32):
            return nc.dram_tensor(name, list(shape), dt, kind="ExternalInput").ap()
        self.d = d = {}
        d["x2"] = inp("x2", [SPC, T, D])
        d["ctx2"] = inp("ctx2", [SPC, TC, D])
        d["cvec"] = inp("cvec", [3, D])
        d["ada_w"] = inp("ada_w", [4, D, 6 * D])
        d["ada_b"] = inp("ada_b", [4, 6 * D])
        d["norm1_w"] = inp("norm1_w", [4, D])
        d["norm2_w"] = inp("norm2_w", [4, D])
        d["a_wqkv"] = inp("a_wqkv", [2, D, 1536])
        d["a_qnorm"] = inp("a_qnorm", [2, 64])
        d["a_knorm"] = inp("a_knorm", [2, 64])
        d["a_wo"] = inp("a_wo", [2, D, D])
        d["b_wdq"] = inp("b_wdq", [1, D, 384])
        d["b_qnorm_lat"] = inp("b_qnorm_lat", [1, 384])
        d["b_wuq"] = inp("b_wuq", [1, 384, 1536])
        d["b_wdkv"] = inp("b_wdkv", [1, D, 288])
        d["b_kvnorm_lat"] = inp("b_kvnorm_lat", [1, 256])
        d["b_wukv"] = inp("b_wukv", [1, 256, 2048])
        d["b_qnorm"] = inp("b_qnorm", [1, 96])
        d["b_knorm"] = inp("b_knorm", [1, 96])
        d["b_wo"] = inp("b_wo", [1, D, D])
        d["c_wqkv"] = inp("c_wqkv", [1, D, 3072])
        d["c_qnorm"] = inp("c_qnorm", [1, 64])
        d["c_knorm"] = inp("c_knorm", [1, 64])
        d["c_wo"] = inp("c_wo", [1, D, D])
        d["moe_router"] = inp("moe_router", [4, D, NE])
        d["moe_wg"] = inp("moe_wg", [4, NE, D, D])
        d["moe_wu"] = inp("moe_wu", [4, NE, D, D])
        d["moe_wd"] = inp("moe_wd", [4, NE, D, D])
        d["cst"] = inp("cst", [128, 388])
        d["selc"] = inp("selc", [16, 2048], BF16)
        d["cosA"] = inp("cosA", [T, 32])
        d["sinA"] = inp("sinA", [T, 32])
        d["cosB"] = inp("cosB", [T, 16])
        d["sinB"] = inp("sinB", [T, 16])
        d["na_bias"] = inp("na_bias", [16, 128, 21 * 128])
        d["na_mask"] = inp("na_mask", [128, 21 * 128])
        self.y2 = nc.dram_tensor("y2", [SPC, T, D], F32, kind="ExternalOutput").ap()

    def dbg_out(self, name, shape, dt=F32):
        ap = self.nc.dram_tensor("dbg_" + name, list(shape), dt, kind="ExternalOutput").ap()
        self.dbg_outs.append("dbg_" + name)
        return ap

    def carve(self, name, off, shape, dt):
        n = int(np.prod(shape[1:]))
        nbytes = n * _dsize(dt)
        assert off % 4 == 0 and nbytes % 4 == 0, (name, off, nbytes)
        assert off + nbytes <= ARENA_BYTES, (name, off, nbytes)
        ap = self.arena[0:shape[0], off // 4:(off + nbytes) // 4]
        if dt != F32:
            ap = ap.bitcast(dt)
        if len(shape) == 3:
            ap = ap.rearrange("p (a b) -> p a b", b=shape[2])
        elif len(shape) == 4:
            ap = ap.rearrange("p (a b c) -> p a b c", b=shape[2], c=shape[3])
        elif len(shape) == 5:
            ap = ap.rearrange("p (a b c d) -> p a b c d", b=shape[2], c=shape[3], d=shape[4])
        return Buf(name, ap)

    def alloc(self, region, name, shape, dt):
        n = int(np.prod(shape[1:])) * _dsize(dt)
        return self.carve(name, region.take(n), shape, dt)

    def psbf(self, i):
        return self.psall[:, i * 512:(i + 1) * 512].bitcast(BF16)

    def mm(self, out, lhsT, rhs, start=True, stop=True, R=(), W=()):
        self.P.op("pe", lambda e: e.matmul(out, lhsT, rhs, start=start, stop=stop), R, W)

    def tr(self, out, in_, ident, R=(), W=()):
        self.P.op("pe", lambda e: e.transpose(out, in_, ident), R, W)

    def act(self, out, in_, func, bias=None, scale=None, accum=None, R=(), W=()):
        kw = {}
        if bias is not None:
            kw["bias"] = bias
        if scale is not None:
            kw["scale"] = scale
        if accum is not None:
            kw["accum_out"] = accum
        self.P.op("act", lambda e: e.activation(out=out, in_=in_, func=func, **kw), R, W)

    def tt(self, eng, out, in0, in1, op, R=(), W=()):
        self.P.op(eng, lambda e: e.tensor_tensor(out=out, in0=in0, in1=in1, op=op), R, W)

    def ts(self, eng, out, in0, s1, op0, s2=None, op1=None, R=(), W=()):
        if op1 is None:
            self.P.op(eng, lambda e: e.tensor_scalar(out=out, in0=in0, scalar1=s1, scalar2=None, op0=op0), R, W)
        else:
            self.P.op(eng, lambda e: e.tensor_scalar(out=out, in0=in0, scalar1=s1, scalar2=s2, op0=op0, op1=op1), R, W)

    def stt(self, out, in0, scalar, in1, op0, op1, R=(), W=()):
        self.P.op("dve", lambda e: e.scalar_tensor_tensor(out=out, in0=in0, scalar=scalar, in1=in1, op0=op0, op1=op1), R, W)

    def cp(self, eng, out, in_, R=(), W=()):
        if eng == "act":
            self.P.op("act", lambda e: e.activation(out=out, in_=in_, func=AF.Copy), R, W)
        else:
            self.P.op(eng, lambda e: e.tensor_copy(out=out, in_=in_), R, W)

    def red(self, out, in_, op, R=(), W=()):
        self.P.op("dve", lambda e: e.tensor_reduce(out=out, in_=in_, axis=AX.X, op=op), R, W)

    def memset(self, eng, ap, val, R=(), W=()):
        self.P.op(eng, lambda e: e.memset(ap, val), R, W)

    def dma(self, queue, slot, out, in_, R=(), W=(), group=False, bar=True, **kw):
        self.P.dma(queue, slot, out, in_, R, W, group=group, bar=bar, **kw)

    def _persistent(self):
        A = self.alloc
        PR = self.PERS
        self.xT = self.carve("xT", XT_OFF, [128, 8, TT], F32)
        self.ring = [self.carve("ring%d" % i, RING_OFF + i * UNIT, [128, 8, 512], BF16) for i in range(NUNIT)]
        self.cstb = A(PR, "cstb", [128, 388], F32)
        self.identf = self.cstb[:, 0:128]
        self.iotac = self.cstb[:, 128:384]
        self.misc = self.cstb[:, 384:388]
        self.identb = A(PR, "identb", [128, 128], BF16)
        self.onesb = A(PR, "onesb", [128, 128], BF16)
        self.sel = A(PR, "sel", [16, 16, 128], BF16)
        self.modv = A(PR, "modv", [128, 4, 3, 6, 8], F32)
        self.cosA = A(PR, "cosA", [128, 16, 32], F32)
        self.sinA = A(PR, "sinA", [128, 16, 32], F32)
        self.cosB = A(PR, "cosB", [128, 16, 16], F32)
        self.sinB = A(PR, "sinB", [128, 16, 16], F32)
        self.smallp = A(PR, "smallp", [128, 64], F32)

    def prologue(self):
        d = self.d
        self.dma("sp", "c0", self.cstb[:, :], d["cst"][:, :], W=[("cst",)])
        self.dma("sp", "c0", self.sel[:, :, :], d["selc"].rearrange("k (e m) -> k e m", m=128), W=[("sel",)], group=True)
        for nm, buf in (("cosA", self.cosA), ("sinA", self.sinA), ("cosB", self.cosB), ("sinB", self.sinB)):
            self.dma("sp", "c0", buf[:, :, :], d[nm].rearrange("(t p) n -> p t n", p=128), W=[(nm,)], group=True)
        self.cp("dve", self.identb[:, :], self.identf, R=[("cst",)], W=[("identb",)])
        self.memset("pool", self.onesb[:, :], 1.0, W=[("onesb",)])
        R = self.R
        R.reset()
        vr = self.alloc(R, "vr", [128, 128], F32)
        vr2 = self.alloc(R, "vr2", [128, 128], F32)
        vr3 = self.alloc(R, "vr3", [128, 128], F32)
        vT = self.alloc(R, "vT", [128, 88], F32)
        abT = self.alloc(R, "abT", [128, 192], F32)
        sT = self.alloc(R, "sT", [128, 8, 4], F32)
        modraw = self.alloc(R, "modraw", [128, 4, 48, 3], F32)
        wslot = [self.carve("adaw0", HT_OFF, [128, 8, 768], F32),
                 self.alloc(R, "adaw1", [128, 8, 768], F32)]
        self.dma("sp", "p0", vr[0:24, :], d["cvec"].rearrange("s (c p) -> (s c) p", p=128), W=[("vr",)])
        self.dma("sp", "p0", vr[24:56, :], d["norm1_w"].rearrange("l (c p) -> (l c) p", p=128), W=[("vr",)], group=True)
        self.dma("sp", "p0", vr[56:88, :], d["norm2_w"].rearrange("l (c p) -> (l c) p", p=128), W=[("vr",)], group=True)
        abrows = d["ada_b"].rearrange("l (k p) -> (l k) p", p=128)
        self.dma("sp", "p0", vr2[:, :], abrows[0:128, :], W=[("vr2",)], group=True)
        self.dma("sp", "p0", vr3[0:64, :], abrows[128:192, :], W=[("vr3",)], group=True)
        ps = self.psb[0]
        self.tr(ps[:, 0:88], vr[0:88, :], self.identf[0:88, 0:88], R=[("vr",), ("cst",)], W=[("ps", 0)])
        self.cp("dve", vT[:, :], ps[:, 0:88], R=[("ps", 0)], W=[("vT",)])
        ps1 = self.psb[1]
        self.tr(ps1[:, 0:128], vr2[:, :], self.identf, R=[("vr2",), ("cst",)], W=[("ps", 1)])
        self.tr(ps1[:, 128:192], vr3[0:64, :], self.identf[0:64, 0:64], R=[("vr3",), ("cst",)], W=[("ps", 1)])
        self.cp("dve", abT[:, :], ps1[:, 0:192], R=[("ps", 1)], W=[("abT",)])
        self.memset("dve", sT[:, :, :], 0.0, W=[("sT",)])
        cTv = vT[:, 0:24].rearrange("p (s c) -> p c s", c=8)
        self.act(sT[:, :, 0:3], cTv, AF.Silu, R=[("vT",)], W=[("sT",)])
        n1 = vT[:, 24:56].rearrange("p (l c) -> p l c", c=8)
        n2 = vT[:, 56:88].rearrange("p (l c) -> p l c", c=8)
        for l in range(4):
            mps = self.psb[2 + (l % 2)]
            for cb in range(8):
                u = l * 8 + cb
                w = wslot[u % 2]
                self.dma("sp", "aw%d" % (u % 2), w[:, :, :],
                         d["ada_w"][l].rearrange("(c p) f -> p c f", p=128)[:, :, cb * 768:(cb + 1) * 768],
                         W=[("adaw", u % 2)])
                for fc in range(6):
                    col = (cb * 6 + fc) * 4
                    for dc in range(8):
                        self.mm(mps[:, col:col + 4], w[:, dc, fc * 128:(fc + 1) * 128], sT[:, dc, :],
                                start=(dc == 0), stop=(dc == 7),
                                R=[("adaw", u % 2), ("sT",)], W=[("ps", 2 + (l % 2))])
            mv = mps[:, 0:192].rearrange("p (k s) -> p k s", s=4)[:, :, 0:3]
            bv = abT[:, l * 48:(l + 1) * 48].unsqueeze(2).to_broadcast([128, 48, 3])
            self.tt("dve", modraw[:, l, :, :], mv, bv, ALU.add, R=[("ps", 2 + (l % 2)), ("abT",)], W=[("modraw", l)])
            mr = modraw[:, l, :, :].rearrange("p (k c) s -> p k s c", c=8)
            for kind, k in ((1, 0), (2, 2), (4, 3), (5, 5)):
                self.cp("dve", self.modv[:, l, :, kind, :], mr[:, k, :, :], R=[("modraw", l)], W=[("modv",)])
            for kind, k, nn in ((0, 1, n1), (3, 4, n2)):
                nb = nn[:, l, :].unsqueeze(1).to_broadcast([128, 3, 8])
                self.stt(self.modv[:, l, :, kind, :], mr[:, k, :, :], 1.0, nb, ALU.add, ALU.mult,
                         R=[("modraw", l), ("vT",)], W=[("modv",)])
        self.P.barrier()

    def mod(self, l, s, kind, c=None):
        if c is None:
            return self.modv[:, l, s, kind, :]
        return self.modv[:, l, s, kind, c:c + 1]

    def load_sample(self, b):
        R = self.R
        R.reset()
        stg = [self.alloc(R, "stg%d" % i, [128, D], F32) for i in range(2)]
        for t in range(NT):
            src = self.d["x2"][b, t * 128:(t + 1) * 128, :] if t < 16 else self.d["ctx2"][b, (t - 16) * 128:(t - 15) * 128, :]
            s = stg[t % 2]
            self.dma("sp", "ld%d" % (t % 2), s[:, :], src, W=[("stg", t % 2)])
            for half in range(2):
                bank = (t % 2) * 2 + half
                ps = self.psb[bank]
                for j in range(4):
                    c = half * 4 + j
                    self.tr(ps[:, j * 128:(j + 1) * 128], s[:, c * 128:(c + 1) * 128], self.identf,
                            R=[("stg", t % 2), ("cst",)], W=[("ps", bank)])
                self.cp("act" if half == 0 else "dve", self.xT[:, half * 4:(half + 1) * 4, t * 128:(t + 1) * 128],
                        ps[:, :].rearrange("p (j n) -> p j n", n=128),
                        R=[("ps", bank)], W=[("xT", min(t // 4, 4), half * 4 + j) for j in range(4)])
        self.P.barrier()

    def store_sample(self, dst, ntiles):
        R = self.R
        R.reset()
        stg = [self.alloc(R, "ostg%d" % i, [128, D], F32) for i in range(2)]
        for t in range(ntiles):
            s = stg[t % 2]
            for half in range(2):
                bank = (t % 2) * 2 + half
                ps = self.psb[bank]
                for j in range(4):
                    c = half * 4 + j
                    self.tr(ps[:, j * 128:(j + 1) * 128], self.xT[:, c, t * 128:(t + 1) * 128], self.identf,
                            R=[("xT", min(t // 4, 4), c), ("cst",)], W=[("ps", bank)])
                self.cp("act" if half == 0 else "dve", s[:, half * 512:(half + 1) * 512], ps[:, :],
                        R=[("ps", bank)], W=[("ostg", t % 2)])
            self.dma("sp", "st%d" % (t % 2), dst[t * 128:(t + 1) * 128, :], s[:, :], R=[("ostg", t % 2)])
        self.P.barrier()

    def norm_bufs(self):
        R = self.R
        self.sq = [self.alloc(R, "sq%d" % i, [128, 8, 512], BF16) for i in range(2)]
        self.lnv = self.alloc(R, "lnv", [128, 512], F32)
        self.rstd = [self.alloc(R, "rstd%d" % i, [128, 512], F32) for i in range(2)]
        self.tmpn = [self.alloc(R, "tmpn%d" % i, [128, 512], F32) for i in range(2)]

    def norm_group(self, gi, l, b, which, dst_of):
        t0, w = GROUPS[gi]
        s = b if gi < 4 else 2
        ka, kb = (0, 1) if which == 1 else (3, 4)
        sq = self.sq[gi % 2]
        self.act(sq[:, :, 0:w], self.xT[:, :, t0:t0 + w], AF.Square, R=[("xT", gi, c) for c in range(8)], W=[("sq", gi % 2)])
        bank = 5 + (gi % 2)
        ps = self.psb[bank]
        for c in range(8):
            self.mm(ps[:, 0:w], self.onesb[:, :], sq[:, c, 0:w], start=(c == 0), stop=(c == 7),
                    R=[("sq", gi % 2), ("onesb",)], W=[("ps", bank)])
        self.act(self.lnv[:, 0:w], ps[:, 0:w], AF.Ln, bias=self.misc[:, 2:3], scale=1.0 / D,
                 R=[("ps", bank), ("cst",)], W=[("lnv",)])
        rstd = self.rstd[gi % 2]
        self.act(rstd[:, 0:w], self.lnv[:, 0:w], AF.Exp, scale=-0.5, R=[("lnv",)], W=[("rstd", gi % 2)])
        for c in range(8):
            tm = self.tmpn[c % 2]
            self.tt("dve", tm[:, 0:w], self.xT[:, c, t0:t0 + w], rstd[:, 0:w], ALU.mult,
                    R=[("xT", gi, c), ("rstd", gi % 2)], W=[("tmpn", c % 2)])
            dst, dres = dst_of(c)
            self.act(dst, tm[:, 0:w], AF.Identity, bias=self.mod(l, s, kb, c), scale=self.mod(l, s, ka, c),
                     R=[("tmpn", c % 2), ("modv",)], W=[dres])

    def attn_core(self, steps_cfg, scale):
        steps = []
        for hi, hc in enumerate(steps_cfg):
            kts = hc["ktiles"]
            npair = (len(kts) + 1) // 2
            for pi in range(npair):
                ks = kts[2 * pi:2 * pi + 2]
                steps.append((hi, hc, ks, pi == 0, pi == npair - 1))
        n = len(steps)

        def qk(i):
            hi, hc, ks, first, last = steps[i]
            s_ = i % 2
            for j, k in enumerate(ks):
                bank = 2 * s_ + j
                self.mm(self.psb[bank][:, 0:hc["w"]], hc["kt"][:, k * 128:(k + 1) * 128], hc["qt"][:, hc["t0"]:hc["t0"] + hc["w"]],
                        R=[hc["kres"], hc["qres"]], W=[("ps", bank)])

        def ex(i):
            hi, hc, ks, first, last = steps[i]
            s_ = i % 2
            w = hc["w"]
            nk = len(ks)
            src = self.psall[:, s_ * 1024:s_ * 1024 + nk * 512].rearrange("p (b n) -> p b n", n=512)[:, :, 0:w]
            dst = self.PT[:, s_, 0:nk * 512].rearrange("p (b n) -> p b n", n=512)[:, :, 0:w]
            self.act(dst, src, AF.Exp, scale=scale, R=[("ps", 2 * s_ + j) for j in range(nk)], W=[("pt", s_)])

        def pv(i):
            hi, hc, ks, first, last = steps[i]
            w = hc["w"]
            s_ = i % 2
            if hc["hs"] == 0:
                self.take_op(hc["gi"], 8 if last else 1)
            ob = 4 + (hi + self.ot_ctr) % 2
            for j, k in enumerate(ks):
                self.mm(self.psb[ob][:, 0:w], hc["va"](k), self.PT[:, s_, j * 512:j * 512 + w],
                        start=(first and j == 0), stop=(last and j == len(ks) - 1),
                        R=[("pt", s_), hc["vres"]], W=[("ps", ob)])
            if last:
                lo, hi_ = (slice(0, 64), slice(64, 128))
                o_sl, d_sl = (lo, hi_) if hc["o_lo"] else (hi_, lo)
                rec_o = self.rec[d_sl, 0:w]
                rec_i = self.psb[ob][d_sl, 0:w]
                self.P.op("dve", lambda e: e.reciprocal(out=rec_o, in_=rec_i), [("ps", ob)], [("rec",)])
                self.tt("dve", hc["att"][o_sl, hc["t0"]:hc["t0"] + w], self.psb[ob][o_sl, 0:w], self.rec[d_sl, 0:w], ALU.mult,
                        R=[("ps", ob), ("rec",)], W=[("attT", hc["gi"])])

        if n == 0:
            return
        qk(0)
        for i in range(n):
            if i + 1 < n:
                qk(i + 1)
            ex(i)
            pv(i)
        self.ot_ctr += len(steps_cfg)

    def out_proj(self, wo, wres, att, ares, l, b, need_ctx):
        self.flush_op()
        ng = 5 if need_ctx else 4
        pend = {}
        for gi in range(ng):
            t0, w = GROUPS[gi]
            s = b if gi < 4 else 2
            lst = []
            for dc in range(8):
                def blk(gi=gi, dc=dc, t0=t0, w=w, s=s):
                    bank = 6 + (self.op_ctr % 2)
                    self.op_ctr += 1
                    ps = self.psb[bank]
                    self.mm(ps[:, 0:w], wo[:, dc * 128:(dc + 1) * 128], att[:, t0:t0 + w], R=[wres, ("attT", gi)], W=[("ps", bank)])
                    xs = self.xT[:, dc, t0:t0 + w]
                    self.stt(xs, ps[:, 0:w], self.mod(l, s, 2, dc), xs, ALU.mult, ALU.add,
                             R=[("ps", bank), ("modv",), ("xT", gi, dc)], W=[("xT", gi, dc)])
                lst.append(blk)
            pend[gi] = lst
        self.pending_op = pend

    def flush_op(self, gi=None):
        pend = getattr(self, "pending_op", None)
        if not pend:
            return
        keys = sorted(pend.keys()) if gi is None else ([gi] if gi in pend else [])
        for k in keys:
            for blk in pend.pop(k):
                blk()

    def take_op(self, gi, n):
        pend = getattr(self, "pending_op", None)
        if not pend or gi not in pend:
            return
        lst = pend[gi]
        for _ in range(min(n, len(lst))):
            lst.pop(0)()
        if not lst:
            pend.pop(gi)

    def head_rstd(self, src3, nh, hd, sqt, ssq, lnv, rs, res_src):
        self.tt("dve", sqt, src3, src3, ALU.mult, R=[res_src], W=[("sqt",)])
        self.red(ssq[:, 0:nh], sqt, ALU.add, R=[("sqt",)], W=[("ssq",)])
        self.act(lnv[:, 0:nh], ssq[:, 0:nh], AF.Ln, bias=self.misc[:, 2:3], scale=1.0 / hd, R=[("ssq",), ("cst",)], W=[("lnvh",)])
        self.act(rs[:, 0:nh], lnv[:, 0:nh], AF.Exp, scale=-0.5, R=[("lnvh",)], W=[("rsh",)])

    def bcast_row(self, queue, slot, dst, src_row, n, W, group=False):
        self.dma(queue, slot, dst, src_row.unsqueeze(0).to_broadcast([128, n]), W=W, group=group)

    def gqa_layer(self, l, b, need_ctx):
        d = self.d
        j = l // 3
        R, R2 = self.R, self.R2
        R.reset()
        R2.reset()
        self.norm_bufs()
        hT = self.carve("hT", HT_OFF, [128, 8, TT], BF16)
        for gi in range(5):
            t0, w = GROUPS[gi]
            self.norm_group(gi, l, b, 1, lambda c, t0=t0, w=w, gi=gi: (hT[:, c, t0:t0 + w], ("hT", gi)))
        self.P.barrier()
        R.reset()
        QKT = self.alloc(R, "QKT", [128, 5, TT], BF16)
        VA = self.alloc(R, "VA", [128, NT, 192], BF16)
        attT = self.alloc(R, "attT", [128, TT], BF16)
        Wg = self.alloc(R2, "Wg", [128, 8, 384], BF16)
        WoP = self.alloc(R2, "WoP", [128, 2, D], BF16)
        self.PT = self.alloc(R, "PT", [128, 2, 1024], BF16)
        self.rec = self.alloc(R, "rec", [128, 512], F32)
        gq = self.alloc(R, "gq", [128, 5, 64], F32)
        raw = [self.alloc(R2, "raw%d" % i, [128, 384], F32) for i in range(3)]
        self.memset("pool", QKT[64:128, 0:2, :], 0.0, W=[("QKTz",)])
        self.memset("pool", QKT[0:64, 2:4, :], 0.0, W=[("QKTz",)])
        sqt = self.alloc(R2, "sqt", [128, 5, 64], F32)
        t1 = self.alloc(R2, "t1", [128, 5, 64], F32)
        t2 = self.alloc(R2, "t2", [128, 5, 64], F32)
        ra = self.alloc(R2, "ra", [128, 5, 32], F32)
        rb = self.alloc(R2, "rb", [128, 5, 32], F32)
        rc = self.alloc(R2, "rc", [128, 5, 32], F32)
        rd = self.alloc(R2, "rd", [128, 5, 32], F32)
        qkb = [self.alloc(R2, "qkb%d" % i, [128, 6, 64], BF16) for i in range(3)]
        ssq = [self.smallp[:, 0:8], self.smallp[:, 8:16]]
        lnv = [self.smallp[:, 16:24], self.smallp[:, 24:32]]
        rs = [self.smallp[:, 32:40], self.smallp[:, 40:48]]
        for i in range(4):
            self.bcast_row("sp", "gq", gq[:, i, :], d["a_qnorm"][j], 64, W=[("gq",)], group=(i > 0))
        self.bcast_row("sp", "gq", gq[:, 4, :], d["a_knorm"][j], 64, W=[("gq",)], group=True)
        self.memset("pool", VA[:, :, 0:64], 1.0, W=[("VA", t) for t in range(NT)])
        self.memset("pool", VA[:, :, 128:192], 1.0, W=[("VA", t) for t in range(NT)])
        wq = d["a_wqkv"][j].rearrange("(c p) f -> p c f", p=128)
        ptb = self.psbf(7)
        ntl = NT
        for g in range(4):
            self.dma("pool", "wg", Wg[:, :, 0:256], wq[:, :, 256 * g:256 * g + 256], W=[("Wg",)], bar=False)
            self.dma("pool", "wg", Wg[:, :, 256:320], wq[:, :, 1024 + 64 * g:1024 + 64 * g + 64], W=[("Wg",)], group=True, bar=False)
            self.dma("pool", "wg", Wg[:, :, 320:384], wq[:, :, 1280 + 64 * g:1280 + 64 * g + 64], W=[("Wg",)], group=True, bar=False)
            def S1(t):
                bank = 5 + (t % 2)
                ps = self.psb[bank]
                for c in range(8):
                    self.mm(ps[:, 0:384], hT[:, c, t * 128:(t + 1) * 128], Wg[:, c, :], start=(c == 0), stop=(c == 7),
                            R=[("hT", min(t // 4, 4)), ("Wg",)], W=[("ps", bank)])
                self.cp("act", raw[t % 3][:, :], ps[:, 0:384], R=[("ps", bank)], W=[("raw", t % 3)])

            def S2(t):
                rw = raw[t % 3]
                r3 = rw[:, 0:320].rearrange("p (h e) -> p h e", e=64)
                for h in range(5):
                    self.act(sqt[:, h, :], r3[:, h, :], AF.Square, scale=0.125, accum=ssq[t % 2][:, h:h + 1],
                             R=[("raw", t % 3)], W=[("sqt",), ("ssq", t % 2)])

            def S3(t):
                self.act(lnv[t % 2][:, 0:5], ssq[t % 2][:, 0:5], AF.Ln, bias=self.misc[:, 2:3], scale=1.0,
                         R=[("ssq", t % 2), ("cst",)], W=[("lnvh", t % 2)])
                self.act(rs[t % 2][:, 0:5], lnv[t % 2][:, 0:5], AF.Exp, scale=-0.5, R=[("lnvh", t % 2)], W=[("rsh", t % 2)])

            def S4(t):
                rw = raw[t % 3]
                r3 = rw[:, 0:320].rearrange("p (h e) -> p h e", e=64)
                self.tt("dve", t1[:, :, :], r3, rs[t % 2][:, 0:5].unsqueeze(2).to_broadcast([128, 5, 64]), ALU.mult,
                        R=[("raw", t % 3), ("rsh", t % 2)], W=[("t1",)])
                self.tt("dve", t2[:, :, :], t1[:, :, :], gq[:, :, :], ALU.mult, R=[("t1",), ("gq",)], W=[("t2",)])
                qb = qkb[t % 3]
                if t < 16:
                    x1 = t2[:, :, 0:32]
                    x2 = t2[:, :, 32:64]
                    cs = self.cosA[:, t, :].unsqueeze(1).to_broadcast([128, 5, 32])
                    sn = self.sinA[:, t, :].unsqueeze(1).to_broadcast([128, 5, 32])
                    self.tt("dve", ra[:, :, :], x1, cs, ALU.mult, R=[("t2",), ("cosA",)], W=[("ra",)])
                    self.tt("dve", rb[:, :, :], x2, sn, ALU.mult, R=[("t2",), ("sinA",)], W=[("rb",)])
                    self.tt("dve", qb[:, 0:5, 0:32], ra[:, :, :], rb[:, :, :], ALU.subtract, R=[("ra",), ("rb",)], W=[("qkb", t % 3)])
                    self.tt("dve", rc[:, :, :], x1, sn, ALU.mult, R=[("t2",), ("sinA",)], W=[("rc",)])
                    self.tt("dve", rd[:, :, :], x2, cs, ALU.mult, R=[("t2",), ("cosA",)], W=[("rd",)])
                    self.tt("dve", qb[:, 0:5, 32:64], rc[:, :, :], rd[:, :, :], ALU.add, R=[("rc",), ("rd",)], W=[("qkb", t % 3)])
                else:
                    self.cp("dve", qb[:, 0:5, :], t2[:, :, :], R=[("t2",)], W=[("qkb", t % 3)])
                self.cp("dve", qb[:, 5, :], qb[:, 4, :], R=[("qkb", t % 3)], W=[("qkb", t % 3)])
                self.cp("pool", VA[:, t, 64:128], rw[:, 320:384], R=[("raw", t % 3)], W=[("VA", t)])

            def S5(t):
                qb = qkb[t % 3]
                qf = qb[:, :, :].rearrange("p h e -> p (h e)")
                for i in range(3):
                    self.tr(ptb[:, i * 128:(i + 1) * 128], qf[:, i * 128:(i + 1) * 128], self.identb[:, :],
                            R=[("qkb", t % 3), ("identb",)], W=[("ps", 7)])
                self.cp("act", QKT[0:64, 0:2, t * 128:(t + 1) * 128], ptb[0:64, 0:256].rearrange("p (i n) -> p i n", n=128),
                        R=[("ps", 7)], W=[("QKT", t)])
                self.cp("act", QKT[64:128, 2:4, t * 128:(t + 1) * 128], ptb[64:128, 0:256].rearrange("p (i n) -> p i n", n=128),
                        R=[("ps", 7)], W=[("QKT", t)])
                self.cp("dve", QKT[:, 4, t * 128:(t + 1) * 128], ptb[:, 256:384], R=[("ps", 7)], W=[("QKT", t)])

            self.pipeline(ntl, S1, S2, S3, S4, S5)
            for p in range(2):
                slot = (g * 2 + p) % 2
                h0 = 4 * g + 2 * p
                self.dma("pool", "wo%d" % slot, WoP[:, slot, :], d["a_wo"][j][h0 * 64:h0 * 64 + 128, :], W=[("WoP", slot)], bar=False)
                cfgs = []
                qgroups = [0, 1, 2, 3] + ([4] if need_ctx else [])
                for gi in qgroups:
                    t0, w = GROUPS[gi]
                    ktiles = list(range(NT)) if gi < 4 else [16, 17]
                    for hs in range(2):
                        psl = slice(0, 64) if hs == 0 else slice(64, 128)
                        cfgs.append(dict(
                            qt=QKT[:, p + 2 * hs, :], kt=QKT[:, 4, :],
                            va=(lambda k, hs=hs: VA[:, k, 64:192] if hs == 0 else VA[:, k, 0:128]),
                            o_lo=(hs == 0), t0=t0, w=w, gi=gi, hs=hs, ktiles=ktiles, att=attT,
                            qres=("QKTall",), kres=("QKTall",), vres=("VAall",), ares=("attT",)))
                self._alias([("QKT", t) for t in range(ntl)] + [("QKTz",)], ("QKTall",))
                self._alias([("VA", t) for t in range(ntl)], ("VAall",))
                self.attn_core(cfgs, 0.125)
                self.out_proj(WoP[:, slot, :], ("WoP", slot), attT, ("attT",), l, b, need_ctx)
            self._alias_release([("QKT", t) for t in range(ntl)], ("QKTall",))
            self._alias_release([("VA", t) for t in range(ntl)], ("VAall",))
        self.flush_op()
        self.P.barrier()

    def pipeline(self, n, S1, S2, S3, S4, S5):
        for i in range(-1, n + 2):
            if 0 <= i + 1 < n:
                S1(i + 1)
            if 0 <= i < n:
                S2(i)
                S3(i)
            if 0 <= i - 1 < n:
                S4(i - 1)
            if 0 <= i - 2 < n:
                S5(i - 2)

    def _alias(self, fine, coarse):
        P = self.P
        toks = set()
        for f in fine:
            st = P.res.get(f)
            if st is not None and st["w"] is not None:
                toks.add(st["w"])
        P.res[coarse] = {"w": None, "r": {}, "ws": toks}
        for e in ("pe",):
            P.pending[e] |= toks

    def _alias_release(self, fine, coarse):
        P = self.P
        st = P.res.get(coarse)
        if st is None:
            return
        for f in fine:
            fs = P.res.get(f)
            if fs is None:
                fs = P.res[f] = {"w": None, "r": {}}
            for k, v in st["r"].items():
                fs["r"][("al", coarse, k)] = v

    def mla_layer(self, l, b, need_ctx):
        d = self.d
        R, R2, HTR = self.R, self.R2, self.HTR
        R.reset()
        R2.reset()
        HTR.reset()
        self.norm_bufs()
        hT = self.carve("hT", HT_OFF, [128, 8, TT], BF16)
        for gi in range(5):
            t0, w = GROUPS[gi]
            self.norm_group(gi, l, b, 1, lambda c, t0=t0, w=w, gi=gi: (hT[:, c, t0:t0 + w], ("hT", gi)))
        self.P.barrier()
        R.reset()
        cT = self.alloc(R, "cT", [128, 5, TT], BF16)
        krope = self.alloc(R, "krope", [128, NT, 32], BF16)
        self.PT = self.alloc(R, "PT", [128, 2, 1024], BF16)
        self.rec = self.alloc(R, "rec", [128, 512], F32)
        gq2 = self.alloc(R, "gq2", [128, 2, 96], F32)
        gk2 = self.alloc(R, "gk2", [128, 2, 64], F32)
        gk = self.alloc(R, "gkr", [128, 32], F32)
        W1 = self.alloc(R, "W1", [128, 8, 672], BF16)
        g1 = self.alloc(R, "g1", [128, 640], F32)
        raw1 = [self.alloc(R2, "rawm%d" % i, [128, 672], F32) for i in range(2)]
        cn = [self.alloc(R2, "cn%d" % i, [128, 640], BF16) for i in range(2)]
        sq1 = self.alloc(R2, "sq1", [128, 384], F32)
        kr1 = self.alloc(R2, "kr1", [128, 32], F32)
        rt = [self.alloc(R2, "rt%d" % i, [128, 2, 16], F32) for i in range(4)]
        raw2 = [self.alloc(R2, "rawn%d" % i, [128, 448], F32) for i in range(3)]
        ssq2 = [self.smallp[:, 36:44], self.smallp[:, 44:52]]
        lnv2 = [self.smallp[:, 52:58], self.smallp[:, 58:64]]
        rs2 = [self.alloc(R2, "rs2_%d" % i, [128, 8], F32) for i in range(2)]
        sqt = self.alloc(R2, "sqt", [128, 2, 64], F32)
        t1 = self.alloc(R2, "t1", [128, 2, 96], F32)
        tq = self.alloc(R2, "tq", [128, 2, 32], F32)
        k1 = self.alloc(R2, "k1", [128, 2, 64], F32)
        qa = [self.alloc(R2, "qa%d" % i, [128, 2, 96], BF16) for i in range(3)]
        ka = [self.alloc(R2, "ka%d" % i, [128, 2, 96], BF16) for i in range(3)]
        ssq = self.smallp[:, 0:8]
        lnv = self.smallp[:, 8:16]
        rs = self.smallp[:, 16:24]
        ssq1 = self.smallp[:, 24:28]
        lnv1 = self.smallp[:, 28:32]
        rs1 = self.smallp[:, 32:36]
        self.bcast_row("sp", "gq", g1[:, 0:384], d["b_qnorm_lat"][0], 384, W=[("g1",)])
        self.bcast_row("sp", "gq", g1[:, 384:640], d["b_kvnorm_lat"][0], 256, W=[("g1",)], group=True)
        self.bcast_row("sp", "gq", gk[:, :], d["b_knorm"][0, 64:96], 32, W=[("gk",)], group=True)
        for i in range(2):
            self.bcast_row("sp", "gq", gq2[:, i, :], d["b_qnorm"][0], 96, W=[("gq2",)], group=True)
            self.bcast_row("sp", "gq", gk2[:, i, :], d["b_knorm"][0, 0:64], 64, W=[("gk2",)], group=True)
        self.dma("pool", "wg", W1[:, :, 0:384], d["b_wdq"][0].rearrange("(c p) f -> p c f", p=128), W=[("W1",)], bar=False)
        self.dma("pool", "wg", W1[:, :, 384:672], d["b_wdkv"][0].rearrange("(c p) f -> p c f", p=128), W=[("W1",)], group=True, bar=False)
        ptb = self.psbf(7)
        for t in range(NT):
            ba = 3 + 2 * (t % 2)
            bb = ba + 1
            for c in range(8):
                self.mm(self.psb[ba][:, :], hT[:, c, t * 128:(t + 1) * 128], W1[:, c, 0:512], start=(c == 0), stop=(c == 7),
                        R=[("hT", min(t // 4, 4)), ("W1",)], W=[("ps", ba)])
            for c in range(8):
                self.mm(self.psb[bb][:, 0:160], hT[:, c, t * 128:(t + 1) * 128], W1[:, c, 512:672], start=(c == 0), stop=(c == 7),
                        R=[("hT", min(t // 4, 4)), ("W1",)], W=[("ps", bb)])
            rw = raw1[t % 2]
            self.cp("act", rw[:, 0:512], self.psb[ba][:, :], R=[("ps", ba)], W=[("rawm", t % 2)])
            self.cp("act", rw[:, 512:672], self.psb[bb][:, 0:160], R=[("ps", bb)], W=[("rawm", t % 2)])
            for i, (c0, n) in enumerate(((0, 384), (384, 256), (640, 32))):
                self.act(sq1[:, 0:n], rw[:, c0:c0 + n], AF.Square, accum=ssq1[:, i:i + 1], R=[("rawm", t % 2)], W=[("sq1",), ("ssq1", i)])
                self.act(lnv1[:, i:i + 1], ssq1[:, i:i + 1], AF.Ln, bias=self.misc[:, 2:3], scale=1.0 / n, R=[("ssq1", i), ("cst",)], W=[("lnv1", i)])
            self.act(rs1[:, 0:3], lnv1[:, 0:3], AF.Exp, scale=-0.5, R=[("lnv1", 0), ("lnv1", 1), ("lnv1", 2)], W=[("rs1",)])
            cnb = cn[t % 2]
            self.stt(cnb[:, 0:384], rw[:, 0:384], rs1[:, 0:1], g1[:, 0:384], ALU.mult, ALU.mult, R=[("rawm", t % 2), ("rs1",), ("g1",)], W=[("cn", t % 2)])
            self.stt(cnb[:, 384:640], rw[:, 384:640], rs1[:, 1:2], g1[:, 384:640], ALU.mult, ALU.mult, R=[("rawm", t % 2), ("rs1",), ("g1",)], W=[("cn", t % 2)])
            self.stt(kr1[:, :], rw[:, 640:672], rs1[:, 2:3], gk[:, :], ALU.mult, ALU.mult, R=[("rawm", t % 2), ("rs1",), ("gk",)], W=[("kr1",)])
            if t < 16:
                x1 = kr1[:, 0:16]
                x2 = kr1[:, 16:32]
                cs = self.cosB[:, t, :]
                sn = self.sinB[:, t, :]
                self.tt("dve", rt[0][:, 0, :], x1, cs, ALU.mult, R=[("kr1",), ("cosB",)], W=[("rt", 0)])
                self.tt("dve", rt[1][:, 0, :], x2, sn, ALU.mult, R=[("kr1",), ("sinB",)], W=[("rt", 1)])
                self.tt("dve", krope[:, t, 0:16], rt[0][:, 0, :], rt[1][:, 0, :], ALU.subtract, R=[("rt", 0), ("rt", 1)], W=[("krope", t)])
                self.tt("dve", rt[2][:, 0, :], x1, sn, ALU.mult, R=[("kr1",), ("sinB",)], W=[("rt", 2)])
                self.tt("dve", rt[3][:, 0, :], x2, cs, ALU.mult, R=[("kr1",), ("cosB",)], W=[("rt", 3)])
                self.tt("dve", krope[:, t, 16:32], rt[2][:, 0, :], rt[3][:, 0, :], ALU.add, R=[("rt", 2), ("rt", 3)], W=[("krope", t)])
            else:
                self.cp("dve", krope[:, t, :], kr1[:, :], R=[("kr1",)], W=[("krope", t)])
            for i in range(5):
                self.tr(ptb[:, i * 128:(i + 1) * 128], cnb[:, i * 128:(i + 1) * 128], self.identb[:, :],
                        R=[("cn", t % 2), ("identb",)], W=[("ps", 7)])
            self.cp("dve", cT[:, :, t * 128:(t + 1) * 128], ptb[:, 0:640].rearrange("p (i n) -> p i n", n=128),
                    R=[("ps", 7)], W=[("cT", t)])
        self.P.barrier()
        QKT = self.alloc(HTR, "QKT", [128, 4, TT], BF16)
        VA = self.alloc(HTR, "VA", [128, NT, 192], BF16)
        attT = self.alloc(HTR, "attT", [128, TT], BF16)
        W2q = self.alloc(HTR, "W2q", [128, 3, 192], BF16)
        W2kv = self.alloc(HTR, "W2kv", [128, 2, 256], BF16)
        WoP = self.alloc(HTR, "WoP", [128, 2, D], BF16)
        self.memset("pool", VA[:, :, 64:128], 1.0, W=[("VA", t) for t in range(NT)])
        wuq = d["b_wuq"][0].rearrange("(c p) f -> p c f", p=128)
        wukv = d["b_wukv"][0].rearrange("(c p) f -> p c f", p=128)
        sc = 96.0 ** -0.5
        for pp in range(8):
            self.dma("pool", "wg", W2q[:, :, :], wuq[:, :, pp * 192:(pp + 1) * 192], W=[("W2",)], bar=False)
            self.dma("pool", "wg", W2kv[:, :, :], wukv[:, :, pp * 256:(pp + 1) * 256], W=[("W2",)], group=True, bar=False)
            def S1(t):
                bank = 5 + (t % 2)
                ps = self.psb[bank]
                for c in range(3):
                    self.mm(ps[:, 0:192], cT[:, c, t * 128:(t + 1) * 128], W2q[:, c, :], start=(c == 0), stop=(c == 2),
                            R=[("cT", t), ("W2",)], W=[("ps", bank)])
                for c in range(2):
                    self.mm(ps[:, 192:448], cT[:, 3 + c, t * 128:(t + 1) * 128], W2kv[:, c, :], start=(c == 0), stop=(c == 1),
                            R=[("cT", t), ("W2",)], W=[("ps", bank)])
                self.cp("act", raw2[t % 3][:, :], ps[:, 0:448], R=[("ps", bank)], W=[("rawn", t % 3)])

            def views(t):
                rw = raw2[t % 3]
                rq = rw[:, 0:192].rearrange("p (h e) -> p h e", e=96)
                rkv = rw[:, 192:448].rearrange("p (h e) -> p h e", e=128)
                return rw, rq, rkv

            def S2(t):
                rw, rq, rkv = views(t)
                sq = ssq2[t % 2]
                for i, (src, hd) in enumerate(((rq[:, :, 0:64], 64), (rq[:, :, 64:96], 32), (rkv[:, :, 0:64], 64))):
                    for h in range(2):
                        self.act(sqt[:, 0, 0:hd], src[:, h, :], AF.Square, scale=float(hd) ** -0.5, accum=sq[:, 2 * i + h:2 * i + h + 1],
                                 R=[("rawn", t % 3)], W=[("sqt",), ("ssq", t % 2)])

            def S3(t):
                self.act(lnv2[t % 2][:, 0:6], ssq2[t % 2][:, 0:6], AF.Ln, bias=self.misc[:, 2:3], scale=1.0,
                         R=[("ssq", t % 2), ("cst",)], W=[("lnvh", t % 2)])
                self.act(rs2[t % 2][:, 0:6], lnv2[t % 2][:, 0:6], AF.Exp, scale=-0.5, R=[("lnvh", t % 2)], W=[("rsh", t % 2)])

            def S4(t):
                rw, rq, rkv = views(t)
                rsv = rs2[t % 2]
                qab = qa[t % 3]
                kab = ka[t % 3]
                self.tt("dve", t1[:, :, 0:64], rq[:, :, 0:64], rsv[:, 0:2].unsqueeze(2).to_broadcast([128, 2, 64]), ALU.mult,
                        R=[("rawn", t % 3), ("rsh", t % 2)], W=[("t1",)])
                self.tt("dve", qab[:, :, 0:64], t1[:, :, 0:64], gq2[:, :, 0:64], ALU.mult, R=[("t1",), ("gq2",)], W=[("qa", t % 3)])
                self.tt("dve", t1[:, :, 64:96], rq[:, :, 64:96], rsv[:, 2:4].unsqueeze(2).to_broadcast([128, 2, 32]), ALU.mult,
                        R=[("rawn", t % 3), ("rsh", t % 2)], W=[("t1",)])
                if t < 16:
                    self.tt("dve", tq[:, :, :], t1[:, :, 64:96], gq2[:, :, 64:96], ALU.mult, R=[("t1",), ("gq2",)], W=[("tq",)])
                    x1 = tq[:, :, 0:16]
                    x2 = tq[:, :, 16:32]
                    cs = self.cosB[:, t, :].unsqueeze(1).to_broadcast([128, 2, 16])
                    sn = self.sinB[:, t, :].unsqueeze(1).to_broadcast([128, 2, 16])
                    self.tt("dve", rt[0][:, :, :], x1, cs, ALU.mult, R=[("tq",), ("cosB",)], W=[("rt", 0)])
                    self.tt("dve", rt[1][:, :, :], x2, sn, ALU.mult, R=[("tq",), ("sinB",)], W=[("rt", 1)])
                    self.tt("dve", qab[:, :, 64:80], rt[0][:, :, :], rt[1][:, :, :], ALU.subtract, R=[("rt", 0), ("rt", 1)], W=[("qa", t % 3)])
                    self.tt("dve", rt[2][:, :, :], x1, sn, ALU.mult, R=[("tq",), ("sinB",)], W=[("rt", 2)])
                    self.tt("dve", rt[3][:, :, :], x2, cs, ALU.mult, R=[("tq",), ("cosB",)], W=[("rt", 3)])
                    self.tt("dve", qab[:, :, 80:96], rt[2][:, :, :], rt[3][:, :, :], ALU.add, R=[("rt", 2), ("rt", 3)], W=[("qa", t % 3)])
                else:
                    self.tt("dve", qab[:, :, 64:96], t1[:, :, 64:96], gq2[:, :, 64:96], ALU.mult, R=[("t1",), ("gq2",)], W=[("qa", t % 3)])
                self.tt("dve", k1[:, :, :], rkv[:, :, 0:64], rsv[:, 4:6].unsqueeze(2).to_broadcast([128, 2, 64]), ALU.mult,
                        R=[("rawn", t % 3), ("rsh", t % 2)], W=[("k1",)])
                self.tt("dve", kab[:, :, 0:64], k1[:, :, :], gk2[:, :, :], ALU.mult, R=[("k1",), ("gk2",)], W=[("ka", t % 3)])
                self.cp("dve", kab[:, :, 64:96], krope[:, t, :].unsqueeze(1).to_broadcast([128, 2, 32]), R=[("krope", t)], W=[("ka", t % 3)])
                self.cp("pool", VA[:, t, 0:64], rkv[:, 0, 64:128], R=[("rawn", t % 3)], W=[("VA", t)])
                self.cp("pool", VA[:, t, 128:192], rkv[:, 1, 64:128], R=[("rawn", t % 3)], W=[("VA", t)])

            def S5(t):
                qab = qa[t % 3]
                kab = ka[t % 3]
                for i in range(2):
                    self.tr(ptb[0:96, i * 128:(i + 1) * 128], qab[:, i, :], self.identb[:, :], R=[("qa", t % 3), ("identb",)], W=[("ps", 7)])
                for i in range(2):
                    self.tr(ptb[0:96, (2 + i) * 128:(3 + i) * 128], kab[:, i, :], self.identb[:, :], R=[("ka", t % 3), ("identb",)], W=[("ps", 7)])
                self.cp("act", QKT[0:96, :, t * 128:(t + 1) * 128], ptb[0:96, 0:512].rearrange("p (i n) -> p i n", n=128),
                        R=[("ps", 7)], W=[("QKT", t)])

            self.pipeline(NT, S1, S2, S3, S4, S5)
            slot = pp % 2
            self.dma("pool", "wo%d" % slot, WoP[:, slot, :], d["b_wo"][0][pp * 128:(pp + 1) * 128, :], W=[("WoP", slot)], bar=False)
            cfgs = []
            qgroups = [0, 1, 2, 3] + ([4] if need_ctx else [])
            for gi in qgroups:
                t0, w = GROUPS[gi]
                ktiles = list(range(NT)) if gi < 4 else [16, 17]
                for hs in range(2):
                    cfgs.append(dict(
                        qt=QKT[0:96, hs, :], kt=QKT[0:96, 2 + hs, :],
                        va=(lambda k, hs=hs: VA[:, k, 0:128] if hs == 0 else VA[:, k, 64:192]),
                        o_lo=(hs == 0), t0=t0, w=w, gi=gi, hs=hs, ktiles=ktiles, att=attT,
                        qres=("QKTall",), kres=("QKTall",), vres=("VAall",), ares=("attT",)))
            self._alias([("QKT", t) for t in range(NT)], ("QKTall",))
            self._alias([("VA", t) for t in range(NT)], ("VAall",))
            self.attn_core(cfgs, sc)
            self.out_proj(WoP[:, slot, :], ("WoP", slot), attT, ("attT",), l, b, need_ctx)
            self._alias_release([("QKT", t) for t in range(NT)], ("QKTall",))
            self._alias_release([("VA", t) for t in range(NT)], ("VAall",))
        self.flush_op()
        self.P.barrier()

    def na_layer(self, l, b, need_ctx):
        d = self.d
        R, R2 = self.R, self.R2
        R.reset()
        R2.reset()
        self.norm_bufs()
        hT = self.carve("hT", HT_OFF, [128, 8, TT], BF16)
        for gi in range(5):
            t0, w = GROUPS[gi]
            self.norm_group(gi, l, b, 1, lambda c, t0=t0, w=w, gi=gi: (hT[:, c, t0:t0 + w], ("hT", gi)))
        self.P.barrier()
        R.reset()
        QKT = self.alloc(R, "QKT", [128, 3, TT], BF16)
        VA = self.alloc(R, "VA", [128, NT, 192], BF16)
        attT = self.alloc(R, "attT", [128, TT], BF16)
        Wp = self.alloc(R, "Wg", [128, 8, 384], BF16)
        WoP = self.alloc(R, "WoP", [128, 2, D], BF16)
        self.PT = self.alloc(R, "PT", [128, 2, 1024], BF16)
        self.rec = self.alloc(R2, "rec", [128, 256], F32)
        gq4 = self.alloc(R2, "gq4", [128, 4, 64], F32)
        maskb = self.alloc(R, "maskb", [128, 21 * 128], BF16)
        self.memset("pool", QKT[64:128, 0, :], 0.0, W=[("QKTz",)])
        self.memset("pool", QKT[0:64, 1, :], 0.0, W=[("QKTz",)])
        BM = self.alloc(R2, "BM", [128, 21 * 128], F32)
        tmpb = [self.alloc(R2, "tmpb%d" % i, [128, 512], F32) for i in range(2)]
        raw = [self.alloc(R2, "raw%d" % i, [128, 384], F32) for i in range(3)]
        sqt = self.alloc(R2, "sqt", [128, 4, 64], F32)
        qkb = [self.alloc(R2, "qkb%d" % i, [128, 4, 64], BF16) for i in range(3)]
        junk = self.alloc(R2, "junk", [128, 64], F32)
        ssq = [self.smallp[:, 0:8], self.smallp[:, 8:16]]
        lnv = [self.smallp[:, 16:24], self.smallp[:, 24:32]]
        rs = [self.smallp[:, 32:40], self.smallp[:, 40:48]]
        for i in range(2):
            self.bcast_row("sp", "gq", gq4[:, i, :], d["c_qnorm"][0], 64, W=[("gq4",)], group=(i > 0))
            self.bcast_row("sp", "gq", gq4[:, 2 + i, :], d["c_knorm"][0], 64, W=[("gq4",)], group=True)
        self.dma("pool", "mk", maskb[:, 0:1344], d["na_mask"][:, 0:1344], W=[("maskb",)])
        self.dma("pool", "mk", maskb[:, 1344:2688], d["na_mask"][:, 1344:2688], W=[("maskb",)], group=True)
        self.memset("pool", VA[:, :, 64:128], 1.0, W=[("VA", t) for t in range(NT)])
        wq = d["c_wqkv"][0].rearrange("(c p) f -> p c f", p=128)
        ptb = self.psbf(7)
        sc = 0.125
        for pp in range(8):
            self.dma("pool", "wg", Wp[:, :, 0:128], wq[:, :, 128 * pp:128 * pp + 128], W=[("Wg",)], bar=False)
            self.dma("pool", "wg", Wp[:, :, 128:256], wq[:, :, 1024 + 128 * pp:1024 + 128 * pp + 128], W=[("Wg",)], group=True, bar=False)
            self.dma("pool", "wg", Wp[:, :, 256:384], wq[:, :, 2048 + 128 * pp:2048 + 128 * pp + 128], W=[("Wg",)], group=True, bar=False)
            def S1(t):
                bank = 5 + (t % 2)
                ps = self.psb[bank]
                for c in range(8):
                    self.mm(ps[:, 0:384], hT[:, c, t * 128:(t + 1) * 128], Wp[:, c, :], start=(c == 0), stop=(c == 7),
                            R=[("hT", min(t // 4, 4)), ("Wg",)], W=[("ps", bank)])
                self.cp("act", raw[t % 3][:, :], ps[:, 0:384], R=[("ps", bank)], W=[("raw", t % 3)])

            def S2(t):
                r3 = raw[t % 3][:, 0:256].rearrange("p (h e) -> p h e", e=64)
                for h in range(4):
                    self.act(junk[:, :], r3[:, h, :], AF.Square, scale=0.125, accum=ssq[t % 2][:, h:h + 1],
                             R=[("raw", t % 3)], W=[("junk",), ("ssq", t % 2)])

            def S3(t):
                self.act(lnv[t % 2][:, 0:4], ssq[t % 2][:, 0:4], AF.Ln, bias=self.misc[:, 2:3], scale=1.0,
                         R=[("ssq", t % 2), ("cst",)], W=[("lnvh", t % 2)])
                self.act(rs[t % 2][:, 0:4], lnv[t % 2][:, 0:4], AF.Exp, scale=-0.5, R=[("lnvh", t % 2)], W=[("rsh", t % 2)])

            def S4(t):
                rw = raw[t % 3]
                r3 = rw[:, 0:256].rearrange("p (h e) -> p h e", e=64)
                self.tt("dve", sqt[:, :, :], r3, rs[t % 2][:, 0:4].unsqueeze(2).to_broadcast([128, 4, 64]), ALU.mult,
                        R=[("raw", t % 3), ("rsh", t % 2)], W=[("sqt",)])
                qb = qkb[t % 3]
                self.tt("dve", qb[:, :, :], sqt[:, :, :], gq4[:, :, :], ALU.mult, R=[("sqt",), ("gq4",)], W=[("qkb", t % 3)])
                self.cp("pool", VA[:, t, 0:64], rw[:, 256:320], R=[("raw", t % 3)], W=[("VA", t)])
                self.cp("pool", VA[:, t, 128:192], rw[:, 320:384], R=[("raw", t % 3)], W=[("VA", t)])

            def S5(t):
                qb = qkb[t % 3]
                qf = qb[:, :, :].rearrange("p h e -> p (h e)")
                for i in range(2):
                    self.tr(ptb[:, i * 128:(i + 1) * 128], qf[:, i * 128:(i + 1) * 128], self.identb[:, :],
                            R=[("qkb", t % 3), ("identb",)], W=[("ps", 7)])
                self.cp("act", QKT[0:64, 0, t * 128:(t + 1) * 128], ptb[0:64, 0:128], R=[("ps", 7)], W=[("QKT", t)])
                self.cp("act", QKT[64:128, 1, t * 128:(t + 1) * 128], ptb[64:128, 0:128], R=[("ps", 7)], W=[("QKT", t)])
                self.cp("dve", QKT[:, 2, t * 128:(t + 1) * 128], ptb[:, 128:256], R=[("ps", 7)], W=[("QKT", t)])

            self.pipeline(NT, S1, S2, S3, S4, S5)
            slot = pp % 2
            self.dma("pool", "wo%d" % slot, WoP[:, slot, :], d["c_wo"][0][pp * 128:(pp + 1) * 128, :], W=[("WoP", slot)], bar=False)
            self._alias([("QKT", t) for t in range(NT)] + [("QKTz",)], ("QKTall",))
            self._alias([("VA", t) for t in range(NT)], ("VAall",))
            for hs in range(2):
                h = 2 * pp + hs
                psl = slice(0, 64) if hs == 0 else slice(64, 128)
                self.dma("sp", "bm", BM[:, :], d["na_bias"][h], W=[("BM",)])
                self.tt("pool", BM[:, :], BM[:, :], maskb[:, :], ALU.add, R=[("BM",), ("maskb",)], W=[("BM",)])
                va = (lambda k, hs=hs: VA[:, k, 0:128] if hs == 0 else VA[:, k, 64:192])
                self.na_core(QKT[:, hs, :], QKT[:, 2, :], va, hs == 0, attT, BM, tmpb, sc)
            if need_ctx:
                cfgs = []
                t0, w = GROUPS[4]
                for hs in range(2):
                    psl = slice(0, 64) if hs == 0 else slice(64, 128)
                    cfgs.append(dict(
                        qt=QKT[:, hs, :], kt=QKT[:, 2, :],
                        va=(lambda k, hs=hs: VA[:, k, 0:128] if hs == 0 else VA[:, k, 64:192]),
                        o_lo=(hs == 0), t0=t0, w=w, gi=4, hs=hs, ktiles=[16, 17], att=attT,
                        qres=("QKTall",), kres=("QKTall",), vres=("VAall",), ares=("attT",)))
                self.attn_core(cfgs, sc)
            self.out_proj(WoP[:, slot, :], ("WoP", slot), attT, ("attT",), l, b, need_ctx)
            self._alias_release([("QKT", t) for t in range(NT)], ("QKTall",))
            self._alias_release([("VA", t) for t in range(NT)], ("VAall",))
        self.flush_op()
        self.P.barrier()

    def na_core(self, qt, kt, va, o_lo, attT, BM, tmpb, sc):
        packs = []
        for qi in range(16):
            blocks = na_block_ids(qi)
            items = [(kt_, blk) for (kt_, blk) in blocks] + [(16, None), (17, None)]
            plist = [items[0:4], items[4:]]
            for pi, pk in enumerate(plist):
                packs.append((qi, pk, pi == 0, pi == len(plist) - 1))
        n = len(packs)
        pt3 = self.PT[:, :, :].rearrange("p a (b n) -> p (a b) n", n=512)
        lo, hi_ = slice(0, 64), slice(64, 128)
        o_sl, d_sl = (lo, hi_) if o_lo else (hi_, lo)

        def qk(i):
            qi, pk, first, last = packs[i]
            bank = i % 3
            for j, (k, blk) in enumerate(pk):
                self.mm(self.psb[bank][:, j * 128:(j + 1) * 128], kt[:, k * 128:(k + 1) * 128], qt[:, qi * 128:(qi + 1) * 128],
                        R=[("QKTall",)], W=[("ps", bank)])

        def ex(i):
            qi, pk, first, last = packs[i]
            bank = i % 3
            nb = sum(1 for (_, blk) in pk if blk is not None)
            nk = len(pk)
            if nb > 0:
                b0 = pk[0][1]
                tb = tmpb[i % 2]
                self.stt(tb[:, 0:nb * 128], self.psb[bank][:, 0:nb * 128], sc, BM[:, b0 * 128:(b0 + nb) * 128], ALU.mult, ALU.add,
                         R=[("ps", bank), ("BM",)], W=[("tmpb", i % 2)])
                self.act(pt3[:, bank, 0:nb * 128], tb[:, 0:nb * 128], AF.Exp, R=[("tmpb", i % 2)], W=[("pt", bank)])
            if nk > nb:
                self.act(pt3[:, bank, nb * 128:nk * 128], self.psb[bank][:, nb * 128:nk * 128], AF.Exp, scale=sc,
                         R=[("ps", bank)], W=[("pt", bank)])

        def pv(i):
            qi, pk, first, last = packs[i]
            if o_lo and first and qi % 4 == 0:
                self.take_op(qi // 4, 8)
            ob = 3 + (qi + self.ot_ctr) % 2
            for j, (k, blk) in enumerate(pk):
                self.mm(self.psb[ob][:, 0:128], va(k), pt3[:, i % 3, j * 128:(j + 1) * 128],
                        start=(first and j == 0), stop=(last and j == len(pk) - 1),
                        R=[("pt", i % 3), ("VAall",)], W=[("ps", ob)])
            if last:
                rec_o = self.rec[d_sl, 0:128]
                rec_i = self.psb[ob][d_sl, 0:128]
                self.P.op("dve", lambda e: e.reciprocal(out=rec_o, in_=rec_i), [("ps", ob)], [("rec",)])
                self.tt("dve", attT[o_sl, qi * 128:(qi + 1) * 128], self.psb[ob][o_sl, 0:128], self.rec[d_sl, 0:128], ALU.mult,
                        R=[("ps", ob), ("rec",)], W=[("attT", qi // 4)])

        qk(0)
        if n > 1:
            qk(1)
        for i in range(n):
            if i + 2 < n:
                qk(i + 2)
            ex(i)
            pv(i)
        self.ot_ctr += 16

    def ring_load(self, src):
        i = self.unit_ctr % NUNIT
        self.unit_ctr += 1
        buf = self.ring[i]
        self.dma("pool", "ring%d" % i, buf[:, :, :], src, W=[("ring", i)], bar=False)
        return buf, ("ring", i)

    def moe_units(self, l, e):
        d = self.d
        wg = d["moe_wg"][l, e].rearrange("(c p) f -> p c f", p=128)
        wu = d["moe_wu"][l, e].rearrange("(c p) f -> p c f", p=128)
        wd = d["moe_wd"][l, e].rearrange("(c p) f -> p c f", p=128)
        return [wg[:, :, 0:512], wu[:, :, 0:512], wg[:, :, 512:1024], wu[:, :, 512:1024], wd[:, :, 0:512], wd[:, :, 512:1024]]

    def moe_layer(self, l, b, need_ctx, pre):
        d = self.d
        R = self.R
        R.reset()
        ROFF = R_OFF
        self.norm_bufs()
        h2T = [self.alloc(R, "h2T%d" % i, [128, 8, 512], BF16) for i in range(2)]
        aff = self.carve("aff", ROFF + 43008, [128, NT, 16], F32)
        rw = self.carve("rw", ROFF + 44160, [128, 8, 16], BF16)
        m8 = self.carve("m8", ROFF + 44160 + 256, [16, 8], F32)
        thr = self.carve("thr", ROFF + 44160 + 288, [16, 2], F32)
        gate = self.carve("gate", ROFF + 44160 + 320, [128, 4], F32)
        ee = self.carve("ee", ROFF + 44160 + 352, [128, 16], F32)
        sst = self.carve("sst", ROFF + 44160 + 416, [128, 8], F32)
        h2 = self.carve("h2", HT_OFF, [128, NT, D], BF16)
        self.dma("pool", "rw", rw[:, :, :], d["moe_router"][l].rearrange("(c p) e -> p c e", p=128), W=[("rw",)])
        ngrp = 5 if need_ctx else 4
        ntl = NT if need_ctx else 16
        ptb = self.psbf(7)
        for gi in range(ngrp):
            t0, w = GROUPS[gi]
            hb = h2T[gi % 2]
            self.norm_group(gi, l, b, 2, lambda c, hb=hb, w=w, gi=gi: (hb[:, c, 0:w], ("h2T", gi % 2)))
            for tt_ in range(w // 128):
                t = t0 // 128 + tt_
                bank = 3 + (t % 2)
                ps = self.psb[bank]
                for c in range(8):
                    self.mm(ps[:, 0:16], hb[:, c, tt_ * 128:(tt_ + 1) * 128], rw[:, c, :], start=(c == 0), stop=(c == 7),
                            R=[("h2T", gi % 2), ("rw",)], W=[("ps", bank)])
                self.red(sst[:, 0:1], ps[:, 0:16], ALU.max, R=[("ps", bank)], W=[("sst",)])
                self.ts("dve", sst[:, 1:2], sst[:, 0:1], -1.0, ALU.mult, R=[("sst",)], W=[("sst",)])
                self.act(ee[:, :], ps[:, 0:16], AF.Exp, bias=sst[:, 1:2], accum=sst[:, 2:3],
                         R=[("ps", bank), ("sst",)], W=[("ee",), ("sst",)])
                self.P.op("dve", lambda e: e.reciprocal(out=sst[:, 3:4], in_=sst[:, 2:3]), [("sst",)], [("sst",)])
                self.ts("dve", aff[:, t, :], ee[:, :], sst[:, 3:4], ALU.mult, R=[("ee",), ("sst",)], W=[("aff",)])
                for c in range(8):
                    self.tr(ptb[:, c * 128:(c + 1) * 128], hb[:, c, tt_ * 128:(tt_ + 1) * 128], self.identb[:, :],
                            R=[("h2T", gi % 2), ("identb",)], W=[("ps", 7)])
                self.cp("act", h2[:, t, :], ptb[:, :], R=[("ps", 7)], W=[("h2", t)])
        self.P.barrier()
        affT = self.carve("affT", ROFF + 0, [16, TT], F32)
        work = self.carve("work", ROFF + 9216, [16, TT], F32)
        B3 = self.carve("B3", ROFF + 18432, [16, TT], F32)
        vb16 = self.carve("vb16", ROFF + 27648, [16, TT], BF16)
        gmtok = self.carve("gmtok", ROFF + 38400, [128, NT, 16], F32)
        vtok = self.carve("vtok", ROFF + 40704, [128, NT, 16], F32)
        gmhl = self.carve("gmhl", ROFF + 41856, [128, NT, 16, 2], BF16)
        for t in range(ntl):
            bank = (t // 4) % 2
            self.tr(self.psb[bank][0:16, (t % 4) * 128:(t % 4 + 1) * 128], aff[:, t, :], self.identf,
                    R=[("aff",), ("cst",)], W=[("ps", bank)])
            if t % 4 == 3 or t == ntl - 1:
                t_lo = (t // 4) * 4
                n = (t - t_lo + 1) * 128
                self.cp("dve", affT[:, t_lo * 128:t_lo * 128 + n], self.psb[bank][0:16, 0:n], R=[("ps", bank)], W=[("affT",)])
        segs = [(0, T, CAP // 8, 0)] + ([(T, TC, CAPC // 8, 1)] if need_ctx else [])
        ncols = T + (TC if need_ctx else 0)
        self.cp("dve", work[:, 0:ncols], affT[:, 0:ncols], R=[("affT",)], W=[("work",)])
        for (c0, n, rounds, ti) in segs:
            wv = work[:, c0:c0 + n]
            for r in range(rounds):
                self.P.op("dve", lambda e, wv=wv: e.max(out=m8[:, :], in_=wv), [("work",)], [("m8",)])
                if r < rounds - 1:
                    self.P.op("dve", lambda e, wv=wv: e.match_replace(out=wv, in_to_replace=m8[:, :], in_values=wv, imm_value=-1.0),
                              [("work",), ("m8",)], [("work",)])
            self.cp("dve", thr[:, ti:ti + 1], m8[:, 7:8], R=[("m8",)], W=[("thr",)])
        ones_col = self.cstb[0:16, 128 + 1:128 + 2]
        for (c0, n, rounds, ti) in segs:
            self.ts("dve", work[:, c0:c0 + n], affT[:, c0:c0 + n], thr[:, ti:ti + 1], ALU.is_ge, R=[("affT",), ("thr",)], W=[("work",)])
            self.P.op("dve", lambda e, c0=c0, n=n: e.tensor_tensor_scan(out=B3[:, c0:c0 + n], data0=ones_col.to_broadcast([16, n]),
                                                                     data1=work[:, c0:c0 + n], initial=0.0, op0=ALU.mult, op1=ALU.add),
                      [("work",), ("cst",)], [("B3",)])
        self.tt("dve", B3[:, 0:ncols], B3[:, 0:ncols], work[:, 0:ncols], ALU.mult, R=[("B3",), ("work",)], W=[("B3",)])
        self.ts("dve", B3[:, 0:ncols], B3[:, 0:ncols], -1.0, ALU.add, R=[("B3",)], W=[("B3",)])
        self.tt("dve", affT[:, 0:ncols], affT[:, 0:ncols], work[:, 0:ncols], ALU.mult, R=[("affT",), ("work",)], W=[("affT",)])
        self.cp("dve", vb16[:, 0:ncols], B3[:, 0:ncols], R=[("B3",)], W=[("vb16",)])
        for (src, dst, nm, bank) in ((B3, vtok, "vtok", 2), (affT, gmtok, "gmtok", 3)):
            for t in range(ntl):
                self.tr(self.psb[bank][:, t * 16:(t + 1) * 16], src[:, t * 128:(t + 1) * 128], self.identf[0:16, 0:16],
                        R=[(src.name,), ("cst",)], W=[("ps", bank)])
            self.cp("dve", dst[:, 0:ntl, :], self.psb[bank][:, 0:ntl * 16].rearrange("p (t e) -> p t e", e=16),
                    R=[("ps", bank)], W=[(nm,)])
        self.cp("dve", gmhl[:, 0:ntl, :, 0], gmtok[:, 0:ntl, :], R=[("gmtok",)], W=[("gmhl",)])
        self.tt("dve", gmtok[:, 0:ntl, :], gmtok[:, 0:ntl, :], gmhl[:, 0:ntl, :, 0], ALU.subtract, R=[("gmtok",), ("gmhl",)], W=[("gmtok",)])
        self.cp("dve", gmhl[:, 0:ntl, :, 1], gmtok[:, 0:ntl, :], R=[("gmtok",)], W=[("gmhl",)])
        self.P.barrier()
        S = self.carve("S", ROFF + 0, [128, 16, 256], BF16)
        Sc = self.carve("Sc", ROFF + 8192, [128, 2, 32], BF16)
        ST = self.carve("ST", ROFF + 9216, [128, 2, T], BF16)
        STc = self.carve("STc", ROFF + 9216 + 8192, [32, 256], BF16)
        xgT = self.carve("xgT", ROFF + 18432, [128, 8, 288], BF16)
        hidT = self.carve("hidT", ROFF + 18432 + 4608, [128, 8, 288], BF16)
        y = self.carve("y", ROFF + 32256, [128, 3, D], BF16)
        sg = self.carve("sg", ROFF + 38400, [128, 2, 288], F32)
        Wn = 288 if need_ctx else 256
        units = list(pre)
        upos = [0]

        def next_units(n):
            out = []
            for _ in range(n):
                out.append(units[upos[0]])
                upos[0] += 1
            return out

        srcs = []
        for e in range(NE):
            srcs += self.moe_units(l, e)
        issued = [len(pre)]

        def issue(upto):
            while issued[0] < min(upto, len(srcs)):
                units.append(self.ring_load(srcs[issued[0]]))
                issued[0] += 1

        chunks = [(0, 128), (128, 128)] + ([(256, 32)] if need_ctx else [])
        nch = 3 if need_ctx else 2

        def build_S(e):
            for t in range(16):
                self.ts("dve", S[:, t, :], self.iotac, vtok[:, t, e:e + 1], ALU.is_equal, R=[("vtok",), ("cst",)], W=[("S", t)])
            if need_ctx:
                for t in range(2):
                    self.ts("dve", Sc[:, t, :], self.iotac[:, 0:32], vtok[:, 16 + t, e:e + 1], ALU.is_equal, R=[("vtok",), ("cst",)], W=[("Sc",)])

        def slot_gates(e):
            gb = 4
            gps = self.psb[gb]
            for ch in range(2):
                for t in range(16):
                    self.mm(gps[:, ch * 2:ch * 2 + 2], S[:, t, ch * 128:(ch + 1) * 128], gmhl[:, t, e, :], start=(t == 0), stop=(t == 15),
                            R=[("S", t), ("gmhl",)], W=[("ps", gb)])
            if need_ctx:
                for t in range(2):
                    self.mm(gps[0:32, 4:6], Sc[:, t, :], gmhl[:, 16 + t, e, :], start=(t == 0), stop=(t == 1),
                            R=[("Sc",), ("gmhl",)], W=[("ps", gb)])
            self.red(gate[:, 0:nch], gps[:, 0:2 * nch].rearrange("p (c two) -> p c two", two=2), ALU.add, R=[("ps", gb)], W=[("gate",)])

        def gather_c(e, c):
            bank = c % 2
            ps = self.psb[bank]
            for t in range(16):
                self.mm(ps[:, 0:256], h2[:, t, c * 128:(c + 1) * 128], S[:, t, :], start=(t == 0), stop=(t == 15),
                        R=[("h2", t), ("S", t)], W=[("ps", bank)])
            if need_ctx:
                for t in range(2):
                    self.mm(ps[:, 256:288], h2[:, 16 + t, c * 128:(c + 1) * 128], Sc[:, t, :], start=(t == 0), stop=(t == 1),
                            R=[("h2", 16 + t), ("Sc",)], W=[("ps", bank)])
            self.cp("act", xgT[:, c, 0:Wn], ps[:, 0:Wn], R=[("ps", bank)], W=[("xgT",)])

        def scatter_blocks(e):
            out = []
            for grp in range(4):
                for dc in range(8):
                    def blk(grp=grp, dc=dc):
                        bank = 2 + (dc % 2)
                        ps = self.psb[bank]
                        for ch in range(2):
                            self.mm(ps[:, :], y[:, ch, dc * 128:(dc + 1) * 128], ST[:, ch, grp * 512:(grp + 1) * 512],
                                    start=(ch == 0), stop=(ch == 1), R=[("y",), ("ST", grp)], W=[("ps", bank)])
                        xs = self.xT[:, dc, grp * 512:(grp + 1) * 512]
                        self.stt(xs, ps[:, :], self.mod(l, b, 5, dc), xs, ALU.mult, ALU.add,
                                 R=[("ps", bank), ("modv",), ("xT", grp, dc)], W=[("xT", grp, dc)])
                    out.append(blk)
            if need_ctx:
                for dc in range(8):
                    def blk(dc=dc):
                        bank = 2 + (dc % 2)
                        ps = self.psb[bank]
                        self.mm(ps[:, 0:256], y[0:32, 2, dc * 128:(dc + 1) * 128], STc[:, :], R=[("y",), ("STc",)], W=[("ps", bank)])
                        xs = self.xT[:, dc, T:TT]
                        self.stt(xs, ps[:, 0:256], self.mod(l, 2, 5, dc), xs, ALU.mult, ALU.add,
                                 R=[("ps", bank), ("modv",), ("xT", 4, dc)], W=[("xT", 4, dc)])
                    out.append(blk)
            return out

        def build_ST(e):
            for grp in range(4):
                bank = 2 + (grp % 2)
                ps = self.psb[bank]
                self.mm(ps[:, :], self.sel[:, e, :], vb16[:, grp * 512:(grp + 1) * 512], R=[("sel",), ("vb16",)], W=[("ps", bank)])
                for ch in range(2):
                    self.ts("dve", ST[:, ch, grp * 512:(grp + 1) * 512], ps[:, :], self.misc[:, ch:ch + 1], ALU.is_equal,
                            R=[("ps", bank), ("cst",)], W=[("ST", grp)])
            if need_ctx:
                ps = self.psb[2]
                self.mm(ps[0:32, 0:256], self.sel[:, e, 0:32], vb16[:, T:TT], R=[("sel",), ("vb16",)], W=[("ps", 2)])
                self.ts("dve", STc[:, :], ps[0:32, 0:256], self.misc[0:32, 0:1], ALU.is_equal, R=[("ps", 2), ("cst",)], W=[("STc",)])

        def gate_up(e, wg0, wu0, wg1, wu1, inject):
            for fc in range(8):
                if fc in inject:
                    inject[fc]()
                gbuf, gres = (wg0 if fc < 4 else wg1)
                ubuf, ures = (wu0 if fc < 4 else wu1)
                fo = (fc % 4) * 128
                bg = 4 + 2 * (fc % 2)
                bu = bg + 1
                for c in range(8):
                    self.mm(self.psb[bg][:, 0:Wn], gbuf[:, c, fo:fo + 128], xgT[:, c, 0:Wn], start=(c == 0), stop=(c == 7),
                            R=[gres, ("xgT",)], W=[("ps", bg)])
                for c in range(8):
                    self.mm(self.psb[bu][:, 0:Wn], ubuf[:, c, fo:fo + 128], xgT[:, c, 0:Wn], start=(c == 0), stop=(c == 7),
                            R=[ures, ("xgT",)], W=[("ps", bu)])
                self.act(sg[:, fc % 2, 0:Wn], self.psb[bg][:, 0:Wn], AF.Silu, R=[("ps", bg)], W=[("sg", fc % 2)])
                self.tt("dve", hidT[:, fc, 0:Wn], sg[:, fc % 2, 0:Wn], self.psb[bu][:, 0:Wn], ALU.mult,
                        R=[("sg", fc % 2), ("ps", bu)], W=[("hidT",)])

        def down(e, wd0, wd1):
            for half, (dbuf, dres) in enumerate((wd0, wd1)):
                for ci, (c0, m) in enumerate(chunks):
                    bank = (half * 3 + ci) % 2
                    ps = self.psb[bank]
                    for fcn in range(8):
                        self.mm(ps[0:m, :], hidT[:, fcn, c0:c0 + m], dbuf[:, fcn, :], start=(fcn == 0), stop=(fcn == 7),
                                R=[("hidT",), dres], W=[("ps", bank)])
                    self.ts("dve", y[0:m, ci, half * 512:(half + 1) * 512], ps[0:m, :], gate2[0:m, ci:ci + 1], ALU.mult,
                            R=[("ps", bank), ("gate2",)], W=[("y",)])

        gate2 = self.carve("gate2", ROFF + 44160 + 480, [128, 4], F32)
        issue(4)
        build_S(0)
        slot_gates(0)
        for c in range(8):
            gather_c(0, c)
        for e in range(NE):
            issue(e * 6 + 4)
            wg0, wu0, wg1, wu1 = next_units(4)
            self.cp("dve", gate2[:, 0:nch], gate[:, 0:nch], R=[("gate",)], W=[("gate2",)])
            inj = {1: (lambda e=e: build_ST(e))}
            if e + 1 < NE:
                inj[3] = (lambda e=e: build_S(e + 1))
                inj[5] = (lambda e=e: slot_gates(e + 1))
            gate_up(e, wg0, wu0, wg1, wu1, inj)
            issue(e * 6 + 6)
            wd0, wd1 = next_units(2)
            down(e, wd0, wd1)
            issue(e * 6 + 10)
            blocks = scatter_blocks(e)
            if e + 1 < NE:
                nb = len(blocks)
                per = (nb + 7) // 8
                bi = 0
                for c in range(8):
                    gather_c(e + 1, c)
                    for _ in range(per):
                        if bi < nb:
                            blocks[bi]()
                            bi += 1
                while bi < nb:
                    blocks[bi]()
                    bi += 1
            else:
                for blk in blocks:
                    blk()
        self.P.barrier()

    def moe_prefetch(self, l):
        self.unit_ctr = 0
        srcs = self.moe_units(l, 0)
        return [self.ring_load(srcs[i]) for i in range(2)]

    def build(self):
        cfg = self.cfg
        self.prologue()
        for b in range(cfg.get("nsamples", SPC)):
            self.load_sample(b)
            for l in cfg.get("layers", [0, 1, 2, 3]):
                need_ctx = l < 3
                pre = self.moe_prefetch(l) if cfg.get("moe", True) else []
                kind = l % 3
                if cfg.get("attn", True):
                    if kind == 0:
                        self.gqa_layer(l, b, need_ctx)
                    elif kind == 1:
                        self.mla_layer(l, b, need_ctx)
                    else:
                        self.na_layer(l, b, need_ctx)
                if cfg.get("dump_xa") == (b, l):
                    self.store_sample(self.dbg_out("xa", [TT, D]), NT)
                if cfg.get("moe", True):
                    self.moe_layer(l, b, need_ctx, pre)
                if cfg.get("dump_x") == (b, l):
                    self.store_sample(self.dbg_out("x", [TT, D]), NT)
            self.store_sample(self.y2[b], 16)
        self.P.emit(self.es)
        return self.nc


def host_inputs(inp):
    cst = np.zeros((128, 388), np.float32)
    cst[:, 0:128] = np.eye(128, dtype=np.float32)
    cst[:, 128:384] = np.arange(256, dtype=np.float32)[None, :]
    cst[:, 384] = np.arange(128, dtype=np.float32)
    cst[:, 385] = np.arange(128, dtype=np.float32) + 128.0
    cst[:, 386] = EPS
    selc = np.zeros((16, 16, 128), np.float32)
    for e in range(16):
        selc[e, e, :] = 1.0
    selc = selc.reshape(16, 2048).astype(ml_dtypes.bfloat16)
    cosA, sinA = rope_tables(64)
    cosB, sinB = rope_tables(32)
    na_bias, na_mask = na_tables(np.asarray(inp["c_rpb"], np.float32)[0])
    shared = {k: np.ascontiguousarray(np.asarray(inp[k], np.float32)) for k in (
        "ada_w", "ada_b", "norm1_w", "norm2_w", "a_wqkv", "a_qnorm", "a_knorm", "a_wo",
        "b_wdq", "b_qnorm_lat", "b_wuq", "b_wdkv", "b_kvnorm_lat", "b_wukv", "b_qnorm", "b_knorm", "b_wo",
        "c_wqkv", "c_qnorm", "c_knorm", "c_wo", "moe_router", "moe_wg", "moe_wu", "moe_wd")}
    shared.update(cst=cst, selc=selc, cosA=cosA, sinA=sinA, cosB=cosB, sinB=sinB, na_bias=na_bias, na_mask=na_mask)
    x = np.asarray(inp["x"], np.float32)
    ctx = np.asarray(inp["ctx"], np.float32)
    c = np.asarray(inp["c"], np.float32)
    c_ctx = np.asarray(inp["c_ctx"], np.float32)
    maps = []
    for core in range(N_CORES):
        m = dict(shared)
        m["x2"] = np.ascontiguousarray(x[SPC * core:SPC * core + SPC])
        m["ctx2"] = np.ascontiguousarray(ctx[SPC * core:SPC * core + SPC])
        m["cvec"] = np.ascontiguousarray(np.concatenate([c[SPC * core:SPC * core + SPC], c_ctx[None, :]], axis=0))
        maps.append(m)
    return maps


def kernel(**inp):
    maps = host_inputs(inp)
    nc = Builder({}).build()
    res = run_bass_kernel_spmd(nc, maps, core_ids=list(range(N_CORES)))
    out = np.concatenate([np.asarray(r["y2"], np.float32) for r in res.results], axis=0)
    return out
```

```python
import numpy as np
import ml_dtypes
from contextlib import ExitStack
import concourse.bass as bass
import concourse.mybir as mybir
from concourse.bass_utils import run_bass_kernel_spmd

F32 = mybir.dt.float32
BF16 = mybir.dt.bfloat16
AF = mybir.ActivationFunctionType
ALU = mybir.AluOpType
AX = mybir.AxisListType

D = 1024
T = 2048
TC = 256
TT = 2304
NT = 18
NE = 16
CAP = 256
CAPC = 32
EPS = 1e-6
NEG = -30000.0
N_CORES = 8
SPC = 2

XT_OFF = 0
RING_OFF = 73728
UNIT = 8192
NUNIT = 5
HT_OFF = RING_OFF + UNIT * NUNIT
R_OFF = HT_OFF + 36864
R_SIZE = 45056
PERS_OFF = R_OFF + R_SIZE
ARENA_BYTES = 212000
R2_OFF = RING_OFF + 2 * UNIT
R2_SIZE = 3 * UNIT


def _dsize(dt):
    return 4 if dt == F32 else 2


class Prog:
    ENG = ("pe", "act", "dve", "pool", "sp")

    def __init__(self, nc):
        self.nc = nc
        self.eng = {"pe": nc.tensor, "act": nc.scalar, "dve": nc.vector, "pool": nc.gpsimd, "sp": nc.sync}
        self.ops = []
        self.eops = {e: [] for e in self.ENG}
        self.res = {}
        self.slots = {}
        self.pending = {e: set() for e in self.ENG}

    def _deps(self, eng, reads, writes):
        deps = set(self.pending[eng])
        self.pending[eng] = set()
        for r in reads:
            st = self.res.get(r)
            if st is not None and st["w"] is not None:
                deps.add(st["w"])
        for w in writes:
            st = self.res.get(w)
            if st is not None:
                if st["w"] is not None:
                    deps.add(st["w"])
                deps.update(st["r"].values())
        return deps

    def _mark(self, tok, key, reads, writes):
        for r in reads:
            st = self.res.get(r)
            if st is None:
                st = self.res[r] = {"w": None, "r": {}}
            st["r"][key] = tok
        for w in writes:
            self.res[w] = {"w": tok, "r": {}}

    def op(self, eng, fn, reads=(), writes=()):
        deps = self._deps(eng, reads, writes)
        idx = len(self.eops[eng])
        rec = {"eng": eng, "kind": "op", "fn": fn, "deps": deps, "idx": idx, "sig": False}
        self.eops[eng].append(rec)
        self.ops.append(rec)
        self._mark(("e", eng, idx), eng, reads, writes)

    def dma(self, queue, slot, out, in_, reads=(), writes=(), group=False, bar=True, **kw):
        deps = self._deps(queue, reads, writes)
        s = self.slots.get(slot)
        if s is None:
            s = self.slots[slot] = {"n": 0, "groups": [], "bar": bar}
        s["n"] += 1
        n = s["n"]
        if group and s["groups"]:
            s["groups"][-1] = n
        else:
            if n > 1:
                deps.add(("d", slot, n - 1))
            s["groups"].append(n)
        idx = len(self.eops[queue])
        rec = {"eng": queue, "kind": "dma", "slot": slot, "n": n, "out": out, "in_": in_, "deps": deps,
               "idx": idx, "kw": kw, "sig": False}
        self.eops[queue].append(rec)
        self.ops.append(rec)
        self._mark(("d", slot, n), ("d", slot), reads, writes)

    def barrier(self):
        toks = set()
        for e in ("pe", "act", "dve", "pool"):
            if self.eops[e]:
                for rec in reversed(self.eops[e]):
                    if rec["kind"] == "op":
                        toks.add(("e", e, rec["idx"]))
                        break
        for name, s in self.slots.items():
            if s["bar"] and s["n"] > 0:
                toks.add(("d", name, s["n"]))
        for e in ("pe", "act", "dve", "pool", "sp"):
            self.pending[e] |= {t for t in toks if not (t[0] == "e" and t[1] == e == "pe")}

    def _gend(self, slot, n):
        for g in self.slots[slot]["groups"]:
            if g >= n:
                return g
        raise AssertionError

    def emit(self, es):
        nc = self.nc
        for rec in self.ops:
            for d in rec["deps"]:
                if d[0] == "e":
                    if d[1] == rec["eng"] == "pe":
                        continue
                    self.eops[d[1]][d[2]]["sig"] = True
        for e in self.ENG:
            c = 0
            for rec in self.eops[e]:
                if rec["sig"]:
                    c += 1
                rec["sigval"] = c
        sem = {}
        for e in self.ENG:
            sem[("e", e)] = es.enter_context(nc.semaphore("g_" + e))
        for name in self.slots:
            sem[("d", name)] = es.enter_context(nc.semaphore("d_" + name))
        waited = {e: {} for e in self.ENG}
        nwait = 0
        for rec in self.ops:
            e = rec["eng"]
            engine = self.eng[e]
            need = {}
            for d in rec["deps"]:
                if d[0] == "e":
                    if d[1] == e == "pe":
                        continue
                    key = ("e", d[1])
                    val = self.eops[d[1]][d[2]]["sigval"]
                else:
                    if rec["kind"] == "dma" and d[1] == rec["slot"] and self._gend(d[1], d[2]) == self._gend(rec["slot"], rec["n"]):
                        continue
                    key = ("d", d[1])
                    val = 16 * self._gend(d[1], d[2])
                if need.get(key, 0) < val:
                    need[key] = val
            for key, val in need.items():
                if waited[e].get(key, 0) < val:
                    engine.wait_ge(sem[key], val)
                    waited[e][key] = val
                    nwait += 1
            if rec["kind"] == "op":
                inst = rec["fn"](engine)
                if rec["sig"]:
                    inst.then_inc(sem[("e", e)], 1)
            else:
                inst = engine.dma_start(out=rec["out"], in_=rec["in_"], **rec["kw"])
                inst.then_inc(sem[("d", rec["slot"])], 16)
        sp = self.eng["sp"]
        for name, s in self.slots.items():
            if s["n"] > 0:
                sp.wait_ge(sem[("d", name)], 16 * s["n"])
        self.stats = {e: len(self.eops[e]) for e in self.ENG}
        self.stats["waits"] = nwait


class Region:
    def __init__(self, off, size):
        self.off, self.size, self.cur = off, size, 0

    def reset(self):
        self.cur = 0

    def take(self, nbytes):
        nbytes = (nbytes + 31) // 32 * 32
        o = self.off + self.cur
        self.cur += nbytes
        assert self.cur <= self.size, (self.cur, self.size)
        return o


class Buf:
    def __init__(self, name, ap):
        self.name = name
        self.ap = ap

    def __getitem__(self, idx):
        return self.ap[idx]

    def r(self, *k):
        return (self.name,) + k


GROUPS = [(0, 512), (512, 512), (1024, 512), (1536, 512), (2048, 256)]


def na_tables(rpb):
    blocks = [(5, 5 + r) for r in (-2, -1, 0, 1, 2)]
    for qi in (0, 1):
        blocks += [(qi, kt) for kt in range(4)]
    for qi in (14, 15):
        blocks += [(qi, kt) for kt in range(12, 16)]
    nb = len(blocks)
    H = rpb.shape[0]
    bias = np.zeros((H, 128, nb, 128), np.float32)
    mask = np.zeros((128, nb, 128), np.float32)
    kk = np.arange(128)
    for bi, (qi, kt) in enumerate(blocks):
        k_r = 2 * kt + kk // 64
        k_c = kk % 64
        q_r = 2 * qi + kk // 64
        q_c = kk % 64
        rs = np.clip(q_r - 4, 0, 24)
        cs = np.clip(q_c - 8, 0, 48)
        ok = ((k_r[:, None] >= rs[None, :]) & (k_r[:, None] < rs[None, :] + 8)
              & (k_c[:, None] >= cs[None, :]) & (k_c[:, None] < cs[None, :] + 16))
        dr = np.clip(k_r[:, None] - q_r[None, :] + 7, 0, 14)
        dc = np.clip(k_c[:, None] - q_c[None, :] + 15, 0, 30)
        bias[:, :, bi, :] = rpb[:, dr, dc]
        mask[:, bi, :] = np.where(ok, 0.0, NEG)
    return bias.reshape(H, 128, nb * 128), mask.reshape(128, nb * 128)


def na_block_ids(qi):
    if 2 <= qi <= 13:
        return [(qi + r, 2 + r) for r in (-2, -1, 0, 1, 2)]
    if qi in (0, 1):
        return [(kt, 5 + 4 * qi + kt) for kt in range(4)]
    base = 13 if qi == 14 else 17
    return [(kt, base + (kt - 12)) for kt in range(12, 16)]


def rope_tables(rot_dim):
    n_freq = rot_dim // 4
    inv = np.float32(10000.0) ** (-np.arange(n_freq, dtype=np.float32) / np.float32(n_freq))
    t = np.arange(T, dtype=np.int32)
    row = (t // 64).astype(np.float32)
    col = (t % 64).astype(np.float32)
    ang = np.concatenate([row[:, None] * inv, col[:, None] * inv], axis=-1).astype(np.float32)
    return np.cos(ang).astype(np.float32), np.sin(ang).astype(np.float32)


class Builder:
    def __init__(self, cfg):
        self.cfg = cfg
        nc = self.nc = bass.Bass("TRN2", target_bir_lowering=False)
        self.es = ExitStack()
        self.P = Prog(nc)
        self.dbg_outs = []
        self._dram()
        self.arena = self.es.enter_context(nc.sbuf_tensor("arena", [128, ARENA_BYTES // 4], F32))
        self.psall = self.es.enter_context(nc.psum_tensor("psall", [128, 4096], F32))
        self.psb = [self.psall[:, i * 512:(i + 1) * 512] for i in range(8)]
        self.R = Region(R_OFF, R_SIZE)
        self.R2 = Region(R2_OFF, R2_SIZE)
        self.HTR = Region(HT_OFF, 36864)
        self.PERS = Region(PERS_OFF, ARENA_BYTES - PERS_OFF)
        self.unit_ctr = 0
        self.ot_ctr = 0
        self.op_ctr = 0
        self._persistent()

    def _dram(self):
        nc = self.nc

        def inp(name, shape, dt=F32):
            return nc.dram_tensor(name, list(shape), dt, kind="ExternalInput").ap()
        self.d = d = {}
        d["x2"] = inp("x2", [SPC, T, D])
        d["ctx2"] = inp("ctx2", [SPC, TC, D])
        d["cvec"] = inp("cvec", [3, D])
        d["ada_w"] = inp("ada_w", [4, D, 6 * D])
        d["ada_b"] = inp("ada_b", [4, 6 * D])
        d["norm1_w"] = inp("norm1_w", [4, D])
        d["norm2_w"] = inp("norm2_w", [4, D])
        d["a_wqkv"] = inp("a_wqkv", [2, D, 1536])
        d["a_qnorm"] = inp("a_qnorm", [2, 64])
        d["a_knorm"] = inp("a_knorm", [2, 64])
        d["a_wo"] = inp("a_wo", [2, D, D])
        d["b_wdq"] = inp("b_wdq", [1, D, 384])
        d["b_qnorm_lat"] = inp("b_qnorm_lat", [1, 384])
        d["b_wuq"] = inp("b_wuq", [1, 384, 1536])
        d["b_wdkv"] = inp("b_wdkv", [1, D, 288])
        d["b_kvnorm_lat"] = inp("b_kvnorm_lat", [1, 256])
        d["b_wukv"] = inp("b_wukv", [1, 256, 2048])
        d["b_qnorm"] = inp("b_qnorm", [1, 96])
        d["b_knorm"] = inp("b_knorm", [1, 96])
        d["b_wo"] = inp("b_wo", [1, D, D])
        d["c_wqkv"] = inp("c_wqkv", [1, D, 3072])
        d["c_qnorm"] = inp("c_qnorm", [1, 64])
        d["c_knorm"] = inp("c_knorm", [1, 64])
        d["c_wo"] = inp("c_wo", [1, D, D])
        d["moe_router"] = inp("moe_router", [4, D, NE])
        d["moe_wg"] = inp("moe_wg", [4, NE, D, D])
        d["moe_wu"] = inp("moe_wu", [4, NE, D, D])
        d["moe_wd"] = inp("moe_wd", [4, NE, D, D])
        d["cst"] = inp("cst", [128, 388])
        d["selc"] = inp("selc", [16, 2048], BF16)
        d["cosA"] = inp("cosA", [T, 32])
        d["sinA"] = inp("sinA", [T, 32])
        d["cosB"] = inp("cosB", [T, 16])
        d["sinB"] = inp("sinB", [T, 16])
        d["na_bias"] = inp("na_bias", [16, 128, 21 * 128])
        d["na_mask"] = inp("na_mask", [128, 21 * 128])
        self.y2 = nc.dram_tensor("y2", [SPC, T, D], F32, kind="ExternalOutput").ap()

    def dbg_out(self, name, shape, dt=F32):
        ap = self.nc.dram_tensor("dbg_" + name, list(shape), dt, kind="ExternalOutput").ap()
        self.dbg_outs.append("dbg_" + name)
        return ap

    def carve(self, name, off, shape, dt):
        n = int(np.prod(shape[1:]))
        nbytes = n * _dsize(dt)
        assert off % 4 == 0 and nbytes % 4 == 0, (name, off, nbytes)
        assert off + nbytes <= ARENA_BYTES, (name, off, nbytes)
        ap = self.arena[0:shape[0], off // 4:(off + nbytes) // 4]
        if dt != F32:
            ap = ap.bitcast(dt)
        if len(shape) == 3:
            ap = ap.rearrange("p (a b) -> p a b", b=shape[2])
        elif len(shape) == 4:
            ap = ap.rearrange("p (a b c) -> p a b c", b=shape[2], c=shape[3])
        elif len(shape) == 5:
            ap = ap.rearrange("p (a b c d) -> p a b c d", b=shape[2], c=shape[3], d=shape[4])
        return Buf(name, ap)

    def alloc(self, region, name, shape, dt):
        n = int(np.prod(shape[1:])) * _dsize(dt)
        return self.carve(name, region.take(n), shape, dt)

    def psbf(self, i):
        return self.psall[:, i * 512:(i + 1) * 512].bitcast(BF16)

    def mm(self, out, lhsT, rhs, start=True, stop=True, R=(), W=()):
        self.P.op("pe", lambda e: e.matmul(out, lhsT, rhs, start=start, stop=stop), R, W)

    def tr(self, out, in_, ident, R=(), W=()):
        self.P.op("pe", lambda e: e.transpose(out, in_, ident), R, W)

    def act(self, out, in_, func, bias=None, scale=None, accum=None, R=(), W=()):
        kw = {}
        if bias is not None:
            kw["bias"] = bias
        if scale is not None:
            kw["scale"] = scale
        if accum is not None:
            kw["accum_out"] = accum
        self.P.op("act", lambda e: e.activation(out=out, in_=in_, func=func, **kw), R, W)

    def tt(self, eng, out, in0, in1, op, R=(), W=()):
        self.P.op(eng, lambda e: e.tensor_tensor(out=out, in0=in0, in1=in1, op=op), R, W)

    def ts(self, eng, out, in0, s1, op0, s2=None, op1=None, R=(), W=()):
        if op1 is None:
            self.P.op(eng, lambda e: e.tensor_scalar(out=out, in0=in0, scalar1=s1, scalar2=None, op0=op0), R, W)
        else:
            self.P.op(eng, lambda e: e.tensor_scalar(out=out, in0=in0, scalar1=s1, scalar2=s2, op0=op0, op1=op1), R, W)

    def stt(self, out, in0, scalar, in1, op0, op1, R=(), W=()):
        self.P.op("dve", lambda e: e.scalar_tensor_tensor(out=out, in0=in0, scalar=scalar, in1=in1, op0=op0, op1=op1), R, W)

    def cp(self, eng, out, in_, R=(), W=()):
        if eng == "act":
            self.P.op("act", lambda e: e.activation(out=out, in_=in_, func=AF.Copy), R, W)
        else:
            self.P.op(eng, lambda e: e.tensor_copy(out=out, in_=in_), R, W)

    def red(self, out, in_, op, R=(), W=()):
        self.P.op("dve", lambda e: e.tensor_reduce(out=out, in_=in_, axis=AX.X, op=op), R, W)

    def memset(self, eng, ap, val, R=(), W=()):
        self.P.op(eng, lambda e: e.memset(ap, val), R, W)

    def dma(self, queue, slot, out, in_, R=(), W=(), group=False, bar=True, **kw):
        self.P.dma(queue, slot, out, in_, R, W, group=group, bar=bar, **kw)

    def _persistent(self):
        A = self.alloc
        PR = self.PERS
        self.xT = self.carve("xT", XT_OFF, [128, 8, TT], F32)
        self.ring = [self.carve("ring%d" % i, RING_OFF + i * UNIT, [128, 8, 512], BF16) for i in range(NUNIT)]
        self.cstb = A(PR, "cstb", [128, 388], F32)
        self.identf = self.cstb[:, 0:128]
        self.iotac = self.cstb[:, 128:384]
        self.misc = self.cstb[:, 384:388]
        self.identb = A(PR, "identb", [128, 128], BF16)
        self.onesb = A(PR, "onesb", [128, 128], BF16)
        self.sel = A(PR, "sel", [16, 16, 128], BF16)
        self.modv = A(PR, "modv", [128, 4, 3, 6, 8], F32)
        self.cosA = A(PR, "cosA", [128, 16, 32], F32)
        self.sinA = A(PR, "sinA", [128, 16, 32], F32)
        self.cosB = A(PR, "cosB", [128, 16, 16], F32)
        self.sinB = A(PR, "sinB", [128, 16, 16], F32)
        self.smallp = A(PR, "smallp", [128, 64], F32)

    def prologue(self):
        d = self.d
        self.dma("sp", "c0", self.cstb[:, :], d["cst"][:, :], W=[("cst",)])
        self.dma("sp", "c0", self.sel[:, :, :], d["selc"].rearrange("k (e m) -> k e m", m=128), W=[("sel",)], group=True)
        for nm, buf in (("cosA", self.cosA), ("sinA", self.sinA), ("cosB", self.cosB), ("sinB", self.sinB)):
            self.dma("sp", "c0", buf[:, :, :], d[nm].rearrange("(t p) n -> p t n", p=128), W=[(nm,)], group=True)
        self.cp("dve", self.identb[:, :], self.identf, R=[("cst",)], W=[("identb",)])
        self.memset("pool", self.onesb[:, :], 1.0, W=[("onesb",)])
        R = self.R
        R.reset()
        vr = self.alloc(R, "vr", [128, 128], F32)
        vr2 = self.alloc(R, "vr2", [128, 128], F32)
        vr3 = self.alloc(R, "vr3", [128, 128], F32)
        vT = self.alloc(R, "vT", [128, 88], F32)
        abT = self.alloc(R, "abT", [128, 192], F32)
        sT = self.alloc(R, "sT", [128, 8, 4], F32)
        modraw = self.alloc(R, "modraw", [128, 4, 48, 3], F32)
        wslot = [self.carve("adaw0", HT_OFF, [128, 8, 768], F32),
                 self.alloc(R, "adaw1", [128, 8, 768], F32)]
        self.dma("sp", "p0", vr[0:24, :], d["cvec"].rearrange("s (c p) -> (s c) p", p=128), W=[("vr",)])
        self.dma("sp", "p0", vr[24:56, :], d["norm1_w"].rearrange("l (c p) -> (l c) p", p=128), W=[("vr",)], group=True)
        self.dma("sp", "p0", vr[56:88, :], d["norm2_w"].rearrange("l (c p) -> (l c) p", p=128), W=[("vr",)], group=True)
        abrows = d["ada_b"].rearrange("l (k p) -> (l k) p", p=128)
        self.dma("sp", "p0", vr2[:, :], abrows[0:128, :], W=[("vr2",)], group=True)
        self.dma("sp", "p0", vr3[0:64, :], abrows[128:192, :], W=[("vr3",)], group=True)
        ps = self.psb[0]
        self.tr(ps[:, 0:88], vr[0:88, :], self.identf[0:88, 0:88], R=[("vr",), ("cst",)], W=[("ps", 0)])
        self.cp("dve", vT[:, :], ps[:, 0:88], R=[("ps", 0)], W=[("vT",)])
        ps1 = self.psb[1]
        self.tr(ps1[:, 0:128], vr2[:, :], self.identf, R=[("vr2",), ("cst",)], W=[("ps", 1)])
        self.tr(ps1[:, 128:192], vr3[0:64, :], self.identf[0:64, 0:64], R=[("vr3",), ("cst",)], W=[("ps", 1)])
        self.cp("dve", abT[:, :], ps1[:, 0:192], R=[("ps", 1)], W=[("abT",)])
        self.memset("dve", sT[:, :, :], 0.0, W=[("sT",)])
        cTv = vT[:, 0:24].rearrange("p (s c) -> p c s", c=8)
        self.act(sT[:, :, 0:3], cTv, AF.Silu, R=[("vT",)], W=[("sT",)])
        n1 = vT[:, 24:56].rearrange("p (l c) -> p l c", c=8)
        n2 = vT[:, 56:88].rearrange("p (l c) -> p l c", c=8)
        for l in range(4):
            mps = self.psb[2 + (l % 2)]
            for cb in range(8):
                u = l * 8 + cb
                w = wslot[u % 2]
                self.dma("sp", "aw%d" % (u % 2), w[:, :, :],
                         d["ada_w"][l].rearrange("(c p) f -> p c f", p=128)[:, :, cb * 768:(cb + 1) * 768],
                         W=[("adaw", u % 2)])
                for fc in range(6):
                    col = (cb * 6 + fc) * 4
                    for dc in range(8):
                        self.mm(mps[:, col:col + 4], w[:, dc, fc * 128:(fc + 1) * 128], sT[:, dc, :],
                                start=(dc == 0), stop=(dc == 7),
                                R=[("adaw", u % 2), ("sT",)], W=[("ps", 2 + (l % 2))])
            mv = mps[:, 0:192].rearrange("p (k s) -> p k s", s=4)[:, :, 0:3]
            bv = abT[:, l * 48:(l + 1) * 48].unsqueeze(2).to_broadcast([128, 48, 3])
            self.tt("dve", modraw[:, l, :, :], mv, bv, ALU.add, R=[("ps", 2 + (l % 2)), ("abT",)], W=[("modraw", l)])
            mr = modraw[:, l, :, :].rearrange("p (k c) s -> p k s c", c=8)
            for kind, k in ((1, 0), (2, 2), (4, 3), (5, 5)):
                self.cp("dve", self.modv[:, l, :, kind, :], mr[:, k, :, :], R=[("modraw", l)], W=[("modv",)])
            for kind, k, nn in ((0, 1, n1), (3, 4, n2)):
                nb = nn[:, l, :].unsqueeze(1).to_broadcast([128, 3, 8])
                self.stt(self.modv[:, l, :, kind, :], mr[:, k, :, :], 1.0, nb, ALU.add, ALU.mult,
                         R=[("modraw", l), ("vT",)], W=[("modv",)])
        self.P.barrier()

    def mod(self, l, s, kind, c=None):
        if c is None:
            return self.modv[:, l, s, kind, :]
        return self.modv[:, l, s, kind, c:c + 1]

    def load_sample(self, b):
        R = self.R
        R.reset()
        stg = [self.alloc(R, "stg%d" % i, [128, D], F32) for i in range(2)]
        for t in range(NT):
            src = self.d["x2"][b, t * 128:(t + 1) * 128, :] if t < 16 else self.d["ctx2"][b, (t - 16) * 128:(t - 15) * 128, :]
            s = stg[t % 2]
            self.dma("sp", "ld%d" % (t % 2), s[:, :], src, W=[("stg", t % 2)])
            for half in range(2):
                bank = (t % 2) * 2 + half
                ps = self.psb[bank]
                for j in range(4):
                    c = half * 4 + j
                    self.tr(ps[:, j * 128:(j + 1) * 128], s[:, c * 128:(c + 1) * 128], self.identf,
                            R=[("stg", t % 2), ("cst",)], W=[("ps", bank)])
                self.cp("act" if half == 0 else "dve", self.xT[:, half * 4:(half + 1) * 4, t * 128:(t + 1) * 128],
                        ps[:, :].rearrange("p (j n) -> p j n", n=128),
                        R=[("ps", bank)], W=[("xT", min(t // 4, 4), half * 4 + j) for j in range(4)])
        self.P.barrier()

    def store_sample(self, dst, ntiles):
        R = self.R
        R.reset()
        stg = [self.alloc(R, "ostg%d" % i, [128, D], F32) for i in range(2)]
        for t in range(ntiles):
            s = stg[t % 2]
            for half in range(2):
                bank = (t % 2) * 2 + half
                ps = self.psb[bank]
                for j in range(4):
                    c = half * 4 + j
                    self.tr(ps[:, j * 128:(j + 1) * 128], self.xT[:, c, t * 128:(t + 1) * 128], self.identf,
                            R=[("xT", min(t // 4, 4), c), ("cst",)], W=[("ps", bank)])
                self.cp("act" if half == 0 else "dve", s[:, half * 512:(half + 1) * 512], ps[:, :],
                        R=[("ps", bank)], W=[("ostg", t % 2)])
            self.dma("sp", "st%d" % (t % 2), dst[t * 128:(t + 1) * 128, :], s[:, :], R=[("ostg", t % 2)])
        self.P.barrier()

    def norm_bufs(self):
        R = self.R
        self.sq = [self.alloc(R, "sq%d" % i, [128, 8, 512], BF16) for i in range(2)]
        self.lnv = self.alloc(R, "lnv", [128, 512], F32)
        self.rstd = [self.alloc(R, "rstd%d" % i, [128, 512], F32) for i in range(2)]
        self.tmpn = [self.alloc(R, "tmpn%d" % i, [128, 512], F32) for i in range(2)]

    def norm_group(self, gi, l, b, which, dst_of):
        t0, w = GROUPS[gi]
        s = b if gi < 4 else 2
        ka, kb = (0, 1) if which == 1 else (3, 4)
        sq = self.sq[gi % 2]
        self.act(sq[:, :, 0:w], self.xT[:, :, t0:t0 + w], AF.Square, R=[("xT", gi, c) for c in range(8)], W=[("sq", gi % 2)])
        bank = 5 + (gi % 2)
        ps = self.psb[bank]
        for c in range(8):
            self.mm(ps[:, 0:w], self.onesb[:, :], sq[:, c, 0:w], start=(c == 0), stop=(c == 7),
                    R=[("sq", gi % 2), ("onesb",)], W=[("ps", bank)])
        self.act(self.lnv[:, 0:w], ps[:, 0:w], AF.Ln, bias=self.misc[:, 2:3], scale=1.0 / D,
                 R=[("ps", bank), ("cst",)], W=[("lnv",)])
        rstd = self.rstd[gi % 2]
        self.act(rstd[:, 0:w], self.lnv[:, 0:w], AF.Exp, scale=-0.5, R=[("lnv",)], W=[("rstd", gi % 2)])
        for c in range(8):
            tm = self.tmpn[c % 2]
            self.tt("dve", tm[:, 0:w], self.xT[:, c, t0:t0 + w], rstd[:, 0:w], ALU.mult,
                    R=[("xT", gi, c), ("rstd", gi % 2)], W=[("tmpn", c % 2)])
            dst, dres = dst_of(c)
            self.act(dst, tm[:, 0:w], AF.Identity, bias=self.mod(l, s, kb, c), scale=self.mod(l, s, ka, c),
                     R=[("tmpn", c % 2), ("modv",)], W=[dres])

    def attn_core(self, steps_cfg, scale):
        steps = []
        for hi, hc in enumerate(steps_cfg):
            kts = hc["ktiles"]
            npair = (len(kts) + 1) // 2
            for pi in range(npair):
                ks = kts[2 * pi:2 * pi + 2]
                steps.append((hi, hc, ks, pi == 0, pi == npair - 1))
        n = len(steps)

        def qk(i):
            hi, hc, ks, first, last = steps[i]
            s_ = i % 2
            for j, k in enumerate(ks):
                bank = 2 * s_ + j
                self.mm(self.psb[bank][:, 0:hc["w"]], hc["kt"][:, k * 128:(k + 1) * 128], hc["qt"][:, hc["t0"]:hc["t0"] + hc["w"]],
                        R=[hc["kres"], hc["qres"]], W=[("ps", bank)])

        def ex(i):
            hi, hc, ks, first, last = steps[i]
            s_ = i % 2
            w = hc["w"]
            nk = len(ks)
            src = self.psall[:, s_ * 1024:s_ * 1024 + nk * 512].rearrange("p (b n) -> p b n", n=512)[:, :, 0:w]
            dst = self.PT[:, s_, 0:nk * 512].rearrange("p (b n) -> p b n", n=512)[:, :, 0:w]
            self.act(dst, src, AF.Exp, scale=scale, R=[("ps", 2 * s_ + j) for j in range(nk)], W=[("pt", s_)])

        def pv(i):
            hi, hc, ks, first, last = steps[i]
            w = hc["w"]
            s_ = i % 2
            if hc["hs"] == 0:
                self.take_op(hc["gi"], 8 if last else 1)
            ob = 4 + (hi + self.ot_ctr) % 2
            for j, k in enumerate(ks):
                self.mm(self.psb[ob][:, 0:w], hc["va"](k), self.PT[:, s_, j * 512:j * 512 + w],
                        start=(first and j == 0), stop=(last and j == len(ks) - 1),
                        R=[("pt", s_), hc["vres"]], W=[("ps", ob)])
            if last:
                lo, hi_ = (slice(0, 64), slice(64, 128))
                o_sl, d_sl = (lo, hi_) if hc["o_lo"] else (hi_, lo)
                rec_o = self.rec[d_sl, 0:w]
                rec_i = self.psb[ob][d_sl, 0:w]
                self.P.op("dve", lambda e: e.reciprocal(out=rec_o, in_=rec_i), [("ps", ob)], [("rec",)])
                self.tt("dve", hc["att"][o_sl, hc["t0"]:hc["t0"] + w], self.psb[ob][o_sl, 0:w], self.rec[d_sl, 0:w], ALU.mult,
                        R=[("ps", ob), ("rec",)], W=[("attT", hc["gi"])])

        if n == 0:
            return
        qk(0)
        for i in range(n):
            if i + 1 < n:
                qk(i + 1)
            ex(i)
            pv(i)
        self.ot_ctr += len(steps_cfg)

    def out_proj(self, wo, wres, att, ares, l, b, need_ctx):
        self.flush_op()
        ng = 5 if need_ctx else 4
        pend = {}
        for gi in range(ng):
            t0, w = GROUPS[gi]
            s = b if gi < 4 else 2
            lst = []
            for dc in range(8):
                def blk(gi=gi, dc=dc, t0=t0, w=w, s=s):
                    bank = 6 + (self.op_ctr % 2)
                    self.op_ctr += 1
                    ps = self.psb[bank]
                    self.mm(ps[:, 0:w], wo[:, dc * 128:(dc + 1) * 128], att[:, t0:t0 + w], R=[wres, ("attT", gi)], W=[("ps", bank)])
                    xs = self.xT[:, dc, t0:t0 + w]
                    self.stt(xs, ps[:, 0:w], self.mod(l, s, 2, dc), xs, ALU.mult, ALU.add,
                             R=[("ps", bank), ("modv",), ("xT", gi, dc)], W=[("xT", gi, dc)])
                lst.append(blk)
            pend[gi] = lst
        self.pending_op = pend

    def flush_op(self, gi=None):
        pend = getattr(self, "pending_op", None)
        if not pend:
            return
        keys = sorted(pend.keys()) if gi is None else ([gi] if gi in pend else [])
        for k in keys:
            for blk in pend.pop(k):
                blk()

    def take_op(self, gi, n):
        pend = getattr(self, "pending_op", None)
        if not pend or gi not in pend:
            return
        lst = pend[gi]
        for _ in range(min(n, len(lst))):
            lst.pop(0)()
        if not lst:
            pend.pop(gi)

    def head_rstd(self, src3, nh, hd, sqt, ssq, lnv, rs, res_src):
        self.tt("dve", sqt, src3, src3, ALU.mult, R=[res_src], W=[("sqt",)])
        self.red(ssq[:, 0:nh], sqt, ALU.add, R=[("sqt",)], W=[("ssq",)])
        self.act(lnv[:, 0:nh], ssq[:, 0:nh], AF.Ln, bias=self.misc[:, 2:3], scale=1.0 / hd, R=[("ssq",), ("cst",)], W=[("lnvh",)])
        self.act(rs[:, 0:nh], lnv[:, 0:nh], AF.Exp, scale=-0.5, R=[("lnvh",)], W=[("rsh",)])

    def bcast_row(self, queue, slot, dst, src_row, n, W, group=False):
        self.dma(queue, slot, dst, src_row.unsqueeze(0).to_broadcast([128, n]), W=W, group=group)

    def gqa_layer(self, l, b, need_ctx):
        d = self.d
        j = l // 3
        R, R2 = self.R, self.R2
        R.reset()
        R2.reset()
        self.norm_bufs()
        hT = self.carve("hT", HT_OFF, [128, 8, TT], BF16)
        for gi in range(5):
            t0, w = GROUPS[gi]
            self.norm_group(gi, l, b, 1, lambda c, t0=t0, w=w, gi=gi: (hT[:, c, t0:t0 + w], ("hT", gi)))
        self.P.barrier()
        R.reset()
        QKT = self.alloc(R, "QKT", [128, 5, TT], BF16)
        VA = self.alloc(R, "VA", [128, NT, 192], BF16)
        attT = self.alloc(R, "attT", [128, TT], BF16)
        Wg = self.alloc(R2, "Wg", [128, 8, 384], BF16)
        WoP = self.alloc(R2, "WoP", [128, 2, D], BF16)
        self.PT = self.alloc(R, "PT", [128, 2, 1024], BF16)
        self.rec = self.alloc(R, "rec", [128, 512], F32)
        gq = self.alloc(R, "gq", [128, 5, 64], F32)
        raw = [self.alloc(R2, "raw%d" % i, [128, 384], F32) for i in range(3)]
        self.memset("pool", QKT[64:128, 0:2, :], 0.0, W=[("QKTz",)])
        self.memset("pool", QKT[0:64, 2:4, :], 0.0, W=[("QKTz",)])
        sqt = self.alloc(R2, "sqt", [128, 5, 64], F32)
        t1 = self.alloc(R2, "t1", [128, 5, 64], F32)
        t2 = self.alloc(R2, "t2", [128, 5, 64], F32)
        ra = self.alloc(R2, "ra", [128, 5, 32], F32)
        rb = self.alloc(R2, "rb", [128, 5, 32], F32)
        rc = self.alloc(R2, "rc", [128, 5, 32], F32)
        rd = self.alloc(R2, "rd", [128, 5, 32], F32)
        qkb = [self.alloc(R2, "qkb%d" % i, [128, 6, 64], BF16) for i in range(3)]
        ssq = [self.smallp[:, 0:8], self.smallp[:, 8:16]]
        lnv = [self.smallp[:, 16:24], self.smallp[:, 24:32]]
        rs = [self.smallp[:, 32:40], self.smallp[:, 40:48]]
        for i in range(4):
            self.bcast_row("sp", "gq", gq[:, i, :], d["a_qnorm"][j], 64, W=[("gq",)], group=(i > 0))
        self.bcast_row("sp", "gq", gq[:, 4, :], d["a_knorm"][j], 64, W=[("gq",)], group=True)
        self.memset("pool", VA[:, :, 0:64], 1.0, W=[("VA", t) for t in range(NT)])
        self.memset("pool", VA[:, :, 128:192], 1.0, W=[("VA", t) for t in range(NT)])
        wq = d["a_wqkv"][j].rearrange("(c p) f -> p c f", p=128)
        ptb = self.psbf(7)
        ntl = NT
        for g in range(4):
            self.dma("pool", "wg", Wg[:, :, 0:256], wq[:, :, 256 * g:256 * g + 256], W=[("Wg",)], bar=False)
            self.dma("pool", "wg", Wg[:, :, 256:320], wq[:, :, 1024 + 64 * g:1024 + 64 * g + 64], W=[("Wg",)], group=True, bar=False)
            self.dma("pool", "wg", Wg[:, :, 320:384], wq[:, :, 1280 + 64 * g:1280 + 64 * g + 64], W=[("Wg",)], group=True, bar=False)
            def S1(t):
                bank = 5 + (t % 2)
                ps = self.psb[bank]
                for c in range(8):
                    self.mm(ps[:, 0:384], hT[:, c, t * 128:(t + 1) * 128], Wg[:, c, :], start=(c == 0), stop=(c == 7),
                            R=[("hT", min(t // 4, 4)), ("Wg",)], W=[("ps", bank)])
                self.cp("act", raw[t % 3][:, :], ps[:, 0:384], R=[("ps", bank)], W=[("raw", t % 3)])

            def S2(t):
                rw = raw[t % 3]
                r3 = rw[:, 0:320].rearrange("p (h e) -> p h e", e=64)
                for h in range(5):
                    self.act(sqt[:, h, :], r3[:, h, :], AF.Square, scale=0.125, accum=ssq[t % 2][:, h:h + 1],
                             R=[("raw", t % 3)], W=[("sqt",), ("ssq", t % 2)])

            def S3(t):
                self.act(lnv[t % 2][:, 0:5], ssq[t % 2][:, 0:5], AF.Ln, bias=self.misc[:, 2:3], scale=1.0,
                         R=[("ssq", t % 2), ("cst",)], W=[("lnvh", t % 2)])
                self.act(rs[t % 2][:, 0:5], lnv[t % 2][:, 0:5], AF.Exp, scale=-0.5, R=[("lnvh", t % 2)], W=[("rsh", t % 2)])

            def S4(t):
                rw = raw[t % 3]
                r3 = rw[:, 0:320].rearrange("p (h e) -> p h e", e=64)
                self.tt("dve", t1[:, :, :], r3, rs[t % 2][:, 0:5].unsqueeze(2).to_broadcast([128, 5, 64]), ALU.mult,
                        R=[("raw", t % 3), ("rsh", t % 2)], W=[("t1",)])
                self.tt("dve", t2[:, :, :], t1[:, :, :], gq[:, :, :], ALU.mult, R=[("t1",), ("gq",)], W=[("t2",)])
                qb = qkb[t % 3]
                if t < 16:
                    x1 = t2[:, :, 0:32]
                    x2 = t2[:, :, 32:64]
                    cs = self.cosA[:, t, :].unsqueeze(1).to_broadcast([128, 5, 32])
                    sn = self.sinA[:, t, :].unsqueeze(1).to_broadcast([128, 5, 32])
                    self.tt("dve", ra[:, :, :], x1, cs, ALU.mult, R=[("t2",), ("cosA",)], W=[("ra",)])
                    self.tt("dve", rb[:, :, :], x2, sn, ALU.mult, R=[("t2",), ("sinA",)], W=[("rb",)])
                    self.tt("dve", qb[:, 0:5, 0:32], ra[:, :, :], rb[:, :, :], ALU.subtract, R=[("ra",), ("rb",)], W=[("qkb", t % 3)])
                    self.tt("dve", rc[:, :, :], x1, sn, ALU.mult, R=[("t2",), ("sinA",)], W=[("rc",)])
                    self.tt("dve", rd[:, :, :], x2, cs, ALU.mult, R=[("t2",), ("cosA",)], W=[("rd",)])
                    self.tt("dve", qb[:, 0:5, 32:64], rc[:, :, :], rd[:, :, :], ALU.add, R=[("rc",), ("rd",)], W=[("qkb", t % 3)])
                else:
                    self.cp("dve", qb[:, 0:5, :], t2[:, :, :], R=[("t2",)], W=[("qkb", t % 3)])
                self.cp("dve", qb[:, 5, :], qb[:, 4, :], R=[("qkb", t % 3)], W=[("qkb", t % 3)])
                self.cp("pool", VA[:, t, 64:128], rw[:, 320:384], R=[("raw", t % 3)], W=[("VA", t)])

            def S5(t):
                qb = qkb[t % 3]
                qf = qb[:, :, :].rearrange("p h e -> p (h e)")
                for i in range(3):
                    self.tr(ptb[:, i * 128:(i + 1) * 128], qf[:, i * 128:(i + 1) * 128], self.identb[:, :],
                            R=[("qkb", t % 3), ("identb",)], W=[("ps", 7)])
                self.cp("act", QKT[0:64, 0:2, t * 128:(t + 1) * 128], ptb[0:64, 0:256].rearrange("p (i n) -> p i n", n=128),
                        R=[("ps", 7)], W=[("QKT", t)])
                self.cp("act", QKT[64:128, 2:4, t * 128:(t + 1) * 128], ptb[64:128, 0:256].rearrange("p (i n) -> p i n", n=128),
                        R=[("ps", 7)], W=[("QKT", t)])
                self.cp("dve", QKT[:, 4, t * 128:(t + 1) * 128], ptb[:, 256:384], R=[("ps", 7)], W=[("QKT", t)])

            self.pipeline(ntl, S1, S2, S3, S4, S5)
            for p in range(2):
                slot = (g * 2 + p) % 2
                h0 = 4 * g + 2 * p
                self.dma("pool", "wo%d" % slot, WoP[:, slot, :], d["a_wo"][j][h0 * 64:h0 * 64 + 128, :], W=[("WoP", slot)], bar=False)
                cfgs = []
                qgroups = [0, 1, 2, 3] + ([4] if need_ctx else [])
                for gi in qgroups:
                    t0, w = GROUPS[gi]
                    ktiles = list(range(NT)) if gi < 4 else [16, 17]
                    for hs in range(2):
                        psl = slice(0, 64) if hs == 0 else slice(64, 128)
                        cfgs.append(dict(
                            qt=QKT[:, p + 2 * hs, :], kt=QKT[:, 4, :],
                            va=(lambda k, hs=hs: VA[:, k, 64:192] if hs == 0 else VA[:, k, 0:128]),
                            o_lo=(hs == 0), t0=t0, w=w, gi=gi, hs=hs, ktiles=ktiles, att=attT,
                            qres=("QKTall",), kres=("QKTall",), vres=("VAall",), ares=("attT",)))
                self._alias([("QKT", t) for t in range(ntl)] + [("QKTz",)], ("QKTall",))
                self._alias([("VA", t) for t in range(ntl)], ("VAall",))
                self.attn_core(cfgs, 0.125)
                self.out_proj(WoP[:, slot, :], ("WoP", slot), attT, ("attT",), l, b, need_ctx)
            self._alias_release([("QKT", t) for t in range(ntl)], ("QKTall",))
            self._alias_release([("VA", t) for t in range(ntl)], ("VAall",))
        self.flush_op()
        self.P.barrier()

    def pipeline(self, n, S1, S2, S3, S4, S5):
        for i in range(-1, n + 2):
            if 0 <= i + 1 < n:
                S1(i + 1)
            if 0 <= i < n:
                S2(i)
                S3(i)
            if 0 <= i - 1 < n:
                S4(i - 1)
            if 0 <= i - 2 < n:
                S5(i - 2)

    def _alias(self, fine, coarse):
        P = self.P
        toks = set()
        for f in fine:
            st = P.res.get(f)
            if st is not None and st["w"] is not None:
                toks.add(st["w"])
        P.res[coarse] = {"w": None, "r": {}, "ws": toks}
        for e in ("pe",):
            P.pending[e] |= toks

    def _alias_release(self, fine, coarse):
        P = self.P
        st = P.res.get(coarse)
        if st is None:
            return
        for f in fine:
            fs = P.res.get(f)
            if fs is None:
                fs = P.res[f] = {"w": None, "r": {}}
            for k, v in st["r"].items():
                fs["r"][("al", coarse, k)] = v

    def mla_layer(self, l, b, need_ctx):
        d = self.d
        R, R2, HTR = self.R, self.R2, self.HTR
        R.reset()
        R2.reset()
        HTR.reset()
        self.norm_bufs()
        hT = self.carve("hT", HT_OFF, [128, 8, TT], BF16)
        for gi in range(5):
            t0, w = GROUPS[gi]
            self.norm_group(gi, l, b, 1, lambda c, t0=t0, w=w, gi=gi: (hT[:, c, t0:t0 + w], ("hT", gi)))
        self.P.barrier()
        R.reset()
        cT = self.alloc(R, "cT", [128, 5, TT], BF16)
        krope = self.alloc(R, "krope", [128, NT, 32], BF16)
        self.PT = self.alloc(R, "PT", [128, 2, 1024], BF16)
        self.rec = self.alloc(R, "rec", [128, 512], F32)
        gq2 = self.alloc(R, "gq2", [128, 2, 96], F32)
        gk2 = self.alloc(R, "gk2", [128, 2, 64], F32)
        gk = self.alloc(R, "gkr", [128, 32], F32)
        W1 = self.alloc(R, "W1", [128, 8, 672], BF16)
        g1 = self.alloc(R, "g1", [128, 640], F32)
        raw1 = [self.alloc(R2, "rawm%d" % i, [128, 672], F32) for i in range(2)]
        cn = [self.alloc(R2, "cn%d" % i, [128, 640], BF16) for i in range(2)]
        sq1 = self.alloc(R2, "sq1", [128, 384], F32)
        kr1 = self.alloc(R2, "kr1", [128, 32], F32)
        rt = [self.alloc(R2, "rt%d" % i, [128, 2, 16], F32) for i in range(4)]
        raw2 = [self.alloc(R2, "rawn%d" % i, [128, 448], F32) for i in range(3)]
        ssq2 = [self.smallp[:, 36:44], self.smallp[:, 44:52]]
        lnv2 = [self.smallp[:, 52:58], self.smallp[:, 58:64]]
        rs2 = [self.alloc(R2, "rs2_%d" % i, [128, 8], F32) for i in range(2)]
        sqt = self.alloc(R2, "sqt", [128, 2, 64], F32)
        t1 = self.alloc(R2, "t1", [128, 2, 96], F32)
        tq = self.alloc(R2, "tq", [128, 2, 32], F32)
        k1 = self.alloc(R2, "k1", [128, 2, 64], F32)
        qa = [self.alloc(R2, "qa%d" % i, [128, 2, 96], BF16) for i in range(3)]
        ka = [self.alloc(R2, "ka%d" % i, [128, 2, 96], BF16) for i in range(3)]
        ssq = self.smallp[:, 0:8]
        lnv = self.smallp[:, 8:16]
        rs = self.smallp[:, 16:24]
        ssq1 = self.smallp[:, 24:28]
        lnv1 = self.smallp[:, 28:32]
        rs1 = self.smallp[:, 32:36]
        self.bcast_row("sp", "gq", g1[:, 0:384], d["b_qnorm_lat"][0], 384, W=[("g1",)])
        self.bcast_row("sp", "gq", g1[:, 384:640], d["b_kvnorm_lat"][0], 256, W=[("g1",)], group=True)
        self.bcast_row("sp", "gq", gk[:, :], d["b_knorm"][0, 64:96], 32, W=[("gk",)], group=True)
        for i in range(2):
            self.bcast_row("sp", "gq", gq2[:, i, :], d["b_qnorm"][0], 96, W=[("gq2",)], group=True)
            self.bcast_row("sp", "gq", gk2[:, i, :], d["b_knorm"][0, 0:64], 64, W=[("gk2",)], group=True)
        self.dma("pool", "wg", W1[:, :, 0:384], d["b_wdq"][0].rearrange("(c p) f -> p c f", p=128), W=[("W1",)], bar=False)
        self.dma("pool", "wg", W1[:, :, 384:672], d["b_wdkv"][0].rearrange("(c p) f -> p c f", p=128), W=[("W1",)], group=True, bar=False)
        ptb = self.psbf(7)
        for t in range(NT):
            ba = 3 + 2 * (t % 2)
            bb = ba + 1
            for c in range(8):
                self.mm(self.psb[ba][:, :], hT[:, c, t * 128:(t + 1) * 128], W1[:, c, 0:512], start=(c == 0), stop=(c == 7),
                        R=[("hT", min(t // 4, 4)), ("W1",)], W=[("ps", ba)])
            for c in range(8):
                self.mm(self.psb[bb][:, 0:160], hT[:, c, t * 128:(t + 1) * 128], W1[:, c, 512:672], start=(c == 0), stop=(c == 7),
                        R=[("hT", min(t // 4, 4)), ("W1",)], W=[("ps", bb)])
            rw = raw1[t % 2]
            self.cp("act", rw[:, 0:512], self.psb[ba][:, :], R=[("ps", ba)], W=[("rawm", t % 2)])
            self.cp("act", rw[:, 512:672], self.psb[bb][:, 0:160], R=[("ps", bb)], W=[("rawm", t % 2)])
            for i, (c0, n) in enumerate(((0, 384), (384, 256), (640, 32))):
                self.act(sq1[:, 0:n], rw[:, c0:c0 + n], AF.Square, accum=ssq1[:, i:i + 1], R=[("rawm", t % 2)], W=[("sq1",), ("ssq1", i)])
                self.act(lnv1[:, i:i + 1], ssq1[:, i:i + 1], AF.Ln, bias=self.misc[:, 2:3], scale=1.0 / n, R=[("ssq1", i), ("cst",)], W=[("lnv1", i)])
            self.act(rs1[:, 0:3], lnv1[:, 0:3], AF.Exp, scale=-0.5, R=[("lnv1", 0), ("lnv1", 1), ("lnv1", 2)], W=[("rs1",)])
            cnb = cn[t % 2]
            self.stt(cnb[:, 0:384], rw[:, 0:384], rs1[:, 0:1], g1[:, 0:384], ALU.mult, ALU.mult, R=[("rawm", t % 2), ("rs1",), ("g1",)], W=[("cn", t % 2)])
            self.stt(cnb[:, 384:640], rw[:, 384:640], rs1[:, 1:2], g1[:, 384:640], ALU.mult, ALU.mult, R=[("rawm", t % 2), ("rs1",), ("g1",)], W=[("cn", t % 2)])
            self.stt(kr1[:, :], rw[:, 640:672], rs1[:, 2:3], gk[:, :], ALU.mult, ALU.mult, R=[("rawm", t % 2), ("rs1",), ("gk",)], W=[("kr1",)])
            if t < 16:
                x1 = kr1[:, 0:16]
                x2 = kr1[:, 16:32]
                cs = self.cosB[:, t, :]
                sn = self.sinB[:, t, :]
                self.tt("dve", rt[0][:, 0, :], x1, cs, ALU.mult, R=[("kr1",), ("cosB",)], W=[("rt", 0)])
                self.tt("dve", rt[1][:, 0, :], x2, sn, ALU.mult, R=[("kr1",), ("sinB",)], W=[("rt", 1)])
                self.tt("dve", krope[:, t, 0:16], rt[0][:, 0, :], rt[1][:, 0, :], ALU.subtract, R=[("rt", 0), ("rt", 1)], W=[("krope", t)])
                self.tt("dve", rt[2][:, 0, :], x1, sn, ALU.mult, R=[("kr1",), ("sinB",)], W=[("rt", 2)])
                self.tt("dve", rt[3][:, 0, :], x2, cs, ALU.mult, R=[("kr1",), ("cosB",)], W=[("rt", 3)])
                self.tt("dve", krope[:, t, 16:32], rt[2][:, 0, :], rt[3][:, 0, :], ALU.add, R=[("rt", 2), ("rt", 3)], W=[("krope", t)])
            else:
                self.cp("dve", krope[:, t, :], kr1[:, :], R=[("kr1",)], W=[("krope", t)])
            for i in range(5):
                self.tr(ptb[:, i * 128:(i + 1) * 128], cnb[:, i * 128:(i + 1) * 128], self.identb[:, :],
                        R=[("cn", t % 2), ("identb",)], W=[("ps", 7)])
            self.cp("dve", cT[:, :, t * 128:(t + 1) * 128], ptb[:, 0:640].rearrange("p (i n) -> p i n", n=128),
                    R=[("ps", 7)], W=[("cT", t)])
        self.P.barrier()
        QKT = self.alloc(HTR, "QKT", [128, 4, TT], BF16)
        VA = self.alloc(HTR, "VA", [128, NT, 192], BF16)
        attT = self.alloc(HTR, "attT", [128, TT], BF16)
        W2q = self.alloc(HTR, "W2q", [128, 3, 192], BF16)
        W2kv = self.alloc(HTR, "W2kv", [128, 2, 256], BF16)
        WoP = self.alloc(HTR, "WoP", [128, 2, D], BF16)
        self.memset("pool", VA[:, :, 64:128], 1.0, W=[("VA", t) for t in range(NT)])
        wuq = d["b_wuq"][0].rearrange("(c p) f -> p c f", p=128)
        wukv = d["b_wukv"][0].rearrange("(c p) f -> p c f", p=128)
        sc = 96.0 ** -0.5
        for pp in range(8):
            self.dma("pool", "wg", W2q[:, :, :], wuq[:, :, pp * 192:(pp + 1) * 192], W=[("W2",)], bar=False)
            self.dma("pool", "wg", W2kv[:, :, :], wukv[:, :, pp * 256:(pp + 1) * 256], W=[("W2",)], group=True, bar=False)
            def S1(t):
                bank = 5 + (t % 2)
                ps = self.psb[bank]
                for c in range(3):
                    self.mm(ps[:, 0:192], cT[:, c, t * 128:(t + 1) * 128], W2q[:, c, :], start=(c == 0), stop=(c == 2),
                            R=[("cT", t), ("W2",)], W=[("ps", bank)])
                for c in range(2):
                    self.mm(ps[:, 192:448], cT[:, 3 + c, t * 128:(t + 1) * 128], W2kv[:, c, :], start=(c == 0), stop=(c == 1),
                            R=[("cT", t), ("W2",)], W=[("ps", bank)])
                self.cp("act", raw2[t % 3][:, :], ps[:, 0:448], R=[("ps", bank)], W=[("rawn", t % 3)])

            def views(t):
                rw = raw2[t % 3]
                rq = rw[:, 0:192].rearrange("p (h e) -> p h e", e=96)
                rkv = rw[:, 192:448].rearrange("p (h e) -> p h e", e=128)
                return rw, rq, rkv

            def S2(t):
                rw, rq, rkv = views(t)
                sq = ssq2[t % 2]
                for i, (src, hd) in enumerate(((rq[:, :, 0:64], 64), (rq[:, :, 64:96], 32), (rkv[:, :, 0:64], 64))):
                    for h in range(2):
                        self.act(sqt[:, 0, 0:hd], src[:, h, :], AF.Square, scale=float(hd) ** -0.5, accum=sq[:, 2 * i + h:2 * i + h + 1],
                                 R=[("rawn", t % 3)], W=[("sqt",), ("ssq", t % 2)])

            def S3(t):
                self.act(lnv2[t % 2][:, 0:6], ssq2[t % 2][:, 0:6], AF.Ln, bias=self.misc[:, 2:3], scale=1.0,
                         R=[("ssq", t % 2), ("cst",)], W=[("lnvh", t % 2)])
                self.act(rs2[t % 2][:, 0:6], lnv2[t % 2][:, 0:6], AF.Exp, scale=-0.5, R=[("lnvh", t % 2)], W=[("rsh", t % 2)])

            def S4(t):
                rw, rq, rkv = views(t)
                rsv = rs2[t % 2]
                qab = qa[t % 3]
                kab = ka[t % 3]
                self.tt("dve", t1[:, :, 0:64], rq[:, :, 0:64], rsv[:, 0:2].unsqueeze(2).to_broadcast([128, 2, 64]), ALU.mult,
                        R=[("rawn", t % 3), ("rsh", t % 2)], W=[("t1",)])
                self.tt("dve", qab[:, :, 0:64], t1[:, :, 0:64], gq2[:, :, 0:64], ALU.mult, R=[("t1",), ("gq2",)], W=[("qa", t % 3)])
                self.tt("dve", t1[:, :, 64:96], rq[:, :, 64:96], rsv[:, 2:4].unsqueeze(2).to_broadcast([128, 2, 32]), ALU.mult,
                        R=[("rawn", t % 3), ("rsh", t % 2)], W=[("t1",)])
                if t < 16:
                    self.tt("dve", tq[:, :, :], t1[:, :, 64:96], gq2[:, :, 64:96], ALU.mult, R=[("t1",), ("gq2",)], W=[("tq",)])
                    x1 = tq[:, :, 0:16]
                    x2 = tq[:, :, 16:32]
                    cs = self.cosB[:, t, :].unsqueeze(1).to_broadcast([128, 2, 16])
                    sn = self.sinB[:, t, :].unsqueeze(1).to_broadcast([128, 2, 16])
                    self.tt("dve", rt[0][:, :, :], x1, cs, ALU.mult, R=[("tq",), ("cosB",)], W=[("rt", 0)])
                    self.tt("dve", rt[1][:, :, :], x2, sn, ALU.mult, R=[("tq",), ("sinB",)], W=[("rt", 1)])
                    self.tt("dve", qab[:, :, 64:80], rt[0][:, :, :], rt[1][:, :, :], ALU.subtract, R=[("rt", 0), ("rt", 1)], W=[("qa", t % 3)])
                    self.tt("dve", rt[2][:, :, :], x1, sn, ALU.mult, R=[("tq",), ("sinB",)], W=[("rt", 2)])
                    self.tt("dve", rt[3][:, :, :], x2, cs, ALU.mult, R=[("tq",), ("cosB",)], W=[("rt", 3)])
                    self.tt("dve", qab[:, :, 80:96], rt[2][:, :, :], rt[3][:, :, :], ALU.add, R=[("rt", 2), ("rt", 3)], W=[("qa", t % 3)])
                else:
                    self.tt("dve", qab[:, :, 64:96], t1[:, :, 64:96], gq2[:, :, 64:96], ALU.mult, R=[("t1",), ("gq2",)], W=[("qa", t % 3)])
                self.tt("dve", k1[:, :, :], rkv[:, :, 0:64], rsv[:, 4:6].unsqueeze(2).to_broadcast([128, 2, 64]), ALU.mult,
                        R=[("rawn", t % 3), ("rsh", t % 2)], W=[("k1",)])
                self.tt("dve", kab[:, :, 0:64], k1[:, :, :], gk2[:, :, :], ALU.mult, R=[("k1",), ("gk2",)], W=[("ka", t % 3)])
                self.cp("dve", kab[:, :, 64:96], krope[:, t, :].unsqueeze(1).to_broadcast([128, 2, 32]), R=[("krope", t)], W=[("ka", t % 3)])
                self.cp("pool", VA[:, t, 0:64], rkv[:, 0, 64:128], R=[("rawn", t % 3)], W=[("VA", t)])
                self.cp("pool", VA[:, t, 128:192], rkv[:, 1, 64:128], R=[("rawn", t % 3)], W=[("VA", t)])

            def S5(t):
                qab = qa[t % 3]
                kab = ka[t % 3]
                for i in range(2):
                    self.tr(ptb[0:96, i * 128:(i + 1) * 128], qab[:, i, :], self.identb[:, :], R=[("qa", t % 3), ("identb",)], W=[("ps", 7)])
                for i in range(2):
                    self.tr(ptb[0:96, (2 + i) * 128:(3 + i) * 128], kab[:, i, :], self.identb[:, :], R=[("ka", t % 3), ("identb",)], W=[("ps", 7)])
                self.cp("act", QKT[0:96, :, t * 128:(t + 1) * 128], ptb[0:96, 0:512].rearrange("p (i n) -> p i n", n=128),
                        R=[("ps", 7)], W=[("QKT", t)])

            self.pipeline(NT, S1, S2, S3, S4, S5)
            slot = pp % 2
            self.dma("pool", "wo%d" % slot, WoP[:, slot, :], d["b_wo"][0][pp * 128:(pp + 1) * 128, :], W=[("WoP", slot)], bar=False)
            cfgs = []
            qgroups = [0, 1, 2, 3] + ([4] if need_ctx else [])
            for gi in qgroups:
                t0, w = GROUPS[gi]
                ktiles = list(range(NT)) if gi < 4 else [16, 17]
                for hs in range(2):
                    cfgs.append(dict(
                        qt=QKT[0:96, hs, :], kt=QKT[0:96, 2 + hs, :],
                        va=(lambda k, hs=hs: VA[:, k, 0:128] if hs == 0 else VA[:, k, 64:192]),
                        o_lo=(hs == 0), t0=t0, w=w, gi=gi, hs=hs, ktiles=ktiles, att=attT,
                        qres=("QKTall",), kres=("QKTall",), vres=("VAall",), ares=("attT",)))
            self._alias([("QKT", t) for t in range(NT)], ("QKTall",))
            self._alias([("VA", t) for t in range(NT)], ("VAall",))
            self.attn_core(cfgs, sc)
            self.out_proj(WoP[:, slot, :], ("WoP", slot), attT, ("attT",), l, b, need_ctx)
            self._alias_release([("QKT", t) for t in range(NT)], ("QKTall",))
            self._alias_release([("VA", t) for t in range(NT)], ("VAall",))
        self.flush_op()
        self.P.barrier()

    def na_layer(self, l, b, need_ctx):
        d = self.d
        R, R2 = self.R, self.R2
        R.reset()
        R2.reset()
        self.norm_bufs()
        hT = self.carve("hT", HT_OFF, [128, 8, TT], BF16)
        for gi in range(5):
            t0, w = GROUPS[gi]
            self.norm_group(gi, l, b, 1, lambda c, t0=t0, w=w, gi=gi: (hT[:, c, t0:t0 + w], ("hT", gi)))
        self.P.barrier()
        R.reset()
        QKT = self.alloc(R, "QKT", [128, 3, TT], BF16)
        VA = self.alloc(R, "VA", [128, NT, 192], BF16)
        attT = self.alloc(R, "attT", [128, TT], BF16)
        Wp = self.alloc(R, "Wg", [128, 8, 384], BF16)
        WoP = self.alloc(R, "WoP", [128, 2, D], BF16)
        self.PT = self.alloc(R, "PT", [128, 2, 1024], BF16)
        self.rec = self.alloc(R2, "rec", [128, 256], F32)
        gq4 = self.alloc(R2, "gq4", [128, 4, 64], F32)
        maskb = self.alloc(R, "maskb", [128, 21 * 128], BF16)
        self.memset("pool", QKT[64:128, 0, :], 0.0, W=[("QKTz",)])
        self.memset("pool", QKT[0:64, 1, :], 0.0, W=[("QKTz",)])
        BM = self.alloc(R2, "BM", [128, 21 * 128], F32)
        tmpb = [self.alloc(R2, "tmpb%d" % i, [128, 512], F32) for i in range(2)]
        raw = [self.alloc(R2, "raw%d" % i, [128, 384], F32) for i in range(3)]
        sqt = self.alloc(R2, "sqt", [128, 4, 64], F32)
        qkb = [self.alloc(R2, "qkb%d" % i, [128, 4, 64], BF16) for i in range(3)]
        junk = self.alloc(R2, "junk", [128, 64], F32)
        ssq = [self.smallp[:, 0:8], self.smallp[:, 8:16]]
        lnv = [self.smallp[:, 16:24], self.smallp[:, 24:32]]
        rs = [self.smallp[:, 32:40], self.smallp[:, 40:48]]
        for i in range(2):
            self.bcast_row("sp", "gq", gq4[:, i, :], d["c_qnorm"][0], 64, W=[("gq4",)], group=(i > 0))
            self.bcast_row("sp", "gq", gq4[:, 2 + i, :], d["c_knorm"][0], 64, W=[("gq4",)], group=True)
        self.dma("pool", "mk", maskb[:, 0:1344], d["na_mask"][:, 0:1344], W=[("maskb",)])
        self.dma("pool", "mk", maskb[:, 1344:2688], d["na_mask"][:, 1344:2688], W=[("maskb",)], group=True)
        self.memset("pool", VA[:, :, 64:128], 1.0, W=[("VA", t) for t in range(NT)])
        wq = d["c_wqkv"][0].rearrange("(c p) f -> p c f", p=128)
        ptb = self.psbf(7)
        sc = 0.125
        for pp in range(8):
            self.dma("pool", "wg", Wp[:, :, 0:128], wq[:, :, 128 * pp:128 * pp + 128], W=[("Wg",)], bar=False)
            self.dma("pool", "wg", Wp[:, :, 128:256], wq[:, :, 1024 + 128 * pp:1024 + 128 * pp + 128], W=[("Wg",)], group=True, bar=False)
            self.dma("pool", "wg", Wp[:, :, 256:384], wq[:, :, 2048 + 128 * pp:2048 + 128 * pp + 128], W=[("Wg",)], group=True, bar=False)
            def S1(t):
                bank = 5 + (t % 2)
                ps = self.psb[bank]
                for c in range(8):
                    self.mm(ps[:, 0:384], hT[:, c, t * 128:(t + 1) * 128], Wp[:, c, :], start=(c == 0), stop=(c == 7),
                            R=[("hT", min(t // 4, 4)), ("Wg",)], W=[("ps", bank)])
                self.cp("act", raw[t % 3][:, :], ps[:, 0:384], R=[("ps", bank)], W=[("raw", t % 3)])

            def S2(t):
                r3 = raw[t % 3][:, 0:256].rearrange("p (h e) -> p h e", e=64)
                for h in range(4):
                    self.act(junk[:, :], r3[:, h, :], AF.Square, scale=0.125, accum=ssq[t % 2][:, h:h + 1],
                             R=[("raw", t % 3)], W=[("junk",), ("ssq", t % 2)])

            def S3(t):
                self.act(lnv[t % 2][:, 0:4], ssq[t % 2][:, 0:4], AF.Ln, bias=self.misc[:, 2:3], scale=1.0,
                         R=[("ssq", t % 2), ("cst",)], W=[("lnvh", t % 2)])
                self.act(rs[t % 2][:, 0:4], lnv[t % 2][:, 0:4], AF.Exp, scale=-0.5, R=[("lnvh", t % 2)], W=[("rsh", t % 2)])

            def S4(t):
                rw = raw[t % 3]
                r3 = rw[:, 0:256].rearrange("p (h e) -> p h e", e=64)
                self.tt("dve", sqt[:, :, :], r3, rs[t % 2][:, 0:4].unsqueeze(2).to_broadcast([128, 4, 64]), ALU.mult,
                        R=[("raw", t % 3), ("rsh", t % 2)], W=[("sqt",)])
                qb = qkb[t % 3]
                self.tt("dve", qb[:, :, :], sqt[:, :, :], gq4[:, :, :], ALU.mult, R=[("sqt",), ("gq4",)], W=[("qkb", t % 3)])
                self.cp("pool", VA[:, t, 0:64], rw[:, 256:320], R=[("raw", t % 3)], W=[("VA", t)])
                self.cp("pool", VA[:, t, 128:192], rw[:, 320:384], R=[("raw", t % 3)], W=[("VA", t)])

            def S5(t):
                qb = qkb[t % 3]
                qf = qb[:, :, :].rearrange("p h e -> p (h e)")
                for i in range(2):
                    self.tr(ptb[:, i * 128:(i + 1) * 128], qf[:, i * 128:(i + 1) * 128], self.identb[:, :],
                            R=[("qkb", t % 3), ("identb",)], W=[("ps", 7)])
                self.cp("act", QKT[0:64, 0, t * 128:(t + 1) * 128], ptb[0:64, 0:128], R=[("ps", 7)], W=[("QKT", t)])
                self.cp("act", QKT[64:128, 1, t * 128:(t + 1) * 128], ptb[64:128, 0:128], R=[("ps", 7)], W=[("QKT", t)])
                self.cp("dve", QKT[:, 2, t * 128:(t + 1) * 128], ptb[:, 128:256], R=[("ps", 7)], W=[("QKT", t)])

            self.pipeline(NT, S1, S2, S3, S4, S5)
            slot = pp % 2
            self.dma("pool", "wo%d" % slot, WoP[:, slot, :], d["c_wo"][0][pp * 128:(pp + 1) * 128, :], W=[("WoP", slot)], bar=False)
            self._alias([("QKT", t) for t in range(NT)] + [("QKTz",)], ("QKTall",))
            self._alias([("VA", t) for t in range(NT)], ("VAall",))
            for hs in range(2):
                h = 2 * pp + hs
                psl = slice(0, 64) if hs == 0 else slice(64, 128)
                self.dma("sp", "bm", BM[:, :], d["na_bias"][h], W=[("BM",)])
                self.tt("pool", BM[:, :], BM[:, :], maskb[:, :], ALU.add, R=[("BM",), ("maskb",)], W=[("BM",)])
                va = (lambda k, hs=hs: VA[:, k, 0:128] if hs == 0 else VA[:, k, 64:192])
                self.na_core(QKT[:, hs, :], QKT[:, 2, :], va, hs == 0, attT, BM, tmpb, sc)
            if need_ctx:
                cfgs = []
                t0, w = GROUPS[4]
                for hs in range(2):
                    psl = slice(0, 64) if hs == 0 else slice(64, 128)
                    cfgs.append(dict(
                        qt=QKT[:, hs, :], kt=QKT[:, 2, :],
                        va=(lambda k, hs=hs: VA[:, k, 0:128] if hs == 0 else VA[:, k, 64:192]),
                        o_lo=(hs == 0), t0=t0, w=w, gi=4, hs=hs, ktiles=[16, 17], att=attT,
                        qres=("QKTall",), kres=("QKTall",), vres=("VAall",), ares=("attT",)))
                self.attn_core(cfgs, sc)
            self.out_proj(WoP[:, slot, :], ("WoP", slot), attT, ("attT",), l, b, need_ctx)
            self._alias_release([("QKT", t) for t in range(NT)], ("QKTall",))
            self._alias_release([("VA", t) for t in range(NT)], ("VAall",))
        self.flush_op()
        self.P.barrier()

    def na_core(self, qt, kt, va, o_lo, attT, BM, tmpb, sc):
        packs = []
        for qi in range(16):
            blocks = na_block_ids(qi)
            items = [(kt_, blk) for (kt_, blk) in blocks] + [(16, None), (17, None)]
            plist = [items[0:4], items[4:]]
            for pi, pk in enumerate(plist):
                packs.append((qi, pk, pi == 0, pi == len(plist) - 1))
        n = len(packs)
        pt3 = self.PT[:, :, :].rearrange("p a (b n) -> p (a b) n", n=512)
        lo, hi_ = slice(0, 64), slice(64, 128)
        o_sl, d_sl = (lo, hi_) if o_lo else (hi_, lo)

        def qk(i):
            qi, pk, first, last = packs[i]
            bank = i % 4
            for j, (k, blk) in enumerate(pk):
                self.mm(self.psb[bank][:, j * 128:(j + 1) * 128], kt[:, k * 128:(k + 1) * 128], qt[:, qi * 128:(qi + 1) * 128],
                        R=[("QKTall",)], W=[("ps", bank)])

        def ex(i):
            qi, pk, first, last = packs[i]
            bank = i % 4
            nb = sum(1 for (_, blk) in pk if blk is not None)
            nk = len(pk)
            if nb > 0:
                b0 = pk[0][1]
                tb = tmpb[i % 2]
                self.stt(tb[:, 0:nb * 128], self.psb[bank][:, 0:nb * 128], sc, BM[:, b0 * 128:(b0 + nb) * 128], ALU.mult, ALU.add,
                         R=[("ps", bank), ("BM",)], W=[("tmpb", i % 2)])
                self.act(pt3[:, bank, 0:nb * 128], tb[:, 0:nb * 128], AF.Exp, R=[("tmpb", i % 2)], W=[("pt", bank)])
            if nk > nb:
                self.act(pt3[:, bank, nb * 128:nk * 128], self.psb[bank][:, nb * 128:nk * 128], AF.Exp, scale=sc,
                         R=[("ps", bank)], W=[("pt", bank)])

        def pv(i):
            qi, pk, first, last = packs[i]
            if o_lo and first and qi % 4 == 0:
                self.take_op(qi // 4, 8)
            ob = 4 + (qi + self.ot_ctr) % 2
            for j, (k, blk) in enumerate(pk):
                self.mm(self.psb[ob][:, 0:128], va(k), pt3[:, i % 4, j * 128:(j + 1) * 128],
                        start=(first and j == 0), stop=(last and j == len(pk) - 1),
                        R=[("pt", i % 4), ("VAall",)], W=[("ps", ob)])
            if last:
                rec_o = self.rec[d_sl, 0:128]
                rec_l = self.rec[d_sl, 128:256]
                rec_i = self.psb[ob][d_sl, 0:128]
                self.act(rec_l, rec_i, AF.Ln, R=[("ps", ob)], W=[("recl",)])
                self.act(rec_o, rec_l, AF.Exp, scale=-1.0, R=[("recl",)], W=[("rec",)])
                self.tt("dve", attT[o_sl, qi * 128:(qi + 1) * 128], self.psb[ob][o_sl, 0:128], self.rec[d_sl, 0:128], ALU.mult,
                        R=[("ps", ob), ("rec",)], W=[("attT", qi // 4)])

        for i0 in range(min(3, n)):
            qk(i0)
        for i in range(n):
            if i + 3 < n:
                qk(i + 3)
            ex(i)
            pv(i)
        self.ot_ctr += 16

    def ring_load(self, src):
        i = self.unit_ctr % NUNIT
        self.unit_ctr += 1
        buf = self.ring[i]
        self.dma("pool", "ring%d" % i, buf[:, :, :], src, W=[("ring", i)], bar=False)
        return buf, ("ring", i)

    def moe_units(self, l, e):
        d = self.d
        wg = d["moe_wg"][l, e].rearrange("(c p) f -> p c f", p=128)
        wu = d["moe_wu"][l, e].rearrange("(c p) f -> p c f", p=128)
        wd = d["moe_wd"][l, e].rearrange("(c p) f -> p c f", p=128)
        return [wg[:, :, 0:512], wu[:, :, 0:512], wg[:, :, 512:1024], wu[:, :, 512:1024], wd[:, :, 0:512], wd[:, :, 512:1024]]

    def moe_layer(self, l, b, need_ctx, pre):
        d = self.d
        R = self.R
        R.reset()
        ROFF = R_OFF
        self.norm_bufs()
        h2T = [self.alloc(R, "h2T%d" % i, [128, 8, 512], BF16) for i in range(2)]
        aff = self.carve("aff", ROFF + 43008, [128, NT, 16], F32)
        rw = self.carve("rw", ROFF + 44160, [128, 8, 16], BF16)
        m8 = self.carve("m8", ROFF + 44160 + 256, [16, 8], F32)
        thr = self.carve("thr", ROFF + 44160 + 288, [16, 2], F32)
        gate = self.carve("gate", ROFF + 44160 + 320, [128, 4], F32)
        ee = self.carve("ee", ROFF + 44160 + 352, [128, 16], F32)
        sst = self.carve("sst", ROFF + 44160 + 416, [128, 8], F32)
        h2 = self.carve("h2", HT_OFF, [128, NT, D], BF16)
        self.dma("pool", "rw", rw[:, :, :], d["moe_router"][l].rearrange("(c p) e -> p c e", p=128), W=[("rw",)])
        ngrp = 5 if need_ctx else 4
        ntl = NT if need_ctx else 16
        ptb = self.psbf(7)
        for gi in range(ngrp):
            t0, w = GROUPS[gi]
            hb = h2T[gi % 2]
            self.norm_group(gi, l, b, 2, lambda c, hb=hb, w=w, gi=gi: (hb[:, c, 0:w], ("h2T", gi % 2)))
            for tt_ in range(w // 128):
                t = t0 // 128 + tt_
                bank = 3 + (t % 2)
                ps = self.psb[bank]
                for c in range(8):
                    self.mm(ps[:, 0:16], hb[:, c, tt_ * 128:(tt_ + 1) * 128], rw[:, c, :], start=(c == 0), stop=(c == 7),
                            R=[("h2T", gi % 2), ("rw",)], W=[("ps", bank)])
                self.red(sst[:, 0:1], ps[:, 0:16], ALU.max, R=[("ps", bank)], W=[("sst",)])
                self.ts("dve", sst[:, 1:2], sst[:, 0:1], -1.0, ALU.mult, R=[("sst",)], W=[("sst",)])
                self.act(ee[:, :], ps[:, 0:16], AF.Exp, bias=sst[:, 1:2], accum=sst[:, 2:3],
                         R=[("ps", bank), ("sst",)], W=[("ee",), ("sst",)])
                self.P.op("dve", lambda e: e.reciprocal(out=sst[:, 3:4], in_=sst[:, 2:3]), [("sst",)], [("sst",)])
                self.ts("dve", aff[:, t, :], ee[:, :], sst[:, 3:4], ALU.mult, R=[("ee",), ("sst",)], W=[("aff",)])
                for c in range(8):
                    self.tr(ptb[:, c * 128:(c + 1) * 128], hb[:, c, tt_ * 128:(tt_ + 1) * 128], self.identb[:, :],
                            R=[("h2T", gi % 2), ("identb",)], W=[("ps", 7)])
                self.cp("act", h2[:, t, :], ptb[:, :], R=[("ps", 7)], W=[("h2", t)])
        self.P.barrier()
        affT = self.carve("affT", ROFF + 0, [16, TT], F32)
        work = self.carve("work", ROFF + 9216, [16, TT], F32)
        B3 = self.carve("B3", ROFF + 18432, [16, TT], F32)
        vb16 = self.carve("vb16", ROFF + 27648, [16, TT], BF16)
        gmtok = self.carve("gmtok", ROFF + 38400, [128, NT, 16], F32)
        vtok = self.carve("vtok", ROFF + 40704, [128, NT, 16], F32)
        gmhl = self.carve("gmhl", ROFF + 41856, [128, NT, 16, 2], BF16)
        for t in range(ntl):
            bank = (t // 4) % 2
            self.tr(self.psb[bank][0:16, (t % 4) * 128:(t % 4 + 1) * 128], aff[:, t, :], self.identf,
                    R=[("aff",), ("cst",)], W=[("ps", bank)])
            if t % 4 == 3 or t == ntl - 1:
                t_lo = (t // 4) * 4
                n = (t - t_lo + 1) * 128
                self.cp("dve", affT[:, t_lo * 128:t_lo * 128 + n], self.psb[bank][0:16, 0:n], R=[("ps", bank)], W=[("affT",)])
        segs = [(0, T, CAP // 8, 0)] + ([(T, TC, CAPC // 8, 1)] if need_ctx else [])
        ncols = T + (TC if need_ctx else 0)
        self.cp("dve", work[:, 0:ncols], affT[:, 0:ncols], R=[("affT",)], W=[("work",)])
        for (c0, n, rounds, ti) in segs:
            wv = work[:, c0:c0 + n]
            for r in range(rounds):
                self.P.op("dve", lambda e, wv=wv: e.max(out=m8[:, :], in_=wv), [("work",)], [("m8",)])
                if r < rounds - 1:
                    self.P.op("dve", lambda e, wv=wv: e.match_replace(out=wv, in_to_replace=m8[:, :], in_values=wv, imm_value=-1.0),
                              [("work",), ("m8",)], [("work",)])
            self.cp("dve", thr[:, ti:ti + 1], m8[:, 7:8], R=[("m8",)], W=[("thr",)])
        ones_col = self.cstb[0:16, 128 + 1:128 + 2]
        for (c0, n, rounds, ti) in segs:
            self.ts("dve", work[:, c0:c0 + n], affT[:, c0:c0 + n], thr[:, ti:ti + 1], ALU.is_ge, R=[("affT",), ("thr",)], W=[("work",)])
            self.P.op("dve", lambda e, c0=c0, n=n: e.tensor_tensor_scan(out=B3[:, c0:c0 + n], data0=ones_col.to_broadcast([16, n]),
                                                                     data1=work[:, c0:c0 + n], initial=0.0, op0=ALU.mult, op1=ALU.add),
                      [("work",), ("cst",)], [("B3",)])
        self.tt("dve", B3[:, 0:ncols], B3[:, 0:ncols], work[:, 0:ncols], ALU.mult, R=[("B3",), ("work",)], W=[("B3",)])
        self.ts("dve", B3[:, 0:ncols], B3[:, 0:ncols], -1.0, ALU.add, R=[("B3",)], W=[("B3",)])
        self.tt("dve", affT[:, 0:ncols], affT[:, 0:ncols], work[:, 0:ncols], ALU.mult, R=[("affT",), ("work",)], W=[("affT",)])
        self.cp("dve", vb16[:, 0:ncols], B3[:, 0:ncols], R=[("B3",)], W=[("vb16",)])
        for (src, dst, nm, bank) in ((B3, vtok, "vtok", 2), (affT, gmtok, "gmtok", 3)):
            for t in range(ntl):
                self.tr(self.psb[bank][:, t * 16:(t + 1) * 16], src[:, t * 128:(t + 1) * 128], self.identf[0:16, 0:16],
                        R=[(src.name,), ("cst",)], W=[("ps", bank)])
            self.cp("dve", dst[:, 0:ntl, :], self.psb[bank][:, 0:ntl * 16].rearrange("p (t e) -> p t e", e=16),
                    R=[("ps", bank)], W=[(nm,)])
        self.cp("dve", gmhl[:, 0:ntl, :, 0], gmtok[:, 0:ntl, :], R=[("gmtok",)], W=[("gmhl",)])
        self.tt("dve", gmtok[:, 0:ntl, :], gmtok[:, 0:ntl, :], gmhl[:, 0:ntl, :, 0], ALU.subtract, R=[("gmtok",), ("gmhl",)], W=[("gmtok",)])
        self.cp("dve", gmhl[:, 0:ntl, :, 1], gmtok[:, 0:ntl, :], R=[("gmtok",)], W=[("gmhl",)])
        self.P.barrier()
        S = self.carve("S", ROFF + 0, [128, 16, 256], BF16)
        Sc = self.carve("Sc", ROFF + 8192, [128, 2, 32], BF16)
        ST = self.carve("ST", ROFF + 9216, [128, 2, T], BF16)
        STc = self.carve("STc", ROFF + 9216 + 8192, [32, 256], BF16)
        xgT = self.carve("xgT", ROFF + 18432, [128, 8, 288], BF16)
        hidT = self.carve("hidT", ROFF + 18432 + 4608, [128, 8, 288], BF16)
        y = self.carve("y", ROFF + 32256, [128, 3, D], BF16)
        sg = self.carve("sg", ROFF + 38400, [128, 2, 288], F32)
        Wn = 288 if need_ctx else 256
        units = list(pre)
        upos = [0]

        def next_units(n):
            out = []
            for _ in range(n):
                out.append(units[upos[0]])
                upos[0] += 1
            return out

        srcs = []
        for e in range(NE):
            srcs += self.moe_units(l, e)
        issued = [len(pre)]

        def issue(upto):
            while issued[0] < min(upto, len(srcs)):
                units.append(self.ring_load(srcs[issued[0]]))
                issued[0] += 1

        chunks = [(0, 128), (128, 128)] + ([(256, 32)] if need_ctx else [])
        nch = 3 if need_ctx else 2

        def build_S(e):
            for t in range(16):
                self.ts("dve", S[:, t, :], self.iotac, vtok[:, t, e:e + 1], ALU.is_equal, R=[("vtok",), ("cst",)], W=[("S", t)])
            if need_ctx:
                for t in range(2):
                    self.ts("dve", Sc[:, t, :], self.iotac[:, 0:32], vtok[:, 16 + t, e:e + 1], ALU.is_equal, R=[("vtok",), ("cst",)], W=[("Sc",)])

        def slot_gates(e):
            gb = 4
            gps = self.psb[gb]
            for ch in range(2):
                for t in range(16):
                    self.mm(gps[:, ch * 2:ch * 2 + 2], S[:, t, ch * 128:(ch + 1) * 128], gmhl[:, t, e, :], start=(t == 0), stop=(t == 15),
                            R=[("S", t), ("gmhl",)], W=[("ps", gb)])
            if need_ctx:
                for t in range(2):
                    self.mm(gps[0:32, 4:6], Sc[:, t, :], gmhl[:, 16 + t, e, :], start=(t == 0), stop=(t == 1),
                            R=[("Sc",), ("gmhl",)], W=[("ps", gb)])
            self.red(gate[:, 0:nch], gps[:, 0:2 * nch].rearrange("p (c two) -> p c two", two=2), ALU.add, R=[("ps", gb)], W=[("gate",)])

        def gather_c(e, c):
            bank = c % 2
            ps = self.psb[bank]
            for t in range(16):
                self.mm(ps[:, 0:256], h2[:, t, c * 128:(c + 1) * 128], S[:, t, :], start=(t == 0), stop=(t == 15),
                        R=[("h2", t), ("S", t)], W=[("ps", bank)])
            if need_ctx:
                for t in range(2):
                    self.mm(ps[:, 256:288], h2[:, 16 + t, c * 128:(c + 1) * 128], Sc[:, t, :], start=(t == 0), stop=(t == 1),
                            R=[("h2", 16 + t), ("Sc",)], W=[("ps", bank)])
            self.cp("act", xgT[:, c, 0:Wn], ps[:, 0:Wn], R=[("ps", bank)], W=[("xgT",)])

        def scatter_blocks(e):
            out = []
            for grp in range(4):
                for dc in range(8):
                    def blk(grp=grp, dc=dc):
                        bank = 2 + (dc % 2)
                        ps = self.psb[bank]
                        for ch in range(2):
                            self.mm(ps[:, :], y[:, ch, dc * 128:(dc + 1) * 128], ST[:, ch, grp * 512:(grp + 1) * 512],
                                    start=(ch == 0), stop=(ch == 1), R=[("y",), ("ST", grp)], W=[("ps", bank)])
                        xs = self.xT[:, dc, grp * 512:(grp + 1) * 512]
                        self.stt(xs, ps[:, :], self.mod(l, b, 5, dc), xs, ALU.mult, ALU.add,
                                 R=[("ps", bank), ("modv",), ("xT", grp, dc)], W=[("xT", grp, dc)])
                    out.append(blk)
            if need_ctx:
                for dc in range(8):
                    def blk(dc=dc):
                        bank = 2 + (dc % 2)
                        ps = self.psb[bank]
                        self.mm(ps[:, 0:256], y[0:32, 2, dc * 128:(dc + 1) * 128], STc[:, :], R=[("y",), ("STc",)], W=[("ps", bank)])
                        xs = self.xT[:, dc, T:TT]
                        self.stt(xs, ps[:, 0:256], self.mod(l, 2, 5, dc), xs, ALU.mult, ALU.add,
                                 R=[("ps", bank), ("modv",), ("xT", 4, dc)], W=[("xT", 4, dc)])
                    out.append(blk)
            return out

        def build_ST(e):
            for grp in range(4):
                bank = 2 + (grp % 2)
                ps = self.psb[bank]
                self.mm(ps[:, :], self.sel[:, e, :], vb16[:, grp * 512:(grp + 1) * 512], R=[("sel",), ("vb16",)], W=[("ps", bank)])
                for ch in range(2):
                    self.ts("dve", ST[:, ch, grp * 512:(grp + 1) * 512], ps[:, :], self.misc[:, ch:ch + 1], ALU.is_equal,
                            R=[("ps", bank), ("cst",)], W=[("ST", grp)])
            if need_ctx:
                ps = self.psb[2]
                self.mm(ps[0:32, 0:256], self.sel[:, e, 0:32], vb16[:, T:TT], R=[("sel",), ("vb16",)], W=[("ps", 2)])
                self.ts("dve", STc[:, :], ps[0:32, 0:256], self.misc[0:32, 0:1], ALU.is_equal, R=[("ps", 2), ("cst",)], W=[("STc",)])

        def gate_up(e, wg0, wu0, wg1, wu1, inject):
            for fc in range(8):
                if fc in inject:
                    inject[fc]()
                gbuf, gres = (wg0 if fc < 4 else wg1)
                ubuf, ures = (wu0 if fc < 4 else wu1)
                fo = (fc % 4) * 128
                bg = 4 + 2 * (fc % 2)
                bu = bg + 1
                for c in range(8):
                    self.mm(self.psb[bg][:, 0:Wn], gbuf[:, c, fo:fo + 128], xgT[:, c, 0:Wn], start=(c == 0), stop=(c == 7),
                            R=[gres, ("xgT",)], W=[("ps", bg)])
                for c in range(8):
                    self.mm(self.psb[bu][:, 0:Wn], ubuf[:, c, fo:fo + 128], xgT[:, c, 0:Wn], start=(c == 0), stop=(c == 7),
                            R=[ures, ("xgT",)], W=[("ps", bu)])
                self.act(sg[:, fc % 2, 0:Wn], self.psb[bg][:, 0:Wn], AF.Silu, R=[("ps", bg)], W=[("sg", fc % 2)])
                self.tt("dve", hidT[:, fc, 0:Wn], sg[:, fc % 2, 0:Wn], self.psb[bu][:, 0:Wn], ALU.mult,
                        R=[("sg", fc % 2), ("ps", bu)], W=[("hidT",)])

        def down(e, wd0, wd1):
            for half, (dbuf, dres) in enumerate((wd0, wd1)):
                for ci, (c0, m) in enumerate(chunks):
                    bank = (half * 3 + ci) % 2
                    ps = self.psb[bank]
                    for fcn in range(8):
                        self.mm(ps[0:m, :], hidT[:, fcn, c0:c0 + m], dbuf[:, fcn, :], start=(fcn == 0), stop=(fcn == 7),
                                R=[("hidT",), dres], W=[("ps", bank)])
                    self.ts("dve", y[0:m, ci, half * 512:(half + 1) * 512], ps[0:m, :], gate2[0:m, ci:ci + 1], ALU.mult,
                            R=[("ps", bank), ("gate2",)], W=[("y",)])

        gate2 = self.carve("gate2", ROFF + 44160 + 480, [128, 4], F32)
        issue(4)
        build_S(0)
        slot_gates(0)
        for c in range(8):
            gather_c(0, c)
        for e in range(NE):
            issue(e * 6 + 4)
            wg0, wu0, wg1, wu1 = next_units(4)
            self.cp("dve", gate2[:, 0:nch], gate[:, 0:nch], R=[("gate",)], W=[("gate2",)])
            inj = {1: (lambda e=e: build_ST(e))}
            if e + 1 < NE:
                inj[3] = (lambda e=e: build_S(e + 1))
                inj[5] = (lambda e=e: slot_gates(e + 1))
            gate_up(e, wg0, wu0, wg1, wu1, inj)
            issue(e * 6 + 6)
            wd0, wd1 = next_units(2)
            down(e, wd0, wd1)
            issue(e * 6 + 10)
            blocks = scatter_blocks(e)
            if e + 1 < NE:
                nb = len(blocks)
                per = (nb + 7) // 8
                bi = 0
                for c in range(8):
                    gather_c(e + 1, c)
                    for _ in range(per):
                        if bi < nb:
                            blocks[bi]()
                            bi += 1
                while bi < nb:
                    blocks[bi]()
                    bi += 1
            else:
                for blk in blocks:
                    blk()
        self.P.barrier()

    def moe_prefetch(self, l):
        self.unit_ctr = 0
        srcs = self.moe_units(l, 0)
        return [self.ring_load(srcs[i]) for i in range(2)]

    def build(self):
        cfg = self.cfg
        self.prologue()
        for b in range(cfg.get("nsamples", SPC)):
            self.load_sample(b)
            for l in cfg.get("layers", [0, 1, 2, 3]):
                need_ctx = l < 3
                pre = self.moe_prefetch(l) if cfg.get("moe", True) else []
                kind = l % 3
                if cfg.get("attn", True):
                    if kind == 0:
                        self.gqa_layer(l, b, need_ctx)
                    elif kind == 1:
                        self.mla_layer(l, b, need_ctx)
                    else:
                        self.na_layer(l, b, need_ctx)
                if cfg.get("dump_xa") == (b, l):
                    self.store_sample(self.dbg_out("xa", [TT, D]), NT)
                if cfg.get("moe", True):
                    self.moe_layer(l, b, need_ctx, pre)
                if cfg.get("dump_x") == (b, l):
                    self.store_sample(self.dbg_out("x", [TT, D]), NT)
            self.store_sample(self.y2[b], 16)
        self.P.emit(self.es)
        return self.nc


def host_inputs(inp):
    cst = np.zeros((128, 388), np.float32)
    cst[:, 0:128] = np.eye(128, dtype=np.float32)
    cst[:, 128:384] = np.arange(256, dtype=np.float32)[None, :]
    cst[:, 384] = np.arange(128, dtype=np.float32)
    cst[:, 385] = np.arange(128, dtype=np.float32) + 128.0
    cst[:, 386] = EPS
    selc = np.zeros((16, 16, 128), np.float32)
    for e in range(16):
        selc[e, e, :] = 1.0
    selc = selc.reshape(16, 2048).astype(ml_dtypes.bfloat16)
    cosA, sinA = rope_tables(64)
    cosB, sinB = rope_tables(32)
    na_bias, na_mask = na_tables(np.asarray(inp["c_rpb"], np.float32)[0])
    shared = {k: np.ascontiguousarray(np.asarray(inp[k], np.float32)) for k in (
        "ada_w", "ada_b", "norm1_w", "norm2_w", "a_wqkv", "a_qnorm", "a_knorm", "a_wo",
        "b_wdq", "b_qnorm_lat", "b_wuq", "b_wdkv", "b_kvnorm_lat", "b_wukv", "b_qnorm", "b_knorm", "b_wo",
        "c_wqkv", "c_qnorm", "c_knorm", "c_wo", "moe_router", "moe_wg", "moe_wu", "moe_wd")}
    shared.update(cst=cst, selc=selc, cosA=cosA, sinA=sinA, cosB=cosB, sinB=sinB, na_bias=na_bias, na_mask=na_mask)
    x = np.asarray(inp["x"], np.float32)
    ctx = np.asarray(inp["ctx"], np.float32)
    c = np.asarray(inp["c"], np.float32)
    c_ctx = np.asarray(inp["c_ctx"], np.float32)
    maps = []
    for core in range(N_CORES):
        m = dict(shared)
        m["x2"] = np.ascontiguousarray(x[SPC * core:SPC * core + SPC])
        m["ctx2"] = np.ascontiguousarray(ctx[SPC * core:SPC * core + SPC])
        m["cvec"] = np.ascontiguousarray(np.concatenate([c[SPC * core:SPC * core + SPC], c_ctx[None, :]], axis=0))
        maps.append(m)
    return maps


def kernel(**inp):
    maps = host_inputs(inp)
    nc = Builder({}).build()
    res = run_bass_kernel_spmd(nc, maps, core_ids=list(range(N_CORES)))
    out = np.concatenate([np.asarray(r["y2"], np.float32) for r in res.results], axis=0)
    return out
```

```python
import numpy as np
import ml_dtypes
from contextlib import ExitStack
import concourse.bass as bass
import concourse.mybir as mybir
from concourse.bass_utils import run_bass_kernel_spmd

F32 = mybir.dt.float32
BF16 = mybir.dt.bfloat16
AF = mybir.ActivationFunctionType
ALU = mybir.AluOpType
AX = mybir.AxisListType

D = 1024
T = 2048
TC = 256
TT = 2304
NT = 18
NE = 16
CAP = 256
CAPC = 32
EPS = 1e-6
NEG = -30000.0
N_CORES = 8
SPC = 2

XT_OFF = 0
RING_OFF = 73728
UNIT = 8192
NUNIT = 5
HT_OFF = RING_OFF + UNIT * NUNIT
R_OFF = HT_OFF + 36864
R_SIZE = 45056
PERS_OFF = R_OFF + R_SIZE
ARENA_BYTES = 212000
R2_OFF = RING_OFF + 2 * UNIT
R2_SIZE = 3 * UNIT


def _dsize(dt):
    return 4 if dt == F32 else 2


class Prog:
    ENG = ("pe", "act", "dve", "pool", "sp")

    def __init__(self, nc):
        self.nc = nc
        self.eng = {"pe": nc.tensor, "act": nc.scalar, "dve": nc.vector, "pool": nc.gpsimd, "sp": nc.sync}
        self.ops = []
        self.eops = {e: [] for e in self.ENG}
        self.res = {}
        self.slots = {}
        self.pending = {e: set() for e in self.ENG}

    def _deps(self, eng, reads, writes):
        deps = set(self.pending[eng])
        self.pending[eng] = set()
        for r in reads:
            st = self.res.get(r)
            if st is not None and st["w"] is not None:
                deps.add(st["w"])
        for w in writes:
            st = self.res.get(w)
            if st is not None:
                if st["w"] is not None:
                    deps.add(st["w"])
                deps.update(st["r"].values())
        return deps

    def _mark(self, tok, key, reads, writes):
        for r in reads:
            st = self.res.get(r)
            if st is None:
                st = self.res[r] = {"w": None, "r": {}}
            st["r"][key] = tok
        for w in writes:
            self.res[w] = {"w": tok, "r": {}}

    def op(self, eng, fn, reads=(), writes=()):
        deps = self._deps(eng, reads, writes)
        idx = len(self.eops[eng])
        rec = {"eng": eng, "kind": "op", "fn": fn, "deps": deps, "idx": idx, "sig": False}
        self.eops[eng].append(rec)
        self.ops.append(rec)
        self._mark(("e", eng, idx), eng, reads, writes)

    def dma(self, queue, slot, out, in_, reads=(), writes=(), group=False, bar=True, **kw):
        deps = self._deps(queue, reads, writes)
        s = self.slots.get(slot)
        if s is None:
            s = self.slots[slot] = {"n": 0, "groups": [], "bar": bar}
        s["n"] += 1
        n = s["n"]
        if group and s["groups"]:
            s["groups"][-1] = n
        else:
            if n > 1:
                deps.add(("d", slot, n - 1))
            s["groups"].append(n)
        idx = len(self.eops[queue])
        rec = {"eng": queue, "kind": "dma", "slot": slot, "n": n, "out": out, "in_": in_, "deps": deps,
               "idx": idx, "kw": kw, "sig": False}
        self.eops[queue].append(rec)
        self.ops.append(rec)
        self._mark(("d", slot, n), ("d", slot), reads, writes)

    def barrier(self):
        toks = set()
        for e in ("pe", "act", "dve", "pool"):
            if self.eops[e]:
                for rec in reversed(self.eops[e]):
                    if rec["kind"] == "op":
                        toks.add(("e", e, rec["idx"]))
                        break
        for name, s in self.slots.items():
            if s["bar"] and s["n"] > 0:
                toks.add(("d", name, s["n"]))
        for e in ("pe", "act", "dve", "pool", "sp"):
            self.pending[e] |= {t for t in toks if not (t[0] == "e" and t[1] == e == "pe")}

    def _gend(self, slot, n):
        for g in self.slots[slot]["groups"]:
            if g >= n:
                return g
        raise AssertionError

    def emit(self, es):
        nc = self.nc
        for rec in self.ops:
            for d in rec["deps"]:
                if d[0] == "e":
                    if d[1] == rec["eng"] == "pe":
                        continue
                    self.eops[d[1]][d[2]]["sig"] = True
        for e in self.ENG:
            c = 0
            for rec in self.eops[e]:
                if rec["sig"]:
                    c += 1
                rec["sigval"] = c
        sem = {}
        for e in self.ENG:
            sem[("e", e)] = es.enter_context(nc.semaphore("g_" + e))
        for name in self.slots:
            sem[("d", name)] = es.enter_context(nc.semaphore("d_" + name))
        waited = {e: {} for e in self.ENG}
        nwait = 0
        for rec in self.ops:
            e = rec["eng"]
            engine = self.eng[e]
            need = {}
            for d in rec["deps"]:
                if d[0] == "e":
                    if d[1] == e == "pe":
                        continue
                    key = ("e", d[1])
                    val = self.eops[d[1]][d[2]]["sigval"]
                else:
                    if rec["kind"] == "dma" and d[1] == rec["slot"] and self._gend(d[1], d[2]) == self._gend(rec["slot"], rec["n"]):
                        continue
                    key = ("d", d[1])
                    val = 16 * self._gend(d[1], d[2])
                if need.get(key, 0) < val:
                    need[key] = val
            for key, val in need.items():
                if waited[e].get(key, 0) < val:
                    engine.wait_ge(sem[key], val)
                    waited[e][key] = val
                    nwait += 1
            if rec["kind"] == "op":
                inst = rec["fn"](engine)
                if rec["sig"]:
                    inst.then_inc(sem[("e", e)], 1)
            else:
                inst = engine.dma_start(out=rec["out"], in_=rec["in_"], **rec["kw"])
                inst.then_inc(sem[("d", rec["slot"])], 16)
        sp = self.eng["sp"]
        for name, s in self.slots.items():
            if s["n"] > 0:
                sp.wait_ge(sem[("d", name)], 16 * s["n"])
        self.stats = {e: len(self.eops[e]) for e in self.ENG}
        self.stats["waits"] = nwait


class Region:
    def __init__(self, off, size):
        self.off, self.size, self.cur = off, size, 0

    def reset(self):
        self.cur = 0

    def take(self, nbytes):
        nbytes = (nbytes + 31) // 32 * 32
        o = self.off + self.cur
        self.cur += nbytes
        assert self.cur <= self.size, (self.cur, self.size)
        return o


class Buf:
    def __init__(self, name, ap):
        self.name = name
        self.ap = ap

    def __getitem__(self, idx):
        return self.ap[idx]

    def r(self, *k):
        return (self.name,) + k


GROUPS = [(0, 512), (512, 512), (1024, 512), (1536, 512), (2048, 256)]


def na_tables(rpb):
    blocks = [(5, 5 + r) for r in (-2, -1, 0, 1, 2)]
    for qi in (0, 1):
        blocks += [(qi, kt) for kt in range(4)]
    for qi in (14, 15):
        blocks += [(qi, kt) for kt in range(12, 16)]
    nb = len(blocks)
    H = rpb.shape[0]
    bias = np.zeros((H, 128, nb, 128), np.float32)
    mask = np.zeros((128, nb, 128), np.float32)
    kk = np.arange(128)
    for bi, (qi, kt) in enumerate(blocks):
        k_r = 2 * kt + kk // 64
        k_c = kk % 64
        q_r = 2 * qi + kk // 64
        q_c = kk % 64
        rs = np.clip(q_r - 4, 0, 24)
        cs = np.clip(q_c - 8, 0, 48)
        ok = ((k_r[:, None] >= rs[None, :]) & (k_r[:, None] < rs[None, :] + 8)
              & (k_c[:, None] >= cs[None, :]) & (k_c[:, None] < cs[None, :] + 16))
        dr = np.clip(k_r[:, None] - q_r[None, :] + 7, 0, 14)
        dc = np.clip(k_c[:, None] - q_c[None, :] + 15, 0, 30)
        bias[:, :, bi, :] = rpb[:, dr, dc]
        mask[:, bi, :] = np.where(ok, 0.0, NEG)
    return bias.reshape(H, 128, nb * 128), mask.reshape(128, nb * 128)


def na_block_ids(qi):
    if 2 <= qi <= 13:
        return [(qi + r, 2 + r) for r in (-2, -1, 0, 1, 2)]
    if qi in (0, 1):
        return [(kt, 5 + 4 * qi + kt) for kt in range(4)]
    base = 13 if qi == 14 else 17
    return [(kt, base + (kt - 12)) for kt in range(12, 16)]


def rope_tables(rot_dim):
    n_freq = rot_dim // 4
    inv = np.float32(10000.0) ** (-np.arange(n_freq, dtype=np.float32) / np.float32(n_freq))
    t = np.arange(T, dtype=np.int32)
    row = (t // 64).astype(np.float32)
    col = (t % 64).astype(np.float32)
    ang = np.concatenate([row[:, None] * inv, col[:, None] * inv], axis=-1).astype(np.float32)
    return np.cos(ang).astype(np.float32), np.sin(ang).astype(np.float32)


class Builder:
    def __init__(self, cfg):
        self.cfg = cfg
        nc = self.nc = bass.Bass("TRN2", target_bir_lowering=False)
        self.es = ExitStack()
        self.P = Prog(nc)
        self.dbg_outs = []
        self._dram()
        self.arena = self.es.enter_context(nc.sbuf_tensor("arena", [128, ARENA_BYTES // 4], F32))
        self.psall = self.es.enter_context(nc.psum_tensor("psall", [128, 4096], F32))
        self.psb = [self.psall[:, i * 512:(i + 1) * 512] for i in range(8)]
        self.R = Region(R_OFF, R_SIZE)
        self.R2 = Region(R2_OFF, R2_SIZE)
        self.HTR = Region(HT_OFF, 36864)
        self.PERS = Region(PERS_OFF, ARENA_BYTES - PERS_OFF)
        self.unit_ctr = 0
        self.ot_ctr = 0
        self.op_ctr = 0
        self._persistent()

    def _dram(self):
        nc = self.nc

        def inp(name, shape, dt=F32):
            return nc.dram_tensor(name, list(shape), dt, kind="ExternalInput").ap()
        self.d = d = {}
        d["x2"] = inp("x2", [SPC, T, D])
        d["ctx2"] = inp("ctx2", [SPC, TC, D])
        d["cvec"] = inp("cvec", [3, D])
        d["ada_w"] = inp("ada_w", [4, D, 6 * D])
        d["ada_b"] = inp("ada_b", [4, 6 * D])
        d["norm1_w"] = inp("norm1_w", [4, D])
        d["norm2_w"] = inp("norm2_w", [4, D])
        d["a_wqkv"] = inp("a_wqkv", [2, D, 1536])
        d["a_qnorm"] = inp("a_qnorm", [2, 64])
        d["a_knorm"] = inp("a_knorm", [2, 64])
        d["a_wo"] = inp("a_wo", [2, D, D])
        d["b_wdq"] = inp("b_wdq", [1, D, 384])
        d["b_qnorm_lat"] = inp("b_qnorm_lat", [1, 384])
        d["b_wuq"] = inp("b_wuq", [1, 384, 1536])
        d["b_wdkv"] = inp("b_wdkv", [1, D, 288])
        d["b_kvnorm_lat"] = inp("b_kvnorm_lat", [1, 256])
        d["b_wukv"] = inp("b_wukv", [1, 256, 2048])
        d["b_qnorm"] = inp("b_qnorm", [1, 96])
        d["b_knorm"] = inp("b_knorm", [1, 96])
        d["b_wo"] = inp("b_wo", [1, D, D])
        d["c_wqkv"] = inp("c_wqkv", [1, D, 3072])
        d["c_qnorm"] = inp("c_qnorm", [1, 64])
        d["c_knorm"] = inp("c_knorm", [1, 64])
        d["c_wo"] = inp("c_wo", [1, D, D])
        d["moe_router"] = inp("moe_router", [4, D, NE])
        d["moe_wg"] = inp("moe_wg", [4, NE, D, D])
        d["moe_wu"] = inp("moe_wu", [4, NE, D, D])
        d["moe_wd"] = inp("moe_wd", [4, NE, D, D])
        d["cst"] = inp("cst", [128, 388])
        d["selc"] = inp("selc", [16, 2048], BF16)
        d["cosA"] = inp("cosA", [T, 32])
        d["sinA"] = inp("sinA", [T, 32])
        d["cosB"] = inp("cosB", [T, 16])
        d["sinB"] = inp("sinB", [T, 16])
        d["na_bias"] = inp("na_bias", [16, 128, 21 * 128])
        d["na_mask"] = inp("na_mask", [128, 21 * 128])
        self.y2 = nc.dram_tensor("y2", [SPC, T, D], F32, kind="ExternalOutput").ap()

    def dbg_out(self, name, shape, dt=F32):
        ap = self.nc.dram_tensor("dbg_" + name, list(shape), dt, kind="ExternalOutput").ap()
        self.dbg_outs.append("dbg_" + name)
        return ap

    def carve(self, name, off, shape, dt):
        n = int(np.prod(shape[1:]))
        nbytes = n * _dsize(dt)
        assert off % 4 == 0 and nbytes % 4 == 0, (name, off, nbytes)
        assert off + nbytes <= ARENA_BYTES, (name, off, nbytes)
        ap = self.arena[0:shape[0], off // 4:(off + nbytes) // 4]
        if dt != F32:
            ap = ap.bitcast(dt)
        if len(shape) == 3:
            ap = ap.rearrange("p (a b) -> p a b", b=shape[2])
        elif len(shape) == 4:
            ap = ap.rearrange("p (a b c) -> p a b c", b=shape[2], c=shape[3])
        elif len(shape) == 5:
            ap = ap.rearrange("p (a b c d) -> p a b c d", b=shape[2], c=shape[3], d=shape[4])
        return Buf(name, ap)

    def alloc(self, region, name, shape, dt):
        n = int(np.prod(shape[1:])) * _dsize(dt)
        return self.carve(name, region.take(n), shape, dt)

    def psbf(self, i):
        return self.psall[:, i * 512:(i + 1) * 512].bitcast(BF16)

    def mm(self, out, lhsT, rhs, start=True, stop=True, R=(), W=()):
        self.P.op("pe", lambda e: e.matmul(out, lhsT, rhs, start=start, stop=stop), R, W)

    def tr(self, out, in_, ident, R=(), W=()):
        self.P.op("pe", lambda e: e.transpose(out, in_, ident), R, W)

    def act(self, out, in_, func, bias=None, scale=None, accum=None, R=(), W=()):
        kw = {}
        if bias is not None:
            kw["bias"] = bias
        if scale is not None:
            kw["scale"] = scale
        if accum is not None:
            kw["accum_out"] = accum
        self.P.op("act", lambda e: e.activation(out=out, in_=in_, func=func, **kw), R, W)

    def tt(self, eng, out, in0, in1, op, R=(), W=()):
        self.P.op(eng, lambda e: e.tensor_tensor(out=out, in0=in0, in1=in1, op=op), R, W)

    def ts(self, eng, out, in0, s1, op0, s2=None, op1=None, R=(), W=()):
        if op1 is None:
            self.P.op(eng, lambda e: e.tensor_scalar(out=out, in0=in0, scalar1=s1, scalar2=None, op0=op0), R, W)
        else:
            self.P.op(eng, lambda e: e.tensor_scalar(out=out, in0=in0, scalar1=s1, scalar2=s2, op0=op0, op1=op1), R, W)

    def stt(self, out, in0, scalar, in1, op0, op1, R=(), W=()):
        self.P.op("dve", lambda e: e.scalar_tensor_tensor(out=out, in0=in0, scalar=scalar, in1=in1, op0=op0, op1=op1), R, W)

    def cp(self, eng, out, in_, R=(), W=()):
        if eng == "act":
            self.P.op("act", lambda e: e.activation(out=out, in_=in_, func=AF.Copy), R, W)
        else:
            self.P.op(eng, lambda e: e.tensor_copy(out=out, in_=in_), R, W)

    def red(self, out, in_, op, R=(), W=()):
        self.P.op("dve", lambda e: e.tensor_reduce(out=out, in_=in_, axis=AX.X, op=op), R, W)

    def memset(self, eng, ap, val, R=(), W=()):
        self.P.op(eng, lambda e: e.memset(ap, val), R, W)

    def dma(self, queue, slot, out, in_, R=(), W=(), group=False, bar=True, **kw):
        self.P.dma(queue, slot, out, in_, R, W, group=group, bar=bar, **kw)

    def _persistent(self):
        A = self.alloc
        PR = self.PERS
        self.xT = self.carve("xT", XT_OFF, [128, 8, TT], F32)
        self.ring = [self.carve("ring%d" % i, RING_OFF + i * UNIT, [128, 8, 512], BF16) for i in range(NUNIT)]
        self.cstb = A(PR, "cstb", [128, 388], F32)
        self.identf = self.cstb[:, 0:128]
        self.iotac = self.cstb[:, 128:384]
        self.misc = self.cstb[:, 384:388]
        self.identb = A(PR, "identb", [128, 128], BF16)
        self.onesb = A(PR, "onesb", [128, 128], BF16)
        self.sel = A(PR, "sel", [16, 16, 128], BF16)
        self.modv = A(PR, "modv", [128, 4, 3, 6, 8], F32)
        self.cosA = A(PR, "cosA", [128, 16, 32], F32)
        self.sinA = A(PR, "sinA", [128, 16, 32], F32)
        self.cosB = A(PR, "cosB", [128, 16, 16], F32)
        self.sinB = A(PR, "sinB", [128, 16, 16], F32)
        self.smallp = A(PR, "smallp", [128, 64], F32)

    def prologue(self):
        d = self.d
        self.dma("sp", "c0", self.cstb[:, :], d["cst"][:, :], W=[("cst",)])
        self.dma("sp", "c0", self.sel[:, :, :], d["selc"].rearrange("k (e m) -> k e m", m=128), W=[("sel",)], group=True)
        for nm, buf in (("cosA", self.cosA), ("sinA", self.sinA), ("cosB", self.cosB), ("sinB", self.sinB)):
            self.dma("sp", "c0", buf[:, :, :], d[nm].rearrange("(t p) n -> p t n", p=128), W=[(nm,)], group=True)
        self.cp("dve", self.identb[:, :], self.identf, R=[("cst",)], W=[("identb",)])
        self.memset("pool", self.onesb[:, :], 1.0, W=[("onesb",)])
        R = self.R
        R.reset()
        vr = self.alloc(R, "vr", [128, 128], F32)
        vr2 = self.alloc(R, "vr2", [128, 128], F32)
        vr3 = self.alloc(R, "vr3", [128, 128], F32)
        vT = self.alloc(R, "vT", [128, 88], F32)
        abT = self.alloc(R, "abT", [128, 192], F32)
        sT = self.alloc(R, "sT", [128, 8, 4], F32)
        modraw = self.alloc(R, "modraw", [128, 4, 48, 3], F32)
        wslot = [self.carve("adaw0", HT_OFF, [128, 8, 768], F32),
                 self.alloc(R, "adaw1", [128, 8, 768], F32)]
        self.dma("sp", "p0", vr[0:24, :], d["cvec"].rearrange("s (c p) -> (s c) p", p=128), W=[("vr",)])
        self.dma("sp", "p0", vr[24:56, :], d["norm1_w"].rearrange("l (c p) -> (l c) p", p=128), W=[("vr",)], group=True)
        self.dma("sp", "p0", vr[56:88, :], d["norm2_w"].rearrange("l (c p) -> (l c) p", p=128), W=[("vr",)], group=True)
        abrows = d["ada_b"].rearrange("l (k p) -> (l k) p", p=128)
        self.dma("sp", "p0", vr2[:, :], abrows[0:128, :], W=[("vr2",)], group=True)
        self.dma("sp", "p0", vr3[0:64, :], abrows[128:192, :], W=[("vr3",)], group=True)
        ps = self.psb[0]
        self.tr(ps[:, 0:88], vr[0:88, :], self.identf[0:88, 0:88], R=[("vr",), ("cst",)], W=[("ps", 0)])
        self.cp("dve", vT[:, :], ps[:, 0:88], R=[("ps", 0)], W=[("vT",)])
        ps1 = self.psb[1]
        self.tr(ps1[:, 0:128], vr2[:, :], self.identf, R=[("vr2",), ("cst",)], W=[("ps", 1)])
        self.tr(ps1[:, 128:192], vr3[0:64, :], self.identf[0:64, 0:64], R=[("vr3",), ("cst",)], W=[("ps", 1)])
        self.cp("dve", abT[:, :], ps1[:, 0:192], R=[("ps", 1)], W=[("abT",)])
        self.memset("dve", sT[:, :, :], 0.0, W=[("sT",)])
        cTv = vT[:, 0:24].rearrange("p (s c) -> p c s", c=8)
        self.act(sT[:, :, 0:3], cTv, AF.Silu, R=[("vT",)], W=[("sT",)])
        n1 = vT[:, 24:56].rearrange("p (l c) -> p l c", c=8)
        n2 = vT[:, 56:88].rearrange("p (l c) -> p l c", c=8)
        for l in range(4):
            mps = self.psb[2 + (l % 2)]
            for cb in range(8):
                u = l * 8 + cb
                w = wslot[u % 2]
                self.dma("sp", "aw%d" % (u % 2), w[:, :, :],
                         d["ada_w"][l].rearrange("(c p) f -> p c f", p=128)[:, :, cb * 768:(cb + 1) * 768],
                         W=[("adaw", u % 2)])
                for fc in range(6):
                    col = (cb * 6 + fc) * 4
                    for dc in range(8):
                        self.mm(mps[:, col:col + 4], w[:, dc, fc * 128:(fc + 1) * 128], sT[:, dc, :],
                                start=(dc == 0), stop=(dc == 7),
                                R=[("adaw", u % 2), ("sT",)], W=[("ps", 2 + (l % 2))])
            mv = mps[:, 0:192].rearrange("p (k s) -> p k s", s=4)[:, :, 0:3]
            bv = abT[:, l * 48:(l + 1) * 48].unsqueeze(2).to_broadcast([128, 48, 3])
            self.tt("dve", modraw[:, l, :, :], mv, bv, ALU.add, R=[("ps", 2 + (l % 2)), ("abT",)], W=[("modraw", l)])
            mr = modraw[:, l, :, :].rearrange("p (k c) s -> p k s c", c=8)
            for kind, k in ((1, 0), (2, 2), (4, 3), (5, 5)):
                self.cp("dve", self.modv[:, l, :, kind, :], mr[:, k, :, :], R=[("modraw", l)], W=[("modv",)])
            for kind, k, nn in ((0, 1, n1), (3, 4, n2)):
                nb = nn[:, l, :].unsqueeze(1).to_broadcast([128, 3, 8])
                self.stt(self.modv[:, l, :, kind, :], mr[:, k, :, :], 1.0, nb, ALU.add, ALU.mult,
                         R=[("modraw", l), ("vT",)], W=[("modv",)])
        self.P.barrier()

    def mod(self, l, s, kind, c=None):
        if c is None:
            return self.modv[:, l, s, kind, :]
        return self.modv[:, l, s, kind, c:c + 1]

    def load_sample(self, b):
        R = self.R
        R.reset()
        stg = [self.alloc(R, "stg%d" % i, [128, D], F32) for i in range(2)]
        for t in range(NT):
            src = self.d["x2"][b, t * 128:(t + 1) * 128, :] if t < 16 else self.d["ctx2"][b, (t - 16) * 128:(t - 15) * 128, :]
            s = stg[t % 2]
            self.dma("sp", "ld%d" % (t % 2), s[:, :], src, W=[("stg", t % 2)])
            for half in range(2):
                bank = (t % 2) * 2 + half
                ps = self.psb[bank]
                for j in range(4):
                    c = half * 4 + j
                    self.tr(ps[:, j * 128:(j + 1) * 128], s[:, c * 128:(c + 1) * 128], self.identf,
                            R=[("stg", t % 2), ("cst",)], W=[("ps", bank)])
                self.cp("act" if half == 0 else "dve", self.xT[:, half * 4:(half + 1) * 4, t * 128:(t + 1) * 128],
                        ps[:, :].rearrange("p (j n) -> p j n", n=128),
                        R=[("ps", bank)], W=[("xT", min(t // 4, 4), half * 4 + j) for j in range(4)])
        self.P.barrier()

    def store_sample(self, dst, ntiles):
        R = self.R
        R.reset()
        stg = [self.alloc(R, "ostg%d" % i, [128, D], F32) for i in range(2)]
        for t in range(ntiles):
            s = stg[t % 2]
            for half in range(2):
                bank = (t % 2) * 2 + half
                ps = self.psb[bank]
                for j in range(4):
                    c = half * 4 + j
                    self.tr(ps[:, j * 128:(j + 1) * 128], self.xT[:, c, t * 128:(t + 1) * 128], self.identf,
                            R=[("xT", min(t // 4, 4), c), ("cst",)], W=[("ps", bank)])
                self.cp("act" if half == 0 else "dve", s[:, half * 512:(half + 1) * 512], ps[:, :],
                        R=[("ps", bank)], W=[("ostg", t % 2)])
            self.dma("sp", "st%d" % (t % 2), dst[t * 128:(t + 1) * 128, :], s[:, :], R=[("ostg", t % 2)])
        self.P.barrier()

    def norm_bufs(self):
        R = self.R
        self.sq = [self.alloc(R, "sq%d" % i, [128, 8, 512], BF16) for i in range(2)]
        self.lnv = self.alloc(R, "lnv", [128, 512], F32)
        self.rstd = [self.alloc(R, "rstd%d" % i, [128, 512], F32) for i in range(2)]
        self.tmpn = [self.alloc(R, "tmpn%d" % i, [128, 512], F32) for i in range(2)]

    def norm_group(self, gi, l, b, which, dst_of):
        t0, w = GROUPS[gi]
        s = b if gi < 4 else 2
        ka, kb = (0, 1) if which == 1 else (3, 4)
        sq = self.sq[gi % 2]
        self.act(sq[:, :, 0:w], self.xT[:, :, t0:t0 + w], AF.Square, R=[("xT", gi, c) for c in range(8)], W=[("sq", gi % 2)])
        bank = 5 + (gi % 2)
        ps = self.psb[bank]
        for c in range(8):
            self.mm(ps[:, 0:w], self.onesb[:, :], sq[:, c, 0:w], start=(c == 0), stop=(c == 7),
                    R=[("sq", gi % 2), ("onesb",)], W=[("ps", bank)])
        self.act(self.lnv[:, 0:w], ps[:, 0:w], AF.Ln, bias=self.misc[:, 2:3], scale=1.0 / D,
                 R=[("ps", bank), ("cst",)], W=[("lnv",)])
        rstd = self.rstd[gi % 2]
        self.act(rstd[:, 0:w], self.lnv[:, 0:w], AF.Exp, scale=-0.5, R=[("lnv",)], W=[("rstd", gi % 2)])
        for c in range(8):
            tm = self.tmpn[c % 2]
            self.tt("dve", tm[:, 0:w], self.xT[:, c, t0:t0 + w], rstd[:, 0:w], ALU.mult,
                    R=[("xT", gi, c), ("rstd", gi % 2)], W=[("tmpn", c % 2)])
            dst, dres = dst_of(c)
            self.act(dst, tm[:, 0:w], AF.Identity, bias=self.mod(l, s, kb, c), scale=self.mod(l, s, ka, c),
                     R=[("tmpn", c % 2), ("modv",)], W=[dres])

    def attn_core(self, steps_cfg, scale):
        steps = []
        for hi, hc in enumerate(steps_cfg):
            kts = hc["ktiles"]
            npair = (len(kts) + 1) // 2
            for pi in range(npair):
                ks = kts[2 * pi:2 * pi + 2]
                steps.append((hi, hc, ks, pi == 0, pi == npair - 1))
        n = len(steps)

        def qk(i):
            hi, hc, ks, first, last = steps[i]
            s_ = i % 2
            for j, k in enumerate(ks):
                bank = 2 * s_ + j
                self.mm(self.psb[bank][:, 0:hc["w"]], hc["kt"][:, k * 128:(k + 1) * 128], hc["qt"][:, hc["t0"]:hc["t0"] + hc["w"]],
                        R=[hc["kres"], hc["qres"]], W=[("ps", bank)])

        def ex(i):
            hi, hc, ks, first, last = steps[i]
            s_ = i % 2
            w = hc["w"]
            nk = len(ks)
            src = self.psall[:, s_ * 1024:s_ * 1024 + nk * 512].rearrange("p (b n) -> p b n", n=512)[:, :, 0:w]
            dst = self.PT[:, s_, 0:nk * 512].rearrange("p (b n) -> p b n", n=512)[:, :, 0:w]
            self.act(dst, src, AF.Exp, scale=scale, R=[("ps", 2 * s_ + j) for j in range(nk)], W=[("pt", s_)])

        def pv(i):
            hi, hc, ks, first, last = steps[i]
            w = hc["w"]
            s_ = i % 2
            if hc["hs"] == 0:
                self.take_op(hc["gi"], 8 if last else 1)
            ob = 4 + (hi + self.ot_ctr) % 2
            for j, k in enumerate(ks):
                self.mm(self.psb[ob][:, 0:w], hc["va"](k), self.PT[:, s_, j * 512:j * 512 + w],
                        start=(first and j == 0), stop=(last and j == len(ks) - 1),
                        R=[("pt", s_), hc["vres"]], W=[("ps", ob)])
            if last:
                lo, hi_ = (slice(0, 64), slice(64, 128))
                o_sl, d_sl = (lo, hi_) if hc["o_lo"] else (hi_, lo)
                rec_o = self.rec[d_sl, 0:w]
                rec_i = self.psb[ob][d_sl, 0:w]
                self.P.op("dve", lambda e: e.reciprocal(out=rec_o, in_=rec_i), [("ps", ob)], [("rec",)])
                self.tt("dve", hc["att"][o_sl, hc["t0"]:hc["t0"] + w], self.psb[ob][o_sl, 0:w], self.rec[d_sl, 0:w], ALU.mult,
                        R=[("ps", ob), ("rec",)], W=[("attT", hc["gi"])])

        if n == 0:
            return
        qk(0)
        for i in range(n):
            if i + 1 < n:
                qk(i + 1)
            ex(i)
            pv(i)
        self.ot_ctr += len(steps_cfg)

    def out_proj(self, wo, wres, att, ares, l, b, need_ctx):
        self.flush_op()
        ng = 5 if need_ctx else 4
        pend = {}
        for gi in range(ng):
            t0, w = GROUPS[gi]
            s = b if gi < 4 else 2
            lst = []
            for dc in range(8):
                def blk(gi=gi, dc=dc, t0=t0, w=w, s=s):
                    bank = 6 + (self.op_ctr % 2)
                    self.op_ctr += 1
                    ps = self.psb[bank]
                    self.mm(ps[:, 0:w], wo[:, dc * 128:(dc + 1) * 128], att[:, t0:t0 + w], R=[wres, ("attT", gi)], W=[("ps", bank)])
                    xs = self.xT[:, dc, t0:t0 + w]
                    self.stt(xs, ps[:, 0:w], self.mod(l, s, 2, dc), xs, ALU.mult, ALU.add,
                             R=[("ps", bank), ("modv",), ("xT", gi, dc)], W=[("xT", gi, dc)])
                lst.append(blk)
            pend[gi] = lst
        self.pending_op = pend

    def flush_op(self, gi=None):
        pend = getattr(self, "pending_op", None)
        if not pend:
            return
        keys = sorted(pend.keys()) if gi is None else ([gi] if gi in pend else [])
        for k in keys:
            for blk in pend.pop(k):
                blk()

    def take_op(self, gi, n):
        pend = getattr(self, "pending_op", None)
        if not pend or gi not in pend:
            return
        lst = pend[gi]
        for _ in range(min(n, len(lst))):
            lst.pop(0)()
        if not lst:
            pend.pop(gi)

    def head_rstd(self, src3, nh, hd, sqt, ssq, lnv, rs, res_src):
        self.tt("dve", sqt, src3, src3, ALU.mult, R=[res_src], W=[("sqt",)])
        self.red(ssq[:, 0:nh], sqt, ALU.add, R=[("sqt",)], W=[("ssq",)])
        self.act(lnv[:, 0:nh], ssq[:, 0:nh], AF.Ln, bias=self.misc[:, 2:3], scale=1.0 / hd, R=[("ssq",), ("cst",)], W=[("lnvh",)])
        self.act(rs[:, 0:nh], lnv[:, 0:nh], AF.Exp, scale=-0.5, R=[("lnvh",)], W=[("rsh",)])

    def bcast_row(self, queue, slot, dst, src_row, n, W, group=False):
        self.dma(queue, slot, dst, src_row.unsqueeze(0).to_broadcast([128, n]), W=W, group=group)

    def gqa_layer(self, l, b, need_ctx):
        d = self.d
        j = l // 3
        R, R2 = self.R, self.R2
        R.reset()
        R2.reset()
        self.norm_bufs()
        hT = self.carve("hT", HT_OFF, [128, 8, TT], BF16)
        for gi in range(5):
            t0, w = GROUPS[gi]
            self.norm_group(gi, l, b, 1, lambda c, t0=t0, w=w, gi=gi: (hT[:, c, t0:t0 + w], ("hT", gi)))
        self.P.barrier()
        R.reset()
        QKT = self.alloc(R, "QKT", [128, 5, TT], BF16)
        VA = self.alloc(R, "VA", [128, NT, 192], BF16)
        attT = self.alloc(R, "attT", [128, TT], BF16)
        Wg = self.alloc(R2, "Wg", [128, 8, 384], BF16)
        WoP = self.alloc(R2, "WoP", [128, 2, D], BF16)
        self.PT = self.alloc(R, "PT", [128, 2, 1024], BF16)
        self.rec = self.alloc(R, "rec", [128, 512], F32)
        gq = self.alloc(R, "gq", [128, 5, 64], F32)
        raw = [self.alloc(R2, "raw%d" % i, [128, 384], F32) for i in range(3)]
        self.memset("pool", QKT[64:128, 0:2, :], 0.0, W=[("QKTz",)])
        self.memset("pool", QKT[0:64, 2:4, :], 0.0, W=[("QKTz",)])
        sqt = self.alloc(R2, "sqt", [128, 5, 64], F32)
        t1 = self.alloc(R2, "t1", [128, 5, 64], F32)
        t2 = self.alloc(R2, "t2", [128, 5, 64], F32)
        ra = self.alloc(R2, "ra", [128, 5, 32], F32)
        rb = self.alloc(R2, "rb", [128, 5, 32], F32)
        rc = self.alloc(R2, "rc", [128, 5, 32], F32)
        rd = self.alloc(R2, "rd", [128, 5, 32], F32)
        qkb = [self.alloc(R2, "qkb%d" % i, [128, 6, 64], BF16) for i in range(3)]
        ssq = [self.smallp[:, 0:8], self.smallp[:, 8:16]]
        lnv = [self.smallp[:, 16:24], self.smallp[:, 24:32]]
        rs = [self.smallp[:, 32:40], self.smallp[:, 40:48]]
        for i in range(4):
            self.bcast_row("sp", "gq", gq[:, i, :], d["a_qnorm"][j], 64, W=[("gq",)], group=(i > 0))
        self.bcast_row("sp", "gq", gq[:, 4, :], d["a_knorm"][j], 64, W=[("gq",)], group=True)
        self.memset("pool", VA[:, :, 0:64], 1.0, W=[("VA", t) for t in range(NT)])
        self.memset("pool", VA[:, :, 128:192], 1.0, W=[("VA", t) for t in range(NT)])
        wq = d["a_wqkv"][j].rearrange("(c p) f -> p c f", p=128)
        ptb = self.psbf(7)
        ntl = NT
        for g in range(4):
            self.dma("pool", "wg", Wg[:, :, 0:256], wq[:, :, 256 * g:256 * g + 256], W=[("Wg",)], bar=False)
            self.dma("pool", "wg", Wg[:, :, 256:320], wq[:, :, 1024 + 64 * g:1024 + 64 * g + 64], W=[("Wg",)], group=True, bar=False)
            self.dma("pool", "wg", Wg[:, :, 320:384], wq[:, :, 1280 + 64 * g:1280 + 64 * g + 64], W=[("Wg",)], group=True, bar=False)
            def S1(t):
                bank = 5 + (t % 2)
                ps = self.psb[bank]
                for c in range(8):
                    self.mm(ps[:, 0:384], hT[:, c, t * 128:(t + 1) * 128], Wg[:, c, :], start=(c == 0), stop=(c == 7),
                            R=[("hT", min(t // 4, 4)), ("Wg",)], W=[("ps", bank)])
                self.cp("act", raw[t % 3][:, :], ps[:, 0:384], R=[("ps", bank)], W=[("raw", t % 3)])

            def S2(t):
                rw = raw[t % 3]
                r3 = rw[:, 0:320].rearrange("p (h e) -> p h e", e=64)
                for h in range(5):
                    self.act(sqt[:, h, :], r3[:, h, :], AF.Square, scale=0.125, accum=ssq[t % 2][:, h:h + 1],
                             R=[("raw", t % 3)], W=[("sqt",), ("ssq", t % 2)])

            def S3(t):
                self.act(lnv[t % 2][:, 0:5], ssq[t % 2][:, 0:5], AF.Ln, bias=self.misc[:, 2:3], scale=1.0,
                         R=[("ssq", t % 2), ("cst",)], W=[("lnvh", t % 2)])
                self.act(rs[t % 2][:, 0:5], lnv[t % 2][:, 0:5], AF.Exp, scale=-0.5, R=[("lnvh", t % 2)], W=[("rsh", t % 2)])

            def S4(t):
                rw = raw[t % 3]
                r3 = rw[:, 0:320].rearrange("p (h e) -> p h e", e=64)
                self.tt("dve", t1[:, :, :], r3, rs[t % 2][:, 0:5].unsqueeze(2).to_broadcast([128, 5, 64]), ALU.mult,
                        R=[("raw", t % 3), ("rsh", t % 2)], W=[("t1",)])
                self.tt("dve", t2[:, :, :], t1[:, :, :], gq[:, :, :], ALU.mult, R=[("t1",), ("gq",)], W=[("t2",)])
                qb = qkb[t % 3]
                if t < 16:
                    x1 = t2[:, :, 0:32]
                    x2 = t2[:, :, 32:64]
                    cs = self.cosA[:, t, :].unsqueeze(1).to_broadcast([128, 5, 32])
                    sn = self.sinA[:, t, :].unsqueeze(1).to_broadcast([128, 5, 32])
                    self.tt("dve", ra[:, :, :], x1, cs, ALU.mult, R=[("t2",), ("cosA",)], W=[("ra",)])
                    self.tt("dve", rb[:, :, :], x2, sn, ALU.mult, R=[("t2",), ("sinA",)], W=[("rb",)])
                    self.tt("dve", qb[:, 0:5, 0:32], ra[:, :, :], rb[:, :, :], ALU.subtract, R=[("ra",), ("rb",)], W=[("qkb", t % 3)])
                    self.tt("dve", rc[:, :, :], x1, sn, ALU.mult, R=[("t2",), ("sinA",)], W=[("rc",)])
                    self.tt("dve", rd[:, :, :], x2, cs, ALU.mult, R=[("t2",), ("cosA",)], W=[("rd",)])
                    self.tt("dve", qb[:, 0:5, 32:64], rc[:, :, :], rd[:, :, :], ALU.add, R=[("rc",), ("rd",)], W=[("qkb", t % 3)])
                else:
                    self.cp("dve", qb[:, 0:5, :], t2[:, :, :], R=[("t2",)], W=[("qkb", t % 3)])
                self.cp("dve", qb[:, 5, :], qb[:, 4, :], R=[("qkb", t % 3)], W=[("qkb", t % 3)])
                self.cp("pool", VA[:, t, 64:128], rw[:, 320:384], R=[("raw", t % 3)], W=[("VA", t)])

            def S5(t):
                qb = qkb[t % 3]
                qf = qb[:, :, :].rearrange("p h e -> p (h e)")
                for i in range(3):
                    self.tr(ptb[:, i * 128:(i + 1) * 128], qf[:, i * 128:(i + 1) * 128], self.identb[:, :],
                            R=[("qkb", t % 3), ("identb",)], W=[("ps", 7)])
                self.cp("act", QKT[0:64, 0:2, t * 128:(t + 1) * 128], ptb[0:64, 0:256].rearrange("p (i n) -> p i n", n=128),
                        R=[("ps", 7)], W=[("QKT", t)])
                self.cp("act", QKT[64:128, 2:4, t * 128:(t + 1) * 128], ptb[64:128, 0:256].rearrange("p (i n) -> p i n", n=128),
                        R=[("ps", 7)], W=[("QKT", t)])
                self.cp("dve", QKT[:, 4, t * 128:(t + 1) * 128], ptb[:, 256:384], R=[("ps", 7)], W=[("QKT", t)])

            self.pipeline(ntl, S1, S2, S3, S4, S5)
            for p in range(2):
                slot = (g * 2 + p) % 2
                h0 = 4 * g + 2 * p
                self.dma("pool", "wo%d" % slot, WoP[:, slot, :], d["a_wo"][j][h0 * 64:h0 * 64 + 128, :], W=[("WoP", slot)], bar=False)
                cfgs = []
                qgroups = [0, 1, 2, 3] + ([4] if need_ctx else [])
                for gi in qgroups:
                    t0, w = GROUPS[gi]
                    ktiles = list(range(NT)) if gi < 4 else [16, 17]
                    for hs in range(2):
                        psl = slice(0, 64) if hs == 0 else slice(64, 128)
                        cfgs.append(dict(
                            qt=QKT[:, p + 2 * hs, :], kt=QKT[:, 4, :],
                            va=(lambda k, hs=hs: VA[:, k, 64:192] if hs == 0 else VA[:, k, 0:128]),
                            o_lo=(hs == 0), t0=t0, w=w, gi=gi, hs=hs, ktiles=ktiles, att=attT,
                            qres=("QKTall",), kres=("QKTall",), vres=("VAall",), ares=("attT",)))
                self._alias([("QKT", t) for t in range(ntl)] + [("QKTz",)], ("QKTall",))
                self._alias([("VA", t) for t in range(ntl)], ("VAall",))
                self.attn_core(cfgs, 0.125)
                self.out_proj(WoP[:, slot, :], ("WoP", slot), attT, ("attT",), l, b, need_ctx)
            self._alias_release([("QKT", t) for t in range(ntl)], ("QKTall",))
            self._alias_release([("VA", t) for t in range(ntl)], ("VAall",))
        self.flush_op()
        self.P.barrier()

    def pipeline(self, n, S1, S2, S3, S4, S5):
        for i in range(-1, n + 2):
            if 0 <= i + 1 < n:
                S1(i + 1)
            if 0 <= i < n:
                S2(i)
                S3(i)
            if 0 <= i - 1 < n:
                S4(i - 1)
            if 0 <= i - 2 < n:
                S5(i - 2)

    def _alias(self, fine, coarse):
        P = self.P
        toks = set()
        for f in fine:
            st = P.res.get(f)
            if st is not None and st["w"] is not None:
                toks.add(st["w"])
        P.res[coarse] = {"w": None, "r": {}, "ws": toks}
        for e in ("pe",):
            P.pending[e] |= toks

    def _alias_release(self, fine, coarse):
        P = self.P
        st = P.res.get(coarse)
        if st is None:
            return
        for f in fine:
            fs = P.res.get(f)
            if fs is None:
                fs = P.res[f] = {"w": None, "r": {}}
            for k, v in st["r"].items():
                fs["r"][("al", coarse, k)] = v

    def mla_layer(self, l, b, need_ctx):
        d = self.d
        R, R2, HTR = self.R, self.R2, self.HTR
        R.reset()
        R2.reset()
        HTR.reset()
        self.norm_bufs()
        hT = self.carve("hT", HT_OFF, [128, 8, TT], BF16)
        for gi in range(5):
            t0, w = GROUPS[gi]
            self.norm_group(gi, l, b, 1, lambda c, t0=t0, w=w, gi=gi: (hT[:, c, t0:t0 + w], ("hT", gi)))
        self.P.barrier()
        R.reset()
        cT = self.alloc(R, "cT", [128, 5, TT], BF16)
        krope = self.alloc(R, "krope", [128, NT, 32], BF16)
        self.PT = self.alloc(R, "PT", [128, 2, 1024], BF16)
        self.rec = self.alloc(R, "rec", [128, 512], F32)
        gq2 = self.alloc(R, "gq2", [128, 2, 96], F32)
        gk2 = self.alloc(R, "gk2", [128, 2, 64], F32)
        gk = self.alloc(R, "gkr", [128, 32], F32)
        W1 = self.alloc(R, "W1", [128, 8, 672], BF16)
        g1 = self.alloc(R, "g1", [128, 640], F32)
        raw1 = [self.alloc(R2, "rawm%d" % i, [128, 672], F32) for i in range(2)]
        cn = [self.alloc(R2, "cn%d" % i, [128, 640], BF16) for i in range(2)]
        sq1 = self.alloc(R2, "sq1", [128, 384], F32)
        kr1 = self.alloc(R2, "kr1", [128, 32], F32)
        rt = [self.alloc(R2, "rt%d" % i, [128, 2, 16], F32) for i in range(4)]
        raw2 = [self.alloc(R2, "rawn%d" % i, [128, 448], F32) for i in range(3)]
        ssq2 = [self.smallp[:, 36:44], self.smallp[:, 44:52]]
        lnv2 = [self.smallp[:, 52:58], self.smallp[:, 58:64]]
        rs2 = [self.alloc(R2, "rs2_%d" % i, [128, 8], F32) for i in range(2)]
        sqt = self.alloc(R2, "sqt", [128, 2, 64], F32)
        t1 = self.alloc(R2, "t1", [128, 2, 96], F32)
        tq = self.alloc(R2, "tq", [128, 2, 32], F32)
        k1 = self.alloc(R2, "k1", [128, 2, 64], F32)
        qa = [self.alloc(R2, "qa%d" % i, [128, 2, 96], BF16) for i in range(3)]
        ka = [self.alloc(R2, "ka%d" % i, [128, 2, 96], BF16) for i in range(3)]
        ssq = self.smallp[:, 0:8]
        lnv = self.smallp[:, 8:16]
        rs = self.smallp[:, 16:24]
        ssq1 = self.smallp[:, 24:28]
        lnv1 = self.smallp[:, 28:32]
        rs1 = self.smallp[:, 32:36]
        self.bcast_row("sp", "gq", g1[:, 0:384], d["b_qnorm_lat"][0], 384, W=[("g1",)])
        self.bcast_row("sp", "gq", g1[:, 384:640], d["b_kvnorm_lat"][0], 256, W=[("g1",)], group=True)
        self.bcast_row("sp", "gq", gk[:, :], d["b_knorm"][0, 64:96], 32, W=[("gk",)], group=True)
        for i in range(2):
            self.bcast_row("sp", "gq", gq2[:, i, :], d["b_qnorm"][0], 96, W=[("gq2",)], group=True)
            self.bcast_row("sp", "gq", gk2[:, i, :], d["b_knorm"][0, 0:64], 64, W=[("gk2",)], group=True)
        self.dma("pool", "wg", W1[:, :, 0:384], d["b_wdq"][0].rearrange("(c p) f -> p c f", p=128), W=[("W1",)], bar=False)
        self.dma("pool", "wg", W1[:, :, 384:672], d["b_wdkv"][0].rearrange("(c p) f -> p c f", p=128), W=[("W1",)], group=True, bar=False)
        ptb = self.psbf(7)
        for t in range(NT):
            ba = 3 + 2 * (t % 2)
            bb = ba + 1
            for c in range(8):
                self.mm(self.psb[ba][:, :], hT[:, c, t * 128:(t + 1) * 128], W1[:, c, 0:512], start=(c == 0), stop=(c == 7),
                        R=[("hT", min(t // 4, 4)), ("W1",)], W=[("ps", ba)])
            for c in range(8):
                self.mm(self.psb[bb][:, 0:160], hT[:, c, t * 128:(t + 1) * 128], W1[:, c, 512:672], start=(c == 0), stop=(c == 7),
                        R=[("hT", min(t // 4, 4)), ("W1",)], W=[("ps", bb)])
            rw = raw1[t % 2]
            self.cp("act", rw[:, 0:512], self.psb[ba][:, :], R=[("ps", ba)], W=[("rawm", t % 2)])
            self.cp("act", rw[:, 512:672], self.psb[bb][:, 0:160], R=[("ps", bb)], W=[("rawm", t % 2)])
            for i, (c0, n) in enumerate(((0, 384), (384, 256), (640, 32))):
                self.act(sq1[:, 0:n], rw[:, c0:c0 + n], AF.Square, accum=ssq1[:, i:i + 1], R=[("rawm", t % 2)], W=[("sq1",), ("ssq1", i)])
                self.act(lnv1[:, i:i + 1], ssq1[:, i:i + 1], AF.Ln, bias=self.misc[:, 2:3], scale=1.0 / n, R=[("ssq1", i), ("cst",)], W=[("lnv1", i)])
            self.act(rs1[:, 0:3], lnv1[:, 0:3], AF.Exp, scale=-0.5, R=[("lnv1", 0), ("lnv1", 1), ("lnv1", 2)], W=[("rs1",)])
            cnb = cn[t % 2]
            self.stt(cnb[:, 0:384], rw[:, 0:384], rs1[:, 0:1], g1[:, 0:384], ALU.mult, ALU.mult, R=[("rawm", t % 2), ("rs1",), ("g1",)], W=[("cn", t % 2)])
            self.stt(cnb[:, 384:640], rw[:, 384:640], rs1[:, 1:2], g1[:, 384:640], ALU.mult, ALU.mult, R=[("rawm", t % 2), ("rs1",), ("g1",)], W=[("cn", t % 2)])
            self.stt(kr1[:, :], rw[:, 640:672], rs1[:, 2:3], gk[:, :], ALU.mult, ALU.mult, R=[("rawm", t % 2), ("rs1",), ("gk",)], W=[("kr1",)])
            if t < 16:
                x1 = kr1[:, 0:16]
                x2 = kr1[:, 16:32]
                cs = self.cosB[:, t, :]
                sn = self.sinB[:, t, :]
                self.tt("dve", rt[0][:, 0, :], x1, cs, ALU.mult, R=[("kr1",), ("cosB",)], W=[("rt", 0)])
                self.tt("dve", rt[1][:, 0, :], x2, sn, ALU.mult, R=[("kr1",), ("sinB",)], W=[("rt", 1)])
                self.tt("dve", krope[:, t, 0:16], rt[0][:, 0, :], rt[1][:, 0, :], ALU.subtract, R=[("rt", 0), ("rt", 1)], W=[("krope", t)])
                self.tt("dve", rt[2][:, 0, :], x1, sn, ALU.mult, R=[("kr1",), ("sinB",)], W=[("rt", 2)])
                self.tt("dve", rt[3][:, 0, :], x2, cs, ALU.mult, R=[("kr1",), ("cosB",)], W=[("rt", 3)])
                self.tt("dve", krope[:, t, 16:32], rt[2][:, 0, :], rt[3][:, 0, :], ALU.add, R=[("rt", 2), ("rt", 3)], W=[("krope", t)])
            else:
                self.cp("dve", krope[:, t, :], kr1[:, :], R=[("kr1",)], W=[("krope", t)])
            for i in range(5):
                self.tr(ptb[:, i * 128:(i + 1) * 128], cnb[:, i * 128:(i + 1) * 128], self.identb[:, :],
                        R=[("cn", t % 2), ("identb",)], W=[("ps", 7)])
            self.cp("dve", cT[:, :, t * 128:(t + 1) * 128], ptb[:, 0:640].rearrange("p (i n) -> p i n", n=128),
                    R=[("ps", 7)], W=[("cT", t)])
        self.P.barrier()
        QKT = self.alloc(HTR, "QKT", [128, 4, TT], BF16)
        VA = self.alloc(HTR, "VA", [128, NT, 192], BF16)
        attT = self.alloc(HTR, "attT", [128, TT], BF16)
        W2q = self.alloc(HTR, "W2q", [128, 3, 192], BF16)
        W2kv = self.alloc(HTR, "W2kv", [128, 2, 256], BF16)
        WoP = self.alloc(HTR, "WoP", [128, 2, D], BF16)
        self.memset("pool", VA[:, :, 64:128], 1.0, W=[("VA", t) for t in range(NT)])
        wuq = d["b_wuq"][0].rearrange("(c p) f -> p c f", p=128)
        wukv = d["b_wukv"][0].rearrange("(c p) f -> p c f", p=128)
        sc = 96.0 ** -0.5
        for pp in range(8):
            self.dma("pool", "wg", W2q[:, :, :], wuq[:, :, pp * 192:(pp + 1) * 192], W=[("W2",)], bar=False)
            self.dma("pool", "wg", W2kv[:, :, :], wukv[:, :, pp * 256:(pp + 1) * 256], W=[("W2",)], group=True, bar=False)
            def S1(t):
                bank = 5 + (t % 2)
                ps = self.psb[bank]
                for c in range(3):
                    self.mm(ps[:, 0:192], cT[:, c, t * 128:(t + 1) * 128], W2q[:, c, :], start=(c == 0), stop=(c == 2),
                            R=[("cT", t), ("W2",)], W=[("ps", bank)])
                for c in range(2):
                    self.mm(ps[:, 192:448], cT[:, 3 + c, t * 128:(t + 1) * 128], W2kv[:, c, :], start=(c == 0), stop=(c == 1),
                            R=[("cT", t), ("W2",)], W=[("ps", bank)])
                self.cp("act", raw2[t % 3][:, :], ps[:, 0:448], R=[("ps", bank)], W=[("rawn", t % 3)])

            def views(t):
                rw = raw2[t % 3]
                rq = rw[:, 0:192].rearrange("p (h e) -> p h e", e=96)
                rkv = rw[:, 192:448].rearrange("p (h e) -> p h e", e=128)
                return rw, rq, rkv

            def S2(t):
                rw, rq, rkv = views(t)
                sq = ssq2[t % 2]
                for i, (src, hd) in enumerate(((rq[:, :, 0:64], 64), (rq[:, :, 64:96], 32), (rkv[:, :, 0:64], 64))):
                    for h in range(2):
                        self.act(sqt[:, 0, 0:hd], src[:, h, :], AF.Square, scale=float(hd) ** -0.5, accum=sq[:, 2 * i + h:2 * i + h + 1],
                                 R=[("rawn", t % 3)], W=[("sqt",), ("ssq", t % 2)])

            def S3(t):
                self.act(lnv2[t % 2][:, 0:6], ssq2[t % 2][:, 0:6], AF.Ln, bias=self.misc[:, 2:3], scale=1.0,
                         R=[("ssq", t % 2), ("cst",)], W=[("lnvh", t % 2)])
                self.act(rs2[t % 2][:, 0:6], lnv2[t % 2][:, 0:6], AF.Exp, scale=-0.5, R=[("lnvh", t % 2)], W=[("rsh", t % 2)])

            def S4(t):
                rw, rq, rkv = views(t)
                rsv = rs2[t % 2]
                qab = qa[t % 3]
                kab = ka[t % 3]
                self.tt("dve", t1[:, :, 0:64], rq[:, :, 0:64], rsv[:, 0:2].unsqueeze(2).to_broadcast([128, 2, 64]), ALU.mult,
                        R=[("rawn", t % 3), ("rsh", t % 2)], W=[("t1",)])
                self.tt("dve", qab[:, :, 0:64], t1[:, :, 0:64], gq2[:, :, 0:64], ALU.mult, R=[("t1",), ("gq2",)], W=[("qa", t % 3)])
                self.tt("dve", t1[:, :, 64:96], rq[:, :, 64:96], rsv[:, 2:4].unsqueeze(2).to_broadcast([128, 2, 32]), ALU.mult,
                        R=[("rawn", t % 3), ("rsh", t % 2)], W=[("t1",)])
                if t < 16:
                    self.tt("dve", tq[:, :, :], t1[:, :, 64:96], gq2[:, :, 64:96], ALU.mult, R=[("t1",), ("gq2",)], W=[("tq",)])
                    x1 = tq[:, :, 0:16]
                    x2 = tq[:, :, 16:32]
                    cs = self.cosB[:, t, :].unsqueeze(1).to_broadcast([128, 2, 16])
                    sn = self.sinB[:, t, :].unsqueeze(1).to_broadcast([128, 2, 16])
                    self.tt("dve", rt[0][:, :, :], x1, cs, ALU.mult, R=[("tq",), ("cosB",)], W=[("rt", 0)])
                    self.tt("dve", rt[1][:, :, :], x2, sn, ALU.mult, R=[("tq",), ("sinB",)], W=[("rt", 1)])
                    self.tt("dve", qab[:, :, 64:80], rt[0][:, :, :], rt[1][:, :, :], ALU.subtract, R=[("rt", 0), ("rt", 1)], W=[("qa", t % 3)])
                    self.tt("dve", rt[2][:, :, :], x1, sn, ALU.mult, R=[("tq",), ("sinB",)], W=[("rt", 2)])
                    self.tt("dve", rt[3][:, :, :], x2, cs, ALU.mult, R=[("tq",), ("cosB",)], W=[("rt", 3)])
                    self.tt("dve", qab[:, :, 80:96], rt[2][:, :, :], rt[3][:, :, :], ALU.add, R=[("rt", 2), ("rt", 3)], W=[("qa", t % 3)])
                else:
                    self.tt("dve", qab[:, :, 64:96], t1[:, :, 64:96], gq2[:, :, 64:96], ALU.mult, R=[("t1",), ("gq2",)], W=[("qa", t % 3)])
                self.tt("dve", k1[:, :, :], rkv[:, :, 0:64], rsv[:, 4:6].unsqueeze(2).to_broadcast([128, 2, 64]), ALU.mult,
                        R=[("rawn", t % 3), ("rsh", t % 2)], W=[("k1",)])
                self.tt("dve", kab[:, :, 0:64], k1[:, :, :], gk2[:, :, :], ALU.mult, R=[("k1",), ("gk2",)], W=[("ka", t % 3)])
                self.cp("dve", kab[:, :, 64:96], krope[:, t, :].unsqueeze(1).to_broadcast([128, 2, 32]), R=[("krope", t)], W=[("ka", t % 3)])
                self.cp("pool", VA[:, t, 0:64], rkv[:, 0, 64:128], R=[("rawn", t % 3)], W=[("VA", t)])
                self.cp("pool", VA[:, t, 128:192], rkv[:, 1, 64:128], R=[("rawn", t % 3)], W=[("VA", t)])

            def S5(t):
                qab = qa[t % 3]
                kab = ka[t % 3]
                for i in range(2):
                    self.tr(ptb[0:96, i * 128:(i + 1) * 128], qab[:, i, :], self.identb[:, :], R=[("qa", t % 3), ("identb",)], W=[("ps", 7)])
                for i in range(2):
                    self.tr(ptb[0:96, (2 + i) * 128:(3 + i) * 128], kab[:, i, :], self.identb[:, :], R=[("ka", t % 3), ("identb",)], W=[("ps", 7)])
                self.cp("act", QKT[0:96, :, t * 128:(t + 1) * 128], ptb[0:96, 0:512].rearrange("p (i n) -> p i n", n=128),
                        R=[("ps", 7)], W=[("QKT", t)])

            self.pipeline(NT, S1, S2, S3, S4, S5)
            slot = pp % 2
            self.dma("pool", "wo%d" % slot, WoP[:, slot, :], d["b_wo"][0][pp * 128:(pp + 1) * 128, :], W=[("WoP", slot)], bar=False)
            cfgs = []
            qgroups = [0, 1, 2, 3] + ([4] if need_ctx else [])
            for gi in qgroups:
                t0, w = GROUPS[gi]
                ktiles = list(range(NT)) if gi < 4 else [16, 17]
                for hs in range(2):
                    cfgs.append(dict(
                        qt=QKT[0:96, hs, :], kt=QKT[0:96, 2 + hs, :],
                        va=(lambda k, hs=hs: VA[:, k, 0:128] if hs == 0 else VA[:, k, 64:192]),
                        o_lo=(hs == 0), t0=t0, w=w, gi=gi, hs=hs, ktiles=ktiles, att=attT,
                        qres=("QKTall",), kres=("QKTall",), vres=("VAall",), ares=("attT",)))
            self._alias([("QKT", t) for t in range(NT)], ("QKTall",))
            self._alias([("VA", t) for t in range(NT)], ("VAall",))
            self.attn_core(cfgs, sc)
            self.out_proj(WoP[:, slot, :], ("WoP", slot), attT, ("attT",), l, b, need_ctx)
            self._alias_release([("QKT", t) for t in range(NT)], ("QKTall",))
            self._alias_release([("VA", t) for t in range(NT)], ("VAall",))
        self.flush_op()
        self.P.barrier()

    def na_layer(self, l, b, need_ctx):
        d = self.d
        R, R2 = self.R, self.R2
        R.reset()
        R2.reset()
        self.norm_bufs()
        hT = self.carve("hT", HT_OFF, [128, 8, TT], BF16)
        for gi in range(5):
            t0, w = GROUPS[gi]
            self.norm_group(gi, l, b, 1, lambda c, t0=t0, w=w, gi=gi: (hT[:, c, t0:t0 + w], ("hT", gi)))
        self.P.barrier()
        R.reset()
        QKT = self.alloc(R, "QKT", [128, 3, TT], BF16)
        VA = self.alloc(R, "VA", [128, NT, 192], BF16)
        attT = self.alloc(R, "attT", [128, TT], BF16)
        Wp = self.alloc(R, "Wg", [128, 8, 384], BF16)
        WoP = self.alloc(R, "WoP", [128, 2, D], BF16)
        self.PT = self.alloc(R, "PT", [128, 2, 1024], BF16)
        self.rec = self.alloc(R2, "rec", [128, 256], F32)
        gq4 = self.alloc(R2, "gq4", [128, 4, 64], F32)
        maskb = self.alloc(R, "maskb", [128, 21 * 128], BF16)
        self.memset("pool", QKT[64:128, 0, :], 0.0, W=[("QKTz",)])
        self.memset("pool", QKT[0:64, 1, :], 0.0, W=[("QKTz",)])
        BM = self.alloc(R2, "BM", [128, 21 * 128], F32)
        tmpb = [self.alloc(R2, "tmpb%d" % i, [128, 512], F32) for i in range(2)]
        raw = [self.alloc(R2, "raw%d" % i, [128, 384], F32) for i in range(3)]
        sqt = self.alloc(R2, "sqt", [128, 4, 64], F32)
        qkb = [self.alloc(R2, "qkb%d" % i, [128, 4, 64], BF16) for i in range(3)]
        junk = self.alloc(R2, "junk", [128, 64], F32)
        ssq = [self.smallp[:, 0:8], self.smallp[:, 8:16]]
        lnv = [self.smallp[:, 16:24], self.smallp[:, 24:32]]
        rs = [self.smallp[:, 32:40], self.smallp[:, 40:48]]
        for i in range(2):
            self.bcast_row("sp", "gq", gq4[:, i, :], d["c_qnorm"][0], 64, W=[("gq4",)], group=(i > 0))
            self.bcast_row("sp", "gq", gq4[:, 2 + i, :], d["c_knorm"][0], 64, W=[("gq4",)], group=True)
        self.dma("pool", "mk", maskb[:, 0:1344], d["na_mask"][:, 0:1344], W=[("maskb",)])
        self.dma("pool", "mk", maskb[:, 1344:2688], d["na_mask"][:, 1344:2688], W=[("maskb",)], group=True)
        self.memset("pool", VA[:, :, 64:128], 1.0, W=[("VA", t) for t in range(NT)])
        wq = d["c_wqkv"][0].rearrange("(c p) f -> p c f", p=128)
        ptb = self.psbf(7)
        sc = 0.125
        for pp in range(8):
            self.dma("pool", "wg", Wp[:, :, 0:128], wq[:, :, 128 * pp:128 * pp + 128], W=[("Wg",)], bar=False)
            self.dma("pool", "wg", Wp[:, :, 128:256], wq[:, :, 1024 + 128 * pp:1024 + 128 * pp + 128], W=[("Wg",)], group=True, bar=False)
            self.dma("pool", "wg", Wp[:, :, 256:384], wq[:, :, 2048 + 128 * pp:2048 + 128 * pp + 128], W=[("Wg",)], group=True, bar=False)
            def S1(t):
                bank = 5 + (t % 2)
                ps = self.psb[bank]
                for c in range(8):
                    self.mm(ps[:, 0:384], hT[:, c, t * 128:(t + 1) * 128], Wp[:, c, :], start=(c == 0), stop=(c == 7),
                            R=[("hT", min(t // 4, 4)), ("Wg",)], W=[("ps", bank)])
                self.cp("act", raw[t % 3][:, :], ps[:, 0:384], R=[("ps", bank)], W=[("raw", t % 3)])

            def S2(t):
                r3 = raw[t % 3][:, 0:256].rearrange("p (h e) -> p h e", e=64)
                for h in range(4):
                    self.act(junk[:, :], r3[:, h, :], AF.Square, scale=0.125, accum=ssq[t % 2][:, h:h + 1],
                             R=[("raw", t % 3)], W=[("junk",), ("ssq", t % 2)])

            def S3(t):
                self.act(lnv[t % 2][:, 0:4], ssq[t % 2][:, 0:4], AF.Ln, bias=self.misc[:, 2:3], scale=1.0,
                         R=[("ssq", t % 2), ("cst",)], W=[("lnvh", t % 2)])
                self.act(rs[t % 2][:, 0:4], lnv[t % 2][:, 0:4], AF.Exp, scale=-0.5, R=[("lnvh", t % 2)], W=[("rsh", t % 2)])

            def S4(t):
                rw = raw[t % 3]
                r3 = rw[:, 0:256].rearrange("p (h e) -> p h e", e=64)
                self.tt("dve", sqt[:, :, :], r3, rs[t % 2][:, 0:4].unsqueeze(2).to_broadcast([128, 4, 64]), ALU.mult,
                        R=[("raw", t % 3), ("rsh", t % 2)], W=[("sqt",)])
                qb = qkb[t % 3]
                self.tt("dve", qb[:, :, :], sqt[:, :, :], gq4[:, :, :], ALU.mult, R=[("sqt",), ("gq4",)], W=[("qkb", t % 3)])
                self.cp("pool", VA[:, t, 0:64], rw[:, 256:320], R=[("raw", t % 3)], W=[("VA", t)])
                self.cp("pool", VA[:, t, 128:192], rw[:, 320:384], R=[("raw", t % 3)], W=[("VA", t)])

            def S5(t):
                qb = qkb[t % 3]
                qf = qb[:, :, :].rearrange("p h e -> p (h e)")
                for i in range(2):
                    self.tr(ptb[:, i * 128:(i + 1) * 128], qf[:, i * 128:(i + 1) * 128], self.identb[:, :],
                            R=[("qkb", t % 3), ("identb",)], W=[("ps", 7)])
                self.cp("act", QKT[0:64, 0, t * 128:(t + 1) * 128], ptb[0:64, 0:128], R=[("ps", 7)], W=[("QKT", t)])
                self.cp("act", QKT[64:128, 1, t * 128:(t + 1) * 128], ptb[64:128, 0:128], R=[("ps", 7)], W=[("QKT", t)])
                self.cp("dve", QKT[:, 2, t * 128:(t + 1) * 128], ptb[:, 128:256], R=[("ps", 7)], W=[("QKT", t)])

            self.pipeline(NT, S1, S2, S3, S4, S5)
            slot = pp % 2
            self.dma("pool", "wo%d" % slot, WoP[:, slot, :], d["c_wo"][0][pp * 128:(pp + 1) * 128, :], W=[("WoP", slot)], bar=False)
            self._alias([("QKT", t) for t in range(NT)] + [("QKTz",)], ("QKTall",))
            self._alias([("VA", t) for t in range(NT)], ("VAall",))
            for hs in range(2):
                h = 2 * pp + hs
                psl = slice(0, 64) if hs == 0 else slice(64, 128)
                self.dma("sp", "bm", BM[:, :], d["na_bias"][h], W=[("BM",)])
                self.tt("pool", BM[:, :], BM[:, :], maskb[:, :], ALU.add, R=[("BM",), ("maskb",)], W=[("BM",)])
                va = (lambda k, hs=hs: VA[:, k, 0:128] if hs == 0 else VA[:, k, 64:192])
                self.na_core(QKT[:, hs, :], QKT[:, 2, :], va, hs == 0, attT, BM, tmpb, sc)
            if need_ctx:
                cfgs = []
                t0, w = GROUPS[4]
                for hs in range(2):
                    psl = slice(0, 64) if hs == 0 else slice(64, 128)
                    cfgs.append(dict(
                        qt=QKT[:, hs, :], kt=QKT[:, 2, :],
                        va=(lambda k, hs=hs: VA[:, k, 0:128] if hs == 0 else VA[:, k, 64:192]),
                        o_lo=(hs == 0), t0=t0, w=w, gi=4, hs=hs, ktiles=[16, 17], att=attT,
                        qres=("QKTall",), kres=("QKTall",), vres=("VAall",), ares=("attT",)))
                self.attn_core(cfgs, sc)
            self.out_proj(WoP[:, slot, :], ("WoP", slot), attT, ("attT",), l, b, need_ctx)
            self._alias_release([("QKT", t) for t in range(NT)], ("QKTall",))
            self._alias_release([("VA", t) for t in range(NT)], ("VAall",))
        self.flush_op()
        self.P.barrier()

    def na_core(self, qt, kt, va, o_lo, attT, BM, tmpb, sc):
        packs = []
        for qi in range(16):
            blocks = na_block_ids(qi)
            items = [(kt_, blk) for (kt_, blk) in blocks] + [(16, None), (17, None)]
            plist = [items[0:4], items[4:]]
            for pi, pk in enumerate(plist):
                packs.append((qi, pk, pi == 0, pi == len(plist) - 1))
        n = len(packs)
        pt3 = self.PT[:, :, :].rearrange("p a (b n) -> p (a b) n", n=512)
        lo, hi_ = slice(0, 64), slice(64, 128)
        o_sl, d_sl = (lo, hi_) if o_lo else (hi_, lo)

        def qk(i):
            qi, pk, first, last = packs[i]
            bank = i % 3
            for j, (k, blk) in enumerate(pk):
                self.mm(self.psb[bank][:, j * 128:(j + 1) * 128], kt[:, k * 128:(k + 1) * 128], qt[:, qi * 128:(qi + 1) * 128],
                        R=[("QKTall",)], W=[("ps", bank)])

        def ex(i):
            qi, pk, first, last = packs[i]
            bank = i % 3
            nb = sum(1 for (_, blk) in pk if blk is not None)
            nk = len(pk)
            if nb > 0:
                b0 = pk[0][1]
                tb = tmpb[i % 2]
                self.stt(tb[:, 0:nb * 128], self.psb[bank][:, 0:nb * 128], sc, BM[:, b0 * 128:(b0 + nb) * 128], ALU.mult, ALU.add,
                         R=[("ps", bank), ("BM",)], W=[("tmpb", i % 2)])
                self.act(pt3[:, bank, 0:nb * 128], tb[:, 0:nb * 128], AF.Exp, R=[("tmpb", i % 2)], W=[("pt", bank)])
            if nk > nb:
                self.act(pt3[:, bank, nb * 128:nk * 128], self.psb[bank][:, nb * 128:nk * 128], AF.Exp, scale=sc,
                         R=[("ps", bank)], W=[("pt", bank)])

        def pv(i):
            qi, pk, first, last = packs[i]
            if o_lo and first and qi % 4 == 0:
                self.take_op(qi // 4, 8)
            ob = 3 + (qi + self.ot_ctr) % 2
            for j, (k, blk) in enumerate(pk):
                self.mm(self.psb[ob][:, 0:128], va(k), pt3[:, i % 3, j * 128:(j + 1) * 128],
                        start=(first and j == 0), stop=(last and j == len(pk) - 1),
                        R=[("pt", i % 3), ("VAall",)], W=[("ps", ob)])
            if last:
                def fin(ob=ob, qi=qi):
                    rec_o = self.rec[d_sl, 0:128]
                    rec_l = self.rec[d_sl, 128:256]
                    rec_i = self.psb[ob][d_sl, 0:128]
                    self.act(rec_l, rec_i, AF.Ln, R=[("ps", ob)], W=[("recl",)])
                    self.act(rec_o, rec_l, AF.Exp, scale=-1.0, R=[("recl",)], W=[("rec",)])
                    self.tt("dve", attT[o_sl, qi * 128:(qi + 1) * 128], self.psb[ob][o_sl, 0:128], self.rec[d_sl, 0:128], ALU.mult,
                            R=[("ps", ob), ("rec",)], W=[("attT", qi // 4)])
                deferred.append((i + 2, fin))

        deferred = []
        qk(0)
        if n > 1:
            qk(1)
        for i in range(n):
            if i + 2 < n:
                qk(i + 2)
            ex(i)
            while deferred and deferred[0][0] <= i:
                deferred.pop(0)[1]()
            pv(i)
        while deferred:
            deferred.pop(0)[1]()
        self.ot_ctr += 16

    def ring_load(self, src):
        i = self.unit_ctr % NUNIT
        self.unit_ctr += 1
        buf = self.ring[i]
        self.dma("pool", "ring%d" % i, buf[:, :, :], src, W=[("ring", i)], bar=False)
        return buf, ("ring", i)

    def moe_units(self, l, e):
        d = self.d
        wg = d["moe_wg"][l, e].rearrange("(c p) f -> p c f", p=128)
        wu = d["moe_wu"][l, e].rearrange("(c p) f -> p c f", p=128)
        wd = d["moe_wd"][l, e].rearrange("(c p) f -> p c f", p=128)
        return [wg[:, :, 0:512], wu[:, :, 0:512], wg[:, :, 512:1024], wu[:, :, 512:1024], wd[:, :, 0:512], wd[:, :, 512:1024]]

    def moe_layer(self, l, b, need_ctx, pre):
        d = self.d
        R = self.R
        R.reset()
        ROFF = R_OFF
        self.norm_bufs()
        h2T = [self.alloc(R, "h2T%d" % i, [128, 8, 512], BF16) for i in range(2)]
        aff = self.carve("aff", ROFF + 43008, [128, NT, 16], F32)
        rw = self.carve("rw", ROFF + 44160, [128, 8, 16], BF16)
        m8 = self.carve("m8", ROFF + 44160 + 256, [16, 8], F32)
        thr = self.carve("thr", ROFF + 44160 + 288, [16, 2], F32)
        gate = self.carve("gate", ROFF + 44160 + 320, [128, 4], F32)
        ee = self.carve("ee", ROFF + 44160 + 352, [128, 16], F32)
        sst = self.carve("sst", ROFF + 44160 + 416, [128, 8], F32)
        h2 = self.carve("h2", HT_OFF, [128, NT, D], BF16)
        self.dma("pool", "rw", rw[:, :, :], d["moe_router"][l].rearrange("(c p) e -> p c e", p=128), W=[("rw",)])
        ngrp = 5 if need_ctx else 4
        ntl = NT if need_ctx else 16
        ptb = self.psbf(7)
        for gi in range(ngrp):
            t0, w = GROUPS[gi]
            hb = h2T[gi % 2]
            self.norm_group(gi, l, b, 2, lambda c, hb=hb, w=w, gi=gi: (hb[:, c, 0:w], ("h2T", gi % 2)))
            for tt_ in range(w // 128):
                t = t0 // 128 + tt_
                bank = 3 + (t % 2)
                ps = self.psb[bank]
                for c in range(8):
                    self.mm(ps[:, 0:16], hb[:, c, tt_ * 128:(tt_ + 1) * 128], rw[:, c, :], start=(c == 0), stop=(c == 7),
                            R=[("h2T", gi % 2), ("rw",)], W=[("ps", bank)])
                self.red(sst[:, 0:1], ps[:, 0:16], ALU.max, R=[("ps", bank)], W=[("sst",)])
                self.ts("dve", sst[:, 1:2], sst[:, 0:1], -1.0, ALU.mult, R=[("sst",)], W=[("sst",)])
                self.act(ee[:, :], ps[:, 0:16], AF.Exp, bias=sst[:, 1:2], accum=sst[:, 2:3],
                         R=[("ps", bank), ("sst",)], W=[("ee",), ("sst",)])
                self.P.op("dve", lambda e: e.reciprocal(out=sst[:, 3:4], in_=sst[:, 2:3]), [("sst",)], [("sst",)])
                self.ts("dve", aff[:, t, :], ee[:, :], sst[:, 3:4], ALU.mult, R=[("ee",), ("sst",)], W=[("aff",)])
                for c in range(8):
                    self.tr(ptb[:, c * 128:(c + 1) * 128], hb[:, c, tt_ * 128:(tt_ + 1) * 128], self.identb[:, :],
                            R=[("h2T", gi % 2), ("identb",)], W=[("ps", 7)])
                self.cp("act", h2[:, t, :], ptb[:, :], R=[("ps", 7)], W=[("h2", t)])
        self.P.barrier()
        affT = self.carve("affT", ROFF + 0, [16, TT], F32)
        work = self.carve("work", ROFF + 9216, [16, TT], F32)
        B3 = self.carve("B3", ROFF + 18432, [16, TT], F32)
        vb16 = self.carve("vb16", ROFF + 27648, [16, TT], BF16)
        gmtok = self.carve("gmtok", ROFF + 38400, [128, NT, 16], F32)
        vtok = self.carve("vtok", ROFF + 40704, [128, NT, 16], F32)
        gmhl = self.carve("gmhl", ROFF + 41856, [128, NT, 16, 2], BF16)
        for t in range(ntl):
            bank = (t // 4) % 2
            self.tr(self.psb[bank][0:16, (t % 4) * 128:(t % 4 + 1) * 128], aff[:, t, :], self.identf,
                    R=[("aff",), ("cst",)], W=[("ps", bank)])
            if t % 4 == 3 or t == ntl - 1:
                t_lo = (t // 4) * 4
                n = (t - t_lo + 1) * 128
                self.cp("dve", affT[:, t_lo * 128:t_lo * 128 + n], self.psb[bank][0:16, 0:n], R=[("ps", bank)], W=[("affT",)])
        segs = [(0, T, CAP // 8, 0)] + ([(T, TC, CAPC // 8, 1)] if need_ctx else [])
        ncols = T + (TC if need_ctx else 0)
        self.cp("dve", work[:, 0:ncols], affT[:, 0:ncols], R=[("affT",)], W=[("work",)])
        for (c0, n, rounds, ti) in segs:
            wv = work[:, c0:c0 + n]
            for r in range(rounds):
                self.P.op("dve", lambda e, wv=wv: e.max(out=m8[:, :], in_=wv), [("work",)], [("m8",)])
                if r < rounds - 1:
                    self.P.op("dve", lambda e, wv=wv: e.match_replace(out=wv, in_to_replace=m8[:, :], in_values=wv, imm_value=-1.0),
                              [("work",), ("m8",)], [("work",)])
            self.cp("dve", thr[:, ti:ti + 1], m8[:, 7:8], R=[("m8",)], W=[("thr",)])
        ones_col = self.cstb[0:16, 128 + 1:128 + 2]
        for (c0, n, rounds, ti) in segs:
            self.ts("dve", work[:, c0:c0 + n], affT[:, c0:c0 + n], thr[:, ti:ti + 1], ALU.is_ge, R=[("affT",), ("thr",)], W=[("work",)])
            self.P.op("dve", lambda e, c0=c0, n=n: e.tensor_tensor_scan(out=B3[:, c0:c0 + n], data0=ones_col.to_broadcast([16, n]),
                                                                     data1=work[:, c0:c0 + n], initial=0.0, op0=ALU.mult, op1=ALU.add),
                      [("work",), ("cst",)], [("B3",)])
        self.tt("dve", B3[:, 0:ncols], B3[:, 0:ncols], work[:, 0:ncols], ALU.mult, R=[("B3",), ("work",)], W=[("B3",)])
        self.ts("dve", B3[:, 0:ncols], B3[:, 0:ncols], -1.0, ALU.add, R=[("B3",)], W=[("B3",)])
        self.tt("dve", affT[:, 0:ncols], affT[:, 0:ncols], work[:, 0:ncols], ALU.mult, R=[("affT",), ("work",)], W=[("affT",)])
        self.cp("dve", vb16[:, 0:ncols], B3[:, 0:ncols], R=[("B3",)], W=[("vb16",)])
        for (src, dst, nm, bank) in ((B3, vtok, "vtok", 2), (affT, gmtok, "gmtok", 3)):
            for t in range(ntl):
                self.tr(self.psb[bank][:, t * 16:(t + 1) * 16], src[:, t * 128:(t + 1) * 128], self.identf[0:16, 0:16],
                        R=[(src.name,), ("cst",)], W=[("ps", bank)])
            self.cp("dve", dst[:, 0:ntl, :], self.psb[bank][:, 0:ntl * 16].rearrange("p (t e) -> p t e", e=16),
                    R=[("ps", bank)], W=[(nm,)])
        self.cp("dve", gmhl[:, 0:ntl, :, 0], gmtok[:, 0:ntl, :], R=[("gmtok",)], W=[("gmhl",)])
        self.tt("dve", gmtok[:, 0:ntl, :], gmtok[:, 0:ntl, :], gmhl[:, 0:ntl, :, 0], ALU.subtract, R=[("gmtok",), ("gmhl",)], W=[("gmtok",)])
        self.cp("dve", gmhl[:, 0:ntl, :, 1], gmtok[:, 0:ntl, :], R=[("gmtok",)], W=[("gmhl",)])
        self.P.barrier()
        S = self.carve("S", ROFF + 0, [128, 16, 256], BF16)
        Sc = self.carve("Sc", ROFF + 8192, [128, 2, 32], BF16)
        ST = self.carve("ST", ROFF + 9216, [128, 2, T], BF16)
        STc = self.carve("STc", ROFF + 9216 + 8192, [32, 256], BF16)
        xgT = self.carve("xgT", ROFF + 18432, [128, 8, 288], BF16)
        hidT = self.carve("hidT", ROFF + 18432 + 4608, [128, 8, 288], BF16)
        y = self.carve("y", ROFF + 32256, [128, 3, D], BF16)
        sg = self.carve("sg", ROFF + 38400, [128, 2, 288], F32)
        Wn = 288 if need_ctx else 256
        units = list(pre)
        upos = [0]

        def next_units(n):
            out = []
            for _ in range(n):
                out.append(units[upos[0]])
                upos[0] += 1
            return out

        srcs = []
        for e in range(NE):
            srcs += self.moe_units(l, e)
        issued = [len(pre)]

        def issue(upto):
            while issued[0] < min(upto, len(srcs)):
                units.append(self.ring_load(srcs[issued[0]]))
                issued[0] += 1

        chunks = [(0, 128), (128, 128)] + ([(256, 32)] if need_ctx else [])
        nch = 3 if need_ctx else 2

        def build_S(e):
            for t in range(16):
                self.ts("dve", S[:, t, :], self.iotac, vtok[:, t, e:e + 1], ALU.is_equal, R=[("vtok",), ("cst",)], W=[("S", t)])
            if need_ctx:
                for t in range(2):
                    self.ts("dve", Sc[:, t, :], self.iotac[:, 0:32], vtok[:, 16 + t, e:e + 1], ALU.is_equal, R=[("vtok",), ("cst",)], W=[("Sc",)])

        def slot_gates(e):
            gb = 4
            gps = self.psb[gb]
            for ch in range(2):
                for t in range(16):
                    self.mm(gps[:, ch * 2:ch * 2 + 2], S[:, t, ch * 128:(ch + 1) * 128], gmhl[:, t, e, :], start=(t == 0), stop=(t == 15),
                            R=[("S", t), ("gmhl",)], W=[("ps", gb)])
            if need_ctx:
                for t in range(2):
                    self.mm(gps[0:32, 4:6], Sc[:, t, :], gmhl[:, 16 + t, e, :], start=(t == 0), stop=(t == 1),
                            R=[("Sc",), ("gmhl",)], W=[("ps", gb)])
            self.red(gate[:, 0:nch], gps[:, 0:2 * nch].rearrange("p (c two) -> p c two", two=2), ALU.add, R=[("ps", gb)], W=[("gate",)])

        def gather_c(e, c):
            bank = c % 2
            ps = self.psb[bank]
            for t in range(16):
                self.mm(ps[:, 0:256], h2[:, t, c * 128:(c + 1) * 128], S[:, t, :], start=(t == 0), stop=(t == 15),
                        R=[("h2", t), ("S", t)], W=[("ps", bank)])
            if need_ctx:
                for t in range(2):
                    self.mm(ps[:, 256:288], h2[:, 16 + t, c * 128:(c + 1) * 128], Sc[:, t, :], start=(t == 0), stop=(t == 1),
                            R=[("h2", 16 + t), ("Sc",)], W=[("ps", bank)])
            self.cp("act", xgT[:, c, 0:Wn], ps[:, 0:Wn], R=[("ps", bank)], W=[("xgT",)])

        def scatter_blocks(e):
            out = []
            for grp in range(4):
                for dc in range(8):
                    def blk(grp=grp, dc=dc):
                        bank = 2 + (dc % 2)
                        ps = self.psb[bank]
                        for ch in range(2):
                            self.mm(ps[:, :], y[:, ch, dc * 128:(dc + 1) * 128], ST[:, ch, grp * 512:(grp + 1) * 512],
                                    start=(ch == 0), stop=(ch == 1), R=[("y",), ("ST", grp)], W=[("ps", bank)])
                        xs = self.xT[:, dc, grp * 512:(grp + 1) * 512]
                        self.stt(xs, ps[:, :], self.mod(l, b, 5, dc), xs, ALU.mult, ALU.add,
                                 R=[("ps", bank), ("modv",), ("xT", grp, dc)], W=[("xT", grp, dc)])
                    out.append(blk)
            if need_ctx:
                for dc in range(8):
                    def blk(dc=dc):
                        bank = 2 + (dc % 2)
                        ps = self.psb[bank]
                        self.mm(ps[:, 0:256], y[0:32, 2, dc * 128:(dc + 1) * 128], STc[:, :], R=[("y",), ("STc",)], W=[("ps", bank)])
                        xs = self.xT[:, dc, T:TT]
                        self.stt(xs, ps[:, 0:256], self.mod(l, 2, 5, dc), xs, ALU.mult, ALU.add,
                                 R=[("ps", bank), ("modv",), ("xT", 4, dc)], W=[("xT", 4, dc)])
                    out.append(blk)
            return out

        def build_ST(e):
            for grp in range(4):
                bank = 2 + (grp % 2)
                ps = self.psb[bank]
                self.mm(ps[:, :], self.sel[:, e, :], vb16[:, grp * 512:(grp + 1) * 512], R=[("sel",), ("vb16",)], W=[("ps", bank)])
                for ch in range(2):
                    self.ts("dve", ST[:, ch, grp * 512:(grp + 1) * 512], ps[:, :], self.misc[:, ch:ch + 1], ALU.is_equal,
                            R=[("ps", bank), ("cst",)], W=[("ST", grp)])
            if need_ctx:
                ps = self.psb[2]
                self.mm(ps[0:32, 0:256], self.sel[:, e, 0:32], vb16[:, T:TT], R=[("sel",), ("vb16",)], W=[("ps", 2)])
                self.ts("dve", STc[:, :], ps[0:32, 0:256], self.misc[0:32, 0:1], ALU.is_equal, R=[("ps", 2), ("cst",)], W=[("STc",)])

        def gate_up(e, wg0, wu0, wg1, wu1, inject):
            for fc in range(8):
                if fc in inject:
                    inject[fc]()
                gbuf, gres = (wg0 if fc < 4 else wg1)
                ubuf, ures = (wu0 if fc < 4 else wu1)
                fo = (fc % 4) * 128
                bg = 4 + 2 * (fc % 2)
                bu = bg + 1
                for c in range(8):
                    self.mm(self.psb[bg][:, 0:Wn], gbuf[:, c, fo:fo + 128], xgT[:, c, 0:Wn], start=(c == 0), stop=(c == 7),
                            R=[gres, ("xgT",)], W=[("ps", bg)])
                for c in range(8):
                    self.mm(self.psb[bu][:, 0:Wn], ubuf[:, c, fo:fo + 128], xgT[:, c, 0:Wn], start=(c == 0), stop=(c == 7),
                            R=[ures, ("xgT",)], W=[("ps", bu)])
                self.act(sg[:, fc % 2, 0:Wn], self.psb[bg][:, 0:Wn], AF.Silu, R=[("ps", bg)], W=[("sg", fc % 2)])
                self.tt("dve", hidT[:, fc, 0:Wn], sg[:, fc % 2, 0:Wn], self.psb[bu][:, 0:Wn], ALU.mult,
                        R=[("sg", fc % 2), ("ps", bu)], W=[("hidT",)])

        def down(e, wd0, wd1):
            for half, (dbuf, dres) in enumerate((wd0, wd1)):
                for ci, (c0, m) in enumerate(chunks):
                    bank = (half * 3 + ci) % 2
                    ps = self.psb[bank]
                    for fcn in range(8):
                        self.mm(ps[0:m, :], hidT[:, fcn, c0:c0 + m], dbuf[:, fcn, :], start=(fcn == 0), stop=(fcn == 7),
                                R=[("hidT",), dres], W=[("ps", bank)])
                    self.ts("dve", y[0:m, ci, half * 512:(half + 1) * 512], ps[0:m, :], gate2[0:m, ci:ci + 1], ALU.mult,
                            R=[("ps", bank), ("gate2",)], W=[("y",)])

        gate2 = self.carve("gate2", ROFF + 44160 + 480, [128, 4], F32)
        issue(4)
        build_S(0)
        slot_gates(0)
        for c in range(8):
            gather_c(0, c)
        for e in range(NE):
            issue(e * 6 + 4)
            wg0, wu0, wg1, wu1 = next_units(4)
            self.cp("dve", gate2[:, 0:nch], gate[:, 0:nch], R=[("gate",)], W=[("gate2",)])
            inj = {1: (lambda e=e: build_ST(e))}
            if e + 1 < NE:
                inj[3] = (lambda e=e: build_S(e + 1))
                inj[5] = (lambda e=e: slot_gates(e + 1))
            gate_up(e, wg0, wu0, wg1, wu1, inj)
            issue(e * 6 + 6)
            wd0, wd1 = next_units(2)
            down(e, wd0, wd1)
            issue(e * 6 + 10)
            blocks = scatter_blocks(e)
            if e + 1 < NE:
                nb = len(blocks)
                per = (nb + 7) // 8
                bi = 0
                for c in range(8):
                    gather_c(e + 1, c)
                    for _ in range(per):
                        if bi < nb:
                            blocks[bi]()
                            bi += 1
                while bi < nb:
                    blocks[bi]()
                    bi += 1
            else:
                for blk in blocks:
                    blk()
        self.P.barrier()

    def moe_prefetch(self, l):
        self.unit_ctr = 0
        srcs = self.moe_units(l, 0)
        return [self.ring_load(srcs[i]) for i in range(2)]

    def build(self):
        cfg = self.cfg
        self.prologue()
        for b in range(cfg.get("nsamples", SPC)):
            self.load_sample(b)
            for l in cfg.get("layers", [0, 1, 2, 3]):
                need_ctx = l < 3
                pre = self.moe_prefetch(l) if cfg.get("moe", True) else []
                kind = l % 3
                if cfg.get("attn", True):
                    if kind == 0:
                        self.gqa_layer(l, b, need_ctx)
                    elif kind == 1:
                        self.mla_layer(l, b, need_ctx)
                    else:
                        self.na_layer(l, b, need_ctx)
                if cfg.get("dump_xa") == (b, l):
                    self.store_sample(self.dbg_out("xa", [TT, D]), NT)
                if cfg.get("moe", True):
                    self.moe_layer(l, b, need_ctx, pre)
                if cfg.get("dump_x") == (b, l):
                    self.store_sample(self.dbg_out("x", [TT, D]), NT)
            self.store_sample(self.y2[b], 16)
        self.P.emit(self.es)
        return self.nc


def host_inputs(inp):
    cst = np.zeros((128, 388), np.float32)
    cst[:, 0:128] = np.eye(128, dtype=np.float32)
    cst[:, 128:384] = np.arange(256, dtype=np.float32)[None, :]
    cst[:, 384] = np.arange(128, dtype=np.float32)
    cst[:, 385] = np.arange(128, dtype=np.float32) + 128.0
    cst[:, 386] = EPS
    selc = np.zeros((16, 16, 128), np.float32)
    for e in range(16):
        selc[e, e, :] = 1.0
    selc = selc.reshape(16, 2048).astype(ml_dtypes.bfloat16)
    cosA, sinA = rope_tables(64)
    cosB, sinB = rope_tables(32)
    na_bias, na_mask = na_tables(np.asarray(inp["c_rpb"], np.float32)[0])
    shared = {k: np.ascontiguousarray(np.asarray(inp[k], np.float32)) for k in (
        "ada_w", "ada_b", "norm1_w", "norm2_w", "a_wqkv", "a_qnorm", "a_knorm", "a_wo",
        "b_wdq", "b_qnorm_lat", "b_wuq", "b_wdkv", "b_kvnorm_lat", "b_wukv", "b_qnorm", "b_knorm", "b_wo",
        "c_wqkv", "c_qnorm", "c_knorm", "c_wo", "moe_router", "moe_wg", "moe_wu", "moe_wd")}
    shared.update(cst=cst, selc=selc, cosA=cosA, sinA=sinA, cosB=cosB, sinB=sinB, na_bias=na_bias, na_mask=na_mask)
    x = np.asarray(inp["x"], np.float32)
    ctx = np.asarray(inp["ctx"], np.float32)
    c = np.asarray(inp["c"], np.float32)
    c_ctx = np.asarray(inp["c_ctx"], np.float32)
    maps = []
    for core in range(N_CORES):
        m = dict(shared)
        m["x2"] = np.ascontiguousarray(x[SPC * core:SPC * core + SPC])
        m["ctx2"] = np.ascontiguousarray(ctx[SPC * core:SPC * core + SPC])
        m["cvec"] = np.ascontiguousarray(np.concatenate([c[SPC * core:SPC * core + SPC], c_ctx[None, :]], axis=0))
        maps.append(m)
    return maps


def kernel(**inp):
    maps = host_inputs(inp)
    nc = Builder({}).build()
    res = run_bass_kernel_spmd(nc, maps, core_ids=list(range(N_CORES)))
    out = np.concatenate([np.asarray(r["y2"], np.float32) for r in res.results], axis=0)
    return out
```

```python
import numpy as np
import ml_dtypes
from contextlib import ExitStack
import concourse.bass as bass
import concourse.mybir as mybir
from concourse.bass_utils import run_bass_kernel_spmd

F32 = mybir.dt.float32
BF16 = mybir.dt.bfloat16
AF = mybir.ActivationFunctionType
ALU = mybir.AluOpType
AX = mybir.AxisListType

D = 1024
T = 2048
TC = 256
TT = 2304
NT = 18
NE = 16
CAP = 256
CAPC = 32
EPS = 1e-6
NEG = -30000.0
N_CORES = 8
SPC = 2

XT_OFF = 0
RING_OFF = 73728
UNIT = 8192
NUNIT = 5
HT_OFF = RING_OFF + UNIT * NUNIT
R_OFF = HT_OFF + 36864
R_SIZE = 45056
PERS_OFF = R_OFF + R_SIZE
ARENA_BYTES = 212000
R2_OFF = RING_OFF + 2 * UNIT
R2_SIZE = 3 * UNIT


def _dsize(dt):
    return 4 if dt == F32 else 2


class Prog:
    ENG = ("pe", "act", "dve", "pool", "sp")

    def __init__(self, nc):
        self.nc = nc
        self.eng = {"pe": nc.tensor, "act": nc.scalar, "dve": nc.vector, "pool": nc.gpsimd, "sp": nc.sync}
        self.ops = []
        self.eops = {e: [] for e in self.ENG}
        self.res = {}
        self.slots = {}
        self.pending = {e: set() for e in self.ENG}

    def _deps(self, eng, reads, writes):
        deps = set(self.pending[eng])
        self.pending[eng] = set()
        for r in reads:
            st = self.res.get(r)
            if st is not None and st["w"] is not None:
                deps.add(st["w"])
        for w in writes:
            st = self.res.get(w)
            if st is not None:
                if st["w"] is not None:
                    deps.add(st["w"])
                deps.update(st["r"].values())
        return deps

    def _mark(self, tok, key, reads, writes):
        for r in reads:
            st = self.res.get(r)
            if st is None:
                st = self.res[r] = {"w": None, "r": {}}
            st["r"][key] = tok
        for w in writes:
            self.res[w] = {"w": tok, "r": {}}

    def op(self, eng, fn, reads=(), writes=()):
        deps = self._deps(eng, reads, writes)
        idx = len(self.eops[eng])
        rec = {"eng": eng, "kind": "op", "fn": fn, "deps": deps, "idx": idx, "sig": False}
        self.eops[eng].append(rec)
        self.ops.append(rec)
        self._mark(("e", eng, idx), eng, reads, writes)

    def dma(self, queue, slot, out, in_, reads=(), writes=(), group=False, bar=True, **kw):
        deps = self._deps(queue, reads, writes)
        s = self.slots.get(slot)
        if s is None:
            s = self.slots[slot] = {"n": 0, "groups": [], "bar": bar}
        s["n"] += 1
        n = s["n"]
        if group and s["groups"]:
            s["groups"][-1] = n
        else:
            if n > 1:
                deps.add(("d", slot, n - 1))
            s["groups"].append(n)
        idx = len(self.eops[queue])
        rec = {"eng": queue, "kind": "dma", "slot": slot, "n": n, "out": out, "in_": in_, "deps": deps,
               "idx": idx, "kw": kw, "sig": False}
        self.eops[queue].append(rec)
        self.ops.append(rec)
        self._mark(("d", slot, n), ("d", slot), reads, writes)

    def barrier(self):
        toks = set()
        for e in ("pe", "act", "dve", "pool"):
            if self.eops[e]:
                for rec in reversed(self.eops[e]):
                    if rec["kind"] == "op":
                        toks.add(("e", e, rec["idx"]))
                        break
        for name, s in self.slots.items():
            if s["bar"] and s["n"] > 0:
                toks.add(("d", name, s["n"]))
        for e in ("pe", "act", "dve", "pool", "sp"):
            self.pending[e] |= {t for t in toks if not (t[0] == "e" and t[1] == e == "pe")}

    def _gend(self, slot, n):
        for g in self.slots[slot]["groups"]:
            if g >= n:
                return g
        raise AssertionError

    def emit(self, es):
        nc = self.nc
        for rec in self.ops:
            for d in rec["deps"]:
                if d[0] == "e":
                    if d[1] == rec["eng"] == "pe":
                        continue
                    self.eops[d[1]][d[2]]["sig"] = True
        for e in self.ENG:
            c = 0
            for rec in self.eops[e]:
                if rec["sig"]:
                    c += 1
                rec["sigval"] = c
        sem = {}
        for e in self.ENG:
            sem[("e", e)] = es.enter_context(nc.semaphore("g_" + e))
        for name in self.slots:
            sem[("d", name)] = es.enter_context(nc.semaphore("d_" + name))
        waited = {e: {} for e in self.ENG}
        nwait = 0
        for rec in self.ops:
            e = rec["eng"]
            engine = self.eng[e]
            need = {}
            for d in rec["deps"]:
                if d[0] == "e":
                    if d[1] == e == "pe":
                        continue
                    key = ("e", d[1])
                    val = self.eops[d[1]][d[2]]["sigval"]
                else:
                    if rec["kind"] == "dma" and d[1] == rec["slot"] and self._gend(d[1], d[2]) == self._gend(rec["slot"], rec["n"]):
                        continue
                    key = ("d", d[1])
                    val = 16 * self._gend(d[1], d[2])
                if need.get(key, 0) < val:
                    need[key] = val
            for key, val in need.items():
                if waited[e].get(key, 0) < val:
                    engine.wait_ge(sem[key], val)
                    waited[e][key] = val
                    nwait += 1
            if rec["kind"] == "op":
                inst = rec["fn"](engine)
                if rec["sig"]:
                    inst.then_inc(sem[("e", e)], 1)
            else:
                inst = engine.dma_start(out=rec["out"], in_=rec["in_"], **rec["kw"])
                inst.then_inc(sem[("d", rec["slot"])], 16)
        sp = self.eng["sp"]
        for name, s in self.slots.items():
            if s["n"] > 0:
                sp.wait_ge(sem[("d", name)], 16 * s["n"])
        self.stats = {e: len(self.eops[e]) for e in self.ENG}
        self.stats["waits"] = nwait


class Region:
    def __init__(self, off, size):
        self.off, self.size, self.cur = off, size, 0

    def reset(self):
        self.cur = 0

    def take(self, nbytes):
        nbytes = (nbytes + 31) // 32 * 32
        o = self.off + self.cur
        self.cur += nbytes
        assert self.cur <= self.size, (self.cur, self.size)
        return o


class Buf:
    def __init__(self, name, ap):
        self.name = name
        self.ap = ap

    def __getitem__(self, idx):
        return self.ap[idx]

    def r(self, *k):
        return (self.name,) + k


GROUPS = [(0, 512), (512, 512), (1024, 512), (1536, 512), (2048, 256)]


def na_tables(rpb):
    blocks = [(5, 5 + r) for r in (-2, -1, 0, 1, 2)]
    for qi in (0, 1):
        blocks += [(qi, kt) for kt in range(4)]
    for qi in (14, 15):
        blocks += [(qi, kt) for kt in range(12, 16)]
    nb = len(blocks)
    H = rpb.shape[0]
    bias = np.zeros((H, 128, nb, 128), np.float32)
    mask = np.zeros((128, nb, 128), np.float32)
    kk = np.arange(128)
    for bi, (qi, kt) in enumerate(blocks):
        k_r = 2 * kt + kk // 64
        k_c = kk % 64
        q_r = 2 * qi + kk // 64
        q_c = kk % 64
        rs = np.clip(q_r - 4, 0, 24)
        cs = np.clip(q_c - 8, 0, 48)
        ok = ((k_r[:, None] >= rs[None, :]) & (k_r[:, None] < rs[None, :] + 8)
              & (k_c[:, None] >= cs[None, :]) & (k_c[:, None] < cs[None, :] + 16))
        dr = np.clip(k_r[:, None] - q_r[None, :] + 7, 0, 14)
        dc = np.clip(k_c[:, None] - q_c[None, :] + 15, 0, 30)
        bias[:, :, bi, :] = rpb[:, dr, dc]
        mask[:, bi, :] = np.where(ok, 0.0, NEG)
    return bias.reshape(H, 128, nb * 128), mask.reshape(128, nb * 128)


def na_block_ids(qi):
    if 2 <= qi <= 13:
        return [(qi + r, 2 + r) for r in (-2, -1, 0, 1, 2)]
    if qi in (0, 1):
        return [(kt, 5 + 4 * qi + kt) for kt in range(4)]
    base = 13 if qi == 14 else 17
    return [(kt, base + (kt - 12)) for kt in range(12, 16)]


def rope_tables(rot_dim):
    n_freq = rot_dim // 4
    inv = np.float32(10000.0) ** (-np.arange(n_freq, dtype=np.float32) / np.float32(n_freq))
    t = np.arange(T, dtype=np.int32)
    row = (t // 64).astype(np.float32)
    col = (t % 64).astype(np.float32)
    ang = np.concatenate([row[:, None] * inv, col[:, None] * inv], axis=-1).astype(np.float32)
    return np.cos(ang).astype(np.float32), np.sin(ang).astype(np.float32)


class Builder:
    def __init__(self, cfg):
        self.cfg = cfg
        nc = self.nc = bass.Bass("TRN2", target_bir_lowering=False)
        self.es = ExitStack()
        self.P = Prog(nc)
        self.dbg_outs = []
        self._dram()
        self.arena = self.es.enter_context(nc.sbuf_tensor("arena", [128, ARENA_BYTES // 4], F32))
        self.psall = self.es.enter_context(nc.psum_tensor("psall", [128, 4096], F32))
        self.psb = [self.psall[:, i * 512:(i + 1) * 512] for i in range(8)]
        self.R = Region(R_OFF, R_SIZE)
        self.R2 = Region(R2_OFF, R2_SIZE)
        self.HTR = Region(HT_OFF, 36864)
        self.PERS = Region(PERS_OFF, ARENA_BYTES - PERS_OFF)
        self.unit_ctr = 0
        self.ot_ctr = 0
        self.op_ctr = 0
        self._persistent()

    def _dram(self):
        nc = self.nc

        def inp(name, shape, dt=F32):
            return nc.dram_tensor(name, list(shape), dt, kind="ExternalInput").ap()
        self.d = d = {}
        d["x2"] = inp("x2", [SPC, T, D])
        d["ctx2"] = inp("ctx2", [SPC, TC, D])
        d["cvec"] = inp("cvec", [3, D])
        d["ada_w"] = inp("ada_w", [4, D, 6 * D])
        d["ada_b"] = inp("ada_b", [4, 6 * D])
        d["norm1_w"] = inp("norm1_w", [4, D])
        d["norm2_w"] = inp("norm2_w", [4, D])
        d["a_wqkv"] = inp("a_wqkv", [2, D, 1536])
        d["a_qnorm"] = inp("a_qnorm", [2, 64])
        d["a_knorm"] = inp("a_knorm", [2, 64])
        d["a_wo"] = inp("a_wo", [2, D, D])
        d["b_wdq"] = inp("b_wdq", [1, D, 384])
        d["b_qnorm_lat"] = inp("b_qnorm_lat", [1, 384])
        d["b_wuq"] = inp("b_wuq", [1, 384, 1536])
        d["b_wdkv"] = inp("b_wdkv", [1, D, 288])
        d["b_kvnorm_lat"] = inp("b_kvnorm_lat", [1, 256])
        d["b_wukv"] = inp("b_wukv", [1, 256, 2048])
        d["b_qnorm"] = inp("b_qnorm", [1, 96])
        d["b_knorm"] = inp("b_knorm", [1, 96])
        d["b_wo"] = inp("b_wo", [1, D, D])
        d["c_wqkv"] = inp("c_wqkv", [1, D, 3072])
        d["c_qnorm"] = inp("c_qnorm", [1, 64])
        d["c_knorm"] = inp("c_knorm", [1, 64])
        d["c_wo"] = inp("c_wo", [1, D, D])
        d["moe_router"] = inp("moe_router", [4, D, NE])
        d["moe_wg"] = inp("moe_wg", [4, NE, D, D])
        d["moe_wu"] = inp("moe_wu", [4, NE, D, D])
        d["moe_wd"] = inp("moe_wd", [4, NE, D, D])
        d["cst"] = inp("cst", [128, 388])
        d["selc"] = inp("selc", [16, 2048], BF16)
        d["cosA"] = inp("cosA", [T, 32])
        d["sinA"] = inp("sinA", [T, 32])
        d["cosB"] = inp("cosB", [T, 16])
        d["sinB"] = inp("sinB", [T, 16])
        d["na_bias"] = inp("na_bias", [16, 128, 21 * 128])
        d["na_mask"] = inp("na_mask", [128, 21 * 128])
        self.y2 = nc.dram_tensor("y2", [SPC, T, D], F32, kind="ExternalOutput").ap()

    def dbg_out(self, name, shape, dt=F32):
        ap = self.nc.dram_tensor("dbg_" + name, list(shape), dt, kind="ExternalOutput").ap()
        self.dbg_outs.append("dbg_" + name)
        return ap

    def carve(self, name, off, shape, dt):
        n = int(np.prod(shape[1:]))
        nbytes = n * _dsize(dt)
        assert off % 4 == 0 and nbytes % 4 == 0, (name, off, nbytes)
        assert off + nbytes <= ARENA_BYTES, (name, off, nbytes)
        ap = self.arena[0:shape[0], off // 4:(off + nbytes) // 4]
        if dt != F32:
            ap = ap.bitcast(dt)
        if len(shape) == 3:
            ap = ap.rearrange("p (a b) -> p a b", b=shape[2])
        elif len(shape) == 4:
            ap = ap.rearrange("p (a b c) -> p a b c", b=shape[2], c=shape[3])
        elif len(shape) == 5:
            ap = ap.rearrange("p (a b c d) -> p a b c d", b=shape[2], c=shape[3], d=shape[4])
        return Buf(name, ap)

    def alloc(self, region, name, shape, dt):
        n = int(np.prod(shape[1:])) * _dsize(dt)
        return self.carve(name, region.take(n), shape, dt)

    def psbf(self, i):
        return self.psall[:, i * 512:(i + 1) * 512].bitcast(BF16)

    def mm(self, out, lhsT, rhs, start=True, stop=True, R=(), W=()):
        self.P.op("pe", lambda e: e.matmul(out, lhsT, rhs, start=start, stop=stop), R, W)

    def tr(self, out, in_, ident, R=(), W=()):
        self.P.op("pe", lambda e: e.transpose(out, in_, ident), R, W)

    def act(self, out, in_, func, bias=None, scale=None, accum=None, R=(), W=()):
        kw = {}
        if bias is not None:
            kw["bias"] = bias
        if scale is not None:
            kw["scale"] = scale
        if accum is not None:
            kw["accum_out"] = accum
        self.P.op("act", lambda e: e.activation(out=out, in_=in_, func=func, **kw), R, W)

    def tt(self, eng, out, in0, in1, op, R=(), W=()):
        self.P.op(eng, lambda e: e.tensor_tensor(out=out, in0=in0, in1=in1, op=op), R, W)

    def ts(self, eng, out, in0, s1, op0, s2=None, op1=None, R=(), W=()):
        if op1 is None:
            self.P.op(eng, lambda e: e.tensor_scalar(out=out, in0=in0, scalar1=s1, scalar2=None, op0=op0), R, W)
        else:
            self.P.op(eng, lambda e: e.tensor_scalar(out=out, in0=in0, scalar1=s1, scalar2=s2, op0=op0, op1=op1), R, W)

    def stt(self, out, in0, scalar, in1, op0, op1, R=(), W=()):
        self.P.op("dve", lambda e: e.scalar_tensor_tensor(out=out, in0=in0, scalar=scalar, in1=in1, op0=op0, op1=op1), R, W)

    def cp(self, eng, out, in_, R=(), W=()):
        if eng == "act":
            self.P.op("act", lambda e: e.activation(out=out, in_=in_, func=AF.Copy), R, W)
        else:
            self.P.op(eng, lambda e: e.tensor_copy(out=out, in_=in_), R, W)

    def red(self, out, in_, op, R=(), W=()):
        self.P.op("dve", lambda e: e.tensor_reduce(out=out, in_=in_, axis=AX.X, op=op), R, W)

    def memset(self, eng, ap, val, R=(), W=()):
        self.P.op(eng, lambda e: e.memset(ap, val), R, W)

    def dma(self, queue, slot, out, in_, R=(), W=(), group=False, bar=True, **kw):
        self.P.dma(queue, slot, out, in_, R, W, group=group, bar=bar, **kw)

    def _persistent(self):
        A = self.alloc
        PR = self.PERS
        self.xT = self.carve("xT", XT_OFF, [128, 8, TT], F32)
        self.ring = [self.carve("ring%d" % i, RING_OFF + i * UNIT, [128, 8, 512], BF16) for i in range(NUNIT)]
        self.cstb = A(PR, "cstb", [128, 388], F32)
        self.identf = self.cstb[:, 0:128]
        self.iotac = self.cstb[:, 128:384]
        self.misc = self.cstb[:, 384:388]
        self.identb = A(PR, "identb", [128, 128], BF16)
        self.onesb = A(PR, "onesb", [128, 128], BF16)
        self.sel = A(PR, "sel", [16, 16, 128], BF16)
        self.modv = A(PR, "modv", [128, 4, 3, 6, 8], F32)
        self.cosA = A(PR, "cosA", [128, 16, 32], F32)
        self.sinA = A(PR, "sinA", [128, 16, 32], F32)
        self.cosB = A(PR, "cosB", [128, 16, 16], F32)
        self.sinB = A(PR, "sinB", [128, 16, 16], F32)
        self.smallp = A(PR, "smallp", [128, 64], F32)

    def prologue(self):
        d = self.d
        self.dma("sp", "c0", self.cstb[:, :], d["cst"][:, :], W=[("cst",)])
        self.dma("sp", "c0", self.sel[:, :, :], d["selc"].rearrange("k (e m) -> k e m", m=128), W=[("sel",)], group=True)
        for nm, buf in (("cosA", self.cosA), ("sinA", self.sinA), ("cosB", self.cosB), ("sinB", self.sinB)):
            self.dma("sp", "c0", buf[:, :, :], d[nm].rearrange("(t p) n -> p t n", p=128), W=[(nm,)], group=True)
        self.cp("dve", self.identb[:, :], self.identf, R=[("cst",)], W=[("identb",)])
        self.memset("pool", self.onesb[:, :], 1.0, W=[("onesb",)])
        R = self.R
        R.reset()
        vr = self.alloc(R, "vr", [128, 128], F32)
        vr2 = self.alloc(R, "vr2", [128, 128], F32)
        vr3 = self.alloc(R, "vr3", [128, 128], F32)
        vT = self.alloc(R, "vT", [128, 88], F32)
        abT = self.alloc(R, "abT", [128, 192], F32)
        sT = self.alloc(R, "sT", [128, 8, 4], F32)
        modraw = self.alloc(R, "modraw", [128, 4, 48, 3], F32)
        wslot = [self.carve("adaw0", HT_OFF, [128, 8, 768], F32),
                 self.alloc(R, "adaw1", [128, 8, 768], F32)]
        self.dma("sp", "p0", vr[0:24, :], d["cvec"].rearrange("s (c p) -> (s c) p", p=128), W=[("vr",)])
        self.dma("sp", "p0", vr[24:56, :], d["norm1_w"].rearrange("l (c p) -> (l c) p", p=128), W=[("vr",)], group=True)
        self.dma("sp", "p0", vr[56:88, :], d["norm2_w"].rearrange("l (c p) -> (l c) p", p=128), W=[("vr",)], group=True)
        abrows = d["ada_b"].rearrange("l (k p) -> (l k) p", p=128)
        self.dma("sp", "p0", vr2[:, :], abrows[0:128, :], W=[("vr2",)], group=True)
        self.dma("sp", "p0", vr3[0:64, :], abrows[128:192, :], W=[("vr3",)], group=True)
        ps = self.psb[0]
        self.tr(ps[:, 0:88], vr[0:88, :], self.identf[0:88, 0:88], R=[("vr",), ("cst",)], W=[("ps", 0)])
        self.cp("dve", vT[:, :], ps[:, 0:88], R=[("ps", 0)], W=[("vT",)])
        ps1 = self.psb[1]
        self.tr(ps1[:, 0:128], vr2[:, :], self.identf, R=[("vr2",), ("cst",)], W=[("ps", 1)])
        self.tr(ps1[:, 128:192], vr3[0:64, :], self.identf[0:64, 0:64], R=[("vr3",), ("cst",)], W=[("ps", 1)])
        self.cp("dve", abT[:, :], ps1[:, 0:192], R=[("ps", 1)], W=[("abT",)])
        self.memset("dve", sT[:, :, :], 0.0, W=[("sT",)])
        cTv = vT[:, 0:24].rearrange("p (s c) -> p c s", c=8)
        self.act(sT[:, :, 0:3], cTv, AF.Silu, R=[("vT",)], W=[("sT",)])
        n1 = vT[:, 24:56].rearrange("p (l c) -> p l c", c=8)
        n2 = vT[:, 56:88].rearrange("p (l c) -> p l c", c=8)
        for l in range(4):
            mps = self.psb[2 + (l % 2)]
            for cb in range(8):
                u = l * 8 + cb
                w = wslot[u % 2]
                self.dma("sp", "aw%d" % (u % 2), w[:, :, :],
                         d["ada_w"][l].rearrange("(c p) f -> p c f", p=128)[:, :, cb * 768:(cb + 1) * 768],
                         W=[("adaw", u % 2)])
                for fc in range(6):
                    col = (cb * 6 + fc) * 4
                    for dc in range(8):
                        self.mm(mps[:, col:col + 4], w[:, dc, fc * 128:(fc + 1) * 128], sT[:, dc, :],
                                start=(dc == 0), stop=(dc == 7),
                                R=[("adaw", u % 2), ("sT",)], W=[("ps", 2 + (l % 2))])
            mv = mps[:, 0:192].rearrange("p (k s) -> p k s", s=4)[:, :, 0:3]
            bv = abT[:, l * 48:(l + 1) * 48].unsqueeze(2).to_broadcast([128, 48, 3])
            self.tt("dve", modraw[:, l, :, :], mv, bv, ALU.add, R=[("ps", 2 + (l % 2)), ("abT",)], W=[("modraw", l)])
            mr = modraw[:, l, :, :].rearrange("p (k c) s -> p k s c", c=8)
            for kind, k in ((1, 0), (2, 2), (4, 3), (5, 5)):
                self.cp("dve", self.modv[:, l, :, kind, :], mr[:, k, :, :], R=[("modraw", l)], W=[("modv",)])
            for kind, k, nn in ((0, 1, n1), (3, 4, n2)):
                nb = nn[:, l, :].unsqueeze(1).to_broadcast([128, 3, 8])
                self.stt(self.modv[:, l, :, kind, :], mr[:, k, :, :], 1.0, nb, ALU.add, ALU.mult,
                         R=[("modraw", l), ("vT",)], W=[("modv",)])
        self.P.barrier()

    def mod(self, l, s, kind, c=None):
        if c is None:
            return self.modv[:, l, s, kind, :]
        return self.modv[:, l, s, kind, c:c + 1]

    def load_sample(self, b):
        R = self.R
        R.reset()
        stg = [self.alloc(R, "stg%d" % i, [128, D], F32) for i in range(2)]
        for t in range(NT):
            src = self.d["x2"][b, t * 128:(t + 1) * 128, :] if t < 16 else self.d["ctx2"][b, (t - 16) * 128:(t - 15) * 128, :]
            s = stg[t % 2]
            self.dma("sp", "ld%d" % (t % 2), s[:, :], src, W=[("stg", t % 2)])
            for half in range(2):
                bank = (t % 2) * 2 + half
                ps = self.psb[bank]
                for j in range(4):
                    c = half * 4 + j
                    self.tr(ps[:, j * 128:(j + 1) * 128], s[:, c * 128:(c + 1) * 128], self.identf,
                            R=[("stg", t % 2), ("cst",)], W=[("ps", bank)])
                self.cp("act" if half == 0 else "dve", self.xT[:, half * 4:(half + 1) * 4, t * 128:(t + 1) * 128],
                        ps[:, :].rearrange("p (j n) -> p j n", n=128),
                        R=[("ps", bank)], W=[("xT", min(t // 4, 4), half * 4 + j) for j in range(4)])
        self.P.barrier()

    def store_sample(self, dst, ntiles):
        R = self.R
        R.reset()
        stg = [self.alloc(R, "ostg%d" % i, [128, D], F32) for i in range(2)]
        for t in range(ntiles):
            s = stg[t % 2]
            for half in range(2):
                bank = (t % 2) * 2 + half
                ps = self.psb[bank]
                for j in range(4):
                    c = half * 4 + j
                    self.tr(ps[:, j * 128:(j + 1) * 128], self.xT[:, c, t * 128:(t + 1) * 128], self.identf,
                            R=[("xT", min(t // 4, 4), c), ("cst",)], W=[("ps", bank)])
                self.cp("act" if half == 0 else "dve", s[:, half * 512:(half + 1) * 512], ps[:, :],
                        R=[("ps", bank)], W=[("ostg", t % 2)])
            self.dma("sp", "st%d" % (t % 2), dst[t * 128:(t + 1) * 128, :], s[:, :], R=[("ostg", t % 2)])
        self.P.barrier()

    def norm_bufs(self):
        R = self.R
        self.sq = [self.alloc(R, "sq%d" % i, [128, 8, 512], BF16) for i in range(2)]
        self.lnv = self.alloc(R, "lnv", [128, 512], F32)
        self.rstd = [self.alloc(R, "rstd%d" % i, [128, 512], F32) for i in range(2)]
        self.tmpn = [self.alloc(R, "tmpn%d" % i, [128, 512], F32) for i in range(2)]

    def norm_group(self, gi, l, b, which, dst_of):
        t0, w = GROUPS[gi]
        s = b if gi < 4 else 2
        ka, kb = (0, 1) if which == 1 else (3, 4)
        sq = self.sq[gi % 2]
        self.act(sq[:, :, 0:w], self.xT[:, :, t0:t0 + w], AF.Square, R=[("xT", gi, c) for c in range(8)], W=[("sq", gi % 2)])
        bank = 5 + (gi % 2)
        ps = self.psb[bank]
        for c in range(8):
            self.mm(ps[:, 0:w], self.onesb[:, :], sq[:, c, 0:w], start=(c == 0), stop=(c == 7),
                    R=[("sq", gi % 2), ("onesb",)], W=[("ps", bank)])
        self.act(self.lnv[:, 0:w], ps[:, 0:w], AF.Ln, bias=self.misc[:, 2:3], scale=1.0 / D,
                 R=[("ps", bank), ("cst",)], W=[("lnv",)])
        rstd = self.rstd[gi % 2]
        self.act(rstd[:, 0:w], self.lnv[:, 0:w], AF.Exp, scale=-0.5, R=[("lnv",)], W=[("rstd", gi % 2)])
        for c in range(8):
            tm = self.tmpn[c % 2]
            self.tt("dve", tm[:, 0:w], self.xT[:, c, t0:t0 + w], rstd[:, 0:w], ALU.mult,
                    R=[("xT", gi, c), ("rstd", gi % 2)], W=[("tmpn", c % 2)])
            dst, dres = dst_of(c)
            self.act(dst, tm[:, 0:w], AF.Identity, bias=self.mod(l, s, kb, c), scale=self.mod(l, s, ka, c),
                     R=[("tmpn", c % 2), ("modv",)], W=[dres])

    def attn_core(self, steps_cfg, scale):
        steps = []
        for hi, hc in enumerate(steps_cfg):
            kts = hc["ktiles"]
            npair = (len(kts) + 1) // 2
            for pi in range(npair):
                ks = kts[2 * pi:2 * pi + 2]
                steps.append((hi, hc, ks, pi == 0, pi == npair - 1))
        n = len(steps)

        def qk(i):
            hi, hc, ks, first, last = steps[i]
            s_ = i % 2
            for j, k in enumerate(ks):
                bank = 2 * s_ + j
                self.mm(self.psb[bank][:, 0:hc["w"]], hc["kt"][:, k * 128:(k + 1) * 128], hc["qt"][:, hc["t0"]:hc["t0"] + hc["w"]],
                        R=[hc["kres"], hc["qres"]], W=[("ps", bank)])

        def ex(i):
            hi, hc, ks, first, last = steps[i]
            s_ = i % 2
            w = hc["w"]
            nk = len(ks)
            src = self.psall[:, s_ * 1024:s_ * 1024 + nk * 512].rearrange("p (b n) -> p b n", n=512)[:, :, 0:w]
            dst = self.PT[:, s_, 0:nk * 512].rearrange("p (b n) -> p b n", n=512)[:, :, 0:w]
            self.act(dst, src, AF.Exp, scale=scale, R=[("ps", 2 * s_ + j) for j in range(nk)], W=[("pt", s_)])

        def pv(i):
            hi, hc, ks, first, last = steps[i]
            w = hc["w"]
            s_ = i % 2
            if hc["hs"] == 0:
                self.take_op(hc["gi"], 8 if last else 1)
            ob = 4 + (hi + self.ot_ctr) % 2
            for j, k in enumerate(ks):
                self.mm(self.psb[ob][:, 0:w], hc["va"](k), self.PT[:, s_, j * 512:j * 512 + w],
                        start=(first and j == 0), stop=(last and j == len(ks) - 1),
                        R=[("pt", s_), hc["vres"]], W=[("ps", ob)])
            if last:
                lo, hi_ = (slice(0, 64), slice(64, 128))
                o_sl, d_sl = (lo, hi_) if hc["o_lo"] else (hi_, lo)
                rec_o = self.rec[d_sl, 0:w]
                rec_i = self.psb[ob][d_sl, 0:w]
                self.P.op("dve", lambda e: e.reciprocal(out=rec_o, in_=rec_i), [("ps", ob)], [("rec",)])
                self.tt("dve", hc["att"][o_sl, hc["t0"]:hc["t0"] + w], self.psb[ob][o_sl, 0:w], self.rec[d_sl, 0:w], ALU.mult,
                        R=[("ps", ob), ("rec",)], W=[("attT", hc["gi"])])

        if n == 0:
            return
        qk(0)
        for i in range(n):
            if i + 1 < n:
                qk(i + 1)
            ex(i)
            pv(i)
        self.ot_ctr += len(steps_cfg)

    def out_proj(self, wo, wres, att, ares, l, b, need_ctx):
        self.flush_op()
        ng = 5 if need_ctx else 4
        pend = {}
        for gi in range(ng):
            t0, w = GROUPS[gi]
            s = b if gi < 4 else 2
            lst = []
            for dc in range(8):
                def blk(gi=gi, dc=dc, t0=t0, w=w, s=s):
                    bank = 6 + (self.op_ctr % 2)
                    self.op_ctr += 1
                    ps = self.psb[bank]
                    self.mm(ps[:, 0:w], wo[:, dc * 128:(dc + 1) * 128], att[:, t0:t0 + w], R=[wres, ("attT", gi)], W=[("ps", bank)])
                    xs = self.xT[:, dc, t0:t0 + w]
                    self.stt(xs, ps[:, 0:w], self.mod(l, s, 2, dc), xs, ALU.mult, ALU.add,
                             R=[("ps", bank), ("modv",), ("xT", gi, dc)], W=[("xT", gi, dc)])
                lst.append(blk)
            pend[gi] = lst
        self.pending_op = pend

    def flush_op(self, gi=None):
        pend = getattr(self, "pending_op", None)
        if not pend:
            return
        keys = sorted(pend.keys()) if gi is None else ([gi] if gi in pend else [])
        for k in keys:
            for blk in pend.pop(k):
                blk()

    def take_op(self, gi, n):
        pend = getattr(self, "pending_op", None)
        if not pend or gi not in pend:
            return
        lst = pend[gi]
        for _ in range(min(n, len(lst))):
            lst.pop(0)()
        if not lst:
            pend.pop(gi)

    def head_rstd(self, src3, nh, hd, sqt, ssq, lnv, rs, res_src):
        self.tt("dve", sqt, src3, src3, ALU.mult, R=[res_src], W=[("sqt",)])
        self.red(ssq[:, 0:nh], sqt, ALU.add, R=[("sqt",)], W=[("ssq",)])
        self.act(lnv[:, 0:nh], ssq[:, 0:nh], AF.Ln, bias=self.misc[:, 2:3], scale=1.0 / hd, R=[("ssq",), ("cst",)], W=[("lnvh",)])
        self.act(rs[:, 0:nh], lnv[:, 0:nh], AF.Exp, scale=-0.5, R=[("lnvh",)], W=[("rsh",)])

    def bcast_row(self, queue, slot, dst, src_row, n, W, group=False):
        self.dma(queue, slot, dst, src_row.unsqueeze(0).to_broadcast([128, n]), W=W, group=group)

    def gqa_layer(self, l, b, need_ctx):
        d = self.d
        j = l // 3
        R, R2 = self.R, self.R2
        R.reset()
        R2.reset()
        self.norm_bufs()
        hT = self.carve("hT", HT_OFF, [128, 8, TT], BF16)
        for gi in range(5):
            t0, w = GROUPS[gi]
            self.norm_group(gi, l, b, 1, lambda c, t0=t0, w=w, gi=gi: (hT[:, c, t0:t0 + w], ("hT", gi)))
        self.P.barrier()
        R.reset()
        QKT = self.alloc(R, "QKT", [128, 5, TT], BF16)
        VA = self.alloc(R, "VA", [128, NT, 192], BF16)
        attT = self.alloc(R, "attT", [128, TT], BF16)
        Wg = self.alloc(R2, "Wg", [128, 8, 384], BF16)
        WoP = self.alloc(R2, "WoP", [128, 2, D], BF16)
        self.PT = self.alloc(R, "PT", [128, 2, 1024], BF16)
        self.rec = self.alloc(R, "rec", [128, 512], F32)
        gq = self.alloc(R, "gq", [128, 5, 64], F32)
        raw = [self.alloc(R2, "raw%d" % i, [128, 384], F32) for i in range(3)]
        self.memset("pool", QKT[64:128, 0:2, :], 0.0, W=[("QKTz",)])
        self.memset("pool", QKT[0:64, 2:4, :], 0.0, W=[("QKTz",)])
        sqt = self.alloc(R2, "sqt", [128, 5, 64], F32)
        t1 = self.alloc(R2, "t1", [128, 5, 64], F32)
        t2 = self.alloc(R2, "t2", [128, 5, 64], F32)
        ra = self.alloc(R2, "ra", [128, 5, 32], F32)
        rb = self.alloc(R2, "rb", [128, 5, 32], F32)
        rc = self.alloc(R2, "rc", [128, 5, 32], F32)
        rd = self.alloc(R2, "rd", [128, 5, 32], F32)
        qkb = [self.alloc(R2, "qkb%d" % i, [128, 6, 64], BF16) for i in range(3)]
        ssq = [self.smallp[:, 0:8], self.smallp[:, 8:16]]
        lnv = [self.smallp[:, 16:24], self.smallp[:, 24:32]]
        rs = [self.smallp[:, 32:40], self.smallp[:, 40:48]]
        for i in range(4):
            self.bcast_row("sp", "gq", gq[:, i, :], d["a_qnorm"][j], 64, W=[("gq",)], group=(i > 0))
        self.bcast_row("sp", "gq", gq[:, 4, :], d["a_knorm"][j], 64, W=[("gq",)], group=True)
        self.memset("pool", VA[:, :, 0:64], 1.0, W=[("VA", t) for t in range(NT)])
        self.memset("pool", VA[:, :, 128:192], 1.0, W=[("VA", t) for t in range(NT)])
        wq = d["a_wqkv"][j].rearrange("(c p) f -> p c f", p=128)
        ptb = self.psbf(7)
        ntl = NT
        for g in range(4):
            self.dma("pool", "wg", Wg[:, :, 0:256], wq[:, :, 256 * g:256 * g + 256], W=[("Wg",)], bar=False)
            self.dma("pool", "wg", Wg[:, :, 256:320], wq[:, :, 1024 + 64 * g:1024 + 64 * g + 64], W=[("Wg",)], group=True, bar=False)
            self.dma("pool", "wg", Wg[:, :, 320:384], wq[:, :, 1280 + 64 * g:1280 + 64 * g + 64], W=[("Wg",)], group=True, bar=False)
            def S1(t):
                bank = 5 + (t % 2)
                ps = self.psb[bank]
                for c in range(8):
                    self.mm(ps[:, 0:384], hT[:, c, t * 128:(t + 1) * 128], Wg[:, c, :], start=(c == 0), stop=(c == 7),
                            R=[("hT", min(t // 4, 4)), ("Wg",)], W=[("ps", bank)])
                self.cp("act", raw[t % 3][:, :], ps[:, 0:384], R=[("ps", bank)], W=[("raw", t % 3)])

            def S2(t):
                rw = raw[t % 3]
                r3 = rw[:, 0:320].rearrange("p (h e) -> p h e", e=64)
                for h in range(5):
                    self.act(sqt[:, h, :], r3[:, h, :], AF.Square, scale=0.125, accum=ssq[t % 2][:, h:h + 1],
                             R=[("raw", t % 3)], W=[("sqt",), ("ssq", t % 2)])

            def S3(t):
                self.act(lnv[t % 2][:, 0:5], ssq[t % 2][:, 0:5], AF.Ln, bias=self.misc[:, 2:3], scale=1.0,
                         R=[("ssq", t % 2), ("cst",)], W=[("lnvh", t % 2)])
                self.act(rs[t % 2][:, 0:5], lnv[t % 2][:, 0:5], AF.Exp, scale=-0.5, R=[("lnvh", t % 2)], W=[("rsh", t % 2)])

            def S4(t):
                rw = raw[t % 3]
                r3 = rw[:, 0:320].rearrange("p (h e) -> p h e", e=64)
                self.tt("dve", t1[:, :, :], r3, rs[t % 2][:, 0:5].unsqueeze(2).to_broadcast([128, 5, 64]), ALU.mult,
                        R=[("raw", t % 3), ("rsh", t % 2)], W=[("t1",)])
                self.tt("dve", t2[:, :, :], t1[:, :, :], gq[:, :, :], ALU.mult, R=[("t1",), ("gq",)], W=[("t2",)])
                qb = qkb[t % 3]
                if t < 16:
                    x1 = t2[:, :, 0:32]
                    x2 = t2[:, :, 32:64]
                    cs = self.cosA[:, t, :].unsqueeze(1).to_broadcast([128, 5, 32])
                    sn = self.sinA[:, t, :].unsqueeze(1).to_broadcast([128, 5, 32])
                    self.tt("dve", ra[:, :, :], x1, cs, ALU.mult, R=[("t2",), ("cosA",)], W=[("ra",)])
                    self.tt("dve", rb[:, :, :], x2, sn, ALU.mult, R=[("t2",), ("sinA",)], W=[("rb",)])
                    self.tt("dve", qb[:, 0:5, 0:32], ra[:, :, :], rb[:, :, :], ALU.subtract, R=[("ra",), ("rb",)], W=[("qkb", t % 3)])
                    self.tt("dve", rc[:, :, :], x1, sn, ALU.mult, R=[("t2",), ("sinA",)], W=[("rc",)])
                    self.tt("dve", rd[:, :, :], x2, cs, ALU.mult, R=[("t2",), ("cosA",)], W=[("rd",)])
                    self.tt("dve", qb[:, 0:5, 32:64], rc[:, :, :], rd[:, :, :], ALU.add, R=[("rc",), ("rd",)], W=[("qkb", t % 3)])
                else:
                    self.cp("dve", qb[:, 0:5, :], t2[:, :, :], R=[("t2",)], W=[("qkb", t % 3)])
                self.cp("dve", qb[:, 5, :], qb[:, 4, :], R=[("qkb", t % 3)], W=[("qkb", t % 3)])
                self.cp("pool", VA[:, t, 64:128], rw[:, 320:384], R=[("raw", t % 3)], W=[("VA", t)])

            def S5(t):
                qb = qkb[t % 3]
                qf = qb[:, :, :].rearrange("p h e -> p (h e)")
                for i in range(3):
                    self.tr(ptb[:, i * 128:(i + 1) * 128], qf[:, i * 128:(i + 1) * 128], self.identb[:, :],
                            R=[("qkb", t % 3), ("identb",)], W=[("ps", 7)])
                self.cp("act", QKT[0:64, 0:2, t * 128:(t + 1) * 128], ptb[0:64, 0:256].rearrange("p (i n) -> p i n", n=128),
                        R=[("ps", 7)], W=[("QKT", t)])
                self.cp("act", QKT[64:128, 2:4, t * 128:(t + 1) * 128], ptb[64:128, 0:256].rearrange("p (i n) -> p i n", n=128),
                        R=[("ps", 7)], W=[("QKT", t)])
                self.cp("dve", QKT[:, 4, t * 128:(t + 1) * 128], ptb[:, 256:384], R=[("ps", 7)], W=[("QKT", t)])

            self.pipeline(ntl, S1, S2, S3, S4, S5)
            for p in range(2):
                slot = (g * 2 + p) % 2
                h0 = 4 * g + 2 * p
                self.dma("pool", "wo%d" % slot, WoP[:, slot, :], d["a_wo"][j][h0 * 64:h0 * 64 + 128, :], W=[("WoP", slot)], bar=False)
                cfgs = []
                qgroups = [0, 1, 2, 3] + ([4] if need_ctx else [])
                for gi in qgroups:
                    t0, w = GROUPS[gi]
                    ktiles = list(range(NT)) if gi < 4 else [16, 17]
                    for hs in range(2):
                        psl = slice(0, 64) if hs == 0 else slice(64, 128)
                        cfgs.append(dict(
                            qt=QKT[:, p + 2 * hs, :], kt=QKT[:, 4, :],
                            va=(lambda k, hs=hs: VA[:, k, 64:192] if hs == 0 else VA[:, k, 0:128]),
                            o_lo=(hs == 0), t0=t0, w=w, gi=gi, hs=hs, ktiles=ktiles, att=attT,
                            qres=("QKTall",), kres=("QKTall",), vres=("VAall",), ares=("attT",)))
                self._alias([("QKT", t) for t in range(ntl)] + [("QKTz",)], ("QKTall",))
                self._alias([("VA", t) for t in range(ntl)], ("VAall",))
                self.attn_core(cfgs, 0.125)
                self.out_proj(WoP[:, slot, :], ("WoP", slot), attT, ("attT",), l, b, need_ctx)
            self._alias_release([("QKT", t) for t in range(ntl)], ("QKTall",))
            self._alias_release([("VA", t) for t in range(ntl)], ("VAall",))
        self.flush_op()
        self.P.barrier()

    def pipeline(self, n, S1, S2, S3, S4, S5):
        for i in range(-1, n + 2):
            if 0 <= i + 1 < n:
                S1(i + 1)
            if 0 <= i < n:
                S2(i)
                S3(i)
            if 0 <= i - 1 < n:
                S4(i - 1)
            if 0 <= i - 2 < n:
                S5(i - 2)

    def _alias(self, fine, coarse):
        P = self.P
        toks = set()
        for f in fine:
            st = P.res.get(f)
            if st is not None and st["w"] is not None:
                toks.add(st["w"])
        P.res[coarse] = {"w": None, "r": {}, "ws": toks}
        for e in ("pe",):
            P.pending[e] |= toks

    def _alias_release(self, fine, coarse):
        P = self.P
        st = P.res.get(coarse)
        if st is None:
            return
        for f in fine:
            fs = P.res.get(f)
            if fs is None:
                fs = P.res[f] = {"w": None, "r": {}}
            for k, v in st["r"].items():
                fs["r"][("al", coarse, k)] = v

    def mla_layer(self, l, b, need_ctx):
        d = self.d
        R, R2, HTR = self.R, self.R2, self.HTR
        R.reset()
        R2.reset()
        HTR.reset()
        self.norm_bufs()
        hT = self.carve("hT", HT_OFF, [128, 8, TT], BF16)
        for gi in range(5):
            t0, w = GROUPS[gi]
            self.norm_group(gi, l, b, 1, lambda c, t0=t0, w=w, gi=gi: (hT[:, c, t0:t0 + w], ("hT", gi)))
        self.P.barrier()
        R.reset()
        cT = self.alloc(R, "cT", [128, 5, TT], BF16)
        krope = self.alloc(R, "krope", [128, NT, 32], BF16)
        self.PT = self.alloc(R, "PT", [128, 2, 1024], BF16)
        self.rec = self.alloc(R, "rec", [128, 512], F32)
        gq2 = self.alloc(R, "gq2", [128, 2, 96], F32)
        gk2 = self.alloc(R, "gk2", [128, 2, 64], F32)
        gk = self.alloc(R, "gkr", [128, 32], F32)
        W1 = self.alloc(R, "W1", [128, 8, 672], BF16)
        g1 = self.alloc(R, "g1", [128, 640], F32)
        raw1 = [self.alloc(R2, "rawm%d" % i, [128, 672], F32) for i in range(2)]
        cn = [self.alloc(R2, "cn%d" % i, [128, 640], BF16) for i in range(2)]
        sq1 = self.alloc(R2, "sq1", [128, 384], F32)
        kr1 = self.alloc(R2, "kr1", [128, 32], F32)
        rt = [self.alloc(R2, "rt%d" % i, [128, 2, 16], F32) for i in range(4)]
        raw2 = [self.alloc(R2, "rawn%d" % i, [128, 448], F32) for i in range(3)]
        ssq2 = [self.smallp[:, 36:44], self.smallp[:, 44:52]]
        lnv2 = [self.smallp[:, 52:58], self.smallp[:, 58:64]]
        rs2 = [self.alloc(R2, "rs2_%d" % i, [128, 8], F32) for i in range(2)]
        sqt = self.alloc(R2, "sqt", [128, 2, 64], F32)
        t1 = self.alloc(R2, "t1", [128, 2, 96], F32)
        tq = self.alloc(R2, "tq", [128, 2, 32], F32)
        k1 = self.alloc(R2, "k1", [128, 2, 64], F32)
        qa = [self.alloc(R2, "qa%d" % i, [128, 2, 96], BF16) for i in range(3)]
        ka = [self.alloc(R2, "ka%d" % i, [128, 2, 96], BF16) for i in range(3)]
        ssq = self.smallp[:, 0:8]
        lnv = self.smallp[:, 8:16]
        rs = self.smallp[:, 16:24]
        ssq1 = self.smallp[:, 24:28]
        lnv1 = self.smallp[:, 28:32]
        rs1 = self.smallp[:, 32:36]
        self.bcast_row("sp", "gq", g1[:, 0:384], d["b_qnorm_lat"][0], 384, W=[("g1",)])
        self.bcast_row("sp", "gq", g1[:, 384:640], d["b_kvnorm_lat"][0], 256, W=[("g1",)], group=True)
        self.bcast_row("sp", "gq", gk[:, :], d["b_knorm"][0, 64:96], 32, W=[("gk",)], group=True)
        for i in range(2):
            self.bcast_row("sp", "gq", gq2[:, i, :], d["b_qnorm"][0], 96, W=[("gq2",)], group=True)
            self.bcast_row("sp", "gq", gk2[:, i, :], d["b_knorm"][0, 0:64], 64, W=[("gk2",)], group=True)
        self.dma("pool", "wg", W1[:, :, 0:384], d["b_wdq"][0].rearrange("(c p) f -> p c f", p=128), W=[("W1",)], bar=False)
        self.dma("pool", "wg", W1[:, :, 384:672], d["b_wdkv"][0].rearrange("(c p) f -> p c f", p=128), W=[("W1",)], group=True, bar=False)
        ptb = self.psbf(7)
        for t in range(NT):
            ba = 3 + 2 * (t % 2)
            bb = ba + 1
            for c in range(8):
                self.mm(self.psb[ba][:, :], hT[:, c, t * 128:(t + 1) * 128], W1[:, c, 0:512], start=(c == 0), stop=(c == 7),
                        R=[("hT", min(t // 4, 4)), ("W1",)], W=[("ps", ba)])
            for c in range(8):
                self.mm(self.psb[bb][:, 0:160], hT[:, c, t * 128:(t + 1) * 128], W1[:, c, 512:672], start=(c == 0), stop=(c == 7),
                        R=[("hT", min(t // 4, 4)), ("W1",)], W=[("ps", bb)])
            rw = raw1[t % 2]
            self.cp("act", rw[:, 0:512], self.psb[ba][:, :], R=[("ps", ba)], W=[("rawm", t % 2)])
            self.cp("act", rw[:, 512:672], self.psb[bb][:, 0:160], R=[("ps", bb)], W=[("rawm", t % 2)])
            for i, (c0, n) in enumerate(((0, 384), (384, 256), (640, 32))):
                self.act(sq1[:, 0:n], rw[:, c0:c0 + n], AF.Square, accum=ssq1[:, i:i + 1], R=[("rawm", t % 2)], W=[("sq1",), ("ssq1", i)])
                self.act(lnv1[:, i:i + 1], ssq1[:, i:i + 1], AF.Ln, bias=self.misc[:, 2:3], scale=1.0 / n, R=[("ssq1", i), ("cst",)], W=[("lnv1", i)])
            self.act(rs1[:, 0:3], lnv1[:, 0:3], AF.Exp, scale=-0.5, R=[("lnv1", 0), ("lnv1", 1), ("lnv1", 2)], W=[("rs1",)])
            cnb = cn[t % 2]
            self.stt(cnb[:, 0:384], rw[:, 0:384], rs1[:, 0:1], g1[:, 0:384], ALU.mult, ALU.mult, R=[("rawm", t % 2), ("rs1",), ("g1",)], W=[("cn", t % 2)])
            self.stt(cnb[:, 384:640], rw[:, 384:640], rs1[:, 1:2], g1[:, 384:640], ALU.mult, ALU.mult, R=[("rawm", t % 2), ("rs1",), ("g1",)], W=[("cn", t % 2)])
            self.stt(kr1[:, :], rw[:, 640:672], rs1[:, 2:3], gk[:, :], ALU.mult, ALU.mult, R=[("rawm", t % 2), ("rs1",), ("gk",)], W=[("kr1",)])
            if t < 16:
                x1 = kr1[:, 0:16]
                x2 = kr1[:, 16:32]
                cs = self.cosB[:, t, :]
                sn = self.sinB[:, t, :]
                self.tt("dve", rt[0][:, 0, :], x1, cs, ALU.mult, R=[("kr1",), ("cosB",)], W=[("rt", 0)])
                self.tt("dve", rt[1][:, 0, :], x2, sn, ALU.mult, R=[("kr1",), ("sinB",)], W=[("rt", 1)])
                self.tt("dve", krope[:, t, 0:16], rt[0][:, 0, :], rt[1][:, 0, :], ALU.subtract, R=[("rt", 0), ("rt", 1)], W=[("krope", t)])
                self.tt("dve", rt[2][:, 0, :], x1, sn, ALU.mult, R=[("kr1",), ("sinB",)], W=[("rt", 2)])
                self.tt("dve", rt[3][:, 0, :], x2, cs, ALU.mult, R=[("kr1",), ("cosB",)], W=[("rt", 3)])
                self.tt("dve", krope[:, t, 16:32], rt[2][:, 0, :], rt[3][:, 0, :], ALU.add, R=[("rt", 2), ("rt", 3)], W=[("krope", t)])
            else:
                self.cp("dve", krope[:, t, :], kr1[:, :], R=[("kr1",)], W=[("krope", t)])
            for i in range(5):
                self.tr(ptb[:, i * 128:(i + 1) * 128], cnb[:, i * 128:(i + 1) * 128], self.identb[:, :],
                        R=[("cn", t % 2), ("identb",)], W=[("ps", 7)])
            self.cp("dve", cT[:, :, t * 128:(t + 1) * 128], ptb[:, 0:640].rearrange("p (i n) -> p i n", n=128),
                    R=[("ps", 7)], W=[("cT", t)])
        self.P.barrier()
        QKT = self.alloc(HTR, "QKT", [128, 4, TT], BF16)
        VA = self.alloc(HTR, "VA", [128, NT, 192], BF16)
        attT = self.alloc(HTR, "attT", [128, TT], BF16)
        W2q = self.alloc(HTR, "W2q", [128, 3, 192], BF16)
        W2kv = self.alloc(HTR, "W2kv", [128, 2, 256], BF16)
        WoP = self.alloc(HTR, "WoP", [128, 2, D], BF16)
        self.memset("pool", VA[:, :, 64:128], 1.0, W=[("VA", t) for t in range(NT)])
        wuq = d["b_wuq"][0].rearrange("(c p) f -> p c f", p=128)
        wukv = d["b_wukv"][0].rearrange("(c p) f -> p c f", p=128)
        sc = 96.0 ** -0.5
        for pp in range(8):
            self.dma("pool", "wg", W2q[:, :, :], wuq[:, :, pp * 192:(pp + 1) * 192], W=[("W2",)], bar=False)
            self.dma("pool", "wg", W2kv[:, :, :], wukv[:, :, pp * 256:(pp + 1) * 256], W=[("W2",)], group=True, bar=False)
            def S1(t):
                bank = 5 + (t % 2)
                ps = self.psb[bank]
                for c in range(3):
                    self.mm(ps[:, 0:192], cT[:, c, t * 128:(t + 1) * 128], W2q[:, c, :], start=(c == 0), stop=(c == 2),
                            R=[("cT", t), ("W2",)], W=[("ps", bank)])
                for c in range(2):
                    self.mm(ps[:, 192:448], cT[:, 3 + c, t * 128:(t + 1) * 128], W2kv[:, c, :], start=(c == 0), stop=(c == 1),
                            R=[("cT", t), ("W2",)], W=[("ps", bank)])
                self.cp("act", raw2[t % 3][:, :], ps[:, 0:448], R=[("ps", bank)], W=[("rawn", t % 3)])

            def views(t):
                rw = raw2[t % 3]
                rq = rw[:, 0:192].rearrange("p (h e) -> p h e", e=96)
                rkv = rw[:, 192:448].rearrange("p (h e) -> p h e", e=128)
                return rw, rq, rkv

            def S2(t):
                rw, rq, rkv = views(t)
                sq = ssq2[t % 2]
                for i, (src, hd) in enumerate(((rq[:, :, 0:64], 64), (rq[:, :, 64:96], 32), (rkv[:, :, 0:64], 64))):
                    for h in range(2):
                        self.act(sqt[:, 0, 0:hd], src[:, h, :], AF.Square, scale=float(hd) ** -0.5, accum=sq[:, 2 * i + h:2 * i + h + 1],
                                 R=[("rawn", t % 3)], W=[("sqt",), ("ssq", t % 2)])

            def S3(t):
                self.act(lnv2[t % 2][:, 0:6], ssq2[t % 2][:, 0:6], AF.Ln, bias=self.misc[:, 2:3], scale=1.0,
                         R=[("ssq", t % 2), ("cst",)], W=[("lnvh", t % 2)])
                self.act(rs2[t % 2][:, 0:6], lnv2[t % 2][:, 0:6], AF.Exp, scale=-0.5, R=[("lnvh", t % 2)], W=[("rsh", t % 2)])

            def S4(t):
                rw, rq, rkv = views(t)
                rsv = rs2[t % 2]
                qab = qa[t % 3]
                kab = ka[t % 3]
                self.tt("dve", t1[:, :, 0:64], rq[:, :, 0:64], rsv[:, 0:2].unsqueeze(2).to_broadcast([128, 2, 64]), ALU.mult,
                        R=[("rawn", t % 3), ("rsh", t % 2)], W=[("t1",)])
                self.tt("dve", qab[:, :, 0:64], t1[:, :, 0:64], gq2[:, :, 0:64], ALU.mult, R=[("t1",), ("gq2",)], W=[("qa", t % 3)])
                self.tt("dve", t1[:, :, 64:96], rq[:, :, 64:96], rsv[:, 2:4].unsqueeze(2).to_broadcast([128, 2, 32]), ALU.mult,
                        R=[("rawn", t % 3), ("rsh", t % 2)], W=[("t1",)])
                if t < 16:
                    self.tt("dve", tq[:, :, :], t1[:, :, 64:96], gq2[:, :, 64:96], ALU.mult, R=[("t1",), ("gq2",)], W=[("tq",)])
                    x1 = tq[:, :, 0:16]
                    x2 = tq[:, :, 16:32]
                    cs = self.cosB[:, t, :].unsqueeze(1).to_broadcast([128, 2, 16])
                    sn = self.sinB[:, t, :].unsqueeze(1).to_broadcast([128, 2, 16])
                    self.tt("dve", rt[0][:, :, :], x1, cs, ALU.mult, R=[("tq",), ("cosB",)], W=[("rt", 0)])
                    self.tt("dve", rt[1][:, :, :], x2, sn, ALU.mult, R=[("tq",), ("sinB",)], W=[("rt", 1)])
                    self.tt("dve", qab[:, :, 64:80], rt[0][:, :, :], rt[1][:, :, :], ALU.subtract, R=[("rt", 0), ("rt", 1)], W=[("qa", t % 3)])
                    self.tt("dve", rt[2][:, :, :], x1, sn, ALU.mult, R=[("tq",), ("sinB",)], W=[("rt", 2)])
                    self.tt("dve", rt[3][:, :, :], x2, cs, ALU.mult, R=[("tq",), ("cosB",)], W=[("rt", 3)])
                    self.tt("dve", qab[:, :, 80:96], rt[2][:, :, :], rt[3][:, :, :], ALU.add, R=[("rt", 2), ("rt", 3)], W=[("qa", t % 3)])
                else:
                    self.tt("dve", qab[:, :, 64:96], t1[:, :, 64:96], gq2[:, :, 64:96], ALU.mult, R=[("t1",), ("gq2",)], W=[("qa", t % 3)])
                self.tt("dve", k1[:, :, :], rkv[:, :, 0:64], rsv[:, 4:6].unsqueeze(2).to_broadcast([128, 2, 64]), ALU.mult,
                        R=[("rawn", t % 3), ("rsh", t % 2)], W=[("k1",)])
                self.tt("dve", kab[:, :, 0:64], k1[:, :, :], gk2[:, :, :], ALU.mult, R=[("k1",), ("gk2",)], W=[("ka", t % 3)])
                self.cp("dve", kab[:, :, 64:96], krope[:, t, :].unsqueeze(1).to_broadcast([128, 2, 32]), R=[("krope", t)], W=[("ka", t % 3)])
                self.cp("pool", VA[:, t, 0:64], rkv[:, 0, 64:128], R=[("rawn", t % 3)], W=[("VA", t)])
                self.cp("pool", VA[:, t, 128:192], rkv[:, 1, 64:128], R=[("rawn", t % 3)], W=[("VA", t)])

            def S5(t):
                qab = qa[t % 3]
                kab = ka[t % 3]
                for i in range(2):
                    self.tr(ptb[0:96, i * 128:(i + 1) * 128], qab[:, i, :], self.identb[:, :], R=[("qa", t % 3), ("identb",)], W=[("ps", 7)])
                for i in range(2):
                    self.tr(ptb[0:96, (2 + i) * 128:(3 + i) * 128], kab[:, i, :], self.identb[:, :], R=[("ka", t % 3), ("identb",)], W=[("ps", 7)])
                self.cp("act", QKT[0:96, :, t * 128:(t + 1) * 128], ptb[0:96, 0:512].rearrange("p (i n) -> p i n", n=128),
                        R=[("ps", 7)], W=[("QKT", t)])

            self.pipeline(NT, S1, S2, S3, S4, S5)
            slot = pp % 2
            self.dma("pool", "wo%d" % slot, WoP[:, slot, :], d["b_wo"][0][pp * 128:(pp + 1) * 128, :], W=[("WoP", slot)], bar=False)
            cfgs = []
            qgroups = [0, 1, 2, 3] + ([4] if need_ctx else [])
            for gi in qgroups:
                t0, w = GROUPS[gi]
                ktiles = list(range(NT)) if gi < 4 else [16, 17]
                for hs in range(2):
                    cfgs.append(dict(
                        qt=QKT[0:96, hs, :], kt=QKT[0:96, 2 + hs, :],
                        va=(lambda k, hs=hs: VA[:, k, 0:128] if hs == 0 else VA[:, k, 64:192]),
                        o_lo=(hs == 0), t0=t0, w=w, gi=gi, hs=hs, ktiles=ktiles, att=attT,
                        qres=("QKTall",), kres=("QKTall",), vres=("VAall",), ares=("attT",)))
            self._alias([("QKT", t) for t in range(NT)], ("QKTall",))
            self._alias([("VA", t) for t in range(NT)], ("VAall",))
            self.attn_core(cfgs, sc)
            self.out_proj(WoP[:, slot, :], ("WoP", slot), attT, ("attT",), l, b, need_ctx)
            self._alias_release([("QKT", t) for t in range(NT)], ("QKTall",))
            self._alias_release([("VA", t) for t in range(NT)], ("VAall",))
        self.flush_op()
        self.P.barrier()

    def na_layer(self, l, b, need_ctx):
        d = self.d
        R, R2 = self.R, self.R2
        R.reset()
        R2.reset()
        self.norm_bufs()
        hT = self.carve("hT", HT_OFF, [128, 8, TT], BF16)
        for gi in range(5):
            t0, w = GROUPS[gi]
            self.norm_group(gi, l, b, 1, lambda c, t0=t0, w=w, gi=gi: (hT[:, c, t0:t0 + w], ("hT", gi)))
        self.P.barrier()
        R.reset()
        QKT = self.alloc(R, "QKT", [128, 3, TT], BF16)
        VA = self.alloc(R, "VA", [128, NT, 192], BF16)
        attT = self.alloc(R, "attT", [128, TT], BF16)
        Wp = self.alloc(R, "Wg", [128, 8, 384], BF16)
        WoP = self.alloc(R, "WoP", [128, 2, D], BF16)
        self.PT = self.alloc(R, "PT", [128, 2, 1024], BF16)
        self.rec = self.alloc(R2, "rec", [128, 256], F32)
        gq4 = self.alloc(R2, "gq4", [128, 4, 64], F32)
        maskb = self.alloc(R, "maskb", [128, 21 * 128], BF16)
        self.memset("pool", QKT[64:128, 0, :], 0.0, W=[("QKTz",)])
        self.memset("pool", QKT[0:64, 1, :], 0.0, W=[("QKTz",)])
        BM = self.alloc(R2, "BM", [128, 21 * 128], F32)
        tmpb = [self.alloc(R2, "tmpb%d" % i, [128, 512], F32) for i in range(2)]
        raw = [self.alloc(R2, "raw%d" % i, [128, 384], F32) for i in range(3)]
        sqt = self.alloc(R2, "sqt", [128, 4, 64], F32)
        qkb = [self.alloc(R2, "qkb%d" % i, [128, 4, 64], BF16) for i in range(3)]
        junk = self.alloc(R2, "junk", [128, 64], F32)
        ssq = [self.smallp[:, 0:8], self.smallp[:, 8:16]]
        lnv = [self.smallp[:, 16:24], self.smallp[:, 24:32]]
        rs = [self.smallp[:, 32:40], self.smallp[:, 40:48]]
        for i in range(2):
            self.bcast_row("sp", "gq", gq4[:, i, :], d["c_qnorm"][0], 64, W=[("gq4",)], group=(i > 0))
            self.bcast_row("sp", "gq", gq4[:, 2 + i, :], d["c_knorm"][0], 64, W=[("gq4",)], group=True)
        self.dma("pool", "mk", maskb[:, 0:1344], d["na_mask"][:, 0:1344], W=[("maskb",)])
        self.dma("pool", "mk", maskb[:, 1344:2688], d["na_mask"][:, 1344:2688], W=[("maskb",)], group=True)
        self.memset("pool", VA[:, :, 64:128], 1.0, W=[("VA", t) for t in range(NT)])
        wq = d["c_wqkv"][0].rearrange("(c p) f -> p c f", p=128)
        ptb = self.psbf(7)
        sc = 0.125
        for pp in range(8):
            self.dma("pool", "wg", Wp[:, :, 0:128], wq[:, :, 128 * pp:128 * pp + 128], W=[("Wg",)], bar=False)
            self.dma("pool", "wg", Wp[:, :, 128:256], wq[:, :, 1024 + 128 * pp:1024 + 128 * pp + 128], W=[("Wg",)], group=True, bar=False)
            self.dma("pool", "wg", Wp[:, :, 256:384], wq[:, :, 2048 + 128 * pp:2048 + 128 * pp + 128], W=[("Wg",)], group=True, bar=False)
            def S1(t):
                bank = 5 + (t % 2)
                ps = self.psb[bank]
                for c in range(8):
                    self.mm(ps[:, 0:384], hT[:, c, t * 128:(t + 1) * 128], Wp[:, c, :], start=(c == 0), stop=(c == 7),
                            R=[("hT", min(t // 4, 4)), ("Wg",)], W=[("ps", bank)])
                self.cp("act", raw[t % 3][:, :], ps[:, 0:384], R=[("ps", bank)], W=[("raw", t % 3)])

            def S2(t):
                r3 = raw[t % 3][:, 0:256].rearrange("p (h e) -> p h e", e=64)
                for h in range(4):
                    self.act(junk[:, :], r3[:, h, :], AF.Square, scale=0.125, accum=ssq[t % 2][:, h:h + 1],
                             R=[("raw", t % 3)], W=[("junk",), ("ssq", t % 2)])

            def S3(t):
                self.act(lnv[t % 2][:, 0:4], ssq[t % 2][:, 0:4], AF.Ln, bias=self.misc[:, 2:3], scale=1.0,
                         R=[("ssq", t % 2), ("cst",)], W=[("lnvh", t % 2)])
                self.act(rs[t % 2][:, 0:4], lnv[t % 2][:, 0:4], AF.Exp, scale=-0.5, R=[("lnvh", t % 2)], W=[("rsh", t % 2)])

            def S4(t):
                rw = raw[t % 3]
                r3 = rw[:, 0:256].rearrange("p (h e) -> p h e", e=64)
                self.tt("dve", sqt[:, :, :], r3, rs[t % 2][:, 0:4].unsqueeze(2).to_broadcast([128, 4, 64]), ALU.mult,
                        R=[("raw", t % 3), ("rsh", t % 2)], W=[("sqt",)])
                qb = qkb[t % 3]
                self.tt("dve", qb[:, :, :], sqt[:, :, :], gq4[:, :, :], ALU.mult, R=[("sqt",), ("gq4",)], W=[("qkb", t % 3)])
                self.cp("pool", VA[:, t, 0:64], rw[:, 256:320], R=[("raw", t % 3)], W=[("VA", t)])
                self.cp("pool", VA[:, t, 128:192], rw[:, 320:384], R=[("raw", t % 3)], W=[("VA", t)])

            def S5(t):
                qb = qkb[t % 3]
                qf = qb[:, :, :].rearrange("p h e -> p (h e)")
                for i in range(2):
                    self.tr(ptb[:, i * 128:(i + 1) * 128], qf[:, i * 128:(i + 1) * 128], self.identb[:, :],
                            R=[("qkb", t % 3), ("identb",)], W=[("ps", 7)])
                self.cp("act", QKT[0:64, 0, t * 128:(t + 1) * 128], ptb[0:64, 0:128], R=[("ps", 7)], W=[("QKT", t)])
                self.cp("act", QKT[64:128, 1, t * 128:(t + 1) * 128], ptb[64:128, 0:128], R=[("ps", 7)], W=[("QKT", t)])
                self.cp("dve", QKT[:, 2, t * 128:(t + 1) * 128], ptb[:, 128:256], R=[("ps", 7)], W=[("QKT", t)])

            self.pipeline(NT, S1, S2, S3, S4, S5)
            slot = pp % 2
            self.dma("pool", "wo%d" % slot, WoP[:, slot, :], d["c_wo"][0][pp * 128:(pp + 1) * 128, :], W=[("WoP", slot)], bar=False)
            self._alias([("QKT", t) for t in range(NT)] + [("QKTz",)], ("QKTall",))
            self._alias([("VA", t) for t in range(NT)], ("VAall",))
            for hs in range(2):
                h = 2 * pp + hs
                psl = slice(0, 64) if hs == 0 else slice(64, 128)
                self.dma("sp", "bm", BM[:, :], d["na_bias"][h], W=[("BM",)])
                self.tt("pool", BM[:, :], BM[:, :], maskb[:, :], ALU.add, R=[("BM",), ("maskb",)], W=[("BM",)])
                va = (lambda k, hs=hs: VA[:, k, 0:128] if hs == 0 else VA[:, k, 64:192])
                self.na_core(QKT[:, hs, :], QKT[:, 2, :], va, hs == 0, attT, BM, tmpb, sc)
            if need_ctx:
                cfgs = []
                t0, w = GROUPS[4]
                for hs in range(2):
                    psl = slice(0, 64) if hs == 0 else slice(64, 128)
                    cfgs.append(dict(
                        qt=QKT[:, hs, :], kt=QKT[:, 2, :],
                        va=(lambda k, hs=hs: VA[:, k, 0:128] if hs == 0 else VA[:, k, 64:192]),
                        o_lo=(hs == 0), t0=t0, w=w, gi=4, hs=hs, ktiles=[16, 17], att=attT,
                        qres=("QKTall",), kres=("QKTall",), vres=("VAall",), ares=("attT",)))
                self.attn_core(cfgs, sc)
            self.out_proj(WoP[:, slot, :], ("WoP", slot), attT, ("attT",), l, b, need_ctx)
            self._alias_release([("QKT", t) for t in range(NT)], ("QKTall",))
            self._alias_release([("VA", t) for t in range(NT)], ("VAall",))
        self.flush_op()
        self.P.barrier()

    def na_core(self, qt, kt, va, o_lo, attT, BM, tmpb, sc):
        packs = []
        for qi in range(16):
            blocks = na_block_ids(qi)
            items = [(kt_, blk) for (kt_, blk) in blocks] + [(16, None), (17, None)]
            plist = [items[0:4], items[4:]]
            for pi, pk in enumerate(plist):
                packs.append((qi, pk, pi == 0, pi == len(plist) - 1))
        n = len(packs)
        pt3 = self.PT[:, :, :].rearrange("p a (b n) -> p (a b) n", n=512)
        lo, hi_ = slice(0, 64), slice(64, 128)
        o_sl, d_sl = (lo, hi_) if o_lo else (hi_, lo)

        def qk(i):
            qi, pk, first, last = packs[i]
            bank = i % 3
            for j, (k, blk) in enumerate(pk):
                self.mm(self.psb[bank][:, j * 128:(j + 1) * 128], kt[:, k * 128:(k + 1) * 128], qt[:, qi * 128:(qi + 1) * 128],
                        R=[("QKTall",)], W=[("ps", bank)])

        def ex(i):
            qi, pk, first, last = packs[i]
            bank = i % 3
            nb = sum(1 for (_, blk) in pk if blk is not None)
            nk = len(pk)
            if nb > 0:
                b0 = pk[0][1]
                tb = tmpb[i % 2]
                self.stt(tb[:, 0:nb * 128], self.psb[bank][:, 0:nb * 128], sc, BM[:, b0 * 128:(b0 + nb) * 128], ALU.mult, ALU.add,
                         R=[("ps", bank), ("BM",)], W=[("tmpb", i % 2)])
                self.act(pt3[:, bank, 0:nb * 128], tb[:, 0:nb * 128], AF.Exp, R=[("tmpb", i % 2)], W=[("pt", bank)])
            if nk > nb:
                self.act(pt3[:, bank, nb * 128:nk * 128], self.psb[bank][:, nb * 128:nk * 128], AF.Exp, scale=sc,
                         R=[("ps", bank)], W=[("pt", bank)])

        def pv(i):
            qi, pk, first, last = packs[i]
            if o_lo and (qi % 4 == 0 or (qi % 4 == 1 and first)):
                self.take_op(qi // 4, 3)
            ob = 3 + (qi + self.ot_ctr) % 2
            for j, (k, blk) in enumerate(pk):
                self.mm(self.psb[ob][:, 0:128], va(k), pt3[:, i % 3, j * 128:(j + 1) * 128],
                        start=(first and j == 0), stop=(last and j == len(pk) - 1),
                        R=[("pt", i % 3), ("VAall",)], W=[("ps", ob)])
            if last:
                def fin(ob=ob, qi=qi):
                    rec_o = self.rec[d_sl, 0:128]
                    rec_l = self.rec[d_sl, 128:256]
                    rec_i = self.psb[ob][d_sl, 0:128]
                    self.act(rec_l, rec_i, AF.Ln, R=[("ps", ob)], W=[("recl",)])
                    self.act(rec_o, rec_l, AF.Exp, scale=-1.0, R=[("recl",)], W=[("rec",)])
                    self.tt("dve", attT[o_sl, qi * 128:(qi + 1) * 128], self.psb[ob][o_sl, 0:128], self.rec[d_sl, 0:128], ALU.mult,
                            R=[("ps", ob), ("rec",)], W=[("attT", qi // 4)])
                deferred.append((i + 2, fin))

        deferred = []
        qk(0)
        if n > 1:
            qk(1)
        for i in range(n):
            if i + 2 < n:
                qk(i + 2)
            ex(i)
            while deferred and deferred[0][0] <= i:
                deferred.pop(0)[1]()
            pv(i)
        while deferred:
            deferred.pop(0)[1]()
        self.ot_ctr += 16

    def ring_load(self, src):
        i = self.unit_ctr % NUNIT
        self.unit_ctr += 1
        buf = self.ring[i]
        self.dma("pool", "ring%d" % i, buf[:, :, :], src, W=[("ring", i)], bar=False)
        return buf, ("ring", i)

    def moe_units(self, l, e):
        d = self.d
        wg = d["moe_wg"][l, e].rearrange("(c p) f -> p c f", p=128)
        wu = d["moe_wu"][l, e].rearrange("(c p) f -> p c f", p=128)
        wd = d["moe_wd"][l, e].rearrange("(c p) f -> p c f", p=128)
        return [wg[:, :, 0:512], wu[:, :, 0:512], wg[:, :, 512:1024], wu[:, :, 512:1024], wd[:, :, 0:512], wd[:, :, 512:1024]]

    def moe_layer(self, l, b, need_ctx, pre):
        d = self.d
        R = self.R
        R.reset()
        ROFF = R_OFF
        self.norm_bufs()
        h2T = [self.alloc(R, "h2T%d" % i, [128, 8, 512], BF16) for i in range(2)]
        aff = self.carve("aff", ROFF + 43008, [128, NT, 16], F32)
        rw = self.carve("rw", ROFF + 44160, [128, 8, 16], BF16)
        m8 = self.carve("m8", ROFF + 44160 + 256, [16, 8], F32)
        thr = self.carve("thr", ROFF + 44160 + 288, [16, 2], F32)
        gate = self.carve("gate", ROFF + 44160 + 320, [128, 4], F32)
        ee = self.carve("ee", ROFF + 44160 + 352, [128, 16], F32)
        sst = self.carve("sst", ROFF + 44160 + 416, [128, 8], F32)
        h2 = self.carve("h2", HT_OFF, [128, NT, D], BF16)
        self.dma("pool", "rw", rw[:, :, :], d["moe_router"][l].rearrange("(c p) e -> p c e", p=128), W=[("rw",)])
        ngrp = 5 if need_ctx else 4
        ntl = NT if need_ctx else 16
        ptb = self.psbf(7)
        for gi in range(ngrp):
            t0, w = GROUPS[gi]
            hb = h2T[gi % 2]
            self.norm_group(gi, l, b, 2, lambda c, hb=hb, w=w, gi=gi: (hb[:, c, 0:w], ("h2T", gi % 2)))
            for tt_ in range(w // 128):
                t = t0 // 128 + tt_
                bank = 3 + (t % 2)
                ps = self.psb[bank]
                for c in range(8):
                    self.mm(ps[:, 0:16], hb[:, c, tt_ * 128:(tt_ + 1) * 128], rw[:, c, :], start=(c == 0), stop=(c == 7),
                            R=[("h2T", gi % 2), ("rw",)], W=[("ps", bank)])
                self.red(sst[:, 0:1], ps[:, 0:16], ALU.max, R=[("ps", bank)], W=[("sst",)])
                self.ts("dve", sst[:, 1:2], sst[:, 0:1], -1.0, ALU.mult, R=[("sst",)], W=[("sst",)])
                self.act(ee[:, :], ps[:, 0:16], AF.Exp, bias=sst[:, 1:2], accum=sst[:, 2:3],
                         R=[("ps", bank), ("sst",)], W=[("ee",), ("sst",)])
                self.P.op("dve", lambda e: e.reciprocal(out=sst[:, 3:4], in_=sst[:, 2:3]), [("sst",)], [("sst",)])
                self.ts("dve", aff[:, t, :], ee[:, :], sst[:, 3:4], ALU.mult, R=[("ee",), ("sst",)], W=[("aff",)])
                for c in range(8):
                    self.tr(ptb[:, c * 128:(c + 1) * 128], hb[:, c, tt_ * 128:(tt_ + 1) * 128], self.identb[:, :],
                            R=[("h2T", gi % 2), ("identb",)], W=[("ps", 7)])
                self.cp("act", h2[:, t, :], ptb[:, :], R=[("ps", 7)], W=[("h2", t)])
        self.P.barrier()
        affT = self.carve("affT", ROFF + 0, [16, TT], F32)
        work = self.carve("work", ROFF + 9216, [16, TT], F32)
        B3 = self.carve("B3", ROFF + 18432, [16, TT], F32)
        vb16 = self.carve("vb16", ROFF + 27648, [16, TT], BF16)
        gmtok = self.carve("gmtok", ROFF + 38400, [128, NT, 16], F32)
        vtok = self.carve("vtok", ROFF + 40704, [128, NT, 16], F32)
        gmhl = self.carve("gmhl", ROFF + 41856, [128, NT, 16, 2], BF16)
        for t in range(ntl):
            bank = (t // 4) % 2
            self.tr(self.psb[bank][0:16, (t % 4) * 128:(t % 4 + 1) * 128], aff[:, t, :], self.identf,
                    R=[("aff",), ("cst",)], W=[("ps", bank)])
            if t % 4 == 3 or t == ntl - 1:
                t_lo = (t // 4) * 4
                n = (t - t_lo + 1) * 128
                self.cp("dve", affT[:, t_lo * 128:t_lo * 128 + n], self.psb[bank][0:16, 0:n], R=[("ps", bank)], W=[("affT",)])
        segs = [(0, T, CAP // 8, 0)] + ([(T, TC, CAPC // 8, 1)] if need_ctx else [])
        ncols = T + (TC if need_ctx else 0)
        self.cp("dve", work[:, 0:ncols], affT[:, 0:ncols], R=[("affT",)], W=[("work",)])
        for (c0, n, rounds, ti) in segs:
            wv = work[:, c0:c0 + n]
            for r in range(rounds):
                self.P.op("dve", lambda e, wv=wv: e.max(out=m8[:, :], in_=wv), [("work",)], [("m8",)])
                if r < rounds - 1:
                    self.P.op("dve", lambda e, wv=wv: e.match_replace(out=wv, in_to_replace=m8[:, :], in_values=wv, imm_value=-1.0),
                              [("work",), ("m8",)], [("work",)])
            self.cp("dve", thr[:, ti:ti + 1], m8[:, 7:8], R=[("m8",)], W=[("thr",)])
        ones_col = self.cstb[0:16, 128 + 1:128 + 2]
        for (c0, n, rounds, ti) in segs:
            self.ts("dve", work[:, c0:c0 + n], affT[:, c0:c0 + n], thr[:, ti:ti + 1], ALU.is_ge, R=[("affT",), ("thr",)], W=[("work",)])
            self.P.op("dve", lambda e, c0=c0, n=n: e.tensor_tensor_scan(out=B3[:, c0:c0 + n], data0=ones_col.to_broadcast([16, n]),
                                                                     data1=work[:, c0:c0 + n], initial=0.0, op0=ALU.mult, op1=ALU.add),
                      [("work",), ("cst",)], [("B3",)])
        self.tt("dve", B3[:, 0:ncols], B3[:, 0:ncols], work[:, 0:ncols], ALU.mult, R=[("B3",), ("work",)], W=[("B3",)])
        self.ts("dve", B3[:, 0:ncols], B3[:, 0:ncols], -1.0, ALU.add, R=[("B3",)], W=[("B3",)])
        self.tt("dve", affT[:, 0:ncols], affT[:, 0:ncols], work[:, 0:ncols], ALU.mult, R=[("affT",), ("work",)], W=[("affT",)])
        self.cp("dve", vb16[:, 0:ncols], B3[:, 0:ncols], R=[("B3",)], W=[("vb16",)])
        for (src, dst, nm, bank) in ((B3, vtok, "vtok", 2), (affT, gmtok, "gmtok", 3)):
            for t in range(ntl):
                self.tr(self.psb[bank][:, t * 16:(t + 1) * 16], src[:, t * 128:(t + 1) * 128], self.identf[0:16, 0:16],
                        R=[(src.name,), ("cst",)], W=[("ps", bank)])
            self.cp("dve", dst[:, 0:ntl, :], self.psb[bank][:, 0:ntl * 16].rearrange("p (t e) -> p t e", e=16),
                    R=[("ps", bank)], W=[(nm,)])
        self.cp("dve", gmhl[:, 0:ntl, :, 0], gmtok[:, 0:ntl, :], R=[("gmtok",)], W=[("gmhl",)])
        self.tt("dve", gmtok[:, 0:ntl, :], gmtok[:, 0:ntl, :], gmhl[:, 0:ntl, :, 0], ALU.subtract, R=[("gmtok",), ("gmhl",)], W=[("gmtok",)])
        self.cp("dve", gmhl[:, 0:ntl, :, 1], gmtok[:, 0:ntl, :], R=[("gmtok",)], W=[("gmhl",)])
        self.P.barrier()
        S = self.carve("S", ROFF + 0, [128, 16, 256], BF16)
        Sc = self.carve("Sc", ROFF + 8192, [128, 2, 32], BF16)
        ST = self.carve("ST", ROFF + 9216, [128, 2, T], BF16)
        STc = self.carve("STc", ROFF + 9216 + 8192, [32, 256], BF16)
        xgT = self.carve("xgT", ROFF + 18432, [128, 8, 288], BF16)
        hidT = self.carve("hidT", ROFF + 18432 + 4608, [128, 8, 288], BF16)
        y = self.carve("y", ROFF + 32256, [128, 3, D], BF16)
        sg = self.carve("sg", ROFF + 38400, [128, 2, 288], F32)
        Wn = 288 if need_ctx else 256
        units = list(pre)
        upos = [0]

        def next_units(n):
            out = []
            for _ in range(n):
                out.append(units[upos[0]])
                upos[0] += 1
            return out

        srcs = []
        for e in range(NE):
            srcs += self.moe_units(l, e)
        issued = [len(pre)]

        def issue(upto):
            while issued[0] < min(upto, len(srcs)):
                units.append(self.ring_load(srcs[issued[0]]))
                issued[0] += 1

        chunks = [(0, 128), (128, 128)] + ([(256, 32)] if need_ctx else [])
        nch = 3 if need_ctx else 2

        def build_S(e):
            for t in range(16):
                self.ts("dve", S[:, t, :], self.iotac, vtok[:, t, e:e + 1], ALU.is_equal, R=[("vtok",), ("cst",)], W=[("S", t)])
            if need_ctx:
                for t in range(2):
                    self.ts("dve", Sc[:, t, :], self.iotac[:, 0:32], vtok[:, 16 + t, e:e + 1], ALU.is_equal, R=[("vtok",), ("cst",)], W=[("Sc",)])

        def slot_gates(e):
            gb = 4
            gps = self.psb[gb]
            for ch in range(2):
                for t in range(16):
                    self.mm(gps[:, ch * 2:ch * 2 + 2], S[:, t, ch * 128:(ch + 1) * 128], gmhl[:, t, e, :], start=(t == 0), stop=(t == 15),
                            R=[("S", t), ("gmhl",)], W=[("ps", gb)])
            if need_ctx:
                for t in range(2):
                    self.mm(gps[0:32, 4:6], Sc[:, t, :], gmhl[:, 16 + t, e, :], start=(t == 0), stop=(t == 1),
                            R=[("Sc",), ("gmhl",)], W=[("ps", gb)])
            self.red(gate[:, 0:nch], gps[:, 0:2 * nch].rearrange("p (c two) -> p c two", two=2), ALU.add, R=[("ps", gb)], W=[("gate",)])

        def gather_c(e, c):
            bank = c % 2
            ps = self.psb[bank]
            for t in range(16):
                self.mm(ps[:, 0:256], h2[:, t, c * 128:(c + 1) * 128], S[:, t, :], start=(t == 0), stop=(t == 15),
                        R=[("h2", t), ("S", t)], W=[("ps", bank)])
            if need_ctx:
                for t in range(2):
                    self.mm(ps[:, 256:288], h2[:, 16 + t, c * 128:(c + 1) * 128], Sc[:, t, :], start=(t == 0), stop=(t == 1),
                            R=[("h2", 16 + t), ("Sc",)], W=[("ps", bank)])
            self.cp("act", xgT[:, c, 0:Wn], ps[:, 0:Wn], R=[("ps", bank)], W=[("xgT",)])

        def scatter_blocks(e):
            out = []
            for grp in range(4):
                for dc in range(8):
                    def blk(grp=grp, dc=dc):
                        bank = 2 + (dc % 2)
                        ps = self.psb[bank]
                        for ch in range(2):
                            self.mm(ps[:, :], y[:, ch, dc * 128:(dc + 1) * 128], ST[:, ch, grp * 512:(grp + 1) * 512],
                                    start=(ch == 0), stop=(ch == 1), R=[("y",), ("ST", grp)], W=[("ps", bank)])
                        xs = self.xT[:, dc, grp * 512:(grp + 1) * 512]
                        self.stt(xs, ps[:, :], self.mod(l, b, 5, dc), xs, ALU.mult, ALU.add,
                                 R=[("ps", bank), ("modv",), ("xT", grp, dc)], W=[("xT", grp, dc)])
                    out.append(blk)
            if need_ctx:
                for dc in range(8):
                    def blk(dc=dc):
                        bank = 2 + (dc % 2)
                        ps = self.psb[bank]
                        self.mm(ps[:, 0:256], y[0:32, 2, dc * 128:(dc + 1) * 128], STc[:, :], R=[("y",), ("STc",)], W=[("ps", bank)])
                        xs = self.xT[:, dc, T:TT]
                        self.stt(xs, ps[:, 0:256], self.mod(l, 2, 5, dc), xs, ALU.mult, ALU.add,
                                 R=[("ps", bank), ("modv",), ("xT", 4, dc)], W=[("xT", 4, dc)])
                    out.append(blk)
            return out

        def build_ST(e):
            for grp in range(4):
                bank = 2 + (grp % 2)
                ps = self.psb[bank]
                self.mm(ps[:, :], self.sel[:, e, :], vb16[:, grp * 512:(grp + 1) * 512], R=[("sel",), ("vb16",)], W=[("ps", bank)])
                for ch in range(2):
                    self.ts("dve", ST[:, ch, grp * 512:(grp + 1) * 512], ps[:, :], self.misc[:, ch:ch + 1], ALU.is_equal,
                            R=[("ps", bank), ("cst",)], W=[("ST", grp)])
            if need_ctx:
                ps = self.psb[2]
                self.mm(ps[0:32, 0:256], self.sel[:, e, 0:32], vb16[:, T:TT], R=[("sel",), ("vb16",)], W=[("ps", 2)])
                self.ts("dve", STc[:, :], ps[0:32, 0:256], self.misc[0:32, 0:1], ALU.is_equal, R=[("ps", 2), ("cst",)], W=[("STc",)])

        def gate_up(e, wg0, wu0, wg1, wu1, inject):
            for fc in range(8):
                if fc in inject:
                    inject[fc]()
                gbuf, gres = (wg0 if fc < 4 else wg1)
                ubuf, ures = (wu0 if fc < 4 else wu1)
                fo = (fc % 4) * 128
                bg = 4 + 2 * (fc % 2)
                bu = bg + 1
                for c in range(8):
                    self.mm(self.psb[bg][:, 0:Wn], gbuf[:, c, fo:fo + 128], xgT[:, c, 0:Wn], start=(c == 0), stop=(c == 7),
                            R=[gres, ("xgT",)], W=[("ps", bg)])
                for c in range(8):
                    self.mm(self.psb[bu][:, 0:Wn], ubuf[:, c, fo:fo + 128], xgT[:, c, 0:Wn], start=(c == 0), stop=(c == 7),
                            R=[ures, ("xgT",)], W=[("ps", bu)])
                self.act(sg[:, fc % 2, 0:Wn], self.psb[bg][:, 0:Wn], AF.Silu, R=[("ps", bg)], W=[("sg", fc % 2)])
                self.tt("dve", hidT[:, fc, 0:Wn], sg[:, fc % 2, 0:Wn], self.psb[bu][:, 0:Wn], ALU.mult,
                        R=[("sg", fc % 2), ("ps", bu)], W=[("hidT",)])

        def down(e, wd0, wd1):
            for half, (dbuf, dres) in enumerate((wd0, wd1)):
                for ci, (c0, m) in enumerate(chunks):
                    bank = (half * 3 + ci) % 2
                    ps = self.psb[bank]
                    for fcn in range(8):
                        self.mm(ps[0:m, :], hidT[:, fcn, c0:c0 + m], dbuf[:, fcn, :], start=(fcn == 0), stop=(fcn == 7),
                                R=[("hidT",), dres], W=[("ps", bank)])
                    self.ts("dve", y[0:m, ci, half * 512:(half + 1) * 512], ps[0:m, :], gate2[0:m, ci:ci + 1], ALU.mult,
                            R=[("ps", bank), ("gate2",)], W=[("y",)])

        gate2 = self.carve("gate2", ROFF + 44160 + 480, [128, 4], F32)
        issue(4)
        build_S(0)
        slot_gates(0)
        for c in range(8):
            gather_c(0, c)
        for e in range(NE):
            issue(e * 6 + 4)
            wg0, wu0, wg1, wu1 = next_units(4)
            self.cp("dve", gate2[:, 0:nch], gate[:, 0:nch], R=[("gate",)], W=[("gate2",)])
            inj = {1: (lambda e=e: build_ST(e))}
            if e + 1 < NE:
                inj[3] = (lambda e=e: build_S(e + 1))
                inj[5] = (lambda e=e: slot_gates(e + 1))
            gate_up(e, wg0, wu0, wg1, wu1, inj)
            issue(e * 6 + 6)
            wd0, wd1 = next_units(2)
            down(e, wd0, wd1)
            issue(e * 6 + 10)
            blocks = scatter_blocks(e)
            if e + 1 < NE:
                nb = len(blocks)
                per = (nb + 7) // 8
                bi = 0
                for c in range(8):
                    gather_c(e + 1, c)
                    for _ in range(per):
                        if bi < nb:
                            blocks[bi]()
                            bi += 1
                while bi < nb:
                    blocks[bi]()
                    bi += 1
            else:
                for blk in blocks:
                    blk()
        self.P.barrier()

    def moe_prefetch(self, l):
        self.unit_ctr = 0
        srcs = self.moe_units(l, 0)
        return [self.ring_load(srcs[i]) for i in range(2)]

    def build(self):
        cfg = self.cfg
        self.prologue()
        for b in range(cfg.get("nsamples", SPC)):
            self.load_sample(b)
            for l in cfg.get("layers", [0, 1, 2, 3]):
                need_ctx = l < 3
                pre = self.moe_prefetch(l) if cfg.get("moe", True) else []
                kind = l % 3
                if cfg.get("attn", True):
                    if kind == 0:
                        self.gqa_layer(l, b, need_ctx)
                    elif kind == 1:
                        self.mla_layer(l, b, need_ctx)
                    else:
                        self.na_layer(l, b, need_ctx)
                if cfg.get("dump_xa") == (b, l):
                    self.store_sample(self.dbg_out("xa", [TT, D]), NT)
                if cfg.get("moe", True):
                    self.moe_layer(l, b, need_ctx, pre)
                if cfg.get("dump_x") == (b, l):
                    self.store_sample(self.dbg_out("x", [TT, D]), NT)
            self.store_sample(self.y2[b], 16)
        self.P.emit(self.es)
        return self.nc


def host_inputs(inp):
    cst = np.zeros((128, 388), np.float32)
    cst[:, 0:128] = np.eye(128, dtype=np.float32)
    cst[:, 128:384] = np.arange(256, dtype=np.float32)[None, :]
    cst[:, 384] = np.arange(128, dtype=np.float32)
    cst[:, 385] = np.arange(128, dtype=np.float32) + 128.0
    cst[:, 386] = EPS
    selc = np.zeros((16, 16, 128), np.float32)
    for e in range(16):
        selc[e, e, :] = 1.0
    selc = selc.reshape(16, 2048).astype(ml_dtypes.bfloat16)
    cosA, sinA = rope_tables(64)
    cosB, sinB = rope_tables(32)
    na_bias, na_mask = na_tables(np.asarray(inp["c_rpb"], np.float32)[0])
    shared = {k: np.ascontiguousarray(np.asarray(inp[k], np.float32)) for k in (
        "ada_w", "ada_b", "norm1_w", "norm2_w", "a_wqkv", "a_qnorm", "a_knorm", "a_wo",
        "b_wdq", "b_qnorm_lat", "b_wuq", "b_wdkv", "b_kvnorm_lat", "b_wukv", "b_qnorm", "b_knorm", "b_wo",
        "c_wqkv", "c_qnorm", "c_knorm", "c_wo", "moe_router", "moe_wg", "moe_wu", "moe_wd")}
    shared.update(cst=cst, selc=selc, cosA=cosA, sinA=sinA, cosB=cosB, sinB=sinB, na_bias=na_bias, na_mask=na_mask)
    x = np.asarray(inp["x"], np.float32)
    ctx = np.asarray(inp["ctx"], np.float32)
    c = np.asarray(inp["c"], np.float32)
    c_ctx = np.asarray(inp["c_ctx"], np.float32)
    maps = []
    for core in range(N_CORES):
        m = dict(shared)
        m["x2"] = np.ascontiguousarray(x[SPC * core:SPC * core + SPC])
        m["ctx2"] = np.ascontiguousarray(ctx[SPC * core:SPC * core + SPC])
        m["cvec"] = np.ascontiguousarray(np.concatenate([c[SPC * core:SPC * core + SPC], c_ctx[None, :]], axis=0))
        maps.append(m)
    return maps


def kernel(**inp):
    maps = host_inputs(inp)
    nc = Builder({}).build()
    res = run_bass_kernel_spmd(nc, maps, core_ids=list(range(N_CORES)))
    out = np.concatenate([np.asarray(r["y2"], np.float32) for r in res.results], axis=0)
    return out
```
